# Optimizing a Trainium2 kernel written in Bass

```python
import jax
import jax.numpy as jnp
from jax import lax
import numpy as np

D_MODEL = 1024
BATCH = 4
SEQ = 4096
DEPTH = 4

CTX_LEN = 256
GRID_W = 64
N_MOD = 6
ADA_SCALE = 0.5
LN_EPS = 1e-5
DN_ALPHA = (2 * DEPTH) ** 0.25
DN_BETA = (8 * DEPTH) ** -0.25

SSD_HEADS = 8
SSD_HEAD_DIM = 64
SSD_INNER = SSD_HEADS * SSD_HEAD_DIM
SSD_GROUPS = 2
SSD_STATE = 128
SSD_CONV = 3
SSD_CHUNK = 128
SSD_XBC = SSD_INNER + 2 * SSD_GROUPS * SSD_STATE

RWKV_HEADS = 4
RWKV_HEAD_DIM = 64
RWKV_WIDTH = RWKV_HEADS * RWKV_HEAD_DIM
RWKV_DECAY_LORA = 64
RWKV_ICLR_LORA = 64
RWKV_GATE_LORA = 128
RWKV_COLS = 3 * RWKV_WIDTH + 2 * RWKV_DECAY_LORA + 2 * RWKV_ICLR_LORA + RWKV_GATE_LORA
RWKV_LNX_EPS = 64e-5

GLA_HEADS = 4
GLA_KEY_DIM = 32
GLA_VAL_DIM = 64
GLA_K_WIDTH = GLA_HEADS * GLA_KEY_DIM
GLA_V_WIDTH = GLA_HEADS * GLA_VAL_DIM
GLA_GATE_LORA = 16
GLA_GATE_NORMALIZER = 16.0
GLA_CHUNK = 16

MIX_WIDTH = SSD_INNER + RWKV_WIDTH + GLA_V_WIDTH
IN_WIDTHS = (SSD_INNER, SSD_XBC, 2 * SSD_HEADS, RWKV_COLS, GLA_K_WIDTH, GLA_K_WIDTH, GLA_V_WIDTH, 2 * GLA_GATE_LORA, GLA_V_WIDTH)
N_IN = SSD_INNER + SSD_XBC + 2 * SSD_HEADS + RWKV_COLS + 2 * GLA_K_WIDTH + 2 * GLA_V_WIDTH + 2 * GLA_GATE_LORA

N_EXPERTS = 64
TOP_K = 8
N_GROUPS = 8
TOPK_GROUPS = 4
EXPERT_DIM = 256
SHARED_DIM = 256
ROUTED_SCALE = 2.5
MOE_BLOCK = 128

kernel_name = 'hybrid_ssd_rwkv7_gla_moe_diffusion'


def _split(z, widths):
    return jnp.split(z, [int(i) for i in np.cumsum(widths)[:-1]], axis=-1)


def _layernorm(x, w=None, b=None, eps=LN_EPS):
    xf = x.astype(jnp.float32)
    mu = jnp.mean(xf, -1, keepdims=True)
    var = jnp.mean(jnp.square(xf - mu), -1, keepdims=True)
    y = (xf - mu) * lax.rsqrt(var + eps)
    if w is not None:
        y = y * w.astype(jnp.float32) + b.astype(jnp.float32)
    return y.astype(x.dtype)


def _rmsnorm_groups(x, w, group, eps=1e-5):
    xf = x.astype(jnp.float32)
    shp = xf.shape
    g = xf.reshape(shp[:-1] + (shp[-1] // group, group))
    g = g * lax.rsqrt(jnp.mean(g * g, -1, keepdims=True) + eps)
    return (g.reshape(shp) * w.astype(jnp.float32)).astype(x.dtype)


def _seg_flip(z, n_ctx):
    return jnp.concatenate([jnp.flip(z[:, :n_ctx], 1), jnp.flip(z[:, n_ctx:], 1)], axis=1)


def _dwconv_centred(z, w, b):
    ch = z.shape[-1]
    pad = (SSD_CONV - 1) // 2
    y = lax.conv_general_dilated(z, w[:, None, :].astype(z.dtype), window_strides=(1,), padding=[(pad, pad)],
                                 dimension_numbers=('NWC', 'WIO', 'NWC'), feature_group_count=ch)
    return y + b


def _seq_shift_mix(z, mu_prev, mu_next):
    prev = jnp.pad(z, ((0, 0), (1, 0), (0, 0)))[:, :-1]
    nxt = jnp.pad(z, ((0, 0), (0, 1), (0, 0)))[:, 1:]
    return z + mu_prev * (prev - z) + mu_next * (nxt - z)


def _grid_shift_mix(z, mu):
    b, s, ch = z.shape
    rows = s // GRID_W
    g = z.reshape(b, rows, GRID_W, ch)
    left = jnp.pad(g, ((0, 0), (0, 0), (1, 0), (0, 0)))[:, :, :-1]
    right = jnp.pad(g, ((0, 0), (0, 0), (0, 1), (0, 0)))[:, :, 1:]
    up = jnp.pad(g, ((0, 0), (1, 0), (0, 0), (0, 0)))[:, :-1]
    down = jnp.pad(g, ((0, 0), (0, 1), (0, 0), (0, 0)))[:, 1:]
    out = g + mu[0] * (left - g) + mu[1] * (right - g) + mu[2] * (up - g) + mu[3] * (down - g)
    return out.reshape(b, s, ch)


def _ssd_chunked(x, dt, log_a, bm, cm):
    n, t, h, p = x.shape
    g, ds = bm.shape[2], bm.shape[3]
    q = SSD_CHUNK
    nc = t // q
    rep = h // g
    b_h = jnp.repeat(bm, rep, axis=2).reshape(n, nc, q, h, ds)
    c_h = jnp.repeat(cm, rep, axis=2).reshape(n, nc, q, h, ds)
    xdt = (x * dt[..., None]).reshape(n, nc, q, h, p)
    acs = jnp.cumsum(log_a.reshape(n, nc, q, h), axis=2)
    acs_h = jnp.moveaxis(acs, 3, 1)
    tril = jnp.tril(jnp.ones((q, q), dtype=bool))
    seg = acs_h[..., :, None] - acs_h[..., None, :]
    lmat = jnp.exp(jnp.where(tril, seg, -jnp.inf))
    scores = jnp.einsum('nclhd,ncshd->nhcls', c_h, b_h) * lmat
    y_diag = jnp.einsum('nhcls,ncshp->nclhp', scores, xdt)
    decay_end = jnp.exp(acs[:, :, -1:, :] - acs)
    states = jnp.einsum('nclhd,nclh,nclhp->nchpd', b_h, decay_end, xdt)
    chunk_decay = jnp.exp(acs[:, :, -1, :])

    def step(hprev, inp):
        st, dec = inp
        return hprev * dec[..., None, None] + st, hprev

    h0 = jnp.zeros((n, h, p, ds), x.dtype)
    _, h_in = lax.scan(step, h0, (jnp.moveaxis(states, 1, 0), jnp.moveaxis(chunk_decay, 1, 0)))
    h_in = jnp.moveaxis(h_in, 0, 1)
    y_off = jnp.einsum('nclhd,nchpd,nclh->nclhp', c_h, h_in, jnp.exp(acs))
    return (y_diag + y_off).reshape(n, t, h, p)


def _rwkv7_scan(r, w, k, v, kk, a):
    n, t, h, dk = r.shape

    def step(S, inp):
        r_t, w_t, k_t, v_t, kk_t, a_t = inp
        sa = -jnp.einsum('nhvk,nhk->nhv', S, kk_t)
        S = S * w_t[:, :, None, :] + sa[..., None] * (kk_t * a_t)[:, :, None, :] + v_t[..., None] * k_t[:, :, None, :]
        return S, jnp.einsum('nhvk,nhk->nhv', S, r_t)

    S0 = jnp.zeros((n, h, dk, dk), r.dtype)
    xs = (jnp.moveaxis(r, 1, 0), jnp.moveaxis(w, 1, 0), jnp.moveaxis(k, 1, 0),
          jnp.moveaxis(v, 1, 0), jnp.moveaxis(kk, 1, 0), jnp.moveaxis(a, 1, 0))
    _, y = lax.scan(step, S0, xs)
    return jnp.moveaxis(y, 0, 1)


def _gla_chunked(q, k, v, log_a):
    n, t, h, dk = q.shape
    dv = v.shape[-1]
    c = GLA_CHUNK
    nc = t // c
    q = q.reshape(n, nc, c, h, dk)
    k = k.reshape(n, nc, c, h, dk)
    v = v.reshape(n, nc, c, h, dv)
    acs = jnp.cumsum(log_a.reshape(n, nc, c, h, dk), axis=2)
    tril = jnp.tril(jnp.ones((c, c), dtype=bool))[:, :, None, None]
    seg = acs[:, :, :, None] - acs[:, :, None, :]
    decay = jnp.exp(jnp.where(tril, seg, -jnp.inf))
    scores = jnp.einsum('ncihd,ncjhd,ncijhd->nchij', q, k, decay)
    o_intra = jnp.einsum('nchij,ncjhv->ncihv', scores, v)
    q_dec = q * jnp.exp(acs)
    k_dec = k * jnp.exp(acs[:, :, -1:] - acs)
    states = jnp.einsum('ncjhd,ncjhv->nchdv', k_dec, v)
    chunk_decay = jnp.exp(acs[:, :, -1])

    def step(S, inp):
        st, dec = inp
        return S * dec[..., None] + st, S

    S0 = jnp.zeros((n, h, dk, dv), q.dtype)
    _, s_in = lax.scan(step, S0, (jnp.moveaxis(states, 1, 0), jnp.moveaxis(chunk_decay, 1, 0)))
    s_in = jnp.moveaxis(s_in, 0, 1)
    o_inter = jnp.einsum('ncihd,nchdv->ncihv', q_dec, s_in)
    return (o_intra + o_inter).reshape(n, t, h, dv)


def _token_mixer(u, n_ctx, w_in, ssd_conv_w, ssd_conv_b, ssd_dt_bias, ssd_a_log, ssd_d, ssd_norm_w,
                 rwkv_mu, rwkv_w0, rwkv_w2, rwkv_a0, rwkv_a2, rwkv_g2, rwkv_kk, rwkv_ka, rwkv_rk,
                 rwkv_lnx_w, rwkv_lnx_b, gla_gu, gla_gb, gla_norm_w, w_out):
    f32 = jnp.float32
    bsz, t, _ = u.shape
    flip = lambda zz: _seg_flip(zz, n_ctx)

    def both(fwd, bwd):
        s = jnp.stack([fwd, flip(bwd)])
        return s.reshape((2 * bsz,) + s.shape[2:])

    def unboth(y):
        y = y.reshape((2, bsz) + y.shape[1:])
        return y[0] + flip(y[1])

    z, xbc, dt, rw, gq, gk, gv, gd, og = _split(u @ w_in, IN_WIDTHS)

    xbc = jax.nn.silu(jnp.concatenate([_dwconv_centred(xbc[:, :n_ctx], ssd_conv_w, ssd_conv_b),
                                       _dwconv_centred(xbc[:, n_ctx:], ssd_conv_w, ssd_conv_b)], axis=1))
    xs, bm, cm = _split(xbc.astype(f32), (SSD_INNER, SSD_GROUPS * SSD_STATE, SSD_GROUPS * SSD_STATE))
    xs = xs.reshape(bsz, t, SSD_HEADS, SSD_HEAD_DIM)
    bm = bm.reshape(bsz, t, SSD_GROUPS, SSD_STATE)
    cm = cm.reshape(bsz, t, SSD_GROUPS, SSD_STATE)
    dt = jax.nn.softplus(dt.astype(f32).reshape(bsz, t, 2, SSD_HEADS) + ssd_dt_bias.astype(f32))
    neg_a = -jnp.exp(ssd_a_log.astype(f32))
    dt_f, dt_b = dt[:, :, 0], dt[:, :, 1]
    y = unboth(_ssd_chunked(both(xs, xs), both(dt_f, dt_b), both(dt_f * neg_a[0], dt_b * neg_a[1]),
                            both(bm, bm), both(cm, cm)))
    y = y + ssd_d.astype(f32)[:, None] * xs
    y_ssd = _rmsnorm_groups(y.reshape(bsz, t, SSD_INNER) * jax.nn.silu(z.astype(f32)), ssd_norm_w,
                            SSD_INNER // SSD_GROUPS)

    rw = jnp.concatenate([_seq_shift_mix(rw[:, :n_ctx], rwkv_mu[0], rwkv_mu[1]),
                          _grid_shift_mix(rw[:, n_ctx:], rwkv_mu)], axis=1)
    r, k, v, wd_f, wd_b, ad_f, ad_b, gd_r = _split(
        rw.astype(f32), (RWKV_WIDTH,) * 3 + (RWKV_DECAY_LORA,) * 2 + (RWKV_ICLR_LORA,) * 2 + (RWKV_GATE_LORA,))
    hd = lambda zz: zz.reshape(bsz, t, RWKV_HEADS, RWKV_HEAD_DIM)
    decay = lambda wd, i: jnp.exp(-jnp.exp(-jax.nn.softplus(-(rwkv_w0[i] + jnp.tanh(wd) @ rwkv_w2[i])) - 0.5))
    iclr = lambda ad, i: jax.nn.sigmoid(rwkv_a0[i] + ad @ rwkv_a2[i])
    w_f, w_b = hd(decay(wd_f, 0)), hd(decay(wd_b, 1))
    a_f, a_b = hd(iclr(ad_f, 0)), hd(iclr(ad_b, 1))
    r, k, v = hd(r), hd(k), hd(v)
    kk = k * rwkv_kk.reshape(RWKV_HEADS, RWKV_HEAD_DIM)
    kk = kk / jnp.maximum(jnp.sqrt(jnp.sum(kk * kk, -1, keepdims=True)), 1e-12)
    ka = rwkv_ka.reshape(RWKV_HEADS, RWKV_HEAD_DIM)
    k_f = k * (1.0 + (a_f - 1.0) * ka)
    k_b = k * (1.0 + (a_b - 1.0) * ka)
    y = unboth(_rwkv7_scan(both(r, r), both(w_f, w_b), both(k_f, k_b), both(v, v), both(kk, kk), both(a_f, a_b)))
    mu = jnp.mean(y, -1, keepdims=True)
    var = jnp.mean(jnp.square(y - mu), -1, keepdims=True)
    y = ((y - mu) * lax.rsqrt(var + RWKV_LNX_EPS)).reshape(bsz, t, RWKV_WIDTH) * rwkv_lnx_w + rwkv_lnx_b
    bonus = (jnp.sum(r * (k_f + k_b) * rwkv_rk, -1, keepdims=True) * v).reshape(bsz, t, RWKV_WIDTH)
    y_rwkv = (y + bonus) * (jax.nn.sigmoid(gd_r) @ rwkv_g2)

    gh = lambda zz, d: zz.reshape(bsz, t, GLA_HEADS, d)
    q = gh(gq.astype(f32), GLA_KEY_DIM) * GLA_KEY_DIM ** -0.5
    kg = gh(gk.astype(f32), GLA_KEY_DIM)
    vg = gh(gv.astype(f32), GLA_VAL_DIM)
    gd_f, gd_b = _split(gd.astype(f32), (GLA_GATE_LORA, GLA_GATE_LORA))
    la = lambda gg, i: gh(jax.nn.log_sigmoid(gg @ gla_gu[i] + gla_gb[i]) / GLA_GATE_NORMALIZER, GLA_KEY_DIM)
    o = unboth(_gla_chunked(both(q, q), both(kg, kg), both(vg, vg), both(la(gd_f, 0), la(gd_b, 1))))
    y_gla = _rmsnorm_groups(o.reshape(bsz, t, GLA_V_WIDTH), gla_norm_w, GLA_VAL_DIM) * jax.nn.silu(og.astype(f32))

    y_mix = jnp.concatenate([y_ssd, y_rwkv, y_gla], axis=-1).astype(u.dtype)
    return y_mix @ w_out


def _moe_ffn(h, router_w, router_b, w1, w3, w2, sw1, sw3, sw2):
    f32 = jnp.float32
    t = h.shape[0]
    per_group = N_EXPERTS // N_GROUPS
    scores = jax.nn.sigmoid((h @ router_w).astype(f32))
    sel = scores + router_b.astype(f32)
    grp_score = lax.top_k(sel.reshape(t, N_GROUPS, per_group), 2)[0].sum(-1)
    _, top_g = lax.top_k(grp_score, TOPK_GROUPS)
    gmask = jax.nn.one_hot(top_g, N_GROUPS, dtype=f32).sum(1) > 0
    sel = jnp.where(jnp.repeat(gmask, per_group, axis=1), sel, -jnp.inf)
    _, idx = lax.top_k(sel, TOP_K)
    wts = jnp.take_along_axis(scores, idx, axis=1)
    wts = wts / jnp.sum(wts, -1, keepdims=True) * ROUTED_SCALE

    n_assign = t * TOP_K
    flat_e = idx.reshape(-1)
    order = jnp.argsort(flat_e)
    sorted_e = flat_e[order]
    counts = jnp.bincount(flat_e, length=N_EXPERTS)
    padded = (counts + MOE_BLOCK - 1) // MOE_BLOCK * MOE_BLOCK
    pend = jnp.cumsum(padded)
    pstart = pend - padded
    sstart = jnp.cumsum(counts) - counts
    dest = pstart[sorted_e] + jnp.arange(n_assign, dtype=jnp.int32) - sstart[sorted_e]
    n_rows = -(-n_assign // MOE_BLOCK) * MOE_BLOCK + N_EXPERTS * MOE_BLOCK
    n_blocks = n_rows // MOE_BLOCK
    row_tok = jnp.zeros((n_rows,), jnp.int32).at[dest].set((order // TOP_K).astype(jnp.int32))
    row_w = jnp.zeros((n_rows,), f32).at[dest].set(wts.reshape(-1)[order])
    block_e = jnp.minimum(jnp.searchsorted(pend, jnp.arange(n_blocks, dtype=jnp.int32) * MOE_BLOCK, side='right'),
                          N_EXPERTS - 1)

    def body(acc, blk):
        tok, wt, e = blk
        xb = h[tok]
        hid = jax.nn.silu(xb @ w1[e]) * (xb @ w3[e])
        return acc.at[tok].add(wt[:, None] * (hid @ w2[e]).astype(f32)), None

    routed, _ = lax.scan(body, jnp.zeros(h.shape, f32),
                         (row_tok.reshape(n_blocks, MOE_BLOCK), row_w.reshape(n_blocks, MOE_BLOCK), block_e))
    shared = (jax.nn.silu(h @ sw1) * (h @ sw3)) @ sw2
    return routed.astype(h.dtype) + shared


def setup_inputs(seed: int = 0) -> dict:
    key = jax.random.key(seed)
    ks = jax.random.split(key, 40)
    L, D, E, F, Fs = DEPTH, D_MODEL, N_EXPERTS, EXPERT_DIM, SHARED_DIM
    nrm = lambda k, shape, s: jax.random.normal(k, shape, jnp.float32) * s
    one = lambda k, shape: 1.0 + 0.1 * jax.random.normal(k, shape, jnp.float32)
    dt0 = jnp.exp(jax.random.uniform(ks[9], (L, 2, SSD_HEADS), jnp.float32, np.log(1e-3), np.log(1e-1)))
    return {
        'x': nrm(ks[0], (BATCH, SEQ, D), 1.0),
        'c': nrm(ks[1], (BATCH, D), 1.0),
        'ctx': nrm(ks[2], (BATCH, CTX_LEN, D), 1.0),
        'c_ctx': nrm(ks[3], (D,), 1.0),
        'ada_w': nrm(ks[4], (L, D, N_MOD * D), D ** -0.5 * ADA_SCALE),
        'ada_b': nrm(ks[5], (L, N_MOD * D), 0.02),
        'w_in': nrm(ks[6], (L, D, N_IN), D ** -0.5),
        'ssd_conv_w': nrm(ks[7], (L, SSD_CONV, SSD_XBC), SSD_CONV ** -0.5),
        'ssd_conv_b': nrm(ks[8], (L, SSD_XBC), 0.02),
        'ssd_dt_bias': dt0 + jnp.log(-jnp.expm1(-dt0)),
        'ssd_a_log': jnp.log(jax.random.uniform(ks[10], (L, 2, SSD_HEADS), jnp.float32, 1.0, 16.0)),
        'ssd_d': one(ks[11], (L, SSD_HEADS)),
        'ssd_norm_w': one(ks[12], (L, SSD_INNER)),
        'rwkv_mu': jax.random.uniform(ks[13], (L, 4, RWKV_COLS), jnp.float32, 0.0, 0.5),
        'rwkv_w0': jax.random.uniform(ks[14], (L, 2, RWKV_WIDTH), jnp.float32, -6.0, 1.0),
        'rwkv_w2': nrm(ks[15], (L, 2, RWKV_DECAY_LORA, RWKV_WIDTH), 0.1),
        'rwkv_a0': nrm(ks[16], (L, 2, RWKV_WIDTH), 0.1),
        'rwkv_a2': nrm(ks[17], (L, 2, RWKV_ICLR_LORA, RWKV_WIDTH), 0.1),
        'rwkv_g2': nrm(ks[18], (L, RWKV_GATE_LORA, RWKV_WIDTH), RWKV_GATE_LORA ** -0.5),
        'rwkv_kk': one(ks[19], (L, RWKV_WIDTH)),
        'rwkv_ka': one(ks[20], (L, RWKV_WIDTH)),
        'rwkv_rk': nrm(ks[21], (L, RWKV_HEADS, RWKV_HEAD_DIM), 0.1),
        'rwkv_lnx_w': one(ks[22], (L, RWKV_WIDTH)),
        'rwkv_lnx_b': nrm(ks[23], (L, RWKV_WIDTH), 0.02),
        'gla_gu': nrm(ks[24], (L, 2, GLA_GATE_LORA, GLA_K_WIDTH), GLA_GATE_LORA ** -0.5),
        'gla_gb': nrm(ks[25], (L, 2, GLA_K_WIDTH), 0.5),
        'gla_norm_w': one(ks[26], (L, GLA_V_WIDTH)),
        'w_out': nrm(ks[27], (L, MIX_WIDTH, D), MIX_WIDTH ** -0.5 * DN_BETA),
        'ln1_w': one(ks[28], (L, D)),
        'ln1_b': nrm(ks[29], (L, D), 0.02),
        'ln2_w': one(ks[30], (L, D)),
        'ln2_b': nrm(ks[31], (L, D), 0.02),
        'router_w': nrm(ks[32], (L, D, E), D ** -0.5),
        'router_b': nrm(ks[33], (L, E), 0.01),
        'exp_w1': nrm(ks[34], (L, E, D, F), D ** -0.5),
        'exp_w3': nrm(ks[35], (L, E, D, F), D ** -0.5),
        'exp_w2': nrm(ks[36], (L, E, F, D), F ** -0.5 * DN_BETA),
        'sh_w1': nrm(ks[37], (L, D, Fs), D ** -0.5),
        'sh_w3': nrm(ks[38], (L, D, Fs), D ** -0.5),
        'sh_w2': nrm(ks[39], (L, Fs, D), Fs ** -0.5 * DN_BETA),
    }


def reference(x, c, ctx, c_ctx, ada_w, ada_b, w_in, ssd_conv_w, ssd_conv_b, ssd_dt_bias, ssd_a_log, ssd_d,
              ssd_norm_w, rwkv_mu, rwkv_w0, rwkv_w2, rwkv_a0, rwkv_a2, rwkv_g2, rwkv_kk, rwkv_ka, rwkv_rk,
              rwkv_lnx_w, rwkv_lnx_b, gla_gu, gla_gb, gla_norm_w, w_out, ln1_w, ln1_b, ln2_w, ln2_b,
              router_w, router_b, exp_w1, exp_w3, exp_w2, sh_w1, sh_w3, sh_w2):
    n_ctx = ctx.shape[1]
    xc = ctx
    for l in range(DEPTH):
        mod = jnp.split(jax.nn.silu(c) @ ada_w[l] + ada_b[l], N_MOD, axis=-1)
        modc = jnp.split(jax.nn.silu(c_ctx) @ ada_w[l] + ada_b[l], N_MOD, axis=-1)
        sh1, sc1, g1, sh2, sc2, g2 = [m[:, None, :] for m in mod]
        sh1c, sc1c, g1c, sh2c, sc2c, g2c = modc

        u = jnp.concatenate([_layernorm(xc) * (1.0 + sc1c) + sh1c,
                             _layernorm(x) * (1.0 + sc1) + sh1], axis=1)
        mix = _token_mixer(u, n_ctx, w_in[l], ssd_conv_w[l], ssd_conv_b[l], ssd_dt_bias[l], ssd_a_log[l], ssd_d[l],
                           ssd_norm_w[l], rwkv_mu[l], rwkv_w0[l], rwkv_w2[l], rwkv_a0[l], rwkv_a2[l], rwkv_g2[l],
                           rwkv_kk[l], rwkv_ka[l], rwkv_rk[l], rwkv_lnx_w[l], rwkv_lnx_b[l], gla_gu[l], gla_gb[l],
                           gla_norm_w[l], w_out[l])
        x = _layernorm(DN_ALPHA * x + g1 * mix[:, n_ctx:], ln1_w[l], ln1_b[l])
        xc = _layernorm(DN_ALPHA * xc + g1c * mix[:, :n_ctx], ln1_w[l], ln1_b[l])

        h = _layernorm(x) * (1.0 + sc2) + sh2
        moe_w = (router_w[l], router_b[l], exp_w1[l], exp_w3[l], exp_w2[l], sh_w1[l], sh_w3[l], sh_w2[l])
        if l < DEPTH - 1:
            hc = _layernorm(xc) * (1.0 + sc2c) + sh2c
            tok = jnp.concatenate([hc, h], axis=1)
            f = _moe_ffn(tok.reshape(-1, D_MODEL), *moe_w).reshape(tok.shape)
            xc = _layernorm(DN_ALPHA * xc + g2c * f[:, :n_ctx], ln2_w[l], ln2_b[l])
            f = f[:, n_ctx:]
        else:
            f = _moe_ffn(h.reshape(-1, D_MODEL), *moe_w).reshape(h.shape)
        x = _layernorm(DN_ALPHA * x + g2 * f, ln2_w[l], ln2_b[l])
    return x
```

```python
import numpy as np
import concourse.bass as bass
import concourse.mybir as mybir

F32 = mybir.dt.float32
BF16 = mybir.dt.bfloat16
I32 = mybir.dt.int32
U32 = mybir.dt.uint32
AF = mybir.ActivationFunctionType
ALU = mybir.AluOpType
AX = mybir.AxisListType

_DSZ = {F32: 4, BF16: 2, I32: 4, U32: 4}

ENGS = ("tensor", "vector", "scalar", "gpsimd", "sync")
DMA_K = {"sync": 16, "gpsimd": 8, "scalar": 4}


def _box(ap):
    t = ap.tensor
    name = t.name
    dsz = _DSZ.get(ap.dtype, 4)
    steps = list(ap.ap)
    off = ap.offset
    space = str(ap.space) if hasattr(ap, "space") else ""
    if "DRAM" in space.upper() or "dram" in space.lower() or type(t).__name__.startswith("DRam"):
        lo = off + sum(min(0, s * (c - 1)) for s, c in steps)
        hi = off + sum(max(0, s * (c - 1)) for s, c in steps) + 1
        return name, 0, 1, lo * dsz, hi * dsz
    pstep, pcnt = steps[0]
    if pstep == 0:
        pstep = 1 << 30
    p_lo = off // pstep
    f_off = off - p_lo * pstep
    rest = steps[1:]
    lo = f_off + sum(min(0, s * (c - 1)) for s, c in rest)
    hi = f_off + sum(max(0, s * (c - 1)) for s, c in rest) + 1
    lo *= dsz
    hi *= dsz
    if name.startswith("ps"):
        lo = (lo // 2048) * 2048
        hi = ((hi + 2047) // 2048) * 2048
        p_lo, pcnt = 0, 128
    return name, p_lo, p_lo + pcnt, lo, hi


class Prog:
    def __init__(self, nc):
        self.nc = nc
        self.ops = {e: [] for e in ENGS}
        self.cnt = {e: 0 for e in ENGS}
        self.dcnt = {e: 0 for e in DMA_K}
        self.waited = {e: {} for e in ENGS}
        self.recs = {}
        self.semkeys = set()
        self.final_tokens = []
        self.n_ops = 0

    def _deps(self, reads, writes, pe_accum=False, eng=None):
        deps = set()
        rb = [_box(a) for a in reads]
        wb = [_box(a) for a in writes]
        for (name, p0, p1, f0, f1) in rb:
            isps = name.startswith("ps")
            for r in self.recs.get(name, ()):
                if r[0] < p1 and p0 < r[1] and r[2] < f1 and f0 < r[3]:
                    if r[4] == "w" or (isps and r[5][0] != eng):
                        deps.add(r[5])
        for (name, p0, p1, f0, f1) in wb:
            for r in self.recs.get(name, ()):
                if r[0] < p1 and p0 < r[1] and r[2] < f1 and f0 < r[3]:
                    if r[4] == "w" and r[5][0] == "tensor" and eng == "tensor":
                        continue
                    deps.add(r[5])
        return deps, rb, wb

    def _update(self, rb, wb, tok):
        for (name, p0, p1, f0, f1) in wb:
            lst = self.recs.setdefault(name, [])
            lst[:] = [r for r in lst if not (p0 <= r[0] and r[1] <= p1 and f0 <= r[2] and r[3] <= f1)]
            lst.append((p0, p1, f0, f1, "w", tok))
        for (name, p0, p1, f0, f1) in rb:
            lst = self.recs.setdefault(name, [])
            lst[:] = [r for r in lst if not (r[4] == "r" and r[5][0] == tok[0] and p0 <= r[0] and r[1] <= p1
                                             and f0 <= r[2] and r[3] <= f1)]
            lst.append((p0, p1, f0, f1, "r", tok))

    def _waits(self, eng, deps):
        w = self.waited[eng]
        best = {}
        for (k, v) in deps:
            if v > best.get(k, 0):
                best[k] = v
        out = []
        for k, v in best.items():
            if w.get(k, 0) >= v:
                continue
            w[k] = v
            out.append((k, v))
        return out

    def op(self, eng, fn, reads=(), writes=(), pe_accum=False):
        deps, rb, wb = self._deps(reads, writes, pe_accum, eng)
        self.cnt[eng] += 1
        tok = (eng, self.cnt[eng])
        waits = self._waits(eng, deps)
        self.semkeys.add(eng)
        self.ops[eng].append((waits, fn, (eng, 1)))
        self._update(rb, wb, tok)
        self.n_ops += 1
        return tok

    def dma(self, q, out, in_, final=False, **kw):
        deps, rb, wb = self._deps([in_], [out])
        n = self.dcnt[q]
        self.dcnt[q] += 1
        K = DMA_K[q]
        key = ("dma", q, n % K)
        tok = (key, 16 * (n // K + 1))
        if n >= K:
            deps.add((key, 16 * (n // K)))
        waits = self._waits(q, deps)
        self.semkeys.add(key)
        self.ops[q].append((waits, (lambda e, o=out, i=in_, kw=kw: e.dma_start(out=o, in_=i, **kw)), (key, 16)))
        self._update(rb, wb, tok)
        if final:
            self.final_tokens.append(tok)
        self.n_ops += 1
        return tok

    def barrier(self):
        toks = set()
        for e in ENGS:
            if self.cnt[e] > 0:
                toks.add((e, self.cnt[e]))
        for q, K in DMA_K.items():
            n = self.dcnt[q]
            for j in range(min(n, K)):
                last = ((n - 1 - j) // K) * K + j
                toks.add((("dma", q, j), 16 * (last // K + 1)))
        for e in ENGS:
            waits = self._waits(e, set(toks))
            if waits:
                self.ops[e].append((waits, None, None))
        self.recs.clear()

    def mm(self, out, lhsT, rhs, start=True, stop=True):
        return self.op("tensor", lambda e: e.matmul(out, lhsT, rhs, start=start, stop=stop),
                       reads=[lhsT, rhs], writes=[out], pe_accum=not start)

    def tr(self, out, in_, ident):
        return self.op("tensor", lambda e: e.transpose(out, in_, ident), reads=[in_, ident], writes=[out])

    def act(self, out, in_, func, bias=None, scale=None, eng="scalar", accum_out=None):
        reads = [in_]
        kw = {}
        if bias is not None:
            kw["bias"] = bias
            if not isinstance(bias, (int, float)):
                reads.append(bias)
        if scale is not None:
            kw["scale"] = scale
            if not isinstance(scale, (int, float)):
                reads.append(scale)
        writes = [out]
        if accum_out is not None:
            kw["accum_out"] = accum_out
            writes.append(accum_out)
        return self.op(eng, lambda e: e.activation(out, in_, func, **kw), reads=reads, writes=writes)

    def tt(self, out, in0, in1, op, eng="vector"):
        return self.op(eng, lambda e: e.tensor_tensor(out, in0, in1, op), reads=[in0, in1], writes=[out])

    def ts(self, out, in0, s1, op0, s2=None, op1=None, eng="vector", accum_out=None):
        reads = [in0]
        if not isinstance(s1, (int, float)):
            reads.append(s1)
        if s2 is not None and not isinstance(s2, (int, float)):
            reads.append(s2)
        writes = [out]
        kw = {}
        if accum_out is not None:
            kw["accum_out"] = accum_out
            writes.append(accum_out)
        if op1 is None:
            return self.op(eng, lambda e: e.tensor_scalar(out, in0, s1, None, op0, **kw), reads=reads, writes=writes)
        return self.op(eng, lambda e: e.tensor_scalar(out, in0, s1, s2, op0, op1, **kw), reads=reads, writes=writes)

    def stt(self, out, in0, scalar, in1, op0, op1, eng="vector"):
        reads = [in0, in1]
        if not isinstance(scalar, (int, float)):
            reads.append(scalar)
        return self.op(eng, lambda e: e.scalar_tensor_tensor(out, in0, scalar, in1, op0, op1), reads=reads, writes=[out])

    def copy(self, out, in_, eng="vector"):
        if eng == "scalar":
            return self.op(eng, lambda e: e.copy(out, in_), reads=[in_], writes=[out])
        return self.op(eng, lambda e: e.tensor_copy(out, in_), reads=[in_], writes=[out])

    def memset(self, out, val, eng="vector"):
        return self.op(eng, lambda e: e.memset(out, val), writes=[out])

    def finish(self):
        nc = self.nc
        sems = {}
        import contextlib
        with contextlib.ExitStack() as st:
            for k in sorted(self.semkeys, key=str):
                nm = "s_" + "_".join(str(x) for x in (k if isinstance(k, tuple) else (k,)))
                sems[k] = st.enter_context(nc.semaphore(nm))
            block = st.enter_context(nc.Block())
            fin = self._waits("sync", set(self.final_tokens))
            ops = self.ops

            def make(engname, extra=None):
                def body(e):
                    for waits, fn, inc in ops[engname]:
                        for (k, v) in waits:
                            e.wait_ge(sems[k], v)
                        if fn is None:
                            continue
                        ins = fn(e)
                        ins.then_inc(sems[inc[0]], inc[1])
                    if extra:
                        for (k, v) in extra:
                            e.wait_ge(sems[k], v)
                return body

            block.tensor(make("tensor"))
            block.vector(make("vector"))
            block.scalar(make("scalar"))
            block.gpsimd(make("gpsimd"))
            block.sync(make("sync", fin))
        return nc
import contextlib

T_ALL = 4352
NT = 34
D = 1024
NIN = 3504
LN_EPS = 1e-5
DN_ALPHA = 8 ** 0.25
NEGM = -30000.0

C_Z = 0
C_XBC = 512
C_DT = 1536
C_RW = 1552
C_GQ = 2704
C_GK = 2832
C_GV = 2960
C_GD = 3216
C_OG = 3248


def seg_of(i):
    return 0 if i < 2 else 1


class Phase:
    _n = [0]

    def __init__(self, nc, P):
        self.nc = nc
        self.P = P
        self.st = contextlib.ExitStack()

    def sb(self, name, shape, dt=F32):
        Phase._n[0] += 1
        return self.st.enter_context(self.nc.sbuf_tensor(f"{name}_{Phase._n[0]}", list(shape), dt))

    def close(self):
        self.P.barrier()
        self.st.close()


def bc_rows(ap2d_row, n=128):
    steps = list(ap2d_row.ap)
    last = steps[-1]
    return bass.AP(ap2d_row.tensor, ap2d_row.offset, [[0, n], [last[0], last[1]]])


class Ctx:
    pass


def make_consts(nc, P, G):
    def sbp(name, shape, dt=F32):
        return nc.alloc_sbuf_tensor(name, list(shape), dt)
    G.identf = sbp("identf", [128, 128])
    G.identb = sbp("identb", [128, 128], BF16)
    G.onesf = sbp("onesf", [128, 128])
    G.tri = [sbp("tri_f", [128, 128]), sbp("tri_b", [128, 128])]
    G.negm = [sbp("negm_f", [128, 4, 128]), sbp("negm_b", [128, 4, 128])]
    G.trib = [sbp("trib_f", [128, 128], BF16), sbp("trib_b", [128, 128], BF16)]
    G.SL = sbp("m_SL", [128, 128]); G.SU = sbp("m_SU", [128, 128]); G.IU = sbp("m_IU", [128, 128]); G.IL = sbp("m_IL", [128, 128])
    gp = "gpsimd"
    G.nSU = sbp("m_nSU", [128, 128]); G.nSL = sbp("m_nSL", [128, 128])
    G.cind = sbp("cind", [128, 2]); G.cmask = sbp("cmask", [128, 2])
    G.hm4 = sbp("hm4", [128, 4])
    P.memset(G.hm4[:], 1.0, eng=gp)
    P.op(gp, lambda e: e.affine_select(G.hm4[:], G.hm4[:], [[-32, 4]], ALU.is_ge, 0.0, base=0, channel_multiplier=1),
         reads=[G.hm4[:]], writes=[G.hm4[:]])
    P.op(gp, lambda e: e.affine_select(G.hm4[:], G.hm4[:], [[32, 4]], ALU.is_ge, 0.0, base=31, channel_multiplier=-1),
         reads=[G.hm4[:]], writes=[G.hm4[:]])

    def asel(t, cm, step, cmp, base=0):
        P.op(gp, lambda e: e.affine_select(t, t, [[step, 128]], cmp, 0.0, base=base, channel_multiplier=cm),
             reads=[t], writes=[t])
    for t in (G.identf, G.onesf, G.tri[0], G.tri[1], G.SL, G.SU, G.IU, G.IL):
        P.memset(t[:], 1.0, eng=gp)
    asel(G.identf[:], -1, 1, ALU.is_equal)
    asel(G.tri[0][:], -1, 1, ALU.is_ge)
    asel(G.tri[1][:], 1, -1, ALU.is_ge)
    asel(G.IU[:], -1, 1, ALU.is_ge)
    asel(G.IL[:], 1, -1, ALU.is_ge)
    asel(G.SU[:], -1, 1, ALU.is_gt)
    asel(G.SL[:], 1, -1, ALU.is_gt)
    for t in (G.SL, G.SU, G.IU, G.IL):
        P.memset(t[0:64, 64:128], 0.0, eng=gp)
        P.memset(t[64:128, 0:64], 0.0, eng=gp)
    P.ts(G.nSU[:], G.SU[:], -1.0, ALU.mult, eng=gp)
    P.ts(G.nSL[:], G.SL[:], -1.0, ALU.mult, eng=gp)
    P.memset(G.cind[:], 0.0, eng=gp)
    P.memset(G.cind[0:64, 0:1], 1.0, eng=gp)
    P.memset(G.cind[64:128, 1:2], 1.0, eng=gp)
    P.memset(G.cmask[:], 1.0, eng=gp)
    for (col, base) in ((0, 0), (0, -64), (1, -63), (1, -127)):
        P.op(gp, lambda e, col=col, base=base: e.affine_select(G.cmask[:, col:col + 1], G.cmask[:, col:col + 1], [[0, 1]],
                                                             ALU.not_equal, 0.0, base=base, channel_multiplier=1),
             reads=[G.cmask[:, col:col + 1]], writes=[G.cmask[:, col:col + 1]])
    P.copy(G.identb[:], G.identf[:])
    for d in range(2):
        P.copy(G.trib[d][:], G.tri[d][:])
        for h in range(4):
            P.ts(G.negm[d][:, h, :], G.tri[d][:], 1.0, ALU.subtract, -NEGM, ALU.mult)


def ln_stats(P, ph, x_ap, tag, st6, mv, rstd, eps=LN_EPS):
    for hh in range(2):
        P.op("vector", lambda e, hh=hh: e.bn_stats(st6[:, hh, :], x_ap[:, hh * 512:(hh + 1) * 512]),
             reads=[x_ap[:, hh * 512:(hh + 1) * 512]], writes=[st6[:, hh, :]])
    P.op("vector", lambda e: e.bn_aggr(mv, st6), reads=[st6], writes=[mv])
    P.act(rstd, mv[:, 1:2], AF.Sqrt, bias=eps)
    P.op("vector", lambda e: e.reciprocal(rstd, rstd), reads=[rstd], writes=[rstd])


def phase_mod(nc, P, G, l):
    ph = Phase(nc, P)
    cT = ph.sb("cT", [128, 2, 8]); csil = ph.sb("csil", [128, 2, 8])
    wst = ph.sb("adaw", [128, 2, 8, 512]); brow = ph.sb("brow", [1, 6144]); mst = ph.sb("mst", [128, 2, 2, 512])
    for s in range(2):
        P.dma("sync", cT[:, s, :], G.cvec[s, :].rearrange("(kc p) -> p kc", p=128), allow_slow_non_contiguous=True)
    P.act(csil[:], cT[:], AF.Silu)
    P.dma("sync", brow[:], G.w["ada_b"][l:l + 1, :])
    for n in range(12):
        b = n % 2
        P.dma("sync", wst[:, b], G.w["ada_w"][l, :, n * 512:(n + 1) * 512].rearrange("(kc p) j -> p kc j", p=128))
        for s in range(2):
            pso = G.psf[:, (n % 2 * 2 + s) * 512:(n % 2 * 2 + s + 1) * 512]
            for kc in range(8):
                P.mm(pso, csil[:, s, kc:kc + 1].to_broadcast([128, 128]), wst[:, b, kc, :], start=(kc == 0), stop=False)
            P.mm(pso, G.onesf[0:1, :], brow[0:1, n * 512:(n + 1) * 512], start=False, stop=True)
            which = n // 2
            if which in (1, 4):
                P.act(mst[:, b, s, :], pso, AF.Identity, bias=1.0)
            else:
                P.copy(mst[:, b, s, :], pso)
            P.dma("sync", G.modd[s, which, :, (n % 2) * 512:(n % 2 + 1) * 512], mst[:, b, s, :])
    ph.close()


def phase_inproj(nc, P, G, l):
    ph = Phase(nc, P)
    winb = ph.sb("winb", [128, 8, NIN], BF16)
    wst = ph.sb("wst", [128, 2, 8, 512])
    msc = ph.sb("msc", [128, 2, 1024]); msh = ph.sb("msh", [128, 2, 1024])
    xt = ph.sb("xt", [128, 2, 1024]); ub = ph.sb("ub", [128, 2, 1024], BF16)
    uT = ph.sb("uT", [128, 2, 8, 128], BF16); zt = ph.sb("zt", [128, 2, NIN])
    st6 = ph.sb("st6", [128, 2, 6]); mv = ph.sb("mv", [128, 2]); rstd = ph.sb("rstd", [128, 1])
    for n in range(7):
        w = min(512, NIN - n * 512)
        P.dma("sync", wst[:, n % 2, :, 0:w], G.w["w_in"][l, :, n * 512:n * 512 + w].rearrange("(kc p) j -> p kc j", p=128))
        P.copy(winb[:, :, n * 512:n * 512 + w], wst[:, n % 2, :, 0:w], eng="gpsimd")
    for s in range(2):
        P.dma("sync", msc[:, s, :], G.modd[s, 1])
        P.dma("sync", msh[:, s, :], G.modd[s, 0])
    for i in range(NT):
        b = i % 2
        s = seg_of(i)
        x = xt[:, b, :]
        P.dma("sync", x, G.xres[i * 128:(i + 1) * 128, :])
        ln_stats(P, ph, x, "a", st6[:], mv[:], rstd[:])
        P.ts(x, x, mv[:, 0:1], ALU.subtract, rstd[:, 0:1], ALU.mult)
        P.tt(x, x, msc[:, s, :], ALU.mult, eng="gpsimd")
        P.tt(ub[:, b, :], x, msh[:, s, :], ALU.add)
        if G.dbg is not None and "u" in G.dbg:
            P.dma("sync", G.dbg["u"][i * 128:(i + 1) * 128, :], ub[:, b, :])
        for kc in range(8):
            P.tr(G.psb[:, kc * 128:(kc + 1) * 128], ub[:, b, kc * 128:(kc + 1) * 128], G.identb[:])
        P.copy(uT[:, b].rearrange("p a b -> p (a b)"), G.psb[:, 0:1024], eng="scalar")
        for n in range(7):
            w = min(512, NIN - n * 512)
            pso = G.psf[:, (n % 4) * 512:(n % 4) * 512 + w]
            for kc in range(8):
                P.mm(pso, uT[:, b, kc, :], winb[:, kc, n * 512:n * 512 + w], start=(kc == 0), stop=(kc == 7))
            P.copy(zt[:, b, n * 512:n * 512 + w], pso, eng=("scalar" if n % 2 else "vector"))
        P.dma("sync", G.zin[i * 128:(i + 1) * 128, :], zt[:, b, :])
    ph.close()
def dir_order(d):
    if d == 0:
        return list(range(NT))
    return [1, 0] + list(range(NT - 1, 1, -1))


def load_shift(P, dst, src2d, i, off, eng_ms="gpsimd"):
    lo, hi = (0, 256) if i < 2 else (256, T_ALL)
    r0 = i * 128 + off
    a = max(r0, lo); bnd = min(r0 + 128, hi)
    if a > r0:
        n = a - r0
        if n >= 128:
            P.memset(dst, 0.0, eng=eng_ms); return
        P.memset(dst[0:((n + 31) // 32) * 32, :], 0.0, eng=eng_ms)
    if bnd < r0 + 128:
        n = r0 + 128 - bnd
        if n >= 128:
            P.memset(dst, 0.0, eng=eng_ms); return
        st = ((128 - n) // 32) * 32
        P.memset(dst[st:128, :], 0.0, eng=eng_ms)
    P.dma("sync", dst[a - r0:bnd - r0, :], src2d[a:bnd, :])


def phase_ssd(nc, P, G, l):
    ssd_prep(nc, P, G, l); ssd_dir(nc, P, G, l); ssd_fin(nc, P, G, l)


def ssd_prep(nc, P, G, l):
    W = G.w
    ph = Phase(nc, P)
    cw = ph.sb("cw", [128, 3, 1024]); cb = ph.sb("cb", [128, 1024])
    dtb = ph.sb("dtb", [128, 16]); nega = ph.sb("nega", [128, 16])
    xc = ph.sb("xc", [128, 2, 1024]); xl = ph.sb("xl", [128, 2, 1024]); xr = ph.sb("xr", [128, 2, 1024])
    acc = ph.sb("acc", [128, 2, 1024]); dl = ph.sb("dl", [128, 2, 32]); dr = ph.sb("dr", [128, 2, 16])
    for j in range(3):
        P.dma("sync", cw[:, j, :], bc_rows(W["ssd_conv_w"][l, j:j + 1, :]))
    P.dma("sync", cb[:], bc_rows(W["ssd_conv_b"][l:l + 1, :]))
    P.dma("sync", dtb[:], bc_rows(W["ssd_dt_bias"][l:l + 1].rearrange("a b c -> a (b c)")))
    P.dma("sync", nega[:], bc_rows(W["ssd_a_log"][l:l + 1].rearrange("a b c -> a (b c)")))
    P.act(nega[:], nega[:], AF.Exp)
    P.ts(nega[:], nega[:], -1.0, ALU.mult)
    zx = G.zin[:, C_XBC:C_XBC + 1024]
    for i in range(NT):
        b = i % 2
        rows = slice(i * 128, (i + 1) * 128)
        P.dma("sync", xc[:, b, :], zx[rows, :])
        load_shift(P, xl[:, b, :], zx, i, -1)
        load_shift(P, xr[:, b, :], zx, i, +1)
        P.dma("sync", dr[:, b, :], G.zin[rows, C_DT:C_DT + 16])
        a = acc[:, b, :]
        P.tt(a, xc[:, b, :], cw[:, 1, :], ALU.mult)
        P.tt(xl[:, b, :], xl[:, b, :], cw[:, 0, :], ALU.mult, eng="gpsimd")
        P.tt(xr[:, b, :], xr[:, b, :], cw[:, 2, :], ALU.mult, eng="gpsimd")
        P.tt(a, a, xl[:, b, :], ALU.add)
        P.tt(a, a, xr[:, b, :], ALU.add)
        P.tt(a, a, cb[:], ALU.add)
        P.act(a, a, AF.Silu)
        P.dma("sync", G.xbca[rows, :], a)
        P.tt(dr[:, b, :], dr[:, b, :], dtb[:], ALU.add)
        P.act(dr[:, b, :], dr[:, b, :], AF.Exp)
        P.act(dl[:, b, 0:16], dr[:, b, :], AF.Ln, bias=1.0)
        P.tt(dl[:, b, 16:32], dl[:, b, 0:16], nega[:], ALU.mult)
        P.dma("sync", G.dtla[rows, :], dl[:, b, :])
    ph.close()


def ssd_dir(nc, P, G, l):
    W = G.w
    ph = Phase(nc, P)
    xa = ph.sb("xa", [128, 2, 1024]); dl = ph.sb("dl2", [128, 2, 32])
    sm = ph.sb("sm", [128, 2, 48])
    R1 = ph.sb("R1", [128, 8, 128]); R3 = ph.sb("R3", [128, 8, 128])
    LT = ph.sb("LT", [128, 8, 128], BF16); MT = ph.sb("MT", [128, 8, 128], BF16)
    bcb = ph.sb("bcb", [128, 2, 512], BF16); bcT = ph.sb("bcT", [128, 2, 4, 128], BF16)
    xdt = ph.sb("xdt", [128, 2, 8, 64], BF16); xdd = ph.sb("xdd", [128, 2, 8, 64], BF16)
    ytmp = ph.sb("ytmp", [128, 8, 64]); yo = ph.sb("yo", [128, 2, 512])
    hin32 = ph.sb("hin32", [128, 512]); hinb = ph.sb("hinb", [128, 512], BF16)
    ps = G.psf

    def bank(k, w=512, o=0):
        return ps[:, k * 512 + o:k * 512 + o + w]
    import os
    for d in [int(x) for x in os.environ.get("KDIRS", "01")]:
        P.memset(hin32[:], 0.0)
        P.memset(hinb[:], 0.0)
        tri = G.tri[d]
        import os
        for i in dir_order(d)[:int(os.environ.get("KCH", "99"))]:
            b = i % 2
            rows = slice(i * 128, (i + 1) * 128)
            P.dma("sync", xa[:, b, :], G.xbca[rows, :])
            P.dma("sync", dl[:, b, :], G.dtla[rows, :])
            dt_d = dl[:, b, d * 8:(d + 1) * 8]
            la_d = dl[:, b, 16 + d * 8:16 + (d + 1) * 8]
            S = sm[:, b, :]
            acs_ps = bank(2, 8, 256); tot_ps = bank(2, 8, 264)
            P.mm(acs_ps, tri[:], la_d)
            P.mm(tot_ps, G.onesf[:], la_d)
            P.copy(S[:, 0:8], acs_ps)
            P.act(S[:, 8:16], acs_ps, AF.Exp)
            P.tt(S[:, 40:48], tot_ps, S[:, 0:8], ALU.subtract)
            P.act(S[:, 16:24], S[:, 40:48], AF.Exp)
            P.act(S[:, 24:32], tot_ps, AF.Exp)
            P.tt(S[:, 32:40], dt_d, S[:, 16:24], ALU.mult)
            if G.stop <= 1: continue
            P.tt(R1[:], tri[:].unsqueeze(1).to_broadcast([128, 8, 128]), la_d.unsqueeze(2).to_broadcast([128, 8, 128]),
                 ALU.mult, eng="gpsimd")
            P.act(R3[:], la_d.unsqueeze(2).to_broadcast([128, 8, 128]), AF.Identity, scale=-1.0)
            for g in range(2):
                sp = bank(g)
                P.mm(sp, G.onesf[:], R1[:, 4 * g:4 * g + 4, :], start=True, stop=False)
                P.mm(sp, tri[:], R3[:, 4 * g:4 * g + 4, :], start=False, stop=False)
                P.mm(sp, G.identf[:], G.negm[d][:], start=False, stop=True)
                P.act(LT[:, 4 * g:4 * g + 4, :], sp, AF.Exp)
            if G.stop <= 2: continue
            P.copy(bcb[:, b, :], xa[:, b, 512:1024])
            for q in range(4):
                P.tr(G.psb[:, q * 128:(q + 1) * 128], bcb[:, b, q * 128:(q + 1) * 128], G.identb[:])
            P.copy(bcT[:, b].rearrange("p a b -> p (a b)"), G.psb[:, 0:512], eng="scalar")
            if G.stop <= 3: continue
            xs3 = xa[:, b, 0:512].rearrange("p (h q) -> p h q", h=8)
            sk = os.environ.get("KSKIP", "")
            if "a" not in sk:
                P.tt(xdt[:, b], xs3, dt_d.unsqueeze(2).to_broadcast([128, 8, 64]), ALU.mult)
            if "b" not in sk:
                P.tt(xdd[:, b], xs3, S[:, 32:40].unsqueeze(2).to_broadcast([128, 8, 64]), ALU.mult)
            if G.stop <= 3.3: continue
            for g in range(2):
                cbp = bank(2, 128, g * 128)
                P.mm(cbp, bcT[:, b, g, :], bcT[:, b, 2 + g, :])
                if G.stop <= 3.6: continue
                P.tt(MT[:, 4 * g:4 * g + 4, :], LT[:, 4 * g:4 * g + 4, :], cbp.unsqueeze(1).to_broadcast([128, 4, 128]), ALU.mult)
            if G.stop <= 4: continue
            for h in range(8):
                P.mm(bank(3, 64, h * 64), MT[:, h, :], xdt[:, b, h, :])
            for g in range(2):
                P.mm(bank(4, 256, g * 256), bcT[:, b, 2 + g, :], hinb[:, g * 256:(g + 1) * 256])
            P.tt(ytmp[:], bank(4).rearrange("p (h q) -> p h q", h=8), S[:, 8:16].unsqueeze(2).to_broadcast([128, 8, 64]), ALU.mult)
            P.tt(yo[:, b, :], ytmp[:].rearrange("p h q -> p (h q)"), bank(3), ALU.add)
            P.dma("sync", G.ydir[d, rows, :], yo[:, b, :])
            if G.stop <= 5: continue
            for g in range(2):
                P.mm(bank(5, 256, g * 256), bcb[:, b, g * 128:(g + 1) * 128], xdd[:, b, 4 * g:4 * g + 4, :])
            h3 = hin32[:].rearrange("p (h q) -> p h q", h=8)
            P.tt(h3, h3, S[:, 24:32].unsqueeze(2).to_broadcast([128, 8, 64]), ALU.mult)
            P.tt(hin32[:], hin32[:], bank(5), ALU.add)
            P.copy(hinb[:], hin32[:], eng="scalar")
    ph.close()


def ssd_fin(nc, P, G, l):
    W = G.w
    ph = Phase(nc, P)
    dsm = ph.sb("dsm", [128, 8]); dex = ph.sb("dex", [128, 8, 64]); nw = ph.sb("nw", [128, 512])
    yf = ph.sb("yf", [128, 2, 512]); yb = ph.sb("yb", [128, 2, 512]); xs = ph.sb("xs", [128, 2, 512]); z = ph.sb("z", [128, 2, 512])
    junk = ph.sb("junk", [128, 256]); ss = ph.sb("ss", [128, 2, 2]); o = ph.sb("o", [128, 2, 512])
    P.dma("sync", dsm[:], bc_rows(W["ssd_d"][l:l + 1, :]))
    P.copy(dex[:], dsm[:].unsqueeze(2).to_broadcast([128, 8, 64]))
    P.dma("sync", nw[:], bc_rows(W["ssd_norm_w"][l:l + 1, :]))
    dexf = dex[:].rearrange("p h q -> p (h q)")
    for i in range(NT):
        b = i % 2
        rows = slice(i * 128, (i + 1) * 128)
        P.dma("sync", yf[:, b, :], G.ydir[0, rows, :])
        P.dma("sync", yb[:, b, :], G.ydir[1, rows, :])
        P.dma("sync", xs[:, b, :], G.xbca[rows, 0:512])
        P.dma("sync", z[:, b, :], G.zin[rows, C_Z:C_Z + 512])
        y = yf[:, b, :]
        P.tt(y, y, yb[:, b, :], ALU.add)
        P.tt(xs[:, b, :], xs[:, b, :], dexf, ALU.mult, eng="gpsimd")
        P.tt(y, y, xs[:, b, :], ALU.add)
        P.act(z[:, b, :], z[:, b, :], AF.Silu)
        P.tt(y, y, z[:, b, :], ALU.mult)
        for g in range(2):
            P.act(junk[:], y[:, g * 256:(g + 1) * 256], AF.Square, accum_out=ss[:, b, g:g + 1])
        P.act(ss[:, b, :], ss[:, b, :], AF.Sqrt, bias=1e-5, scale=1.0 / 256)
        P.op("vector", lambda e, b=b: e.reciprocal(ss[:, b, :], ss[:, b, :]), reads=[ss[:, b, :]], writes=[ss[:, b, :]])
        for g in range(2):
            P.stt(o[:, b, g * 256:(g + 1) * 256], y[:, g * 256:(g + 1) * 256], ss[:, b, g:g + 1], nw[:, g * 256:(g + 1) * 256],
                  ALU.mult, ALU.mult)
        P.dma("sync", G.ymix[rows, 0:512], o[:, b, :])
    ph.close()
def phase_gla(nc, P, G, l):
    gla_prep(nc, P, G, l); gla_dir(nc, P, G, l); gla_fin(nc, P, G, l)


def gla_prep(nc, P, G, l):
    W = G.w
    ph = Phase(nc, P)
    GU = ph.sb("GU", [32, 256]); gbrow = ph.sb("gbrow", [1, 256])
    gd = ph.sb("gd", [128, 2, 32]); gdT = ph.sb("gdT", [32, 2, 128]); e = ph.sb("ge", [128, 2, 256])
    P.memset(GU[:], 0.0)
    P.dma("sync", GU[0:16, 0:128], W["gla_gu"][l, 0])
    P.dma("sync", GU[16:32, 128:256], W["gla_gu"][l, 1])
    P.dma("sync", gbrow[:], W["gla_gb"][l:l + 1].rearrange("a b c -> a (b c)"))
    for i in range(NT):
        b = i % 2
        rows = slice(i * 128, (i + 1) * 128)
        P.dma("sync", gd[:, b, :], G.zin[rows, C_GD:C_GD + 32])
        tp = G.psf[0:32, 0:128]
        P.tr(tp, gd[:, b, :], G.identf[:])
        P.copy(gdT[:, b, :], tp)
        gp = G.psf[:, 512:768]
        P.mm(gp, gdT[:, b, :], GU[:], start=True, stop=False)
        P.mm(gp, G.onesf[0:1, :], gbrow[:], start=False, stop=True)
        P.act(e[:, b, :], gp, AF.Exp, scale=-1.0)
        P.act(e[:, b, :], e[:, b, :], AF.Ln, bias=1.0)
        P.ts(e[:, b, :], e[:, b, :], -1.0 / 16.0, ALU.mult)
        P.dma("sync", G.gla_la[rows, :], e[:, b, :])
    ph.close()


def gla_dir(nc, P, G, l):
    ph = Phase(nc, P)
    qk = ph.sb("qk", [128, 2, 256]); v = ph.sb("gv", [128, 2, 256]); la = ph.sb("gla", [128, 2, 128])
    acs = ph.sb("gacs", [128, 128]); ee = ph.sb("gee", [128, 3, 128]); ecd = ph.sb("gecd", [128, 1])
    qkt = ph.sb("qkt", [128, 2, 128], BF16); kdec = ph.sb("kdec", [128, 128], BF16)
    qkT = ph.sb("qkT", [128, 2, 128], BF16); smb = ph.sb("smb", [128, 4, 128], BF16); vb = ph.sb("vb", [128, 256], BF16)
    kTm = ph.sb("kTm", [128, 4, 128], BF16); qTm = ph.sb("qTm", [128, 4, 128], BF16)
    stm = ph.sb("stm", [128, 4, 64]); sred = ph.sb("sred", [128, 64])
    ot = ph.sb("got", [128, 2, 256]); S32 = ph.sb("S32", [128, 64]); Sb = ph.sb("Sb", [128, 64], BF16)
    ps = G.psf

    def bank(k, w=512, o=0):
        return ps[:, k * 512 + o:k * 512 + o + w]
    for d in range(2):
        P.memset(S32[:], 0.0)
        P.memset(Sb[:], 0.0)
        tri = G.tri[d]
        for i in dir_order(d):
            b = i % 2
            rows = slice(i * 128, (i + 1) * 128)
            P.dma("sync", qk[:, b, :], G.zin[rows, C_GQ:C_GQ + 256])
            P.dma("sync", v[:, b, :], G.zin[rows, C_GV:C_GV + 256])
            P.dma("sync", la[:, b, :], G.gla_la[rows, d * 128:(d + 1) * 128])
            acs_ps = bank(0, 128, 0); tot_ps = bank(0, 128, 128); totT = bank(0, 2, 256)
            P.mm(acs_ps, tri[:], la[:, b, :])
            P.mm(tot_ps, G.onesf[:], la[:, b, :])
            P.mm(totT, la[:, b, :], G.onesf[:, 0:2])
            P.copy(acs[:], acs_ps, eng="scalar")
            P.act(ee[:, 0, :], acs_ps, AF.Exp)
            P.act(ee[:, 1, :], acs_ps, AF.Exp, scale=-1.0)
            P.tt(ee[:, 2, :], tot_ps, acs[:], ALU.subtract)
            P.act(ee[:, 2, :], ee[:, 2, :], AF.Exp)
            P.act(ecd[:], totT[:, 0:1], AF.Exp)
            P.stt(qkt[:, 0, :], qk[:, b, 0:128], 32 ** -0.5, ee[:, 0, :], ALU.mult, ALU.mult)
            P.tt(qkt[:, 1, :], qk[:, b, 128:256], ee[:, 1, :], ALU.mult)
            P.tt(kdec[:], qk[:, b, 128:256], ee[:, 2, :], ALU.mult, eng="gpsimd")
            P.copy(vb[:], v[:, b, :], eng="gpsimd")
            for j in range(2):
                P.tr(G.psb[:, j * 128:(j + 1) * 128], qkt[:, j, :], G.identb[:])
            P.copy(qkT[:].rearrange("p a b -> p (a b)"), G.psb[:, 0:256], eng="scalar")
            P.tt(kTm[:], qkT[:, 1, :].unsqueeze(1).to_broadcast([128, 4, 128]), G.hm4[:].unsqueeze(2).to_broadcast([128, 4, 128]), ALU.mult)
            P.tt(qTm[:], qkT[:, 0, :].unsqueeze(1).to_broadcast([128, 4, 128]), G.hm4[:].unsqueeze(2).to_broadcast([128, 4, 128]), ALU.mult, eng="gpsimd")
            for h in range(4):
                P.mm(bank(1, 128, h * 128), kTm[:, h, :], qkT[:, 0, :])
            P.tt(smb[:], bank(1).rearrange("p (h q) -> p h q", h=4), G.tri[d][:].unsqueeze(1).to_broadcast([128, 4, 128]), ALU.mult)
            for h in range(4):
                hs = slice(32 * h, 32 * h + 32)
                o_ps = bank(2, 64, h * 64)
                P.mm(o_ps, smb[:, h, :], vb[:, h * 64:(h + 1) * 64], start=True, stop=False)
                P.mm(o_ps, qTm[:, h, :], Sb[:], start=False, stop=True)
            P.copy(ot[:, b, :], bank(2, 256, 0), eng="scalar")
            P.dma("sync", G.gla_o[d, rows, :], ot[:, b, :])
            P.mm(bank(3, 256, 0), kdec[:], vb[:])
            P.tt(stm[:], bank(3, 256, 0).rearrange("p (h q) -> p h q", h=4), G.hm4[:].unsqueeze(2).to_broadcast([128, 4, 64]), ALU.mult)
            P.op("vector", lambda e: e.tensor_reduce(sred[:], stm[:].rearrange("p h q -> p q h"), AX.X, ALU.add),
                 reads=[stm[:]], writes=[sred[:]])
            P.stt(S32[:], S32[:], ecd[:, 0:1], sred[:], ALU.mult, ALU.add)
            P.copy(Sb[:], S32[:])
    ph.close()


def gla_fin(nc, P, G, l):
    W = G.w
    ph = Phase(nc, P)
    nw = ph.sb("gnw", [128, 256]); of = ph.sb("gof", [128, 2, 256]); ob = ph.sb("gob", [128, 2, 256]); og = ph.sb("gog", [128, 2, 256])
    sq = ph.sb("gsq", [128, 256]); ss = ph.sb("gss", [128, 2, 4]); y = ph.sb("gy", [128, 2, 256])
    P.dma("sync", nw[:], bc_rows(W["gla_norm_w"][l:l + 1, :]))
    for i in range(NT):
        b = i % 2
        rows = slice(i * 128, (i + 1) * 128)
        P.dma("sync", of[:, b, :], G.gla_o[0, rows, :])
        P.dma("sync", ob[:, b, :], G.gla_o[1, rows, :])
        P.dma("sync", og[:, b, :], G.zin[rows, C_OG:C_OG + 256])
        o = of[:, b, :]
        P.tt(o, o, ob[:, b, :], ALU.add)
        P.tt(sq[:], o, o, ALU.mult, eng="gpsimd")
        P.op("vector", lambda e, b=b: e.tensor_reduce(ss[:, b, :], sq[:].rearrange("p (h q) -> p h q", h=4), AX.X, ALU.add),
             reads=[sq[:]], writes=[ss[:, b, :]])
        P.act(ss[:, b, :], ss[:, b, :], AF.Sqrt, bias=1e-5, scale=1.0 / 64)
        P.op("vector", lambda e, b=b: e.reciprocal(ss[:, b, :], ss[:, b, :]), reads=[ss[:, b, :]], writes=[ss[:, b, :]])
        y3 = y[:, b, :].rearrange("p (h q) -> p h q", h=4)
        P.tt(y3, o.rearrange("p (h q) -> p h q", h=4), ss[:, b, :].unsqueeze(2).to_broadcast([128, 4, 64]), ALU.mult)
        P.tt(y[:, b, :], y[:, b, :], nw[:], ALU.mult, eng="gpsimd")
        P.act(og[:, b, :], og[:, b, :], AF.Silu)
        P.tt(y[:, b, :], y[:, b, :], og[:, b, :], ALU.mult)
        P.dma("sync", G.ymix[rows, 768:1024], y[:, b, :])
    ph.close()
def phase_rwkv(nc, P, G, l):
    rwkv_prep(nc, P, G, l); rwkv_dir(nc, P, G, l); rwkv_fin(nc, P, G, l)


def rwkv_prep(nc, P, G, l):
    W = G.w
    ph = Phase(nc, P)
    mu = ph.sb("mu", [128, 4, 1152])
    w2a2 = ph.sb("w2a2", [128, 2, 256]); g2 = ph.sb("g2", [128, 256]); brow = ph.sb("rbrow", [1, 4, 256])
    kkw = ph.sb("kkw", [128, 256]); ka = ph.sb("ka", [128, 256]); rk = ph.sb("rk", [128, 256])
    c = ph.sb("rc", [128, 2, 1152]); nb = ph.sb("rnb", [128, 4, 1152])
    tws = ph.sb("tws", [128, 3, 128]); twT = ph.sb("twT", [128, 3, 128])
    pk = ph.sb("pk", [128, 2, 11, 256]); tmp = ph.sb("rtmp", [128, 3, 256]); s4 = ph.sb("rs4", [128, 8])
    for j in range(4):
        P.dma("sync", mu[:, j, :], bc_rows(W["rwkv_mu"][l, j:j + 1, :]))
    for d in range(2):
        P.dma("sync", w2a2[64 * d:64 * d + 64, 0, :], W["rwkv_w2"][l, d])
        P.dma("sync", w2a2[64 * d:64 * d + 64, 1, :], W["rwkv_a2"][l, d])
    P.dma("sync", g2[:], W["rwkv_g2"][l])
    P.dma("sync", brow[0:1, 0:2, :], W["rwkv_w0"][l:l + 1])
    P.dma("sync", brow[0:1, 2:4, :], W["rwkv_a0"][l:l + 1])
    P.dma("sync", kkw[:], bc_rows(W["rwkv_kk"][l:l + 1, :]))
    P.dma("sync", ka[:], bc_rows(W["rwkv_ka"][l:l + 1, :]))
    P.dma("sync", rk[:], bc_rows(W["rwkv_rk"][l:l + 1].rearrange("a b c -> a (b c)")))
    zr = G.zin[:, C_RW:C_RW + 1152]
    for i in range(NT):
        b = i % 2
        rows = slice(i * 128, (i + 1) * 128)
        lat = i >= 2
        cc = c[:, b, :]
        P.dma("sync", cc, zr[rows, :])
        offs = [-1, 1, -64, 64] if lat else [-1, 1]
        for j, off in enumerate(offs):
            X = nb[:, j, :]
            load_shift(P, X, zr, i, off)
            if lat and j < 2:
                P.stt(X, X, G.cmask[:, j:j + 1], cc, ALU.mult, ALU.subtract, eng=("vector"))
            else:
                P.tt(X, X, cc, ALU.subtract, eng="gpsimd")
            P.tt(X, X, mu[:, j, :], ALU.mult, eng=("gpsimd" if j % 2 else "vector"))
        for j in range(len(offs)):
            P.tt(cc, cc, nb[:, j, :], ALU.add)
        PK = pk[:, b]
        P.act(tws[:, 0, :], cc[:, 768:896], AF.Tanh)
        P.copy(tws[:, 1, :], cc[:, 896:1024], eng="gpsimd")
        P.act(tws[:, 2, :], cc[:, 1024:1152], AF.Sigmoid)
        for q in range(3):
            P.tr(G.psf[:, q * 128:(q + 1) * 128], tws[:, q, :], G.identf[:])
        P.copy(twT[:].rearrange("p a b -> p (a b)"), G.psf[:, 0:384])
        for d in range(2):
            ds = slice(64 * d, 64 * d + 64)
            xp = G.psf[:, (1 + 2 * d) * 512:(1 + 2 * d) * 512 + 256]
            P.mm(xp, twT[ds, 0, :], w2a2[ds, 0, :], start=True, stop=False)
            P.mm(xp, G.onesf[0:1, :], brow[0:1, d, :], start=False, stop=True)
            ap_ = G.psf[:, (1 + 2 * d) * 512 + 256:(1 + 2 * d) * 512 + 512]
            P.mm(ap_, twT[ds, 1, :], w2a2[ds, 1, :], start=True, stop=False)
            P.mm(ap_, G.onesf[0:1, :], brow[0:1, 2 + d, :], start=False, stop=True)
        gp = G.psf[:, 2048:2304]
        P.mm(gp, twT[:, 2, :], g2[:])
        for d in range(2):
            P.act(PK[:, 7 + d, :], G.psf[:, (1 + 2 * d) * 512:(1 + 2 * d) * 512 + 256], AF.Sigmoid)
        P.ts(PK[:, 7:9, :], PK[:, 7:9, :], -0.6065306597126334, ALU.mult, eng="gpsimd")
        adir = tmp[:, 0:2, :]
        for d in range(2):
            P.act(adir[:, d, :], G.psf[:, (1 + 2 * d) * 512 + 256:(1 + 2 * d) * 512 + 512], AF.Sigmoid)
        P.copy(PK[:, 9, :], gp, eng="scalar")
        P.copy(PK[:, 0, :], cc[:, 0:256], eng="gpsimd")
        P.copy(PK[:, 1, :], cc[:, 512:768], eng="gpsimd")
        kx = cc[:, 256:512]
        kk = PK[:, 2, :]
        P.tt(kk, kx, kkw[:], ALU.mult)
        P.tt(tmp[:, 2, :], kk, kk, ALU.mult, eng="gpsimd")
        P.op("vector", lambda e: e.tensor_reduce(s4[:, 0:4], tmp[:, 2, :].rearrange("p (h q) -> p h q", h=4), AX.X, ALU.add),
             reads=[tmp[:, 2, :]], writes=[s4[:, 0:4]])
        P.act(s4[:, 0:4], s4[:, 0:4], AF.Sqrt)
        P.ts(s4[:, 0:4], s4[:, 0:4], 1e-12, ALU.max)
        P.op("vector", lambda e: e.reciprocal(s4[:, 0:4], s4[:, 0:4]), reads=[s4[:, 0:4]], writes=[s4[:, 0:4]])
        kk3 = kk.rearrange("p (h q) -> p h q", h=4)
        P.tt(kk3, kk3, s4[:, 0:4].unsqueeze(2).to_broadcast([128, 4, 64]), ALU.mult)
        for d in range(2):
            t1 = tmp[:, 2, :]
            P.stt(t1, adir[:, d, :], -1.0, ka[:], ALU.add, ALU.mult)
            P.stt(PK[:, 3 + d, :], t1, 1.0, kx, ALU.add, ALU.mult)
            P.tt(PK[:, 5 + d, :], kk, adir[:, d, :], ALU.mult, eng="gpsimd")
        t1 = tmp[:, 2, :]
        P.tt(t1, PK[:, 3, :], PK[:, 4, :], ALU.add)
        P.tt(t1, t1, PK[:, 0, :], ALU.mult)
        P.tt(t1, t1, rk[:], ALU.mult)
        P.op("vector", lambda e: e.tensor_reduce(s4[:, 4:8], tmp[:, 2, :].rearrange("p (h q) -> p h q", h=4), AX.X, ALU.add),
             reads=[tmp[:, 2, :]], writes=[s4[:, 4:8]])
        P.tt(PK[:, 10, :].rearrange("p (h q) -> p h q", h=4), PK[:, 1, :].rearrange("p (h q) -> p h q", h=4),
             s4[:, 4:8].unsqueeze(2).to_broadcast([128, 4, 64]), ALU.mult)
        P.dma("sync", G.rwp[rows, :], PK.rearrange("p a b -> p (a b)"))
    ph.close()


def rwkv_dir(nc, P, G, l):
    ph = Phase(nc, P)
    ld = ph.sb("rld", [128, 2, 6, 256])
    ee = ph.sb("ree", [128, 3, 256])
    tok = ph.sb("rtok", [128, 4, 256])
    fmh = ph.sb("fmh", [64, 4, 4, 128])
    Aarb = ph.sb("Aarb", [128, 4, 128]); Aak = ph.sb("Aak", [128, 4, 128]); Aark = ph.sb("Aark", [128, 4, 128])
    Q = ph.sb("rQ", [128, 2, 4, 128]); QT = ph.sb("rQT", [128, 2, 4, 128]); IQT = ph.sb("rIQT", [128, 4, 128])
    Z = ph.sb("rZ", [128, 2, 4, 128])
    Wsb = ph.sb("rW", [128, 4, 64]); nAkV = ph.sb("nAkV", [128, 4, 64]); Uloc = ph.sb("Uloc", [128, 4, 64])
    GpT = ph.sb("GpT", [64, 4, 2, 64]); RwT = ph.sb("RwT", [64, 4, 128]); gC = ph.sb("gC", [64, 4, 2])
    H32 = ph.sb("H32", [64, 4, 64]); Yt = ph.sb("Yt", [64, 2, 2, 256])
    btc = ph.sb("btc", [128, 2, 256]); ktc = ph.sb("ktc", [128, 2, 256])
    ps = G.psf

    def bank(k, w=512, o=0, p0=0, p1=128):
        return ps[p0:p1, k * 512 + o:k * 512 + o + w]
    for d in range(2):
        P.memset(H32[:], 0.0)
        BD = G.IU if d == 0 else G.IL
        m_abT = G.nSU if d == 0 else G.nSL
        m_ab = G.nSL if d == 0 else G.nSU
        m_s = G.SU if d == 0 else G.SL
        m_i = G.IU if d == 0 else G.IL
        import os
        for i in dir_order(d)[:int(os.environ.get("KCH", "99"))]:
            b = i % 2
            rows = slice(i * 128, (i + 1) * 128)
            L = ld[:, b]
            for q, col in enumerate([0, 1, 2, 3 + d, 5 + d, 7 + d]):
                P.dma("sync", L[:, q, :], G.rwp[rows, col * 256:(col + 1) * 256])
            r_, v_, kk_, k_, b_, lw_ = [L[:, q, :] for q in range(6)]
            cs_ps = bank(5, 256)
            P.mm(cs_ps, BD[:], lw_)
            P.act(ee[:, 0, :], cs_ps, AF.Exp)
            P.act(ee[:, 1, :], cs_ps, AF.Exp, scale=-1.0)
            P.copy(ee[:, 2, :], cs_ps, eng="scalar")
            P.tt(ee[:, 2, :], ee[:, 2, :], lw_, ALU.subtract)
            P.act(ee[:, 2, :], ee[:, 2, :], AF.Exp)
            P.tt(tok[:, 0, :], kk_, ee[:, 2, :], ALU.mult)
            P.tt(tok[:, 1, :], r_, ee[:, 0, :], ALU.mult, eng="gpsimd")
            P.tt(tok[:, 2, :], b_, ee[:, 1, :], ALU.mult)
            P.tt(tok[:, 3, :], k_, ee[:, 1, :], ALU.mult, eng="gpsimd")
            if G.stop <= 1: continue
            for c in range(2):
                P.ts(btc[:, c, :], tok[:, 2, :], G.cind[:, c:c + 1], ALU.mult, eng=("gpsimd" if c else "vector"))
                P.ts(ktc[:, c, :], tok[:, 3, :], G.cind[:, c:c + 1], ALU.mult, eng=("gpsimd" if c else "vector"))
            for rnd in range(2):
                for qq in range(2):
                    q = rnd * 2 + qq
                    for h in range(4):
                        P.tr(bank(rnd * 2 + qq, 128, h * 128, 0, 64), tok[:, q, h * 64:(h + 1) * 64], G.identf[:])
                P.copy(fmh[:, rnd * 2:rnd * 2 + 2].rearrange("p a h t -> p (a h t)"), ps[0:64, rnd * 1024:(rnd + 1) * 1024],
                       eng=("scalar" if rnd else "vector"))
            if G.stop <= 2: continue
            for h in range(4):
                P.mm(bank(5, 2, 256 + 2 * h, 0, 64), lw_[:, h * 64:(h + 1) * 64], G.cind[:])
            P.act(gC[:].rearrange("p h c -> p (h c)"), bank(5, 8, 256, 0, 64), AF.Exp)
            if G.stop <= 3: continue
            for h in range(4):
                khT = fmh[:, 0, h, :]; rtT = fmh[:, 1, h, :]; btT = fmh[:, 2, h, :]; ktT = fmh[:, 3, h, :]
                P.mm(bank(0, 128, h * 128), btT, khT)
                P.mm(bank(1, 128, h * 128), btT, rtT)
                P.mm(bank(2, 128, h * 128), ktT, khT)
                P.mm(bank(3, 128, h * 128), ktT, rtT)
                P.mm(bank(4, 128, h * 128), khT, btT)

            def b4(k):
                return bank(k).rearrange("p (h q) -> p h q", h=4)

            def bc4(m):
                return m[:].unsqueeze(1).to_broadcast([128, 4, 128])
            idb4 = G.identf[:].unsqueeze(1).to_broadcast([128, 4, 128])
            P.tt(Q[:, 0], b4(0), bc4(m_abT), ALU.mult)
            P.tt(QT[:, 0], b4(4), bc4(m_ab), ALU.mult)
            P.tt(Aarb[:], b4(1), bc4(m_i), ALU.mult)
            P.tt(Aak[:], b4(2), bc4(m_s), ALU.mult)
            P.tt(Aark[:], b4(3), bc4(m_i), ALU.mult)
            P.tt(IQT[:], QT[:, 0], idb4, ALU.add, eng="gpsimd")
            P.tt(Z[:, 0], Q[:, 0], idb4, ALU.add, eng="gpsimd")
            if G.stop <= 4: continue
            zc = 0
            for k in range(1, 6):
                a, bq = (k - 1) % 2, k % 2
                for h in range(4):
                    if k < 5:
                        P.mm(bank(0, 128, h * 128), QT[:, a, h, :], Q[:, a, h, :])
                    P.mm(bank(1, 128, h * 128), Q[:, a, h, :], QT[:, a, h, :])
                if k >= 2:
                    for h in range(4):
                        P.mm(bank(2, 128, h * 128), IQT[:, h, :], Z[:, zc, h, :])
                if k < 5:
                    P.copy(Q[:, bq], b4(0), eng="scalar")
                P.copy(QT[:, bq], b4(1))
                if k >= 2:
                    P.copy(Z[:, 1 - zc], b4(2), eng="scalar")
                    zc = 1 - zc
                P.tt(IQT[:], QT[:, bq], idb4, ALU.add, eng="gpsimd")
            for h in range(4):
                P.mm(bank(2, 128, h * 128), IQT[:, h, :], Z[:, zc, h, :])
            P.copy(Z[:, 1 - zc], b4(2), eng="scalar")
            zc = 1 - zc
            TT = Z[:, zc]
            if G.stop <= 5: continue
            for h in range(4):
                hc = slice(h * 64, (h + 1) * 64)
                P.mm(bank(0, 64, h * 64), TT[:, h, :], tok[:, 0, hc])
                P.mm(bank(1, 64, h * 64), Aak[:, h, :], v_[:, hc])
            P.copy(Wsb[:].rearrange("p h q -> p (h q)"), bank(0, 256), eng="scalar")
            P.ts(nAkV[:].rearrange("p h q -> p (h q)"), bank(1, 256), -1.0, ALU.mult)
            for h in range(4):
                P.mm(bank(2, 64, h * 64), TT[:, h, :], nAkV[:, h, :])
            P.copy(Uloc[:].rearrange("p h q -> p (h q)"), bank(2, 256), eng="scalar")
            if G.stop <= 6: continue
            for h in range(4):
                hc = slice(h * 64, (h + 1) * 64)
                for c in range(2):
                    P.mm(bank(3, 64, (h * 2 + c) * 64, 0, 64), Wsb[:, h, :], btc[:, c, hc])
                P.mm(bank(4, 128, h * 128, 0, 64), Wsb[:, h, :], Aarb[:, h, :])
            id8 = G.identf[0:64, 0:64].unsqueeze(1).to_broadcast([64, 8, 64])
            P.tt(GpT[:].rearrange("p h c q -> p (h c) q"), id8, bank(3, 512, 0, 0, 64).rearrange("p (a q) -> p a q", a=8), ALU.subtract)
            P.tt(RwT[:], fmh[:, 1], bank(4, 512, 0, 0, 64).rearrange("p (h q) -> p h q", h=4), ALU.subtract)
            if G.stop <= 7: continue
            for c in ([0, 1] if d == 0 else [1, 0]):
                cs_ = slice(64 * c, 64 * c + 64)
                for h in range(4):
                    hc = slice(h * 64, (h + 1) * 64)
                    yp = bank(1, 64, (c * 4 + h) * 64, 0, 64)
                    P.mm(yp, Aarb[:, h, cs_], Uloc[:, h, :], start=True, stop=False)
                    P.mm(yp, Aark[:, h, cs_], v_[:, hc], start=False, stop=False)
                    P.mm(yp, RwT[:, h, cs_], H32[:, h, :], start=False, stop=True)
                for h in range(4):
                    hc = slice(h * 64, (h + 1) * 64)
                    hp = bank(0, 64, h * 64, 0, 64)
                    P.mm(hp, btc[:, c, hc], Uloc[:, h, :], start=True, stop=False)
                    P.mm(hp, ktc[:, c, hc], v_[:, hc], start=False, stop=False)
                    P.mm(hp, GpT[:, h, c, :], H32[:, h, :], start=False, stop=True)
                P.tt(H32[:], bank(0, 256, 0, 0, 64).rearrange("p (h q) -> p h q", h=4),
                     gC[:, :, c].unsqueeze(2).to_broadcast([64, 4, 64]), ALU.mult)
            P.copy(Yt[:, b].rearrange("p c q -> p (c q)"), bank(1, 512, 0, 0, 64), eng="scalar")
            P.dma("sync", G.rwy[d, rows, :].rearrange("(c p) q -> p c q", p=64), Yt[:, b])
    ph.close()


def rwkv_fin(nc, P, G, l):
    W = G.w
    ph = Phase(nc, P)
    lw_ = ph.sb("lnxw", [128, 256]); lb_ = ph.sb("lnxb", [128, 256])
    yf = ph.sb("ryf", [128, 2, 256]); yb = ph.sb("ryb", [128, 2, 256]); gb = ph.sb("rgb", [128, 2, 2, 256])
    sq = ph.sb("rsq", [128, 256]); s4 = ph.sb("rfs4", [128, 2, 8])
    P.dma("sync", lw_[:], bc_rows(W["rwkv_lnx_w"][l:l + 1, :]))
    P.dma("sync", lb_[:], bc_rows(W["rwkv_lnx_b"][l:l + 1, :]))
    for i in range(NT):
        b = i % 2
        rows = slice(i * 128, (i + 1) * 128)
        P.dma("sync", yf[:, b, :], G.rwy[0, rows, :])
        P.dma("sync", yb[:, b, :], G.rwy[1, rows, :])
        P.dma("sync", gb[:, b].rearrange("p a b -> p (a b)"), G.rwp[rows, 9 * 256:11 * 256])
        y = yf[:, b, :]
        y3 = y.rearrange("p (h q) -> p h q", h=4)
        S = s4[:, b, :]
        P.tt(y, y, yb[:, b, :], ALU.add)
        P.op("vector", lambda e, y3=y3, S=S: e.tensor_reduce(S[:, 0:4], y3, AX.X, ALU.add), reads=[y], writes=[S[:, 0:4]])
        P.ts(S[:, 0:4], S[:, 0:4], 1.0 / 64, ALU.mult)
        P.tt(y3, y3, S[:, 0:4].unsqueeze(2).to_broadcast([128, 4, 64]), ALU.subtract)
        P.tt(sq[:], y, y, ALU.mult, eng="gpsimd")
        P.op("vector", lambda e, S=S: e.tensor_reduce(S[:, 4:8], sq[:].rearrange("p (h q) -> p h q", h=4), AX.X, ALU.add),
             reads=[sq[:]], writes=[S[:, 4:8]])
        P.act(S[:, 4:8], S[:, 4:8], AF.Sqrt, bias=64e-5, scale=1.0 / 64)
        P.op("vector", lambda e, S=S: e.reciprocal(S[:, 4:8], S[:, 4:8]), reads=[S[:, 4:8]], writes=[S[:, 4:8]])
        P.tt(y3, y3, S[:, 4:8].unsqueeze(2).to_broadcast([128, 4, 64]), ALU.mult)
        P.tt(y, y, lw_[:], ALU.mult, eng="gpsimd")
        P.tt(y, y, lb_[:], ALU.add)
        P.tt(y, y, gb[:, b, 1, :], ALU.add)
        P.tt(y, y, gb[:, b, 0, :], ALU.mult)
        P.dma("sync", G.ymix[rows, 512:768], y)
    ph.close()
def tiles_for(l, n_layers_total=4):
    return list(range(2, NT)) if l == n_layers_total - 1 else list(range(NT))


def phase_outproj(nc, P, G, l):
    W = G.w
    ph = Phase(nc, P)
    woutb = ph.sb("woutb", [128, 8, 1024], BF16); wst = ph.sb("owst", [128, 8, 512])
    rw32 = ph.sb("rw32", [128, 8, 64]); rbias = ph.sb("rbias", [128, 64])
    mg1 = ph.sb("mg1", [128, 2, 1024]); msc2 = ph.sb("msc2", [128, 2, 1024]); msh2 = ph.sb("msh2", [128, 2, 1024])
    l1w = ph.sb("l1w", [128, 1024]); l1b = ph.sb("l1b", [128, 1024])
    ym = ph.sb("oym", [128, 2, 1024]); ymb = ph.sb("oymb", [128, 1024], BF16); ymT = ph.sb("oymT", [128, 8, 128], BF16)
    xt = ph.sb("oxt", [128, 2, 1024]); t = ph.sb("ot", [128, 1024]); hh = ph.sb("ohh", [128, 1024])
    hT32 = ph.sb("hT32", [128, 8, 128]); hTb = ph.sb("hTb", [128, 2, 8, 128], BF16)
    st6 = ph.sb("ost6", [128, 2, 6]); mv = ph.sb("omv", [128, 2]); rstd = ph.sb("orstd", [128, 1])
    sc = ph.sb("rsc", [128, 64]); sel = ph.sb("rsel", [128, 64]); m8 = ph.sb("rm8", [128, 8, 8]); gs = ph.sb("rgs", [128, 8])
    g8 = ph.sb("rg8", [128, 8]); gm = ph.sb("rgm", [128, 8]); pen = ph.sb("rpen", [128, 8]); s8 = ph.sb("rs8", [128, 8])
    selm = ph.sb("rselm", [128, 64]); wts = ph.sb("rwts", [128, 64]); den = ph.sb("rden", [128, 1]); gT = ph.sb("rgT", [64, 2, 128])
    for n in range(2):
        P.dma("sync", wst[:], W["w_out"][l, :, n * 512:(n + 1) * 512].rearrange("(kc p) j -> p kc j", p=128))
        P.copy(woutb[:, :, n * 512:(n + 1) * 512], wst[:], eng="gpsimd")
    P.dma("sync", rw32[:], W["router_w"][l].rearrange("(kc p) e -> p kc e", p=128))
    P.dma("sync", rbias[:], bc_rows(W["router_b"][l:l + 1, :]))
    for s in range(2):
        P.dma("sync", mg1[:, s, :], G.modd[s, 2])
        P.dma("sync", msc2[:, s, :], G.modd[s, 4])
        P.dma("sync", msh2[:, s, :], G.modd[s, 3])
    P.dma("sync", l1w[:], bc_rows(W["ln1_w"][l:l + 1, :]))
    P.dma("sync", l1b[:], bc_rows(W["ln1_b"][l:l + 1, :]))
    for i in tiles_for(l):
        b = i % 2
        s = seg_of(i)
        rows = slice(i * 128, (i + 1) * 128)
        P.dma("sync", ym[:, b, :], G.ymix[rows, :])
        P.dma("sync", xt[:, b, :], G.xres[rows, :])
        P.copy(ymb[:], ym[:, b, :], eng="gpsimd")
        for kc in range(8):
            P.tr(G.psb[:, kc * 128:(kc + 1) * 128], ymb[:, kc * 128:(kc + 1) * 128], G.identb[:])
        P.copy(ymT[:].rearrange("p a b -> p (a b)"), G.psb[:, 0:1024], eng="scalar")
        for n in range(2):
            pso = G.psf[:, n * 512:(n + 1) * 512]
            for kc in range(8):
                P.mm(pso, ymT[:, kc, :], woutb[:, kc, n * 512:(n + 1) * 512], start=(kc == 0), stop=(kc == 7))
            P.tt(t[:, n * 512:(n + 1) * 512], pso, mg1[:, s, n * 512:(n + 1) * 512], ALU.mult)
        x = xt[:, b, :]
        P.stt(t[:], x, DN_ALPHA, t[:], ALU.mult, ALU.add)
        ln_stats(P, ph, t[:], "o1", st6[:], mv[:], rstd[:])
        P.ts(t[:], t[:], mv[:, 0:1], ALU.subtract, rstd[:, 0:1], ALU.mult)
        P.tt(t[:], t[:], l1w[:], ALU.mult, eng="gpsimd")
        P.tt(x, t[:], l1b[:], ALU.add)
        P.dma("sync", G.xres[rows, :], x)
        ln_stats(P, ph, x, "o2", st6[:], mv[:], rstd[:])
        P.ts(hh[:], x, mv[:, 0:1], ALU.subtract, rstd[:, 0:1], ALU.mult)
        P.tt(hh[:], hh[:], msc2[:, s, :], ALU.mult, eng="gpsimd")
        P.tt(hh[:], hh[:], msh2[:, s, :], ALU.add)
        if G.dbg is not None and "hdbg" in G.dbg:
            P.dma("sync", G.dbg["hdbg"][rows, :], hh[:])
        for half in range(2):
            for kk in range(4):
                kc = half * 4 + kk
                P.tr(G.psf[:, (2 + half) * 512 + kk * 128:(2 + half) * 512 + (kk + 1) * 128], hh[:, kc * 128:(kc + 1) * 128], G.identf[:])
            P.copy(hT32[:, half * 4:half * 4 + 4, :].rearrange("p a b -> p (a b)"), G.psf[:, (2 + half) * 512:(3 + half) * 512], eng="scalar")
        P.copy(hTb[:, b], hT32[:], eng="gpsimd")
        P.dma("sync", G.hT[:, :, rows].rearrange("kc p t -> p kc t"), hTb[:, b])
        lg = G.psf[:, 4 * 512:4 * 512 + 64]
        for kc in range(8):
            P.mm(lg, hT32[:, kc, :], rw32[:, kc, :], start=(kc == 0), stop=(kc == 7))
        P.act(sc[:], lg, AF.Sigmoid)
        P.tt(sel[:], sc[:], rbias[:], ALU.add)
        for g in range(8):
            P.op("vector", lambda e, g=g: e.max(m8[:, g, :], sel[:, g * 8:(g + 1) * 8]), reads=[sel[:, g * 8:(g + 1) * 8]], writes=[m8[:, g, :]])
        P.tt(gs[:], m8[:, :, 0], m8[:, :, 1], ALU.add)
        P.op("vector", lambda e: e.max(g8[:], gs[:]), reads=[gs[:]], writes=[g8[:]])
        P.ts(gm[:], gs[:], g8[:, 3:4], ALU.is_ge)
        P.ts(pen[:], gm[:], 1.0, ALU.subtract, 1e30, ALU.mult)
        sel3 = sel[:].rearrange("p (g q) -> p g q", g=8)
        selm3 = selm[:].rearrange("p (g q) -> p g q", g=8)
        P.tt(selm3, sel3, gm[:].unsqueeze(2).to_broadcast([128, 8, 8]), ALU.mult)
        P.tt(selm3, selm3, pen[:].unsqueeze(2).to_broadcast([128, 8, 8]), ALU.add)
        P.op("vector", lambda e: e.max(s8[:], selm[:]), reads=[selm[:]], writes=[s8[:]])
        P.ts(wts[:], selm[:], s8[:, 7:8], ALU.is_ge)
        P.tt(wts[:], wts[:], sc[:], ALU.mult)
        P.op("vector", lambda e: e.tensor_reduce(den[:], wts[:], AX.X, ALU.add), reads=[wts[:]], writes=[den[:]])
        P.op("vector", lambda e: e.reciprocal(den[:], den[:]), reads=[den[:]], writes=[den[:]])
        P.ts(wts[:], wts[:], den[:, 0:1], ALU.mult, 2.5, ALU.mult)
        gtp = G.psf[0:64, 5 * 512:5 * 512 + 128]
        P.tr(gtp, wts[:], G.identf[:])
        P.copy(gT[:, b, :], gtp)
        P.dma("sync", G.gT[:, rows], gT[:, b, :])
    ph.close()


def phase_moe(nc, P, G, l):
    W = G.w
    tl = tiles_for(l)
    half = (len(tl) + 1) // 2
    for (pa, tiles) in enumerate((tl[:half], tl[half:])):
        ph = Phase(nc, P)
        ntl = len(tiles)
        ntok = ntl * 128
        t0 = tiles[0] * 128
        acc = ph.sb("macc", [128, ntl, 1024])
        hTp = ph.sb("mhT", [128, 8, ntok], BF16); gTp = ph.sb("mgT", [64, ntok])
        w13s = ph.sb("w13s", [128, 8, 512]); w13b = ph.sb("w13b", [128, 2, 8, 512], BF16)
        w2s = ph.sb("w2s", [128, 2, 1024]); w2b = ph.sb("w2b", [128, 2, 2, 1024], BF16)
        hid = ph.sb("mhid", [128, 2, 2, 512], BF16); sil = ph.sb("msil", [128, 2, 512])
        P.dma("sync", hTp[:], G.hT[:, :, t0:t0 + ntok].rearrange("kc p t -> p kc t"))
        P.dma("sync", gTp[:], G.gT[:, t0:t0 + ntok])
        groups = []
        c0 = 0
        while c0 < ntok:
            n = min(512, ntok - c0)
            groups.append((c0, n)); c0 += n
        NE = 65
        for e in range(NE):
            eb = e % 2
            if e < 64:
                s1, s3, s2 = W["exp_w1"][l, e], W["exp_w3"][l, e], W["exp_w2"][l, e]
            else:
                s1, s3, s2 = W["sh_w1"][l], W["sh_w3"][l], W["sh_w2"][l]
            P.dma("sync", w13s[:, :, 0:256], s1.rearrange("(kc p) f -> p kc f", p=128))
            P.dma("sync", w13s[:, :, 256:512], s3.rearrange("(kc p) f -> p kc f", p=128))
            P.dma("sync", w2s[:], s2.rearrange("(fc p) c -> p fc c", p=128))
            P.copy(w13b[:, eb], w13s[:], eng="gpsimd")
            P.copy(w2b[:, eb], w2s[:], eng="gpsimd")
            for gi, (c0, n) in enumerate(groups):
                hb = (e * len(groups) + gi) % 2
                cols = slice(c0, c0 + n)
                if e < 64:
                    gb = G.psf[:, 4 * 512:4 * 512 + n]
                    P.mm(gb, G.identf[0:64, e:e + 1].to_broadcast([64, 128]), gTp[:, cols])
                for fc in range(2):
                    h1 = G.psf[:, fc * 512:fc * 512 + n]
                    h3 = G.psf[:, (2 + fc) * 512:(2 + fc) * 512 + n]
                    for kc in range(8):
                        P.mm(h1, w13b[:, eb, kc, fc * 128:(fc + 1) * 128], hTp[:, kc, cols], start=(kc == 0), stop=(kc == 7))
                    for kc in range(8):
                        P.mm(h3, w13b[:, eb, kc, 256 + fc * 128:256 + (fc + 1) * 128], hTp[:, kc, cols], start=(kc == 0), stop=(kc == 7))
                    P.act(sil[:, fc, 0:n], h1, AF.Silu)
                    if e < 64:
                        P.tt(sil[:, fc, 0:n], sil[:, fc, 0:n], h3, ALU.mult)
                        P.tt(hid[:, hb, fc, 0:n], sil[:, fc, 0:n], gb, ALU.mult)
                    else:
                        P.tt(hid[:, hb, fc, 0:n], sil[:, fc, 0:n], h3, ALU.mult)
                for sub in range(n // 128):
                    ti = (c0 // 128) + sub
                    for cc in range(2):
                        o = G.psf[:, 5 * 512:6 * 512]
                        for fc in range(2):
                            P.mm(o, hid[:, hb, fc, sub * 128:(sub + 1) * 128], w2b[:, eb, fc, cc * 512:(cc + 1) * 512],
                                 start=(fc == 0), stop=(fc == 1))
                        a = acc[:, ti, cc * 512:(cc + 1) * 512]
                        if e == 0:
                            P.copy(a, o)
                        else:
                            P.tt(a, a, o, ALU.add)
        mg2 = ph2 = None
        fin = Phase(nc, P)
        mg2 = fin.sb("mg2", [128, 2, 1024]); l2w = fin.sb("l2w", [128, 1024]); l2b = fin.sb("l2b", [128, 1024])
        xt = fin.sb("mxt", [128, 2, 1024]); st6 = fin.sb("mst6", [128, 2, 6]); mv = fin.sb("mmv", [128, 2]); rstd = fin.sb("mrstd", [128, 1])
        for s in range(2):
            P.dma("sync", mg2[:, s, :], G.modd[s, 5])
        P.dma("sync", l2w[:], bc_rows(W["ln2_w"][l:l + 1, :]))
        P.dma("sync", l2b[:], bc_rows(W["ln2_b"][l:l + 1, :]))
        for k, i in enumerate(tiles):
            b = k % 2
            s = seg_of(i)
            rows = slice(i * 128, (i + 1) * 128)
            x = xt[:, b, :]
            P.dma("sync", x, G.xres[rows, :])
            f = acc[:, k, :]
            if G.dbg is not None and "fdbg" in G.dbg:
                P.dma("sync", G.dbg["fdbg"][rows, :], f)
            P.tt(f, f, mg2[:, s, :], ALU.mult, eng="gpsimd")
            P.stt(f, x, DN_ALPHA, f, ALU.mult, ALU.add)
            ln_stats(P, fin, f, "m", st6[:], mv[:], rstd[:])
            P.ts(f, f, mv[:, 0:1], ALU.subtract, rstd[:, 0:1], ALU.mult)
            P.tt(f, f, l2w[:], ALU.mult, eng="gpsimd")
            P.tt(x, f, l2b[:], ALU.add)
            if G.dbg is not None and "xout" in G.dbg:
                P.dma("sync", G.dbg["xout"][rows, :], x)
            if l == 3 and G.final_out:
                P.dma("sync", G.out[(i - 2) * 128:(i - 1) * 128, :], x, final=True)
            else:
                P.dma("sync", G.xres[rows, :], x)
        fin.st.close()
        ph.close()
def alloc_scratch(nc, G, scratch):
    G.xbca = scratch("xbca", [T_ALL, 1024])
    G.dtla = scratch("dtla", [T_ALL, 32])
    G.ydir = scratch("ydir", [2, T_ALL, 512])
    G.gla_la = scratch("gla_la", [T_ALL, 256])
    G.gla_o = scratch("gla_o", [2, T_ALL, 256])
    G.rwp = scratch("rwp", [T_ALL, 11 * 256])
    G.rwy = scratch("rwy", [2, T_ALL, 256])
    G.hT = scratch("hT", [8, 128, T_ALL], BF16)
    G.gT = scratch("gT", [64, T_ALL])

PHASES = {"mod": phase_mod, "inproj": phase_inproj, "ssd": phase_ssd, "ssd_prep": ssd_prep, "ssd_dir": ssd_dir, "ssd_fin": ssd_fin,
          "gla": phase_gla, "rwkv": phase_rwkv, "rwkv_prep": rwkv_prep, "rwkv_dir": rwkv_dir, "rwkv_fin": rwkv_fin, "outproj": phase_outproj, "moe": phase_moe}
WNAMES = ["ada_w", "ada_b", "w_in", "ssd_conv_w", "ssd_conv_b", "ssd_dt_bias", "ssd_a_log", "ssd_d", "ssd_norm_w",
          "rwkv_mu", "rwkv_w0", "rwkv_w2", "rwkv_a0", "rwkv_a2", "rwkv_g2", "rwkv_kk", "rwkv_ka", "rwkv_rk",
          "rwkv_lnx_w", "rwkv_lnx_b", "gla_gu", "gla_gb", "gla_norm_w", "w_out", "ln1_w", "ln1_b", "ln2_w", "ln2_b",
          "router_w", "router_b", "exp_w1", "exp_w3", "exp_w2", "sh_w1", "sh_w3", "sh_w2"]
WSHAPES = {"ada_w": [4, 1024, 6144], "ada_b": [4, 6144], "w_in": [4, 1024, 3504], "ssd_conv_w": [4, 3, 1024],
           "ssd_conv_b": [4, 1024], "ssd_dt_bias": [4, 2, 8], "ssd_a_log": [4, 2, 8], "ssd_d": [4, 8],
           "ssd_norm_w": [4, 512], "rwkv_mu": [4, 4, 1152], "rwkv_w0": [4, 2, 256], "rwkv_w2": [4, 2, 64, 256],
           "rwkv_a0": [4, 2, 256], "rwkv_a2": [4, 2, 64, 256], "rwkv_g2": [4, 128, 256], "rwkv_kk": [4, 256],
           "rwkv_ka": [4, 256], "rwkv_rk": [4, 4, 64], "rwkv_lnx_w": [4, 256], "rwkv_lnx_b": [4, 256],
           "gla_gu": [4, 2, 16, 128], "gla_gb": [4, 2, 128], "gla_norm_w": [4, 256], "w_out": [4, 1024, 1024],
           "ln1_w": [4, 1024], "ln1_b": [4, 1024], "ln2_w": [4, 1024], "ln2_b": [4, 1024],
           "router_w": [4, 1024, 64], "router_b": [4, 64], "exp_w1": [4, 64, 1024, 256], "exp_w3": [4, 64, 1024, 256],
           "exp_w2": [4, 64, 256, 1024], "sh_w1": [4, 1024, 256], "sh_w3": [4, 1024, 256], "sh_w2": [4, 256, 1024]}


def build_program(layers=(0, 1, 2, 3), phases=None, dbg=(), wnames=None, feed=()):
    nc = bass.Bass("TRN2", target_bir_lowering=False)
    P = Prog(nc)
    G = Ctx()
    G.w = {}
    import os
    G.stop = float(os.environ.get('KSTOP', '99'))
    for n in (wnames or WNAMES):
        G.w[n] = nc.dram_tensor(n, WSHAPES[n], F32, kind="ExternalInput").ap()
    G.x_in = nc.dram_tensor("x", [4096, 1024], F32, kind="ExternalInput").ap()
    G.ctx_in = nc.dram_tensor("ctx", [256, 1024], F32, kind="ExternalInput").ap()
    G.cvec = nc.dram_tensor("cvec", [2, 1024], F32, kind="ExternalInput").ap()
    G.out = nc.dram_tensor("out", [4096, 1024], F32, kind="ExternalOutput").ap()
    DBG_SHAPES = {"u": ([T_ALL, 1024], BF16), "modd": ([2, 6, 128, 1024], F32), "zin": ([T_ALL, NIN], F32),
                  "ymix": ([T_ALL, 1024], F32), "xres": ([T_ALL, 1024], F32)}
    G.dbg = {}
    G.final_out = (phases is None or "moe" in phases)

    def scratch(name, shape, dt=F32):
        kind = "ExternalOutput" if name in dbg else ("ExternalInput" if name in feed else "Internal")
        t = nc.dram_tensor(name, list(shape), dt, kind=kind).ap()
        return t
    for n in dbg:
        if n in ("u",):
            G.dbg[n] = nc.dram_tensor(n, DBG_SHAPES[n][0], DBG_SHAPES[n][1], kind="ExternalOutput").ap()
        if n in ("hdbg", "fdbg", "xout"):
            G.dbg[n] = nc.dram_tensor(n, [T_ALL, 1024], F32, kind="ExternalOutput").ap()
    G.xres = scratch("xres", [T_ALL, 1024])
    G.modd = scratch("modd", [2, 6, 128, 1024])
    G.zin = scratch("zin", [T_ALL, NIN])
    G.ymix = scratch("ymix", [T_ALL, 1024])
    G.psf = nc.alloc_psum_tensor("psf", [128, 6 * 512], F32)
    G.psb = nc.alloc_psum_tensor("psb", [128, 2 * 1024], BF16)
    make_consts(nc, P, G)
    alloc_scratch(nc, G, scratch)
    if "xres" not in feed:
        P.dma("sync", G.xres[0:256, :], G.ctx_in)
        P.dma("sync", G.xres[256:T_ALL, :], G.x_in)
    P.barrier()
    allp = ["mod", "inproj", "ssd", "gla", "rwkv", "outproj", "moe"]
    for l in layers:
        for pn in (phases or allp):
            PHASES[pn](nc, P, G, l)
    if phases is None or "moe" in phases:
        pass
    else:
        pass
    P.barrier()
    P.dma("sync", G.out[0:128, 0:8], G.xres[0:128, 0:8], final=True) if (phases is not None and "moe" not in phases) else None
    P.finish()
    return nc, P


_NC_CACHE = {}


def kernel(**inputs):
    from concourse.bass_utils import run_bass_kernel_spmd
    if "nc" not in _NC_CACHE:
        nc, P = build_program()
        _NC_CACHE["nc"] = nc
    nc = _NC_CACHE["nc"]
    B = 4
    base = {n: np.ascontiguousarray(np.asarray(inputs[n], dtype=np.float32)) for n in WNAMES}
    x = np.asarray(inputs["x"], dtype=np.float32)
    ctx = np.asarray(inputs["ctx"], dtype=np.float32)
    c = np.asarray(inputs["c"], dtype=np.float32)
    c_ctx = np.asarray(inputs["c_ctx"], dtype=np.float32)
    in_maps = []
    for b in range(B):
        m = dict(base)
        m["x"] = np.ascontiguousarray(x[b])
        m["ctx"] = np.ascontiguousarray(ctx[b])
        m["cvec"] = np.ascontiguousarray(np.stack([c_ctx, c[b]]))
        in_maps.append(m)
    res = run_bass_kernel_spmd(nc, in_maps, core_ids=list(range(B)))
    out = np.stack([np.asarray(res.results[b]["out"], dtype=np.float32) for b in range(B)])
    return out
```

```python
import numpy as np
import concourse.bass as bass
import concourse.mybir as mybir

F32 = mybir.dt.float32
BF16 = mybir.dt.bfloat16
I32 = mybir.dt.int32
U32 = mybir.dt.uint32
AF = mybir.ActivationFunctionType
ALU = mybir.AluOpType
AX = mybir.AxisListType

_DSZ = {F32: 4, BF16: 2, I32: 4, U32: 4}

ENGS = ("tensor", "vector", "scalar", "gpsimd", "sync")
DMA_K = {"sync": 16, "gpsimd": 8, "scalar": 4}


def _box(ap):
    t = ap.tensor
    name = t.name
    dsz = _DSZ.get(ap.dtype, 4)
    steps = list(ap.ap)
    off = ap.offset
    space = str(ap.space) if hasattr(ap, "space") else ""
    if "DRAM" in space.upper() or "dram" in space.lower() or type(t).__name__.startswith("DRam"):
        lo = off + sum(min(0, s * (c - 1)) for s, c in steps)
        hi = off + sum(max(0, s * (c - 1)) for s, c in steps) + 1
        return name, 0, 1, lo * dsz, hi * dsz
    pstep, pcnt = steps[0]
    if pstep == 0:
        pstep = 1 << 30
    p_lo = off // pstep
    f_off = off - p_lo * pstep
    rest = steps[1:]
    lo = f_off + sum(min(0, s * (c - 1)) for s, c in rest)
    hi = f_off + sum(max(0, s * (c - 1)) for s, c in rest) + 1
    lo *= dsz
    hi *= dsz
    if name.startswith("ps"):
        lo = (lo // 2048) * 2048
        hi = ((hi + 2047) // 2048) * 2048
        p_lo, pcnt = 0, 128
    return name, p_lo, p_lo + pcnt, lo, hi


class Prog:
    def __init__(self, nc):
        self.nc = nc
        self.ops = {e: [] for e in ENGS}
        self.cnt = {e: 0 for e in ENGS}
        self.dcnt = {e: 0 for e in DMA_K}
        self.waited = {e: {} for e in ENGS}
        self.recs = {}
        self.semkeys = set()
        self.final_tokens = []
        self.n_ops = 0

    def _deps(self, reads, writes, pe_accum=False, eng=None):
        deps = set()
        rb = [_box(a) for a in reads]
        wb = [_box(a) for a in writes]
        for (name, p0, p1, f0, f1) in rb:
            isps = name.startswith("ps")
            for r in self.recs.get(name, ()):
                if r[0] < p1 and p0 < r[1] and r[2] < f1 and f0 < r[3]:
                    if r[4] == "w" or (isps and r[5][0] != eng):
                        deps.add(r[5])
        for (name, p0, p1, f0, f1) in wb:
            for r in self.recs.get(name, ()):
                if r[0] < p1 and p0 < r[1] and r[2] < f1 and f0 < r[3]:
                    if r[4] == "w" and r[5][0] == "tensor" and eng == "tensor":
                        continue
                    deps.add(r[5])
        return deps, rb, wb

    def _update(self, rb, wb, tok):
        for (name, p0, p1, f0, f1) in wb:
            lst = self.recs.setdefault(name, [])
            lst[:] = [r for r in lst if not (p0 <= r[0] and r[1] <= p1 and f0 <= r[2] and r[3] <= f1)]
            lst.append((p0, p1, f0, f1, "w", tok))
        for (name, p0, p1, f0, f1) in rb:
            lst = self.recs.setdefault(name, [])
            lst[:] = [r for r in lst if not (r[4] == "r" and r[5][0] == tok[0] and p0 <= r[0] and r[1] <= p1
                                             and f0 <= r[2] and r[3] <= f1)]
            lst.append((p0, p1, f0, f1, "r", tok))

    def _waits(self, eng, deps):
        w = self.waited[eng]
        best = {}
        for (k, v) in deps:
            if v > best.get(k, 0):
                best[k] = v
        out = []
        for k, v in best.items():
            if w.get(k, 0) >= v:
                continue
            w[k] = v
            out.append((k, v))
        return out

    def op(self, eng, fn, reads=(), writes=(), pe_accum=False):
        deps, rb, wb = self._deps(reads, writes, pe_accum, eng)
        self.cnt[eng] += 1
        tok = (eng, self.cnt[eng])
        waits = self._waits(eng, deps)
        self.semkeys.add(eng)
        self.ops[eng].append((waits, fn, (eng, 1)))
        self._update(rb, wb, tok)
        self.n_ops += 1
        return tok

    def dma(self, q, out, in_, final=False, **kw):
        deps, rb, wb = self._deps([in_], [out])
        n = self.dcnt[q]
        self.dcnt[q] += 1
        K = DMA_K[q]
        key = ("dma", q, n % K)
        tok = (key, 16 * (n // K + 1))
        if n >= K:
            deps.add((key, 16 * (n // K)))
        waits = self._waits(q, deps)
        self.semkeys.add(key)
        self.ops[q].append((waits, (lambda e, o=out, i=in_, kw=kw: e.dma_start(out=o, in_=i, **kw)), (key, 16)))
        self._update(rb, wb, tok)
        if final:
            self.final_tokens.append(tok)
        self.n_ops += 1
        return tok

    def barrier(self):
        toks = set()
        for e in ENGS:
            if self.cnt[e] > 0:
                toks.add((e, self.cnt[e]))
        for q, K in DMA_K.items():
            n = self.dcnt[q]
            for j in range(min(n, K)):
                last = ((n - 1 - j) // K) * K + j
                toks.add((("dma", q, j), 16 * (last // K + 1)))
        for e in ENGS:
            waits = self._waits(e, set(toks))
            if waits:
                self.ops[e].append((waits, None, None))
        self.recs.clear()

    def mm(self, out, lhsT, rhs, start=True, stop=True):
        return self.op("tensor", lambda e: e.matmul(out, lhsT, rhs, start=start, stop=stop),
                       reads=[lhsT, rhs], writes=[out], pe_accum=not start)

    def tr(self, out, in_, ident):
        return self.op("tensor", lambda e: e.transpose(out, in_, ident), reads=[in_, ident], writes=[out])

    def act(self, out, in_, func, bias=None, scale=None, eng="scalar", accum_out=None):
        reads = [in_]
        kw = {}
        if bias is not None:
            kw["bias"] = bias
            if not isinstance(bias, (int, float)):
                reads.append(bias)
        if scale is not None:
            kw["scale"] = scale
            if not isinstance(scale, (int, float)):
                reads.append(scale)
        writes = [out]
        if accum_out is not None:
            kw["accum_out"] = accum_out
            writes.append(accum_out)
        return self.op(eng, lambda e: e.activation(out, in_, func, **kw), reads=reads, writes=writes)

    def tt(self, out, in0, in1, op, eng="vector"):
        return self.op(eng, lambda e: e.tensor_tensor(out, in0, in1, op), reads=[in0, in1], writes=[out])

    def ts(self, out, in0, s1, op0, s2=None, op1=None, eng="vector", accum_out=None):
        reads = [in0]
        if not isinstance(s1, (int, float)):
            reads.append(s1)
        if s2 is not None and not isinstance(s2, (int, float)):
            reads.append(s2)
        writes = [out]
        kw = {}
        if accum_out is not None:
            kw["accum_out"] = accum_out
            writes.append(accum_out)
        if op1 is None:
            return self.op(eng, lambda e: e.tensor_scalar(out, in0, s1, None, op0, **kw), reads=reads, writes=writes)
        return self.op(eng, lambda e: e.tensor_scalar(out, in0, s1, s2, op0, op1, **kw), reads=reads, writes=writes)

    def stt(self, out, in0, scalar, in1, op0, op1, eng="vector"):
        reads = [in0, in1]
        if not isinstance(scalar, (int, float)):
            reads.append(scalar)
        return self.op(eng, lambda e: e.scalar_tensor_tensor(out, in0, scalar, in1, op0, op1), reads=reads, writes=[out])

    def copy(self, out, in_, eng="vector"):
        if eng == "scalar":
            return self.op(eng, lambda e: e.copy(out, in_), reads=[in_], writes=[out])
        return self.op(eng, lambda e: e.tensor_copy(out, in_), reads=[in_], writes=[out])

    def memset(self, out, val, eng="vector"):
        return self.op(eng, lambda e: e.memset(out, val), writes=[out])

    def finish(self):
        nc = self.nc
        sems = {}
        import contextlib
        with contextlib.ExitStack() as st:
            for k in sorted(self.semkeys, key=str):
                nm = "s_" + "_".join(str(x) for x in (k if isinstance(k, tuple) else (k,)))
                sems[k] = st.enter_context(nc.semaphore(nm))
            block = st.enter_context(nc.Block())
            fin = self._waits("sync", set(self.final_tokens))
            ops = self.ops

            def make(engname, extra=None):
                def body(e):
                    for waits, fn, inc in ops[engname]:
                        for (k, v) in waits:
                            e.wait_ge(sems[k], v)
                        if fn is None:
                            continue
                        ins = fn(e)
                        ins.then_inc(sems[inc[0]], inc[1])
                    if extra:
                        for (k, v) in extra:
                            e.wait_ge(sems[k], v)
                return body

            block.tensor(make("tensor"))
            block.vector(make("vector"))
            block.scalar(make("scalar"))
            block.gpsimd(make("gpsimd"))
            block.sync(make("sync", fin))
        return nc
import contextlib

T_ALL = 4352
NT = 34
D = 1024
NIN = 3504
LN_EPS = 1e-5
DN_ALPHA = 8 ** 0.25
NEGM = -30000.0

C_Z = 0
C_XBC = 512
C_DT = 1536
C_RW = 1552
C_GQ = 2704
C_GK = 2832
C_GV = 2960
C_GD = 3216
C_OG = 3248


def seg_of(i):
    return 0 if i < 2 else 1


class Phase:
    _n = [0]

    def __init__(self, nc, P):
        self.nc = nc
        self.P = P
        self.st = contextlib.ExitStack()

    def sb(self, name, shape, dt=F32):
        Phase._n[0] += 1
        return self.st.enter_context(self.nc.sbuf_tensor(f"{name}_{Phase._n[0]}", list(shape), dt))

    def close(self):
        self.P.barrier()
        self.st.close()


def bc_rows(ap2d_row, n=128):
    steps = list(ap2d_row.ap)
    last = steps[-1]
    return bass.AP(ap2d_row.tensor, ap2d_row.offset, [[0, n], [last[0], last[1]]])


class Ctx:
    pass


def make_consts(nc, P, G):
    def sbp(name, shape, dt=F32):
        return nc.alloc_sbuf_tensor(name, list(shape), dt)
    G.identf = sbp("identf", [128, 128])
    G.identb = sbp("identb", [128, 128], BF16)
    G.onesf = sbp("onesf", [128, 128])
    G.tri = [sbp("tri_f", [128, 128]), sbp("tri_b", [128, 128])]
    G.negm = [sbp("negm_f", [128, 4, 128]), sbp("negm_b", [128, 4, 128])]
    G.trib = [sbp("trib_f", [128, 128], BF16), sbp("trib_b", [128, 128], BF16)]
    G.SL = sbp("m_SL", [128, 128]); G.SU = sbp("m_SU", [128, 128]); G.IU = sbp("m_IU", [128, 128]); G.IL = sbp("m_IL", [128, 128])
    gp = "gpsimd"
    G.nSU = sbp("m_nSU", [128, 128]); G.nSL = sbp("m_nSL", [128, 128])
    G.cind = sbp("cind", [128, 2]); G.cmask = sbp("cmask", [128, 2])
    G.hm4 = sbp("hm4", [128, 4])
    P.memset(G.hm4[:], 1.0, eng=gp)
    P.op(gp, lambda e: e.affine_select(G.hm4[:], G.hm4[:], [[-32, 4]], ALU.is_ge, 0.0, base=0, channel_multiplier=1),
         reads=[G.hm4[:]], writes=[G.hm4[:]])
    P.op(gp, lambda e: e.affine_select(G.hm4[:], G.hm4[:], [[32, 4]], ALU.is_ge, 0.0, base=31, channel_multiplier=-1),
         reads=[G.hm4[:]], writes=[G.hm4[:]])

    def asel(t, cm, step, cmp, base=0):
        P.op(gp, lambda e: e.affine_select(t, t, [[step, 128]], cmp, 0.0, base=base, channel_multiplier=cm),
             reads=[t], writes=[t])
    for t in (G.identf, G.onesf, G.tri[0], G.tri[1], G.SL, G.SU, G.IU, G.IL):
        P.memset(t[:], 1.0, eng=gp)
    asel(G.identf[:], -1, 1, ALU.is_equal)
    asel(G.tri[0][:], -1, 1, ALU.is_ge)
    asel(G.tri[1][:], 1, -1, ALU.is_ge)
    asel(G.IU[:], -1, 1, ALU.is_ge)
    asel(G.IL[:], 1, -1, ALU.is_ge)
    asel(G.SU[:], -1, 1, ALU.is_gt)
    asel(G.SL[:], 1, -1, ALU.is_gt)
    for t in (G.SL, G.SU, G.IU, G.IL):
        P.memset(t[0:64, 64:128], 0.0, eng=gp)
        P.memset(t[64:128, 0:64], 0.0, eng=gp)
    P.ts(G.nSU[:], G.SU[:], -1.0, ALU.mult, eng=gp)
    P.ts(G.nSL[:], G.SL[:], -1.0, ALU.mult, eng=gp)
    P.memset(G.cind[:], 0.0, eng=gp)
    P.memset(G.cind[0:64, 0:1], 1.0, eng=gp)
    P.memset(G.cind[64:128, 1:2], 1.0, eng=gp)
    P.memset(G.cmask[:], 1.0, eng=gp)
    for (col, base) in ((0, 0), (0, -64), (1, -63), (1, -127)):
        P.op(gp, lambda e, col=col, base=base: e.affine_select(G.cmask[:, col:col + 1], G.cmask[:, col:col + 1], [[0, 1]],
                                                             ALU.not_equal, 0.0, base=base, channel_multiplier=1),
             reads=[G.cmask[:, col:col + 1]], writes=[G.cmask[:, col:col + 1]])
    P.copy(G.identb[:], G.identf[:])
    for d in range(2):
        P.copy(G.trib[d][:], G.tri[d][:])
        for h in range(4):
            P.ts(G.negm[d][:, h, :], G.tri[d][:], 1.0, ALU.subtract, -NEGM, ALU.mult)


def ln_stats(P, ph, x_ap, tag, st6, mv, rstd, eps=LN_EPS):
    for hh in range(2):
        P.op("vector", lambda e, hh=hh: e.bn_stats(st6[:, hh, :], x_ap[:, hh * 512:(hh + 1) * 512]),
             reads=[x_ap[:, hh * 512:(hh + 1) * 512]], writes=[st6[:, hh, :]])
    P.op("vector", lambda e: e.bn_aggr(mv, st6), reads=[st6], writes=[mv])
    P.act(rstd, mv[:, 1:2], AF.Sqrt, bias=eps)
    P.op("vector", lambda e: e.reciprocal(rstd, rstd), reads=[rstd], writes=[rstd])


def phase_mod(nc, P, G, l):
    ph = Phase(nc, P)
    cT = ph.sb("cT", [128, 2, 8]); csil = ph.sb("csil", [128, 2, 8])
    wst = ph.sb("adaw", [128, 2, 8, 512]); brow = ph.sb("brow", [1, 6144]); mst = ph.sb("mst", [128, 2, 2, 512])
    for s in range(2):
        P.dma("sync", cT[:, s, :], G.cvec[s, :].rearrange("(kc p) -> p kc", p=128), allow_slow_non_contiguous=True)
    P.act(csil[:], cT[:], AF.Silu)
    P.dma("sync", brow[:], G.w["ada_b"][l:l + 1, :])
    for n in range(12):
        b = n % 2
        P.dma("sync", wst[:, b], G.w["ada_w"][l, :, n * 512:(n + 1) * 512].rearrange("(kc p) j -> p kc j", p=128))
        for s in range(2):
            pso = G.psf[:, (n % 2 * 2 + s) * 512:(n % 2 * 2 + s + 1) * 512]
            for kc in range(8):
                P.mm(pso, csil[:, s, kc:kc + 1].to_broadcast([128, 128]), wst[:, b, kc, :], start=(kc == 0), stop=False)
            P.mm(pso, G.onesf[0:1, :], brow[0:1, n * 512:(n + 1) * 512], start=False, stop=True)
            which = n // 2
            if which in (1, 4):
                P.act(mst[:, b, s, :], pso, AF.Identity, bias=1.0)
            else:
                P.copy(mst[:, b, s, :], pso)
            P.dma("sync", G.modd[s, which, :, (n % 2) * 512:(n % 2 + 1) * 512], mst[:, b, s, :])
    ph.close()


def phase_inproj(nc, P, G, l):
    ph = Phase(nc, P)
    winb = ph.sb("winb", [128, 8, NIN], BF16)
    wst = ph.sb("wst", [128, 2, 8, 512])
    msc = ph.sb("msc", [128, 2, 1024]); msh = ph.sb("msh", [128, 2, 1024])
    xt = ph.sb("xt", [128, 2, 1024]); ub = ph.sb("ub", [128, 2, 1024], BF16)
    uT = ph.sb("uT", [128, 2, 8, 128], BF16); zt = ph.sb("zt", [128, 2, NIN])
    st6 = ph.sb("st6", [128, 2, 6]); mv = ph.sb("mv", [128, 2]); rstd = ph.sb("rstd", [128, 1])
    for n in range(7):
        w = min(512, NIN - n * 512)
        P.dma("sync", wst[:, n % 2, :, 0:w], G.w["w_in"][l, :, n * 512:n * 512 + w].rearrange("(kc p) j -> p kc j", p=128))
        P.copy(winb[:, :, n * 512:n * 512 + w], wst[:, n % 2, :, 0:w], eng="gpsimd")
    for s in range(2):
        P.dma("sync", msc[:, s, :], G.modd[s, 1])
        P.dma("sync", msh[:, s, :], G.modd[s, 0])
    for i in range(NT):
        b = i % 2
        s = seg_of(i)
        x = xt[:, b, :]
        P.dma("sync", x, G.xres[i * 128:(i + 1) * 128, :])
        ln_stats(P, ph, x, "a", st6[:], mv[:], rstd[:])
        P.ts(x, x, mv[:, 0:1], ALU.subtract, rstd[:, 0:1], ALU.mult)
        P.tt(x, x, msc[:, s, :], ALU.mult, eng="gpsimd")
        P.tt(ub[:, b, :], x, msh[:, s, :], ALU.add)
        if G.dbg is not None and "u" in G.dbg:
            P.dma("sync", G.dbg["u"][i * 128:(i + 1) * 128, :], ub[:, b, :])
        for kc in range(8):
            P.tr(G.psb[:, kc * 128:(kc + 1) * 128], ub[:, b, kc * 128:(kc + 1) * 128], G.identb[:])
        P.copy(uT[:, b].rearrange("p a b -> p (a b)"), G.psb[:, 0:1024], eng="scalar")
        for n in range(7):
            w = min(512, NIN - n * 512)
            pso = G.psf[:, (n % 4) * 512:(n % 4) * 512 + w]
            for kc in range(8):
                P.mm(pso, uT[:, b, kc, :], winb[:, kc, n * 512:n * 512 + w], start=(kc == 0), stop=(kc == 7))
            P.copy(zt[:, b, n * 512:n * 512 + w], pso, eng=("scalar" if n % 2 else "vector"))
        P.dma("sync", G.zin[i * 128:(i + 1) * 128, :], zt[:, b, :])
    ph.close()
def dir_order(d):
    if d == 0:
        return list(range(NT))
    return [1, 0] + list(range(NT - 1, 1, -1))


def load_shift(P, dst, src2d, i, off, eng_ms="gpsimd"):
    lo, hi = (0, 256) if i < 2 else (256, T_ALL)
    r0 = i * 128 + off
    a = max(r0, lo); bnd = min(r0 + 128, hi)
    if a > r0:
        n = a - r0
        if n >= 128:
            P.memset(dst, 0.0, eng=eng_ms); return
        P.memset(dst[0:((n + 31) // 32) * 32, :], 0.0, eng=eng_ms)
    if bnd < r0 + 128:
        n = r0 + 128 - bnd
        if n >= 128:
            P.memset(dst, 0.0, eng=eng_ms); return
        st = ((128 - n) // 32) * 32
        P.memset(dst[st:128, :], 0.0, eng=eng_ms)
    P.dma("sync", dst[a - r0:bnd - r0, :], src2d[a:bnd, :])


def phase_ssd(nc, P, G, l):
    ssd_prep(nc, P, G, l); ssd_dir(nc, P, G, l); ssd_fin(nc, P, G, l)


def ssd_prep(nc, P, G, l):
    W = G.w
    ph = Phase(nc, P)
    cw = ph.sb("cw", [128, 3, 1024]); cb = ph.sb("cb", [128, 1024])
    dtb = ph.sb("dtb", [128, 16]); nega = ph.sb("nega", [128, 16])
    xc = ph.sb("xc", [128, 2, 1024]); xl = ph.sb("xl", [128, 2, 1024]); xr = ph.sb("xr", [128, 2, 1024])
    acc = ph.sb("acc", [128, 2, 1024]); dl = ph.sb("dl", [128, 2, 32]); dr = ph.sb("dr", [128, 2, 16])
    for j in range(3):
        P.dma("sync", cw[:, j, :], bc_rows(W["ssd_conv_w"][l, j:j + 1, :]))
    P.dma("sync", cb[:], bc_rows(W["ssd_conv_b"][l:l + 1, :]))
    P.dma("sync", dtb[:], bc_rows(W["ssd_dt_bias"][l:l + 1].rearrange("a b c -> a (b c)")))
    P.dma("sync", nega[:], bc_rows(W["ssd_a_log"][l:l + 1].rearrange("a b c -> a (b c)")))
    P.act(nega[:], nega[:], AF.Exp)
    P.ts(nega[:], nega[:], -1.0, ALU.mult)
    zx = G.zin[:, C_XBC:C_XBC + 1024]
    for i in range(NT):
        b = i % 2
        rows = slice(i * 128, (i + 1) * 128)
        P.dma("sync", xc[:, b, :], zx[rows, :])
        load_shift(P, xl[:, b, :], zx, i, -1)
        load_shift(P, xr[:, b, :], zx, i, +1)
        P.dma("sync", dr[:, b, :], G.zin[rows, C_DT:C_DT + 16])
        a = acc[:, b, :]
        P.tt(a, xc[:, b, :], cw[:, 1, :], ALU.mult)
        P.tt(xl[:, b, :], xl[:, b, :], cw[:, 0, :], ALU.mult, eng="gpsimd")
        P.tt(xr[:, b, :], xr[:, b, :], cw[:, 2, :], ALU.mult, eng="gpsimd")
        P.tt(a, a, xl[:, b, :], ALU.add)
        P.tt(a, a, xr[:, b, :], ALU.add)
        P.tt(a, a, cb[:], ALU.add)
        P.act(a, a, AF.Silu)
        P.dma("sync", G.xbca[rows, :], a)
        P.tt(dr[:, b, :], dr[:, b, :], dtb[:], ALU.add)
        P.act(dr[:, b, :], dr[:, b, :], AF.Exp)
        P.act(dl[:, b, 0:16], dr[:, b, :], AF.Ln, bias=1.0)
        P.tt(dl[:, b, 16:32], dl[:, b, 0:16], nega[:], ALU.mult)
        P.dma("sync", G.dtla[rows, :], dl[:, b, :])
    ph.close()


def ssd_dir(nc, P, G, l):
    W = G.w
    ph = Phase(nc, P)
    xa = ph.sb("xa", [128, 2, 1024]); dl = ph.sb("dl2", [128, 2, 32])
    sm = ph.sb("sm", [128, 2, 48])
    R1 = ph.sb("R1", [128, 8, 128]); R3 = ph.sb("R3", [128, 8, 128])
    LT = ph.sb("LT", [128, 8, 128], BF16); MT = ph.sb("MT", [128, 8, 128], BF16)
    bcb = ph.sb("bcb", [128, 2, 512], BF16); bcT = ph.sb("bcT", [128, 2, 4, 128], BF16)
    xdt = ph.sb("xdt", [128, 2, 8, 64], BF16); xdd = ph.sb("xdd", [128, 2, 8, 64], BF16)
    ytmp = ph.sb("ytmp", [128, 8, 64]); yo = ph.sb("yo", [128, 2, 512])
    hin32 = ph.sb("hin32", [128, 512]); hinb = ph.sb("hinb", [128, 512], BF16)
    ps = G.psf

    def bank(k, w=512, o=0):
        return ps[:, k * 512 + o:k * 512 + o + w]
    import os
    for d in [int(x) for x in os.environ.get("KDIRS", "01")]:
        P.memset(hin32[:], 0.0)
        P.memset(hinb[:], 0.0)
        tri = G.tri[d]
        import os
        for i in dir_order(d)[:int(os.environ.get("KCH", "99"))]:
            b = i % 2
            rows = slice(i * 128, (i + 1) * 128)
            P.dma("sync", xa[:, b, :], G.xbca[rows, :])
            P.dma("sync", dl[:, b, :], G.dtla[rows, :])
            dt_d = dl[:, b, d * 8:(d + 1) * 8]
            la_d = dl[:, b, 16 + d * 8:16 + (d + 1) * 8]
            S = sm[:, b, :]
            acs_ps = bank(2, 8, 256); tot_ps = bank(2, 8, 264)
            P.mm(acs_ps, tri[:], la_d)
            P.mm(tot_ps, G.onesf[:], la_d)
            P.copy(S[:, 0:8], acs_ps)
            P.act(S[:, 8:16], acs_ps, AF.Exp)
            P.tt(S[:, 40:48], tot_ps, S[:, 0:8], ALU.subtract)
            P.act(S[:, 16:24], S[:, 40:48], AF.Exp)
            P.act(S[:, 24:32], tot_ps, AF.Exp)
            P.tt(S[:, 32:40], dt_d, S[:, 16:24], ALU.mult)
            if G.stop <= 1: continue
            P.tt(R1[:], tri[:].unsqueeze(1).to_broadcast([128, 8, 128]), la_d.unsqueeze(2).to_broadcast([128, 8, 128]),
                 ALU.mult, eng="gpsimd")
            P.act(R3[:], la_d.unsqueeze(2).to_broadcast([128, 8, 128]), AF.Identity, scale=-1.0)
            for g in range(2):
                sp = bank(g)
                P.mm(sp, G.onesf[:], R1[:, 4 * g:4 * g + 4, :], start=True, stop=False)
                P.mm(sp, tri[:], R3[:, 4 * g:4 * g + 4, :], start=False, stop=False)
                P.mm(sp, G.identf[:], G.negm[d][:], start=False, stop=True)
                P.act(LT[:, 4 * g:4 * g + 4, :], sp, AF.Exp)
            if G.stop <= 2: continue
            P.copy(bcb[:, b, :], xa[:, b, 512:1024])
            for q in range(4):
                P.tr(G.psb[:, q * 128:(q + 1) * 128], bcb[:, b, q * 128:(q + 1) * 128], G.identb[:])
            P.copy(bcT[:, b].rearrange("p a b -> p (a b)"), G.psb[:, 0:512], eng="scalar")
            if G.stop <= 3: continue
            xs3 = xa[:, b, 0:512].rearrange("p (h q) -> p h q", h=8)
            sk = os.environ.get("KSKIP", "")
            if "a" not in sk:
                P.tt(xdt[:, b], xs3, dt_d.unsqueeze(2).to_broadcast([128, 8, 64]), ALU.mult)
            if "b" not in sk:
                P.tt(xdd[:, b], xs3, S[:, 32:40].unsqueeze(2).to_broadcast([128, 8, 64]), ALU.mult)
            if G.stop <= 3.3: continue
            for g in range(2):
                cbp = bank(2, 128, g * 128)
                P.mm(cbp, bcT[:, b, g, :], bcT[:, b, 2 + g, :])
                if G.stop <= 3.6: continue
                P.tt(MT[:, 4 * g:4 * g + 4, :], LT[:, 4 * g:4 * g + 4, :], cbp.unsqueeze(1).to_broadcast([128, 4, 128]), ALU.mult)
            if G.stop <= 4: continue
            for h in range(8):
                P.mm(bank(3, 64, h * 64), MT[:, h, :], xdt[:, b, h, :])
            for g in range(2):
                P.mm(bank(4, 256, g * 256), bcT[:, b, 2 + g, :], hinb[:, g * 256:(g + 1) * 256])
            P.tt(ytmp[:], bank(4).rearrange("p (h q) -> p h q", h=8), S[:, 8:16].unsqueeze(2).to_broadcast([128, 8, 64]), ALU.mult)
            P.tt(yo[:, b, :], ytmp[:].rearrange("p h q -> p (h q)"), bank(3), ALU.add)
            P.dma("sync", G.ydir[d, rows, :], yo[:, b, :])
            if G.stop <= 5: continue
            for g in range(2):
                P.mm(bank(5, 256, g * 256), bcb[:, b, g * 128:(g + 1) * 128], xdd[:, b, 4 * g:4 * g + 4, :])
            h3 = hin32[:].rearrange("p (h q) -> p h q", h=8)
            P.tt(h3, h3, S[:, 24:32].unsqueeze(2).to_broadcast([128, 8, 64]), ALU.mult)
            P.tt(hin32[:], hin32[:], bank(5), ALU.add)
            P.copy(hinb[:], hin32[:], eng="scalar")
    ph.close()


def ssd_fin(nc, P, G, l):
    W = G.w
    ph = Phase(nc, P)
    dsm = ph.sb("dsm", [128, 8]); dex = ph.sb("dex", [128, 8, 64]); nw = ph.sb("nw", [128, 512])
    yf = ph.sb("yf", [128, 2, 512]); yb = ph.sb("yb", [128, 2, 512]); xs = ph.sb("xs", [128, 2, 512]); z = ph.sb("z", [128, 2, 512])
    junk = ph.sb("junk", [128, 256]); ss = ph.sb("ss", [128, 2, 2]); o = ph.sb("o", [128, 2, 512])
    P.dma("sync", dsm[:], bc_rows(W["ssd_d"][l:l + 1, :]))
    P.copy(dex[:], dsm[:].unsqueeze(2).to_broadcast([128, 8, 64]))
    P.dma("sync", nw[:], bc_rows(W["ssd_norm_w"][l:l + 1, :]))
    dexf = dex[:].rearrange("p h q -> p (h q)")
    for i in range(NT):
        b = i % 2
        rows = slice(i * 128, (i + 1) * 128)
        P.dma("sync", yf[:, b, :], G.ydir[0, rows, :])
        P.dma("sync", yb[:, b, :], G.ydir[1, rows, :])
        P.dma("sync", xs[:, b, :], G.xbca[rows, 0:512])
        P.dma("sync", z[:, b, :], G.zin[rows, C_Z:C_Z + 512])
        y = yf[:, b, :]
        P.tt(y, y, yb[:, b, :], ALU.add)
        P.tt(xs[:, b, :], xs[:, b, :], dexf, ALU.mult, eng="gpsimd")
        P.tt(y, y, xs[:, b, :], ALU.add)
        P.act(z[:, b, :], z[:, b, :], AF.Silu)
        P.tt(y, y, z[:, b, :], ALU.mult)
        for g in range(2):
            P.act(junk[:], y[:, g * 256:(g + 1) * 256], AF.Square, accum_out=ss[:, b, g:g + 1])
        P.act(ss[:, b, :], ss[:, b, :], AF.Sqrt, bias=1e-5, scale=1.0 / 256)
        P.op("vector", lambda e, b=b: e.reciprocal(ss[:, b, :], ss[:, b, :]), reads=[ss[:, b, :]], writes=[ss[:, b, :]])
        for g in range(2):
            P.stt(o[:, b, g * 256:(g + 1) * 256], y[:, g * 256:(g + 1) * 256], ss[:, b, g:g + 1], nw[:, g * 256:(g + 1) * 256],
                  ALU.mult, ALU.mult)
        P.dma("sync", G.ymix[rows, 0:512], o[:, b, :])
    ph.close()
def phase_gla(nc, P, G, l):
    gla_prep(nc, P, G, l); gla_dir(nc, P, G, l); gla_fin(nc, P, G, l)


def gla_prep(nc, P, G, l):
    W = G.w
    ph = Phase(nc, P)
    GU = ph.sb("GU", [32, 256]); gbrow = ph.sb("gbrow", [1, 256])
    gd = ph.sb("gd", [128, 2, 32]); gdT = ph.sb("gdT", [32, 2, 128]); e = ph.sb("ge", [128, 2, 256])
    P.memset(GU[:], 0.0)
    P.dma("sync", GU[0:16, 0:128], W["gla_gu"][l, 0])
    P.dma("sync", GU[16:32, 128:256], W["gla_gu"][l, 1])
    P.dma("sync", gbrow[:], W["gla_gb"][l:l + 1].rearrange("a b c -> a (b c)"))
    for i in range(NT):
        b = i % 2
        rows = slice(i * 128, (i + 1) * 128)
        P.dma("sync", gd[:, b, :], G.zin[rows, C_GD:C_GD + 32])
        tp = G.psf[0:32, 0:128]
        P.tr(tp, gd[:, b, :], G.identf[:])
        P.copy(gdT[:, b, :], tp)
        gp = G.psf[:, 512:768]
        P.mm(gp, gdT[:, b, :], GU[:], start=True, stop=False)
        P.mm(gp, G.onesf[0:1, :], gbrow[:], start=False, stop=True)
        P.act(e[:, b, :], gp, AF.Exp, scale=-1.0)
        P.act(e[:, b, :], e[:, b, :], AF.Ln, bias=1.0)
        P.ts(e[:, b, :], e[:, b, :], -1.0 / 16.0, ALU.mult)
        P.dma("sync", G.gla_la[rows, :], e[:, b, :])
    ph.close()


def gla_dir(nc, P, G, l):
    ph = Phase(nc, P)
    qk = ph.sb("qk", [128, 2, 256]); v = ph.sb("gv", [128, 2, 256]); la = ph.sb("gla", [128, 2, 128])
    acs = ph.sb("gacs", [128, 128]); ee = ph.sb("gee", [128, 3, 128]); ecd = ph.sb("gecd", [128, 1])
    qkt = ph.sb("qkt", [128, 2, 128], BF16); kdec = ph.sb("kdec", [128, 128], BF16)
    qkT = ph.sb("qkT", [128, 2, 128], BF16); smb = ph.sb("smb", [128, 4, 128], BF16); vb = ph.sb("vb", [128, 256], BF16)
    kTm = ph.sb("kTm", [128, 4, 128], BF16); qTm = ph.sb("qTm", [128, 4, 128], BF16)
    stm = ph.sb("stm", [128, 4, 64]); sred = ph.sb("sred", [128, 64])
    ot = ph.sb("got", [128, 2, 256]); S32 = ph.sb("S32", [128, 64]); Sb = ph.sb("Sb", [128, 64], BF16)
    ps = G.psf

    def bank(k, w=512, o=0):
        return ps[:, k * 512 + o:k * 512 + o + w]
    for d in range(2):
        P.memset(S32[:], 0.0)
        P.memset(Sb[:], 0.0)
        tri = G.tri[d]
        for i in dir_order(d):
            b = i % 2
            rows = slice(i * 128, (i + 1) * 128)
            P.dma("sync", qk[:, b, :], G.zin[rows, C_GQ:C_GQ + 256])
            P.dma("sync", v[:, b, :], G.zin[rows, C_GV:C_GV + 256])
            P.dma("sync", la[:, b, :], G.gla_la[rows, d * 128:(d + 1) * 128])
            acs_ps = bank(0, 128, 0); tot_ps = bank(0, 128, 128); totT = bank(0, 2, 256)
            P.mm(acs_ps, tri[:], la[:, b, :])
            P.mm(tot_ps, G.onesf[:], la[:, b, :])
            P.mm(totT, la[:, b, :], G.onesf[:, 0:2])
            P.copy(acs[:], acs_ps, eng="scalar")
            P.act(ee[:, 0, :], acs_ps, AF.Exp)
            P.act(ee[:, 1, :], acs_ps, AF.Exp, scale=-1.0)
            P.tt(ee[:, 2, :], tot_ps, acs[:], ALU.subtract)
            P.act(ee[:, 2, :], ee[:, 2, :], AF.Exp)
            P.act(ecd[:], totT[:, 0:1], AF.Exp)
            P.stt(qkt[:, 0, :], qk[:, b, 0:128], 32 ** -0.5, ee[:, 0, :], ALU.mult, ALU.mult)
            P.tt(qkt[:, 1, :], qk[:, b, 128:256], ee[:, 1, :], ALU.mult)
            P.tt(kdec[:], qk[:, b, 128:256], ee[:, 2, :], ALU.mult, eng="gpsimd")
            P.copy(vb[:], v[:, b, :], eng="gpsimd")
            for j in range(2):
                P.tr(G.psb[:, j * 128:(j + 1) * 128], qkt[:, j, :], G.identb[:])
            P.copy(qkT[:].rearrange("p a b -> p (a b)"), G.psb[:, 0:256], eng="scalar")
            P.tt(kTm[:], qkT[:, 1, :].unsqueeze(1).to_broadcast([128, 4, 128]), G.hm4[:].unsqueeze(2).to_broadcast([128, 4, 128]), ALU.mult)
            P.tt(qTm[:], qkT[:, 0, :].unsqueeze(1).to_broadcast([128, 4, 128]), G.hm4[:].unsqueeze(2).to_broadcast([128, 4, 128]), ALU.mult, eng="gpsimd")
            for h in range(4):
                P.mm(bank(1, 128, h * 128), kTm[:, h, :], qkT[:, 0, :])
            P.tt(smb[:], bank(1).rearrange("p (h q) -> p h q", h=4), G.tri[d][:].unsqueeze(1).to_broadcast([128, 4, 128]), ALU.mult)
            for h in range(4):
                hs = slice(32 * h, 32 * h + 32)
                o_ps = bank(2, 64, h * 64)
                P.mm(o_ps, smb[:, h, :], vb[:, h * 64:(h + 1) * 64], start=True, stop=False)
                P.mm(o_ps, qTm[:, h, :], Sb[:], start=False, stop=True)
            P.copy(ot[:, b, :], bank(2, 256, 0), eng="scalar")
            P.dma("sync", G.gla_o[d, rows, :], ot[:, b, :])
            P.mm(bank(3, 256, 0), kdec[:], vb[:])
            P.tt(stm[:], bank(3, 256, 0).rearrange("p (h q) -> p h q", h=4), G.hm4[:].unsqueeze(2).to_broadcast([128, 4, 64]), ALU.mult)
            P.op("vector", lambda e: e.tensor_reduce(sred[:], stm[:].rearrange("p h q -> p q h"), AX.X, ALU.add),
                 reads=[stm[:]], writes=[sred[:]])
            P.stt(S32[:], S32[:], ecd[:, 0:1], sred[:], ALU.mult, ALU.add)
            P.copy(Sb[:], S32[:])
    ph.close()


def gla_fin(nc, P, G, l):
    W = G.w
    ph = Phase(nc, P)
    nw = ph.sb("gnw", [128, 256]); of = ph.sb("gof", [128, 2, 256]); ob = ph.sb("gob", [128, 2, 256]); og = ph.sb("gog", [128, 2, 256])
    sq = ph.sb("gsq", [128, 256]); ss = ph.sb("gss", [128, 2, 4]); y = ph.sb("gy", [128, 2, 256])
    P.dma("sync", nw[:], bc_rows(W["gla_norm_w"][l:l + 1, :]))
    for i in range(NT):
        b = i % 2
        rows = slice(i * 128, (i + 1) * 128)
        P.dma("sync", of[:, b, :], G.gla_o[0, rows, :])
        P.dma("sync", ob[:, b, :], G.gla_o[1, rows, :])
        P.dma("sync", og[:, b, :], G.zin[rows, C_OG:C_OG + 256])
        o = of[:, b, :]
        P.tt(o, o, ob[:, b, :], ALU.add)
        P.tt(sq[:], o, o, ALU.mult, eng="gpsimd")
        P.op("vector", lambda e, b=b: e.tensor_reduce(ss[:, b, :], sq[:].rearrange("p (h q) -> p h q", h=4), AX.X, ALU.add),
             reads=[sq[:]], writes=[ss[:, b, :]])
        P.act(ss[:, b, :], ss[:, b, :], AF.Sqrt, bias=1e-5, scale=1.0 / 64)
        P.op("vector", lambda e, b=b: e.reciprocal(ss[:, b, :], ss[:, b, :]), reads=[ss[:, b, :]], writes=[ss[:, b, :]])
        y3 = y[:, b, :].rearrange("p (h q) -> p h q", h=4)
        P.tt(y3, o.rearrange("p (h q) -> p h q", h=4), ss[:, b, :].unsqueeze(2).to_broadcast([128, 4, 64]), ALU.mult)
        P.tt(y[:, b, :], y[:, b, :], nw[:], ALU.mult, eng="gpsimd")
        P.act(og[:, b, :], og[:, b, :], AF.Silu)
        P.tt(y[:, b, :], y[:, b, :], og[:, b, :], ALU.mult)
        P.dma("sync", G.ymix[rows, 768:1024], y[:, b, :])
    ph.close()
def phase_rwkv(nc, P, G, l):
    rwkv_prep(nc, P, G, l); rwkv_dir(nc, P, G, l); rwkv_fin(nc, P, G, l)


def rwkv_prep(nc, P, G, l):
    W = G.w
    ph = Phase(nc, P)
    mu = ph.sb("mu", [128, 4, 1152])
    w2a2 = ph.sb("w2a2", [128, 2, 256]); g2 = ph.sb("g2", [128, 256]); brow = ph.sb("rbrow", [1, 4, 256])
    kkw = ph.sb("kkw", [128, 256]); ka = ph.sb("ka", [128, 256]); rk = ph.sb("rk", [128, 256])
    c = ph.sb("rc", [128, 2, 1152]); nb = ph.sb("rnb", [128, 4, 1152])
    tws = ph.sb("tws", [128, 3, 128]); twT = ph.sb("twT", [128, 3, 128])
    pk = ph.sb("pk", [128, 2, 11, 256]); tmp = ph.sb("rtmp", [128, 3, 256]); s4 = ph.sb("rs4", [128, 8])
    for j in range(4):
        P.dma("sync", mu[:, j, :], bc_rows(W["rwkv_mu"][l, j:j + 1, :]))
    for d in range(2):
        P.dma("sync", w2a2[64 * d:64 * d + 64, 0, :], W["rwkv_w2"][l, d])
        P.dma("sync", w2a2[64 * d:64 * d + 64, 1, :], W["rwkv_a2"][l, d])
    P.dma("sync", g2[:], W["rwkv_g2"][l])
    P.dma("sync", brow[0:1, 0:2, :], W["rwkv_w0"][l:l + 1])
    P.dma("sync", brow[0:1, 2:4, :], W["rwkv_a0"][l:l + 1])
    P.dma("sync", kkw[:], bc_rows(W["rwkv_kk"][l:l + 1, :]))
    P.dma("sync", ka[:], bc_rows(W["rwkv_ka"][l:l + 1, :]))
    P.dma("sync", rk[:], bc_rows(W["rwkv_rk"][l:l + 1].rearrange("a b c -> a (b c)")))
    zr = G.zin[:, C_RW:C_RW + 1152]
    for i in range(NT):
        b = i % 2
        rows = slice(i * 128, (i + 1) * 128)
        lat = i >= 2
        cc = c[:, b, :]
        P.dma("sync", cc, zr[rows, :])
        offs = [-1, 1, -64, 64] if lat else [-1, 1]
        for j, off in enumerate(offs):
            X = nb[:, j, :]
            load_shift(P, X, zr, i, off)
            if lat and j < 2:
                P.stt(X, X, G.cmask[:, j:j + 1], cc, ALU.mult, ALU.subtract, eng=("vector"))
            else:
                P.tt(X, X, cc, ALU.subtract, eng="gpsimd")
            P.tt(X, X, mu[:, j, :], ALU.mult, eng=("gpsimd" if j % 2 else "vector"))
        for j in range(len(offs)):
            P.tt(cc, cc, nb[:, j, :], ALU.add)
        PK = pk[:, b]
        P.act(tws[:, 0, :], cc[:, 768:896], AF.Tanh)
        P.copy(tws[:, 1, :], cc[:, 896:1024], eng="gpsimd")
        P.act(tws[:, 2, :], cc[:, 1024:1152], AF.Sigmoid)
        for q in range(3):
            P.tr(G.psf[:, q * 128:(q + 1) * 128], tws[:, q, :], G.identf[:])
        P.copy(twT[:].rearrange("p a b -> p (a b)"), G.psf[:, 0:384])
        for d in range(2):
            ds = slice(64 * d, 64 * d + 64)
            xp = G.psf[:, (1 + 2 * d) * 512:(1 + 2 * d) * 512 + 256]
            P.mm(xp, twT[ds, 0, :], w2a2[ds, 0, :], start=True, stop=False)
            P.mm(xp, G.onesf[0:1, :], brow[0:1, d, :], start=False, stop=True)
            ap_ = G.psf[:, (1 + 2 * d) * 512 + 256:(1 + 2 * d) * 512 + 512]
            P.mm(ap_, twT[ds, 1, :], w2a2[ds, 1, :], start=True, stop=False)
            P.mm(ap_, G.onesf[0:1, :], brow[0:1, 2 + d, :], start=False, stop=True)
        gp = G.psf[:, 2048:2304]
        P.mm(gp, twT[:, 2, :], g2[:])
        for d in range(2):
            P.act(PK[:, 7 + d, :], G.psf[:, (1 + 2 * d) * 512:(1 + 2 * d) * 512 + 256], AF.Sigmoid)
        P.ts(PK[:, 7:9, :], PK[:, 7:9, :], -0.6065306597126334, ALU.mult, eng="gpsimd")
        adir = tmp[:, 0:2, :]
        for d in range(2):
            P.act(adir[:, d, :], G.psf[:, (1 + 2 * d) * 512 + 256:(1 + 2 * d) * 512 + 512], AF.Sigmoid)
        P.copy(PK[:, 9, :], gp, eng="scalar")
        P.copy(PK[:, 0, :], cc[:, 0:256], eng="gpsimd")
        P.copy(PK[:, 1, :], cc[:, 512:768], eng="gpsimd")
        kx = cc[:, 256:512]
        kk = PK[:, 2, :]
        P.tt(kk, kx, kkw[:], ALU.mult)
        P.tt(tmp[:, 2, :], kk, kk, ALU.mult, eng="gpsimd")
        P.op("vector", lambda e: e.tensor_reduce(s4[:, 0:4], tmp[:, 2, :].rearrange("p (h q) -> p h q", h=4), AX.X, ALU.add),
             reads=[tmp[:, 2, :]], writes=[s4[:, 0:4]])
        P.act(s4[:, 0:4], s4[:, 0:4], AF.Sqrt)
        P.ts(s4[:, 0:4], s4[:, 0:4], 1e-12, ALU.max)
        P.op("vector", lambda e: e.reciprocal(s4[:, 0:4], s4[:, 0:4]), reads=[s4[:, 0:4]], writes=[s4[:, 0:4]])
        kk3 = kk.rearrange("p (h q) -> p h q", h=4)
        P.tt(kk3, kk3, s4[:, 0:4].unsqueeze(2).to_broadcast([128, 4, 64]), ALU.mult)
        for d in range(2):
            t1 = tmp[:, 2, :]
            P.stt(t1, adir[:, d, :], -1.0, ka[:], ALU.add, ALU.mult)
            P.stt(PK[:, 3 + d, :], t1, 1.0, kx, ALU.add, ALU.mult)
            P.tt(PK[:, 5 + d, :], kk, adir[:, d, :], ALU.mult, eng="gpsimd")
        t1 = tmp[:, 2, :]
        P.tt(t1, PK[:, 3, :], PK[:, 4, :], ALU.add)
        P.tt(t1, t1, PK[:, 0, :], ALU.mult)
        P.tt(t1, t1, rk[:], ALU.mult)
        P.op("vector", lambda e: e.tensor_reduce(s4[:, 4:8], tmp[:, 2, :].rearrange("p (h q) -> p h q", h=4), AX.X, ALU.add),
             reads=[tmp[:, 2, :]], writes=[s4[:, 4:8]])
        P.tt(PK[:, 10, :].rearrange("p (h q) -> p h q", h=4), PK[:, 1, :].rearrange("p (h q) -> p h q", h=4),
             s4[:, 4:8].unsqueeze(2).to_broadcast([128, 4, 64]), ALU.mult)
        P.dma("sync", G.rwp[rows, :], PK.rearrange("p a b -> p (a b)"))
    ph.close()


def rwkv_dir(nc, P, G, l):
    ph = Phase(nc, P)
    ld = ph.sb("rld", [128, 2, 6, 256])
    ee = ph.sb("ree", [128, 3, 256])
    tok = ph.sb("rtok", [128, 4, 256])
    fmh = ph.sb("fmh", [64, 4, 4, 128])
    Aarb = ph.sb("Aarb", [128, 4, 128]); Aak = ph.sb("Aak", [128, 4, 128]); Aark = ph.sb("Aark", [128, 4, 128])
    Q = ph.sb("rQ", [128, 2, 4, 128]); QT = ph.sb("rQT", [128, 2, 4, 128]); IQT = ph.sb("rIQT", [128, 4, 128])
    Z = ph.sb("rZ", [128, 2, 4, 128])
    Wsb = ph.sb("rW", [128, 4, 64]); nAkV = ph.sb("nAkV", [128, 4, 64]); Uloc = ph.sb("Uloc", [128, 4, 64])
    GpT = ph.sb("GpT", [64, 4, 2, 64]); RwT = ph.sb("RwT", [64, 4, 128]); gC = ph.sb("gC", [64, 4, 2])
    H32 = ph.sb("H32", [64, 4, 64]); Yt = ph.sb("Yt", [64, 2, 2, 256])
    btc = ph.sb("btc", [128, 2, 256]); ktc = ph.sb("ktc", [128, 2, 256])
    ps = G.psf

    def bank(k, w=512, o=0, p0=0, p1=128):
        return ps[p0:p1, k * 512 + o:k * 512 + o + w]
    for d in range(2):
        P.memset(H32[:], 0.0)
        BD = G.IU if d == 0 else G.IL
        m_abT = G.nSU if d == 0 else G.nSL
        m_ab = G.nSL if d == 0 else G.nSU
        m_s = G.SU if d == 0 else G.SL
        m_i = G.IU if d == 0 else G.IL
        import os
        for i in dir_order(d)[:int(os.environ.get("KCH", "99"))]:
            b = i % 2
            rows = slice(i * 128, (i + 1) * 128)
            L = ld[:, b]
            for q, col in enumerate([0, 1, 2, 3 + d, 5 + d, 7 + d]):
                P.dma("sync", L[:, q, :], G.rwp[rows, col * 256:(col + 1) * 256])
            r_, v_, kk_, k_, b_, lw_ = [L[:, q, :] for q in range(6)]
            cs_ps = bank(5, 256)
            P.mm(cs_ps, BD[:], lw_)
            P.act(ee[:, 0, :], cs_ps, AF.Exp)
            P.act(ee[:, 1, :], cs_ps, AF.Exp, scale=-1.0)
            P.copy(ee[:, 2, :], cs_ps, eng="scalar")
            P.tt(ee[:, 2, :], ee[:, 2, :], lw_, ALU.subtract)
            P.act(ee[:, 2, :], ee[:, 2, :], AF.Exp)
            P.tt(tok[:, 0, :], kk_, ee[:, 2, :], ALU.mult)
            P.tt(tok[:, 1, :], r_, ee[:, 0, :], ALU.mult, eng="gpsimd")
            P.tt(tok[:, 2, :], b_, ee[:, 1, :], ALU.mult)
            P.tt(tok[:, 3, :], k_, ee[:, 1, :], ALU.mult, eng="gpsimd")
            if G.stop <= 1: continue
            for c in range(2):
                P.ts(btc[:, c, :], tok[:, 2, :], G.cind[:, c:c + 1], ALU.mult, eng=("gpsimd" if c else "vector"))
                P.ts(ktc[:, c, :], tok[:, 3, :], G.cind[:, c:c + 1], ALU.mult, eng=("gpsimd" if c else "vector"))
            for rnd in range(2):
                for qq in range(2):
                    q = rnd * 2 + qq
                    for h in range(4):
                        P.tr(bank(rnd * 2 + qq, 128, h * 128, 0, 64), tok[:, q, h * 64:(h + 1) * 64], G.identf[:])
                P.copy(fmh[:, rnd * 2:rnd * 2 + 2].rearrange("p a h t -> p (a h t)"), ps[0:64, rnd * 1024:(rnd + 1) * 1024],
                       eng=("scalar" if rnd else "vector"))
            if G.stop <= 2: continue
            for h in range(4):
                P.mm(bank(5, 2, 256 + 2 * h, 0, 64), lw_[:, h * 64:(h + 1) * 64], G.cind[:])
            P.act(gC[:].rearrange("p h c -> p (h c)"), bank(5, 8, 256, 0, 64), AF.Exp)
            if G.stop <= 3: continue
            for h in range(4):
                khT = fmh[:, 0, h, :]; rtT = fmh[:, 1, h, :]; btT = fmh[:, 2, h, :]; ktT = fmh[:, 3, h, :]
                P.mm(bank(0, 128, h * 128), btT, khT)
                P.mm(bank(1, 128, h * 128), btT, rtT)
                P.mm(bank(2, 128, h * 128), ktT, khT)
                P.mm(bank(3, 128, h * 128), ktT, rtT)
                P.mm(bank(4, 128, h * 128), khT, btT)

            def b4(k):
                return bank(k).rearrange("p (h q) -> p h q", h=4)

            def bc4(m):
                return m[:].unsqueeze(1).to_broadcast([128, 4, 128])
            idb4 = G.identf[:].unsqueeze(1).to_broadcast([128, 4, 128])
            P.tt(Q[:, 0], b4(0), bc4(m_abT), ALU.mult)
            P.tt(QT[:, 0], b4(4), bc4(m_ab), ALU.mult)
            P.tt(Aarb[:], b4(1), bc4(m_i), ALU.mult)
            P.tt(Aak[:], b4(2), bc4(m_s), ALU.mult)
            P.tt(Aark[:], b4(3), bc4(m_i), ALU.mult)
            P.tt(IQT[:], QT[:, 0], idb4, ALU.add, eng="gpsimd")
            P.tt(Z[:, 0], Q[:, 0], idb4, ALU.add, eng="gpsimd")
            if G.stop <= 4: continue
            zc = 0
            for k in range(1, 6):
                a, bq = (k - 1) % 2, k % 2
                for h in range(4):
                    if k < 5:
                        P.mm(bank(0, 128, h * 128), QT[:, a, h, :], Q[:, a, h, :])
                    P.mm(bank(1, 128, h * 128), Q[:, a, h, :], QT[:, a, h, :])
                if k >= 2:
                    for h in range(4):
                        P.mm(bank(2, 128, h * 128), IQT[:, h, :], Z[:, zc, h, :])
                if k < 5:
                    P.copy(Q[:, bq], b4(0), eng="scalar")
                P.copy(QT[:, bq], b4(1))
                if k >= 2:
                    P.copy(Z[:, 1 - zc], b4(2), eng="scalar")
                    zc = 1 - zc
                P.tt(IQT[:], QT[:, bq], idb4, ALU.add, eng="gpsimd")
            for h in range(4):
                P.mm(bank(2, 128, h * 128), IQT[:, h, :], Z[:, zc, h, :])
            P.copy(Z[:, 1 - zc], b4(2), eng="scalar")
            zc = 1 - zc
            TT = Z[:, zc]
            if G.stop <= 5: continue
            for h in range(4):
                hc = slice(h * 64, (h + 1) * 64)
                P.mm(bank(0, 64, h * 64), TT[:, h, :], tok[:, 0, hc])
                P.mm(bank(1, 64, h * 64), Aak[:, h, :], v_[:, hc])
            P.copy(Wsb[:].rearrange("p h q -> p (h q)"), bank(0, 256), eng="scalar")
            P.ts(nAkV[:].rearrange("p h q -> p (h q)"), bank(1, 256), -1.0, ALU.mult)
            for h in range(4):
                P.mm(bank(2, 64, h * 64), TT[:, h, :], nAkV[:, h, :])
            P.copy(Uloc[:].rearrange("p h q -> p (h q)"), bank(2, 256), eng="scalar")
            if G.stop <= 6: continue
            for h in range(4):
                hc = slice(h * 64, (h + 1) * 64)
                for c in range(2):
                    P.mm(bank(3, 64, (h * 2 + c) * 64, 0, 64), Wsb[:, h, :], btc[:, c, hc])
                P.mm(bank(4, 128, h * 128, 0, 64), Wsb[:, h, :], Aarb[:, h, :])
            id8 = G.identf[0:64, 0:64].unsqueeze(1).to_broadcast([64, 8, 64])
            P.tt(GpT[:].rearrange("p h c q -> p (h c) q"), id8, bank(3, 512, 0, 0, 64).rearrange("p (a q) -> p a q", a=8), ALU.subtract)
            P.tt(RwT[:], fmh[:, 1], bank(4, 512, 0, 0, 64).rearrange("p (h q) -> p h q", h=4), ALU.subtract)
            if G.stop <= 7: continue
            for c in ([0, 1] if d == 0 else [1, 0]):
                cs_ = slice(64 * c, 64 * c + 64)
                for h in range(4):
                    hc = slice(h * 64, (h + 1) * 64)
                    yp = bank(1, 64, (c * 4 + h) * 64, 0, 64)
                    P.mm(yp, Aarb[:, h, cs_], Uloc[:, h, :], start=True, stop=False)
                    P.mm(yp, Aark[:, h, cs_], v_[:, hc], start=False, stop=False)
                    P.mm(yp, RwT[:, h, cs_], H32[:, h, :], start=False, stop=True)
                for h in range(4):
                    hc = slice(h * 64, (h + 1) * 64)
                    hp = bank(0, 64, h * 64, 0, 64)
                    P.mm(hp, btc[:, c, hc], Uloc[:, h, :], start=True, stop=False)
                    P.mm(hp, ktc[:, c, hc], v_[:, hc], start=False, stop=False)
                    P.mm(hp, GpT[:, h, c, :], H32[:, h, :], start=False, stop=True)
                P.tt(H32[:], bank(0, 256, 0, 0, 64).rearrange("p (h q) -> p h q", h=4),
                     gC[:, :, c].unsqueeze(2).to_broadcast([64, 4, 64]), ALU.mult)
            P.copy(Yt[:, b].rearrange("p c q -> p (c q)"), bank(1, 512, 0, 0, 64), eng="scalar")
            P.dma("sync", G.rwy[d, rows, :].rearrange("(c p) q -> p c q", p=64), Yt[:, b])
    ph.close()


def rwkv_fin(nc, P, G, l):
    W = G.w
    ph = Phase(nc, P)
    lw_ = ph.sb("lnxw", [128, 256]); lb_ = ph.sb("lnxb", [128, 256])
    yf = ph.sb("ryf", [128, 2, 256]); yb = ph.sb("ryb", [128, 2, 256]); gb = ph.sb("rgb", [128, 2, 2, 256])
    sq = ph.sb("rsq", [128, 256]); s4 = ph.sb("rfs4", [128, 2, 8])
    P.dma("sync", lw_[:], bc_rows(W["rwkv_lnx_w"][l:l + 1, :]))
    P.dma("sync", lb_[:], bc_rows(W["rwkv_lnx_b"][l:l + 1, :]))
    for i in range(NT):
        b = i % 2
        rows = slice(i * 128, (i + 1) * 128)
        P.dma("sync", yf[:, b, :], G.rwy[0, rows, :])
        P.dma("sync", yb[:, b, :], G.rwy[1, rows, :])
        P.dma("sync", gb[:, b].rearrange("p a b -> p (a b)"), G.rwp[rows, 9 * 256:11 * 256])
        y = yf[:, b, :]
        y3 = y.rearrange("p (h q) -> p h q", h=4)
        S = s4[:, b, :]
        P.tt(y, y, yb[:, b, :], ALU.add)
        P.op("vector", lambda e, y3=y3, S=S: e.tensor_reduce(S[:, 0:4], y3, AX.X, ALU.add), reads=[y], writes=[S[:, 0:4]])
        P.ts(S[:, 0:4], S[:, 0:4], 1.0 / 64, ALU.mult)
        P.tt(y3, y3, S[:, 0:4].unsqueeze(2).to_broadcast([128, 4, 64]), ALU.subtract)
        P.tt(sq[:], y, y, ALU.mult, eng="gpsimd")
        P.op("vector", lambda e, S=S: e.tensor_reduce(S[:, 4:8], sq[:].rearrange("p (h q) -> p h q", h=4), AX.X, ALU.add),
             reads=[sq[:]], writes=[S[:, 4:8]])
        P.act(S[:, 4:8], S[:, 4:8], AF.Sqrt, bias=64e-5, scale=1.0 / 64)
        P.op("vector", lambda e, S=S: e.reciprocal(S[:, 4:8], S[:, 4:8]), reads=[S[:, 4:8]], writes=[S[:, 4:8]])
        P.tt(y3, y3, S[:, 4:8].unsqueeze(2).to_broadcast([128, 4, 64]), ALU.mult)
        P.tt(y, y, lw_[:], ALU.mult, eng="gpsimd")
        P.tt(y, y, lb_[:], ALU.add)
        P.tt(y, y, gb[:, b, 1, :], ALU.add)
        P.tt(y, y, gb[:, b, 0, :], ALU.mult)
        P.dma("sync", G.ymix[rows, 512:768], y)
    ph.close()
def tiles_for(l, n_layers_total=4):
    return list(range(2, NT)) if l == n_layers_total - 1 else list(range(NT))


def phase_outproj(nc, P, G, l):
    W = G.w
    ph = Phase(nc, P)
    woutb = ph.sb("woutb", [128, 8, 1024], BF16); wst = ph.sb("owst", [128, 8, 512])
    rw32 = ph.sb("rw32", [128, 8, 64]); rbias = ph.sb("rbias", [128, 64])
    mg1 = ph.sb("mg1", [128, 2, 1024]); msc2 = ph.sb("msc2", [128, 2, 1024]); msh2 = ph.sb("msh2", [128, 2, 1024])
    l1w = ph.sb("l1w", [128, 1024]); l1b = ph.sb("l1b", [128, 1024])
    ym = ph.sb("oym", [128, 2, 1024]); ymb = ph.sb("oymb", [128, 1024], BF16); ymT = ph.sb("oymT", [128, 8, 128], BF16)
    xt = ph.sb("oxt", [128, 2, 1024]); t = ph.sb("ot", [128, 1024]); hh = ph.sb("ohh", [128, 1024])
    hT32 = ph.sb("hT32", [128, 8, 128]); hTb = ph.sb("hTb", [128, 2, 8, 128], BF16)
    st6 = ph.sb("ost6", [128, 2, 6]); mv = ph.sb("omv", [128, 2]); rstd = ph.sb("orstd", [128, 1])
    sc = ph.sb("rsc", [128, 64]); sel = ph.sb("rsel", [128, 64]); m8 = ph.sb("rm8", [128, 8, 8]); gs = ph.sb("rgs", [128, 8])
    g8 = ph.sb("rg8", [128, 8]); gm = ph.sb("rgm", [128, 8]); pen = ph.sb("rpen", [128, 8]); s8 = ph.sb("rs8", [128, 8])
    selm = ph.sb("rselm", [128, 64]); wts = ph.sb("rwts", [128, 64]); den = ph.sb("rden", [128, 1]); gT = ph.sb("rgT", [64, 2, 128])
    for n in range(2):
        P.dma("sync", wst[:], W["w_out"][l, :, n * 512:(n + 1) * 512].rearrange("(kc p) j -> p kc j", p=128))
        P.copy(woutb[:, :, n * 512:(n + 1) * 512], wst[:], eng="gpsimd")
    P.dma("sync", rw32[:], W["router_w"][l].rearrange("(kc p) e -> p kc e", p=128))
    P.dma("sync", rbias[:], bc_rows(W["router_b"][l:l + 1, :]))
    for s in range(2):
        P.dma("sync", mg1[:, s, :], G.modd[s, 2])
        P.dma("sync", msc2[:, s, :], G.modd[s, 4])
        P.dma("sync", msh2[:, s, :], G.modd[s, 3])
    P.dma("sync", l1w[:], bc_rows(W["ln1_w"][l:l + 1, :]))
    P.dma("sync", l1b[:], bc_rows(W["ln1_b"][l:l + 1, :]))
    for i in tiles_for(l):
        b = i % 2
        s = seg_of(i)
        rows = slice(i * 128, (i + 1) * 128)
        P.dma("sync", ym[:, b, :], G.ymix[rows, :])
        P.dma("sync", xt[:, b, :], G.xres[rows, :])
        P.copy(ymb[:], ym[:, b, :], eng="gpsimd")
        for kc in range(8):
            P.tr(G.psb[:, kc * 128:(kc + 1) * 128], ymb[:, kc * 128:(kc + 1) * 128], G.identb[:])
        P.copy(ymT[:].rearrange("p a b -> p (a b)"), G.psb[:, 0:1024], eng="scalar")
        for n in range(2):
            pso = G.psf[:, n * 512:(n + 1) * 512]
            for kc in range(8):
                P.mm(pso, ymT[:, kc, :], woutb[:, kc, n * 512:(n + 1) * 512], start=(kc == 0), stop=(kc == 7))
            P.tt(t[:, n * 512:(n + 1) * 512], pso, mg1[:, s, n * 512:(n + 1) * 512], ALU.mult)
        x = xt[:, b, :]
        P.stt(t[:], x, DN_ALPHA, t[:], ALU.mult, ALU.add)
        ln_stats(P, ph, t[:], "o1", st6[:], mv[:], rstd[:])
        P.ts(t[:], t[:], mv[:, 0:1], ALU.subtract, rstd[:, 0:1], ALU.mult)
        P.tt(t[:], t[:], l1w[:], ALU.mult, eng="gpsimd")
        P.tt(x, t[:], l1b[:], ALU.add)
        P.dma("sync", G.xres[rows, :], x)
        ln_stats(P, ph, x, "o2", st6[:], mv[:], rstd[:])
        P.ts(hh[:], x, mv[:, 0:1], ALU.subtract, rstd[:, 0:1], ALU.mult)
        P.tt(hh[:], hh[:], msc2[:, s, :], ALU.mult, eng="gpsimd")
        P.tt(hh[:], hh[:], msh2[:, s, :], ALU.add)
        if G.dbg is not None and "hdbg" in G.dbg:
            P.dma("sync", G.dbg["hdbg"][rows, :], hh[:])
        for half in range(2):
            for kk in range(4):
                kc = half * 4 + kk
                P.tr(G.psf[:, (2 + half) * 512 + kk * 128:(2 + half) * 512 + (kk + 1) * 128], hh[:, kc * 128:(kc + 1) * 128], G.identf[:])
            P.copy(hT32[:, half * 4:half * 4 + 4, :].rearrange("p a b -> p (a b)"), G.psf[:, (2 + half) * 512:(3 + half) * 512], eng="scalar")
        P.copy(hTb[:, b], hT32[:], eng="gpsimd")
        P.dma("sync", G.hT[:, :, rows].rearrange("kc p t -> p kc t"), hTb[:, b])
        lg = G.psf[:, 4 * 512:4 * 512 + 64]
        for kc in range(8):
            P.mm(lg, hT32[:, kc, :], rw32[:, kc, :], start=(kc == 0), stop=(kc == 7))
        P.act(sc[:], lg, AF.Sigmoid)
        P.tt(sel[:], sc[:], rbias[:], ALU.add)
        for g in range(8):
            P.op("vector", lambda e, g=g: e.max(m8[:, g, :], sel[:, g * 8:(g + 1) * 8]), reads=[sel[:, g * 8:(g + 1) * 8]], writes=[m8[:, g, :]])
        P.tt(gs[:], m8[:, :, 0], m8[:, :, 1], ALU.add)
        P.op("vector", lambda e: e.max(g8[:], gs[:]), reads=[gs[:]], writes=[g8[:]])
        P.ts(gm[:], gs[:], g8[:, 3:4], ALU.is_ge)
        P.ts(pen[:], gm[:], 1.0, ALU.subtract, 1e30, ALU.mult)
        sel3 = sel[:].rearrange("p (g q) -> p g q", g=8)
        selm3 = selm[:].rearrange("p (g q) -> p g q", g=8)
        P.tt(selm3, sel3, gm[:].unsqueeze(2).to_broadcast([128, 8, 8]), ALU.mult)
        P.tt(selm3, selm3, pen[:].unsqueeze(2).to_broadcast([128, 8, 8]), ALU.add)
        P.op("vector", lambda e: e.max(s8[:], selm[:]), reads=[selm[:]], writes=[s8[:]])
        P.ts(wts[:], selm[:], s8[:, 7:8], ALU.is_ge)
        P.tt(wts[:], wts[:], sc[:], ALU.mult)
        P.op("vector", lambda e: e.tensor_reduce(den[:], wts[:], AX.X, ALU.add), reads=[wts[:]], writes=[den[:]])
        P.op("vector", lambda e: e.reciprocal(den[:], den[:]), reads=[den[:]], writes=[den[:]])
        P.ts(wts[:], wts[:], den[:, 0:1], ALU.mult, 2.5, ALU.mult)
        gtp = G.psf[0:64, 5 * 512:5 * 512 + 128]
        P.tr(gtp, wts[:], G.identf[:])
        P.copy(gT[:, b, :], gtp)
        P.dma("sync", G.gT[:, rows], gT[:, b, :])
    ph.close()


def phase_moe(nc, P, G, l):
    W = G.w
    tl = tiles_for(l)
    half = (len(tl) + 1) // 2
    for (pa, tiles) in enumerate((tl[:half], tl[half:])):
        ph = Phase(nc, P)
        ntl = len(tiles)
        ntok = ntl * 128
        t0 = tiles[0] * 128
        acc = ph.sb("macc", [128, ntl, 1024])
        hTp = ph.sb("mhT", [128, 8, ntok], BF16); gTp = ph.sb("mgT", [64, ntok])
        w13s = ph.sb("w13s", [128, 8, 512]); w13b = ph.sb("w13b", [128, 2, 8, 512], BF16)
        w2s = ph.sb("w2s", [128, 2, 1024]); w2b = ph.sb("w2b", [128, 2, 2, 1024], BF16)
        hid = ph.sb("mhid", [128, 2, 2, 512], BF16); sil = ph.sb("msil", [128, 2, 512])
        P.dma("sync", hTp[:], G.hT[:, :, t0:t0 + ntok].rearrange("kc p t -> p kc t"))
        P.dma("sync", gTp[:], G.gT[:, t0:t0 + ntok])
        groups = []
        c0 = 0
        while c0 < ntok:
            n = min(512, ntok - c0)
            groups.append((c0, n)); c0 += n
        NE = 65
        orot = [0]
        pending = [None]

        def emit_down(e, eb, hb, c0, n):
            for sub in range(n // 128):
                ti = (c0 // 128) + sub
                for cc in range(2):
                    ob = 5 + (orot[0] % 3)
                    orot[0] += 1
                    o = G.psf[:, ob * 512:(ob + 1) * 512]
                    for fc in range(2):
                        P.mm(o, hid[:, hb, fc, sub * 128:(sub + 1) * 128], w2b[:, eb, fc, cc * 512:(cc + 1) * 512],
                             start=(fc == 0), stop=(fc == 1))
                    a = acc[:, ti, cc * 512:(cc + 1) * 512]
                    if e == 0:
                        P.copy(a, o)
                    else:
                        P.tt(a, a, o, ALU.add)
        for e in range(NE):
            eb = e % 2
            if e < 64:
                s1, s3, s2 = W["exp_w1"][l, e], W["exp_w3"][l, e], W["exp_w2"][l, e]
            else:
                s1, s3, s2 = W["sh_w1"][l], W["sh_w3"][l], W["sh_w2"][l]
            import os
            if e < 2 or not os.environ.get("KNOW"):
                P.dma("sync", w13s[:, :, 0:256], s1.rearrange("(kc p) f -> p kc f", p=128))
                P.dma("sync", w13s[:, :, 256:512], s3.rearrange("(kc p) f -> p kc f", p=128))
                P.dma("sync", w2s[:], s2.rearrange("(fc p) c -> p fc c", p=128))
                P.copy(w13b[:, eb], w13s[:], eng="gpsimd")
                P.copy(w2b[:, eb], w2s[:], eng="gpsimd")
            for gi, (c0, n) in enumerate(groups):
                hb = (e * len(groups) + gi) % 2
                cols = slice(c0, c0 + n)
                if e < 64:
                    gb = G.psf[:, 4 * 512:4 * 512 + n]
                    P.mm(gb, G.identf[0:64, e:e + 1].to_broadcast([64, 128]), gTp[:, cols])
                for fc in range(2):
                    h1 = G.psf[:, fc * 512:fc * 512 + n]
                    h3 = G.psf[:, (2 + fc) * 512:(2 + fc) * 512 + n]
                    for kc in range(8):
                        P.mm(h1, w13b[:, eb, kc, fc * 128:(fc + 1) * 128], hTp[:, kc, cols], start=(kc == 0), stop=(kc == 7))
                    for kc in range(8):
                        P.mm(h3, w13b[:, eb, kc, 256 + fc * 128:256 + (fc + 1) * 128], hTp[:, kc, cols], start=(kc == 0), stop=(kc == 7))
                    P.act(sil[:, fc, 0:n], h1, AF.Silu)
                    if e < 64:
                        P.tt(sil[:, fc, 0:n], sil[:, fc, 0:n], h3, ALU.mult)
                        P.tt(hid[:, hb, fc, 0:n], sil[:, fc, 0:n], gb, ALU.mult)
                    else:
                        P.tt(hid[:, hb, fc, 0:n], sil[:, fc, 0:n], h3, ALU.mult)
                if pending[0] is not None:
                    emit_down(*pending[0])
                pending[0] = (e, eb, hb, c0, n)
        emit_down(*pending[0])
        pending[0] = None
        mg2 = ph2 = None
        fin = Phase(nc, P)
        mg2 = fin.sb("mg2", [128, 2, 1024]); l2w = fin.sb("l2w", [128, 1024]); l2b = fin.sb("l2b", [128, 1024])
        xt = fin.sb("mxt", [128, 2, 1024]); st6 = fin.sb("mst6", [128, 2, 6]); mv = fin.sb("mmv", [128, 2]); rstd = fin.sb("mrstd", [128, 1])
        for s in range(2):
            P.dma("sync", mg2[:, s, :], G.modd[s, 5])
        P.dma("sync", l2w[:], bc_rows(W["ln2_w"][l:l + 1, :]))
        P.dma("sync", l2b[:], bc_rows(W["ln2_b"][l:l + 1, :]))
        for k, i in enumerate(tiles):
            b = k % 2
            s = seg_of(i)
            rows = slice(i * 128, (i + 1) * 128)
            x = xt[:, b, :]
            P.dma("sync", x, G.xres[rows, :])
            f = acc[:, k, :]
            if G.dbg is not None and "fdbg" in G.dbg:
                P.dma("sync", G.dbg["fdbg"][rows, :], f)
            P.tt(f, f, mg2[:, s, :], ALU.mult, eng="gpsimd")
            P.stt(f, x, DN_ALPHA, f, ALU.mult, ALU.add)
            ln_stats(P, fin, f, "m", st6[:], mv[:], rstd[:])
            P.ts(f, f, mv[:, 0:1], ALU.subtract, rstd[:, 0:1], ALU.mult)
            P.tt(f, f, l2w[:], ALU.mult, eng="gpsimd")
            P.tt(x, f, l2b[:], ALU.add)
            if G.dbg is not None and "xout" in G.dbg:
                P.dma("sync", G.dbg["xout"][rows, :], x)
            if l == 3 and G.final_out:
                P.dma("sync", G.out[(i - 2) * 128:(i - 1) * 128, :], x, final=True)
            else:
                P.dma("sync", G.xres[rows, :], x)
        fin.st.close()
        ph.close()
def alloc_scratch(nc, G, scratch):
    G.xbca = scratch("xbca", [T_ALL, 1024])
    G.dtla = scratch("dtla", [T_ALL, 32])
    G.ydir = scratch("ydir", [2, T_ALL, 512])
    G.gla_la = scratch("gla_la", [T_ALL, 256])
    G.gla_o = scratch("gla_o", [2, T_ALL, 256])
    G.rwp = scratch("rwp", [T_ALL, 11 * 256])
    G.rwy = scratch("rwy", [2, T_ALL, 256])
    G.hT = scratch("hT", [8, 128, T_ALL], BF16)
    G.gT = scratch("gT", [64, T_ALL])

PHASES = {"mod": phase_mod, "inproj": phase_inproj, "ssd": phase_ssd, "ssd_prep": ssd_prep, "ssd_dir": ssd_dir, "ssd_fin": ssd_fin,
          "gla": phase_gla, "rwkv": phase_rwkv, "rwkv_prep": rwkv_prep, "rwkv_dir": rwkv_dir, "rwkv_fin": rwkv_fin, "outproj": phase_outproj, "moe": phase_moe}
WNAMES = ["ada_w", "ada_b", "w_in", "ssd_conv_w", "ssd_conv_b", "ssd_dt_bias", "ssd_a_log", "ssd_d", "ssd_norm_w",
          "rwkv_mu", "rwkv_w0", "rwkv_w2", "rwkv_a0", "rwkv_a2", "rwkv_g2", "rwkv_kk", "rwkv_ka", "rwkv_rk",
          "rwkv_lnx_w", "rwkv_lnx_b", "gla_gu", "gla_gb", "gla_norm_w", "w_out", "ln1_w", "ln1_b", "ln2_w", "ln2_b",
          "router_w", "router_b", "exp_w1", "exp_w3", "exp_w2", "sh_w1", "sh_w3", "sh_w2"]
WSHAPES = {"ada_w": [4, 1024, 6144], "ada_b": [4, 6144], "w_in": [4, 1024, 3504], "ssd_conv_w": [4, 3, 1024],
           "ssd_conv_b": [4, 1024], "ssd_dt_bias": [4, 2, 8], "ssd_a_log": [4, 2, 8], "ssd_d": [4, 8],
           "ssd_norm_w": [4, 512], "rwkv_mu": [4, 4, 1152], "rwkv_w0": [4, 2, 256], "rwkv_w2": [4, 2, 64, 256],
           "rwkv_a0": [4, 2, 256], "rwkv_a2": [4, 2, 64, 256], "rwkv_g2": [4, 128, 256], "rwkv_kk": [4, 256],
           "rwkv_ka": [4, 256], "rwkv_rk": [4, 4, 64], "rwkv_lnx_w": [4, 256], "rwkv_lnx_b": [4, 256],
           "gla_gu": [4, 2, 16, 128], "gla_gb": [4, 2, 128], "gla_norm_w": [4, 256], "w_out": [4, 1024, 1024],
           "ln1_w": [4, 1024], "ln1_b": [4, 1024], "ln2_w": [4, 1024], "ln2_b": [4, 1024],
           "router_w": [4, 1024, 64], "router_b": [4, 64], "exp_w1": [4, 64, 1024, 256], "exp_w3": [4, 64, 1024, 256],
           "exp_w2": [4, 64, 256, 1024], "sh_w1": [4, 1024, 256], "sh_w3": [4, 1024, 256], "sh_w2": [4, 256, 1024]}


def build_program(layers=(0, 1, 2, 3), phases=None, dbg=(), wnames=None, feed=()):
    nc = bass.Bass("TRN2", target_bir_lowering=False)
    P = Prog(nc)
    G = Ctx()
    G.w = {}
    import os
    G.stop = float(os.environ.get('KSTOP', '99'))
    for n in (wnames or WNAMES):
        G.w[n] = nc.dram_tensor(n, WSHAPES[n], F32, kind="ExternalInput").ap()
    G.x_in = nc.dram_tensor("x", [4096, 1024], F32, kind="ExternalInput").ap()
    G.ctx_in = nc.dram_tensor("ctx", [256, 1024], F32, kind="ExternalInput").ap()
    G.cvec = nc.dram_tensor("cvec", [2, 1024], F32, kind="ExternalInput").ap()
    G.out = nc.dram_tensor("out", [4096, 1024], F32, kind="ExternalOutput").ap()
    DBG_SHAPES = {"u": ([T_ALL, 1024], BF16), "modd": ([2, 6, 128, 1024], F32), "zin": ([T_ALL, NIN], F32),
                  "ymix": ([T_ALL, 1024], F32), "xres": ([T_ALL, 1024], F32)}
    G.dbg = {}
    G.final_out = (phases is None or "moe" in phases)

    def scratch(name, shape, dt=F32):
        kind = "ExternalOutput" if name in dbg else ("ExternalInput" if name in feed else "Internal")
        t = nc.dram_tensor(name, list(shape), dt, kind=kind).ap()
        return t
    for n in dbg:
        if n in ("u",):
            G.dbg[n] = nc.dram_tensor(n, DBG_SHAPES[n][0], DBG_SHAPES[n][1], kind="ExternalOutput").ap()
        if n in ("hdbg", "fdbg", "xout"):
            G.dbg[n] = nc.dram_tensor(n, [T_ALL, 1024], F32, kind="ExternalOutput").ap()
    G.xres = scratch("xres", [T_ALL, 1024])
    G.modd = scratch("modd", [2, 6, 128, 1024])
    G.zin = scratch("zin", [T_ALL, NIN])
    G.ymix = scratch("ymix", [T_ALL, 1024])
    G.psf = nc.alloc_psum_tensor("psa", [128, 8 * 512], F32)
    G.psb = G.psf.bitcast(BF16)[:, 6 * 1024:8 * 1024]
    make_consts(nc, P, G)
    alloc_scratch(nc, G, scratch)
    if "xres" not in feed:
        P.dma("sync", G.xres[0:256, :], G.ctx_in)
        P.dma("sync", G.xres[256:T_ALL, :], G.x_in)
    P.barrier()
    allp = ["mod", "inproj", "ssd", "gla", "rwkv", "outproj", "moe"]
    for l in layers:
        for pn in (phases or allp):
            PHASES[pn](nc, P, G, l)
    if phases is None or "moe" in phases:
        pass
    else:
        pass
    P.barrier()
    P.dma("sync", G.out[0:128, 0:8], G.xres[0:128, 0:8], final=True) if (phases is not None and "moe" not in phases) else None
    P.finish()
    return nc, P


_NC_CACHE = {}


def kernel(**inputs):
    from concourse.bass_utils import run_bass_kernel_spmd
    if "nc" not in _NC_CACHE:
        nc, P = build_program()
        _NC_CACHE["nc"] = nc
    nc = _NC_CACHE["nc"]
    B = 4
    base = {n: np.ascontiguousarray(np.asarray(inputs[n], dtype=np.float32)) for n in WNAMES}
    x = np.asarray(inputs["x"], dtype=np.float32)
    ctx = np.asarray(inputs["ctx"], dtype=np.float32)
    c = np.asarray(inputs["c"], dtype=np.float32)
    c_ctx = np.asarray(inputs["c_ctx"], dtype=np.float32)
    in_maps = []
    for b in range(B):
        m = dict(base)
        m["x"] = np.ascontiguousarray(x[b])
        m["ctx"] = np.ascontiguousarray(ctx[b])
        m["cvec"] = np.ascontiguousarray(np.stack([c_ctx, c[b]]))
        in_maps.append(m)
    res = run_bass_kernel_spmd(nc, in_maps, core_ids=list(range(B)))
    out = np.stack([np.asarray(res.results[b]["out"], dtype=np.float32) for b in range(B)])
    return out
```

```python
import numpy as np
import concourse.bass as bass
import concourse.mybir as mybir

F32 = mybir.dt.float32
BF16 = mybir.dt.bfloat16
I32 = mybir.dt.int32
U32 = mybir.dt.uint32
AF = mybir.ActivationFunctionType
ALU = mybir.AluOpType
AX = mybir.AxisListType

_DSZ = {F32: 4, BF16: 2, I32: 4, U32: 4}

ENGS = ("tensor", "vector", "scalar", "gpsimd", "sync")
DMA_K = {"sync": 16, "gpsimd": 8, "scalar": 4}


def _box(ap):
    t = ap.tensor
    name = t.name
    dsz = _DSZ.get(ap.dtype, 4)
    steps = list(ap.ap)
    off = ap.offset
    space = str(ap.space) if hasattr(ap, "space") else ""
    if "DRAM" in space.upper() or "dram" in space.lower() or type(t).__name__.startswith("DRam"):
        lo = off + sum(min(0, s * (c - 1)) for s, c in steps)
        hi = off + sum(max(0, s * (c - 1)) for s, c in steps) + 1
        return name, 0, 1, lo * dsz, hi * dsz
    pstep, pcnt = steps[0]
    if pstep == 0:
        pstep = 1 << 30
    p_lo = off // pstep
    f_off = off - p_lo * pstep
    rest = steps[1:]
    lo = f_off + sum(min(0, s * (c - 1)) for s, c in rest)
    hi = f_off + sum(max(0, s * (c - 1)) for s, c in rest) + 1
    lo *= dsz
    hi *= dsz
    if name.startswith("ps"):
        lo = (lo // 2048) * 2048
        hi = ((hi + 2047) // 2048) * 2048
        p_lo, pcnt = 0, 128
    return name, p_lo, p_lo + pcnt, lo, hi


class Prog:
    def __init__(self, nc):
        self.nc = nc
        self.ops = {e: [] for e in ENGS}
        self.cnt = {e: 0 for e in ENGS}
        self.dcnt = {e: 0 for e in DMA_K}
        self.waited = {e: {} for e in ENGS}
        self.recs = {}
        self.semkeys = set()
        self.final_tokens = []
        self.n_ops = 0

    def _deps(self, reads, writes, pe_accum=False, eng=None):
        deps = set()
        rb = [_box(a) for a in reads]
        wb = [_box(a) for a in writes]
        for (name, p0, p1, f0, f1) in rb:
            isps = name.startswith("ps")
            for r in self.recs.get(name, ()):
                if r[0] < p1 and p0 < r[1] and r[2] < f1 and f0 < r[3]:
                    if r[4] == "w" or (isps and r[5][0] != eng):
                        deps.add(r[5])
        for (name, p0, p1, f0, f1) in wb:
            for r in self.recs.get(name, ()):
                if r[0] < p1 and p0 < r[1] and r[2] < f1 and f0 < r[3]:
                    if r[4] == "w" and r[5][0] == "tensor" and eng == "tensor":
                        continue
                    deps.add(r[5])
        return deps, rb, wb

    def _update(self, rb, wb, tok):
        for (name, p0, p1, f0, f1) in wb:
            lst = self.recs.setdefault(name, [])
            lst[:] = [r for r in lst if not (p0 <= r[0] and r[1] <= p1 and f0 <= r[2] and r[3] <= f1)]
            lst.append((p0, p1, f0, f1, "w", tok))
        for (name, p0, p1, f0, f1) in rb:
            lst = self.recs.setdefault(name, [])
            lst[:] = [r for r in lst if not (r[4] == "r" and r[5][0] == tok[0] and p0 <= r[0] and r[1] <= p1
                                             and f0 <= r[2] and r[3] <= f1)]
            lst.append((p0, p1, f0, f1, "r", tok))

    def _waits(self, eng, deps):
        w = self.waited[eng]
        best = {}
        for (k, v) in deps:
            if v > best.get(k, 0):
                best[k] = v
        out = []
        for k, v in best.items():
            if w.get(k, 0) >= v:
                continue
            w[k] = v
            out.append((k, v))
        return out

    def op(self, eng, fn, reads=(), writes=(), pe_accum=False):
        deps, rb, wb = self._deps(reads, writes, pe_accum, eng)
        self.cnt[eng] += 1
        tok = (eng, self.cnt[eng])
        waits = self._waits(eng, deps)
        self.semkeys.add(eng)
        self.ops[eng].append((waits, fn, (eng, 1)))
        self._update(rb, wb, tok)
        self.n_ops += 1
        return tok

    def dma(self, q, out, in_, final=False, **kw):
        deps, rb, wb = self._deps([in_], [out])
        n = self.dcnt[q]
        self.dcnt[q] += 1
        K = DMA_K[q]
        key = ("dma", q, n % K)
        tok = (key, 16 * (n // K + 1))
        if n >= K:
            deps.add((key, 16 * (n // K)))
        waits = self._waits(q, deps)
        self.semkeys.add(key)
        self.ops[q].append((waits, (lambda e, o=out, i=in_, kw=kw: e.dma_start(out=o, in_=i, **kw)), (key, 16)))
        self._update(rb, wb, tok)
        if final:
            self.final_tokens.append(tok)
        self.n_ops += 1
        return tok

    def barrier(self):
        toks = set()
        for e in ENGS:
            if self.cnt[e] > 0:
                toks.add((e, self.cnt[e]))
        for q, K in DMA_K.items():
            n = self.dcnt[q]
            for j in range(min(n, K)):
                last = ((n - 1 - j) // K) * K + j
                toks.add((("dma", q, j), 16 * (last // K + 1)))
        for e in ENGS:
            waits = self._waits(e, set(toks))
            if waits:
                self.ops[e].append((waits, None, None))
        self.recs.clear()

    def mm(self, out, lhsT, rhs, start=True, stop=True):
        return self.op("tensor", lambda e: e.matmul(out, lhsT, rhs, start=start, stop=stop),
                       reads=[lhsT, rhs], writes=[out], pe_accum=not start)

    def tr(self, out, in_, ident):
        return self.op("tensor", lambda e: e.transpose(out, in_, ident), reads=[in_, ident], writes=[out])

    def act(self, out, in_, func, bias=None, scale=None, eng="scalar", accum_out=None):
        reads = [in_]
        kw = {}
        if bias is not None:
            kw["bias"] = bias
            if not isinstance(bias, (int, float)):
                reads.append(bias)
        if scale is not None:
            kw["scale"] = scale
            if not isinstance(scale, (int, float)):
                reads.append(scale)
        writes = [out]
        if accum_out is not None:
            kw["accum_out"] = accum_out
            writes.append(accum_out)
        return self.op(eng, lambda e: e.activation(out, in_, func, **kw), reads=reads, writes=writes)

    def tt(self, out, in0, in1, op, eng="vector"):
        return self.op(eng, lambda e: e.tensor_tensor(out, in0, in1, op), reads=[in0, in1], writes=[out])

    def ts(self, out, in0, s1, op0, s2=None, op1=None, eng="vector", accum_out=None):
        reads = [in0]
        if not isinstance(s1, (int, float)):
            reads.append(s1)
        if s2 is not None and not isinstance(s2, (int, float)):
            reads.append(s2)
        writes = [out]
        kw = {}
        if accum_out is not None:
            kw["accum_out"] = accum_out
            writes.append(accum_out)
        if op1 is None:
            return self.op(eng, lambda e: e.tensor_scalar(out, in0, s1, None, op0, **kw), reads=reads, writes=writes)
        return self.op(eng, lambda e: e.tensor_scalar(out, in0, s1, s2, op0, op1, **kw), reads=reads, writes=writes)

    def stt(self, out, in0, scalar, in1, op0, op1, eng="vector"):
        reads = [in0, in1]
        if not isinstance(scalar, (int, float)):
            reads.append(scalar)
        return self.op(eng, lambda e: e.scalar_tensor_tensor(out, in0, scalar, in1, op0, op1), reads=reads, writes=[out])

    def copy(self, out, in_, eng="vector"):
        if eng == "scalar":
            return self.op(eng, lambda e: e.copy(out, in_), reads=[in_], writes=[out])
        return self.op(eng, lambda e: e.tensor_copy(out, in_), reads=[in_], writes=[out])

    def memset(self, out, val, eng="vector"):
        return self.op(eng, lambda e: e.memset(out, val), writes=[out])

    def finish(self):
        nc = self.nc
        sems = {}
        import contextlib
        with contextlib.ExitStack() as st:
            for k in sorted(self.semkeys, key=str):
                nm = "s_" + "_".join(str(x) for x in (k if isinstance(k, tuple) else (k,)))
                sems[k] = st.enter_context(nc.semaphore(nm))
            block = st.enter_context(nc.Block())
            fin = self._waits("sync", set(self.final_tokens))
            ops = self.ops

            def make(engname, extra=None):
                def body(e):
                    for waits, fn, inc in ops[engname]:
                        for (k, v) in waits:
                            e.wait_ge(sems[k], v)
                        if fn is None:
                            continue
                        ins = fn(e)
                        ins.then_inc(sems[inc[0]], inc[1])
                    if extra:
                        for (k, v) in extra:
                            e.wait_ge(sems[k], v)
                return body

            block.tensor(make("tensor"))
            block.vector(make("vector"))
            block.scalar(make("scalar"))
            block.gpsimd(make("gpsimd"))
            block.sync(make("sync", fin))
        return nc
import contextlib

T_ALL = 4352
NT = 34
D = 1024
NIN = 3504
LN_EPS = 1e-5
DN_ALPHA = 8 ** 0.25
NEGM = -30000.0

C_Z = 0
C_XBC = 512
C_DT = 1536
C_RW = 1552
C_GQ = 2704
C_GK = 2832
C_GV = 2960
C_GD = 3216
C_OG = 3248


def seg_of(i):
    return 0 if i < 2 else 1


class Phase:
    _n = [0]

    def __init__(self, nc, P):
        self.nc = nc
        self.P = P
        self.st = contextlib.ExitStack()

    def sb(self, name, shape, dt=F32):
        Phase._n[0] += 1
        return self.st.enter_context(self.nc.sbuf_tensor(f"{name}_{Phase._n[0]}", list(shape), dt))

    def close(self):
        self.P.barrier()
        self.st.close()


def bc_rows(ap2d_row, n=128):
    steps = list(ap2d_row.ap)
    last = steps[-1]
    return bass.AP(ap2d_row.tensor, ap2d_row.offset, [[0, n], [last[0], last[1]]])


class Ctx:
    pass


def make_consts(nc, P, G):
    def sbp(name, shape, dt=F32):
        return nc.alloc_sbuf_tensor(name, list(shape), dt)
    G.identf = sbp("identf", [128, 128])
    G.identb = sbp("identb", [128, 128], BF16)
    G.onesf = sbp("onesf", [128, 128])
    G.tri = [sbp("tri_f", [128, 128]), sbp("tri_b", [128, 128])]
    G.negm = [sbp("negm_f", [128, 4, 128]), sbp("negm_b", [128, 4, 128])]
    G.trib = [sbp("trib_f", [128, 128], BF16), sbp("trib_b", [128, 128], BF16)]
    G.SL = sbp("m_SL", [128, 128]); G.SU = sbp("m_SU", [128, 128]); G.IU = sbp("m_IU", [128, 128]); G.IL = sbp("m_IL", [128, 128])
    gp = "gpsimd"
    G.nSU = sbp("m_nSU", [128, 128]); G.nSL = sbp("m_nSL", [128, 128])
    G.cind = sbp("cind", [128, 2]); G.cmask = sbp("cmask", [128, 2])
    G.hm4 = sbp("hm4", [128, 4])
    P.memset(G.hm4[:], 1.0, eng=gp)
    P.op(gp, lambda e: e.affine_select(G.hm4[:], G.hm4[:], [[-32, 4]], ALU.is_ge, 0.0, base=0, channel_multiplier=1),
         reads=[G.hm4[:]], writes=[G.hm4[:]])
    P.op(gp, lambda e: e.affine_select(G.hm4[:], G.hm4[:], [[32, 4]], ALU.is_ge, 0.0, base=31, channel_multiplier=-1),
         reads=[G.hm4[:]], writes=[G.hm4[:]])

    def asel(t, cm, step, cmp, base=0):
        P.op(gp, lambda e: e.affine_select(t, t, [[step, 128]], cmp, 0.0, base=base, channel_multiplier=cm),
             reads=[t], writes=[t])
    for t in (G.identf, G.onesf, G.tri[0], G.tri[1], G.SL, G.SU, G.IU, G.IL):
        P.memset(t[:], 1.0, eng=gp)
    asel(G.identf[:], -1, 1, ALU.is_equal)
    asel(G.tri[0][:], -1, 1, ALU.is_ge)
    asel(G.tri[1][:], 1, -1, ALU.is_ge)
    asel(G.IU[:], -1, 1, ALU.is_ge)
    asel(G.IL[:], 1, -1, ALU.is_ge)
    asel(G.SU[:], -1, 1, ALU.is_gt)
    asel(G.SL[:], 1, -1, ALU.is_gt)
    for t in (G.SL, G.SU, G.IU, G.IL):
        P.memset(t[0:64, 64:128], 0.0, eng=gp)
        P.memset(t[64:128, 0:64], 0.0, eng=gp)
    P.ts(G.nSU[:], G.SU[:], -1.0, ALU.mult, eng=gp)
    P.ts(G.nSL[:], G.SL[:], -1.0, ALU.mult, eng=gp)
    P.memset(G.cind[:], 0.0, eng=gp)
    P.memset(G.cind[0:64, 0:1], 1.0, eng=gp)
    P.memset(G.cind[64:128, 1:2], 1.0, eng=gp)
    P.memset(G.cmask[:], 1.0, eng=gp)
    for (col, base) in ((0, 0), (0, -64), (1, -63), (1, -127)):
        P.op(gp, lambda e, col=col, base=base: e.affine_select(G.cmask[:, col:col + 1], G.cmask[:, col:col + 1], [[0, 1]],
                                                             ALU.not_equal, 0.0, base=base, channel_multiplier=1),
             reads=[G.cmask[:, col:col + 1]], writes=[G.cmask[:, col:col + 1]])
    P.copy(G.identb[:], G.identf[:])
    for d in range(2):
        P.copy(G.trib[d][:], G.tri[d][:])
        for h in range(4):
            P.ts(G.negm[d][:, h, :], G.tri[d][:], 1.0, ALU.subtract, -NEGM, ALU.mult)


def ln_stats(P, ph, x_ap, tag, st6, mv, rstd, eps=LN_EPS):
    for hh in range(2):
        P.op("vector", lambda e, hh=hh: e.bn_stats(st6[:, hh, :], x_ap[:, hh * 512:(hh + 1) * 512]),
             reads=[x_ap[:, hh * 512:(hh + 1) * 512]], writes=[st6[:, hh, :]])
    P.op("vector", lambda e: e.bn_aggr(mv, st6), reads=[st6], writes=[mv])
    P.act(rstd, mv[:, 1:2], AF.Sqrt, bias=eps)
    P.op("vector", lambda e: e.reciprocal(rstd, rstd), reads=[rstd], writes=[rstd])


def phase_mod(nc, P, G, l):
    ph = Phase(nc, P)
    cT = ph.sb("cT", [128, 2, 8]); csil = ph.sb("csil", [128, 2, 8])
    wst = ph.sb("adaw", [128, 2, 8, 512]); brow = ph.sb("brow", [1, 6144]); mst = ph.sb("mst", [128, 2, 2, 512])
    for s in range(2):
        P.dma("sync", cT[:, s, :], G.cvec[s, :].rearrange("(kc p) -> p kc", p=128), allow_slow_non_contiguous=True)
    P.act(csil[:], cT[:], AF.Silu)
    P.dma("sync", brow[:], G.w["ada_b"][l:l + 1, :])
    for n in range(12):
        b = n % 2
        P.dma("sync", wst[:, b], G.w["ada_w"][l, :, n * 512:(n + 1) * 512].rearrange("(kc p) j -> p kc j", p=128))
        for s in range(2):
            pso = G.psf[:, (n % 2 * 2 + s) * 512:(n % 2 * 2 + s + 1) * 512]
            for kc in range(8):
                P.mm(pso, csil[:, s, kc:kc + 1].to_broadcast([128, 128]), wst[:, b, kc, :], start=(kc == 0), stop=False)
            P.mm(pso, G.onesf[0:1, :], brow[0:1, n * 512:(n + 1) * 512], start=False, stop=True)
            which = n // 2
            if which in (1, 4):
                P.act(mst[:, b, s, :], pso, AF.Identity, bias=1.0)
            else:
                P.copy(mst[:, b, s, :], pso)
            P.dma("sync", G.modd[s, which, :, (n % 2) * 512:(n % 2 + 1) * 512], mst[:, b, s, :])
    ph.close()


def phase_inproj(nc, P, G, l):
    ph = Phase(nc, P)
    winb = ph.sb("winb", [128, 8, NIN], BF16)
    wst = ph.sb("wst", [128, 2, 8, 512])
    msc = ph.sb("msc", [128, 2, 1024]); msh = ph.sb("msh", [128, 2, 1024])
    xt = ph.sb("xt", [128, 2, 1024]); ub = ph.sb("ub", [128, 2, 1024], BF16)
    uT = ph.sb("uT", [128, 2, 8, 128], BF16); zt = ph.sb("zt", [128, 2, NIN])
    st6 = ph.sb("st6", [128, 2, 6]); mv = ph.sb("mv", [128, 2]); rstd = ph.sb("rstd", [128, 1])
    for n in range(7):
        w = min(512, NIN - n * 512)
        P.dma("sync", wst[:, n % 2, :, 0:w], G.w["w_in"][l, :, n * 512:n * 512 + w].rearrange("(kc p) j -> p kc j", p=128))
        P.copy(winb[:, :, n * 512:n * 512 + w], wst[:, n % 2, :, 0:w], eng="gpsimd")
    for s in range(2):
        P.dma("sync", msc[:, s, :], G.modd[s, 1])
        P.dma("sync", msh[:, s, :], G.modd[s, 0])
    for i in range(NT):
        b = i % 2
        s = seg_of(i)
        x = xt[:, b, :]
        P.dma("sync", x, G.xres[i * 128:(i + 1) * 128, :])
        ln_stats(P, ph, x, "a", st6[:], mv[:], rstd[:])
        P.ts(x, x, mv[:, 0:1], ALU.subtract, rstd[:, 0:1], ALU.mult)
        P.tt(x, x, msc[:, s, :], ALU.mult, eng="gpsimd")
        P.tt(ub[:, b, :], x, msh[:, s, :], ALU.add)
        if G.dbg is not None and "u" in G.dbg:
            P.dma("sync", G.dbg["u"][i * 128:(i + 1) * 128, :], ub[:, b, :])
        for kc in range(8):
            P.tr(G.psb[:, kc * 128:(kc + 1) * 128], ub[:, b, kc * 128:(kc + 1) * 128], G.identb[:])
        P.copy(uT[:, b].rearrange("p a b -> p (a b)"), G.psb[:, 0:1024], eng="scalar")
        for n in range(7):
            w = min(512, NIN - n * 512)
            pso = G.psf[:, (n % 4) * 512:(n % 4) * 512 + w]
            for kc in range(8):
                P.mm(pso, uT[:, b, kc, :], winb[:, kc, n * 512:n * 512 + w], start=(kc == 0), stop=(kc == 7))
            P.copy(zt[:, b, n * 512:n * 512 + w], pso, eng=("scalar" if n % 2 else "vector"))
        P.dma("sync", G.zin[i * 128:(i + 1) * 128, :], zt[:, b, :])
    ph.close()
def dir_order(d):
    if d == 0:
        return list(range(NT))
    return [1, 0] + list(range(NT - 1, 1, -1))


def load_shift(P, dst, src2d, i, off, eng_ms="gpsimd"):
    lo, hi = (0, 256) if i < 2 else (256, T_ALL)
    r0 = i * 128 + off
    a = max(r0, lo); bnd = min(r0 + 128, hi)
    if a > r0:
        n = a - r0
        if n >= 128:
            P.memset(dst, 0.0, eng=eng_ms); return
        P.memset(dst[0:((n + 31) // 32) * 32, :], 0.0, eng=eng_ms)
    if bnd < r0 + 128:
        n = r0 + 128 - bnd
        if n >= 128:
            P.memset(dst, 0.0, eng=eng_ms); return
        st = ((128 - n) // 32) * 32
        P.memset(dst[st:128, :], 0.0, eng=eng_ms)
    P.dma("sync", dst[a - r0:bnd - r0, :], src2d[a:bnd, :])


def phase_ssd(nc, P, G, l):
    ssd_prep(nc, P, G, l); ssd_dir(nc, P, G, l); ssd_fin(nc, P, G, l)


def ssd_prep(nc, P, G, l):
    W = G.w
    ph = Phase(nc, P)
    cw = ph.sb("cw", [128, 3, 1024]); cb = ph.sb("cb", [128, 1024])
    dtb = ph.sb("dtb", [128, 16]); nega = ph.sb("nega", [128, 16])
    xc = ph.sb("xc", [128, 2, 1024]); xl = ph.sb("xl", [128, 2, 1024]); xr = ph.sb("xr", [128, 2, 1024])
    acc = ph.sb("acc", [128, 2, 1024]); dl = ph.sb("dl", [128, 2, 32]); dr = ph.sb("dr", [128, 2, 16])
    for j in range(3):
        P.dma("sync", cw[:, j, :], bc_rows(W["ssd_conv_w"][l, j:j + 1, :]))
    P.dma("sync", cb[:], bc_rows(W["ssd_conv_b"][l:l + 1, :]))
    P.dma("sync", dtb[:], bc_rows(W["ssd_dt_bias"][l:l + 1].rearrange("a b c -> a (b c)")))
    P.dma("sync", nega[:], bc_rows(W["ssd_a_log"][l:l + 1].rearrange("a b c -> a (b c)")))
    P.act(nega[:], nega[:], AF.Exp)
    P.ts(nega[:], nega[:], -1.0, ALU.mult)
    zx = G.zin[:, C_XBC:C_XBC + 1024]
    for i in range(NT):
        b = i % 2
        rows = slice(i * 128, (i + 1) * 128)
        P.dma("sync", xc[:, b, :], zx[rows, :])
        load_shift(P, xl[:, b, :], zx, i, -1)
        load_shift(P, xr[:, b, :], zx, i, +1)
        P.dma("sync", dr[:, b, :], G.zin[rows, C_DT:C_DT + 16])
        a = acc[:, b, :]
        P.tt(a, xc[:, b, :], cw[:, 1, :], ALU.mult)
        P.tt(xl[:, b, :], xl[:, b, :], cw[:, 0, :], ALU.mult, eng="gpsimd")
        P.tt(xr[:, b, :], xr[:, b, :], cw[:, 2, :], ALU.mult, eng="gpsimd")
        P.tt(a, a, xl[:, b, :], ALU.add)
        P.tt(a, a, xr[:, b, :], ALU.add)
        P.tt(a, a, cb[:], ALU.add)
        P.act(a, a, AF.Silu)
        P.dma("sync", G.xbca[rows, :], a)
        P.tt(dr[:, b, :], dr[:, b, :], dtb[:], ALU.add)
        P.act(dr[:, b, :], dr[:, b, :], AF.Exp)
        P.act(dl[:, b, 0:16], dr[:, b, :], AF.Ln, bias=1.0)
        P.tt(dl[:, b, 16:32], dl[:, b, 0:16], nega[:], ALU.mult)
        P.dma("sync", G.dtla[rows, :], dl[:, b, :])
    ph.close()


def run_streams(gens):
    gens = list(gens)
    while gens:
        for g in list(gens):
            try:
                next(g)
            except StopIteration:
                gens.remove(g)


def ssd_dir(nc, P, G, l):
    ph = Phase(nc, P)
    ps = G.psf
    psbf = G.psf.bitcast(BF16)

    def stream(d, kb):
        sfx = "f" if d == 0 else "b"
        xa = ph.sb("xa" + sfx, [128, 2, 1024]); dl = ph.sb("dl2" + sfx, [128, 2, 32])
        sm = ph.sb("sm" + sfx, [128, 2, 48])
        R1 = ph.sb("R1" + sfx, [128, 8, 128]); R3 = ph.sb("R3" + sfx, [128, 8, 128])
        LT = ph.sb("LT" + sfx, [128, 8, 128], BF16); MT = ph.sb("MT" + sfx, [128, 8, 128], BF16)
        bcb = ph.sb("bcb" + sfx, [128, 2, 512], BF16); bcT = ph.sb("bcT" + sfx, [128, 2, 4, 128], BF16)
        xdt = ph.sb("xdt" + sfx, [128, 2, 8, 64], BF16); xdd = ph.sb("xdd" + sfx, [128, 2, 8, 64], BF16)
        ytmp = ph.sb("ytmp" + sfx, [128, 8, 64]); yo = ph.sb("yo" + sfx, [128, 2, 512])
        hin32 = ph.sb("hin32" + sfx, [128, 512]); hinb = ph.sb("hinb" + sfx, [128, 512], BF16)

        def bank(k, w=512, o=0):
            return ps[:, kb[k] * 512 + o:kb[k] * 512 + o + w]
        P.memset(hin32[:], 0.0)
        P.memset(hinb[:], 0.0)
        tri = G.tri[d]
        for i in dir_order(d):
            b = i % 2
            rows = slice(i * 128, (i + 1) * 128)
            P.dma("sync", xa[:, b, :], G.xbca[rows, :])
            P.dma("sync", dl[:, b, :], G.dtla[rows, :])
            dt_d = dl[:, b, d * 8:(d + 1) * 8]
            la_d = dl[:, b, 16 + d * 8:16 + (d + 1) * 8]
            S = sm[:, b, :]
            acs_ps = bank(0, 8, 256); tot_ps = bank(0, 8, 264)
            P.mm(acs_ps, tri[:], la_d)
            P.mm(tot_ps, G.onesf[:], la_d)
            P.tt(R1[:], tri[:].unsqueeze(1).to_broadcast([128, 8, 128]), la_d.unsqueeze(2).to_broadcast([128, 8, 128]),
                 ALU.mult, eng="gpsimd")
            P.act(R3[:], la_d.unsqueeze(2).to_broadcast([128, 8, 128]), AF.Identity, scale=-1.0)
            P.copy(bcb[:, b, :], xa[:, b, 512:1024])
            yield
            P.copy(S[:, 0:8], acs_ps)
            P.act(S[:, 8:16], acs_ps, AF.Exp)
            P.tt(S[:, 40:48], tot_ps, S[:, 0:8], ALU.subtract)
            P.act(S[:, 16:24], S[:, 40:48], AF.Exp)
            P.act(S[:, 24:32], tot_ps, AF.Exp)
            P.tt(S[:, 32:40], dt_d, S[:, 16:24], ALU.mult)
            for q in range(4):
                P.tr(psbf[:, kb[2] * 1024 + q * 128:kb[2] * 1024 + (q + 1) * 128], bcb[:, b, q * 128:(q + 1) * 128], G.identb[:])
            for g in range(2):
                sp = bank(1)
                P.mm(sp, G.onesf[:], R1[:, 4 * g:4 * g + 4, :], start=True, stop=False)
                P.mm(sp, tri[:], R3[:, 4 * g:4 * g + 4, :], start=False, stop=False)
                P.mm(sp, G.identf[:], G.negm[d][:], start=False, stop=True)
                if g == 0:
                    yield
                    P.copy(bcT[:, b].rearrange("p a b -> p (a b)"), psbf[:, kb[2] * 1024:kb[2] * 1024 + 512], eng="scalar")
                P.act(LT[:, 4 * g:4 * g + 4, :], sp, AF.Exp)
            xs3 = xa[:, b, 0:512].rearrange("p (h q) -> p h q", h=8)
            P.tt(xdt[:, b], xs3, dt_d.unsqueeze(2).to_broadcast([128, 8, 64]), ALU.mult, eng="gpsimd")
            P.tt(xdd[:, b], xs3, S[:, 32:40].unsqueeze(2).to_broadcast([128, 8, 64]), ALU.mult, eng="gpsimd")
            for g in range(2):
                cbp = bank(0, 128, g * 128)
                P.mm(cbp, bcT[:, b, g, :], bcT[:, b, 2 + g, :])
            for g in range(2):
                P.mm(bank(3, 256, g * 256), bcT[:, b, 2 + g, :], hinb[:, g * 256:(g + 1) * 256])
            yield
            for g in range(2):
                cbp = bank(0, 128, g * 128)
                P.tt(MT[:, 4 * g:4 * g + 4, :], LT[:, 4 * g:4 * g + 4, :], cbp.unsqueeze(1).to_broadcast([128, 4, 128]), ALU.mult)
            P.tt(ytmp[:], bank(3).rearrange("p (h q) -> p h q", h=8), S[:, 8:16].unsqueeze(2).to_broadcast([128, 8, 64]), ALU.mult)
            for h in range(8):
                P.mm(bank(2, 64, h * 64), MT[:, h, :], xdt[:, b, h, :])
            for g in range(2):
                P.mm(bank(3, 256, g * 256), bcb[:, b, g * 128:(g + 1) * 128], xdd[:, b, 4 * g:4 * g + 4, :])
            yield
            P.tt(yo[:, b, :], ytmp[:].rearrange("p h q -> p (h q)"), bank(2), ALU.add)
            P.dma("sync", G.ydir[d, rows, :], yo[:, b, :])
            h3 = hin32[:].rearrange("p (h q) -> p h q", h=8)
            P.tt(h3, h3, S[:, 24:32].unsqueeze(2).to_broadcast([128, 8, 64]), ALU.mult)
            P.tt(hin32[:], hin32[:], bank(3), ALU.add)
            P.copy(hinb[:], hin32[:], eng="scalar")
            yield

    run_streams([stream(0, [0, 1, 2, 3]), stream(1, [4, 5, 6, 7])])
    ph.close()


def ssd_fin(nc, P, G, l):
    W = G.w
    ph = Phase(nc, P)
    dsm = ph.sb("dsm", [128, 8]); dex = ph.sb("dex", [128, 8, 64]); nw = ph.sb("nw", [128, 512])
    yf = ph.sb("yf", [128, 2, 512]); yb = ph.sb("yb", [128, 2, 512]); xs = ph.sb("xs", [128, 2, 512]); z = ph.sb("z", [128, 2, 512])
    junk = ph.sb("junk", [128, 256]); ss = ph.sb("ss", [128, 2, 2]); o = ph.sb("o", [128, 2, 512])
    P.dma("sync", dsm[:], bc_rows(W["ssd_d"][l:l + 1, :]))
    P.copy(dex[:], dsm[:].unsqueeze(2).to_broadcast([128, 8, 64]))
    P.dma("sync", nw[:], bc_rows(W["ssd_norm_w"][l:l + 1, :]))
    dexf = dex[:].rearrange("p h q -> p (h q)")
    for i in range(NT):
        b = i % 2
        rows = slice(i * 128, (i + 1) * 128)
        P.dma("sync", yf[:, b, :], G.ydir[0, rows, :])
        P.dma("sync", yb[:, b, :], G.ydir[1, rows, :])
        P.dma("sync", xs[:, b, :], G.xbca[rows, 0:512])
        P.dma("sync", z[:, b, :], G.zin[rows, C_Z:C_Z + 512])
        y = yf[:, b, :]
        P.tt(y, y, yb[:, b, :], ALU.add)
        P.tt(xs[:, b, :], xs[:, b, :], dexf, ALU.mult, eng="gpsimd")
        P.tt(y, y, xs[:, b, :], ALU.add)
        P.act(z[:, b, :], z[:, b, :], AF.Silu)
        P.tt(y, y, z[:, b, :], ALU.mult)
        for g in range(2):
            P.act(junk[:], y[:, g * 256:(g + 1) * 256], AF.Square, accum_out=ss[:, b, g:g + 1])
        P.act(ss[:, b, :], ss[:, b, :], AF.Sqrt, bias=1e-5, scale=1.0 / 256)
        P.op("vector", lambda e, b=b: e.reciprocal(ss[:, b, :], ss[:, b, :]), reads=[ss[:, b, :]], writes=[ss[:, b, :]])
        for g in range(2):
            P.stt(o[:, b, g * 256:(g + 1) * 256], y[:, g * 256:(g + 1) * 256], ss[:, b, g:g + 1], nw[:, g * 256:(g + 1) * 256],
                  ALU.mult, ALU.mult)
        P.dma("sync", G.ymix[rows, 0:512], o[:, b, :])
    ph.close()
def phase_gla(nc, P, G, l):
    gla_prep(nc, P, G, l); gla_dir(nc, P, G, l); gla_fin(nc, P, G, l)


def gla_prep(nc, P, G, l):
    W = G.w
    ph = Phase(nc, P)
    GU = ph.sb("GU", [32, 256]); gbrow = ph.sb("gbrow", [1, 256])
    gd = ph.sb("gd", [128, 2, 32]); gdT = ph.sb("gdT", [32, 2, 128]); e = ph.sb("ge", [128, 2, 256])
    P.memset(GU[:], 0.0)
    P.dma("sync", GU[0:16, 0:128], W["gla_gu"][l, 0])
    P.dma("sync", GU[16:32, 128:256], W["gla_gu"][l, 1])
    P.dma("sync", gbrow[:], W["gla_gb"][l:l + 1].rearrange("a b c -> a (b c)"))
    for i in range(NT):
        b = i % 2
        rows = slice(i * 128, (i + 1) * 128)
        P.dma("sync", gd[:, b, :], G.zin[rows, C_GD:C_GD + 32])
        tp = G.psf[0:32, 0:128]
        P.tr(tp, gd[:, b, :], G.identf[:])
        P.copy(gdT[:, b, :], tp)
        gp = G.psf[:, 512:768]
        P.mm(gp, gdT[:, b, :], GU[:], start=True, stop=False)
        P.mm(gp, G.onesf[0:1, :], gbrow[:], start=False, stop=True)
        P.act(e[:, b, :], gp, AF.Exp, scale=-1.0)
        P.act(e[:, b, :], e[:, b, :], AF.Ln, bias=1.0)
        P.ts(e[:, b, :], e[:, b, :], -1.0 / 16.0, ALU.mult)
        P.dma("sync", G.gla_la[rows, :], e[:, b, :])
    ph.close()


def gla_dir(nc, P, G, l):
    ph = Phase(nc, P)
    ps = G.psf
    psbf = G.psf.bitcast(BF16)

    def stream(d, kb):
        sfx = "f" if d == 0 else "b"
        qk = ph.sb("qk" + sfx, [128, 2, 256]); v = ph.sb("gv" + sfx, [128, 2, 256]); la = ph.sb("gla" + sfx, [128, 2, 128])
        acs = ph.sb("gacs" + sfx, [128, 128]); ee = ph.sb("gee" + sfx, [128, 3, 128]); ecd = ph.sb("gecd" + sfx, [128, 1])
        qkt = ph.sb("qkt" + sfx, [128, 2, 128], BF16); kdec = ph.sb("kdec" + sfx, [128, 128], BF16)
        qkT = ph.sb("qkT" + sfx, [128, 2, 128], BF16); smb = ph.sb("smb" + sfx, [128, 4, 128], BF16); vb = ph.sb("vb" + sfx, [128, 256], BF16)
        kTm = ph.sb("kTm" + sfx, [128, 4, 128], BF16); qTm = ph.sb("qTm" + sfx, [128, 4, 128], BF16)
        stm = ph.sb("stm" + sfx, [128, 4, 64]); sred = ph.sb("sred" + sfx, [128, 64])
        ot = ph.sb("got" + sfx, [128, 2, 256]); S32 = ph.sb("S32" + sfx, [128, 64]); Sb = ph.sb("Sb" + sfx, [128, 64], BF16)

        def bank(k, w=512, o=0):
            return ps[:, kb[k] * 512 + o:kb[k] * 512 + o + w]
        P.memset(S32[:], 0.0)
        P.memset(Sb[:], 0.0)
        tri = G.tri[d]
        for i in dir_order(d):
            b = i % 2
            rows = slice(i * 128, (i + 1) * 128)
            P.dma("sync", qk[:, b, :], G.zin[rows, C_GQ:C_GQ + 256])
            P.dma("sync", v[:, b, :], G.zin[rows, C_GV:C_GV + 256])
            P.dma("sync", la[:, b, :], G.gla_la[rows, d * 128:(d + 1) * 128])
            acs_ps = bank(0, 128, 0); tot_ps = bank(0, 128, 128); totT = bank(0, 2, 256)
            P.mm(acs_ps, tri[:], la[:, b, :])
            P.mm(tot_ps, G.onesf[:], la[:, b, :])
            P.mm(totT, la[:, b, :], G.onesf[:, 0:2])
            P.copy(vb[:], v[:, b, :], eng="gpsimd")
            yield
            P.copy(acs[:], acs_ps, eng="scalar")
            P.act(ee[:, 0, :], acs_ps, AF.Exp)
            P.act(ee[:, 1, :], acs_ps, AF.Exp, scale=-1.0)
            P.tt(ee[:, 2, :], tot_ps, acs[:], ALU.subtract)
            P.act(ee[:, 2, :], ee[:, 2, :], AF.Exp)
            P.act(ecd[:], totT[:, 0:1], AF.Exp)
            P.stt(qkt[:, 0, :], qk[:, b, 0:128], 32 ** -0.5, ee[:, 0, :], ALU.mult, ALU.mult)
            P.tt(qkt[:, 1, :], qk[:, b, 128:256], ee[:, 1, :], ALU.mult)
            P.tt(kdec[:], qk[:, b, 128:256], ee[:, 2, :], ALU.mult, eng="gpsimd")
            for j in range(2):
                P.tr(psbf[:, kb[0] * 1024 + j * 128:kb[0] * 1024 + (j + 1) * 128], qkt[:, j, :], G.identb[:])
            P.mm(bank(3, 256, 0), kdec[:], vb[:])
            yield
            P.copy(qkT[:].rearrange("p a b -> p (a b)"), psbf[:, kb[0] * 1024:kb[0] * 1024 + 256], eng="scalar")
            P.tt(kTm[:], qkT[:, 1, :].unsqueeze(1).to_broadcast([128, 4, 128]), G.hm4[:].unsqueeze(2).to_broadcast([128, 4, 128]), ALU.mult)
            P.tt(qTm[:], qkT[:, 0, :].unsqueeze(1).to_broadcast([128, 4, 128]), G.hm4[:].unsqueeze(2).to_broadcast([128, 4, 128]), ALU.mult, eng="gpsimd")
            for h in range(4):
                P.mm(bank(1, 128, h * 128), kTm[:, h, :], qkT[:, 0, :])
            yield
            P.tt(smb[:], bank(1).rearrange("p (h q) -> p h q", h=4), G.tri[d][:].unsqueeze(1).to_broadcast([128, 4, 128]), ALU.mult)
            for h in range(4):
                o_ps = bank(2, 64, h * 64)
                P.mm(o_ps, smb[:, h, :], vb[:, h * 64:(h + 1) * 64], start=True, stop=False)
                P.mm(o_ps, qTm[:, h, :], Sb[:], start=False, stop=True)
            yield
            P.copy(ot[:, b, :], bank(2, 256, 0), eng="scalar")
            P.dma("sync", G.gla_o[d, rows, :], ot[:, b, :])
            P.tt(stm[:], bank(3, 256, 0).rearrange("p (h q) -> p h q", h=4), G.hm4[:].unsqueeze(2).to_broadcast([128, 4, 64]), ALU.mult)
            P.op("vector", lambda e: e.tensor_reduce(sred[:], stm[:].rearrange("p h q -> p q h"), AX.X, ALU.add),
                 reads=[stm[:]], writes=[sred[:]])
            P.stt(S32[:], S32[:], ecd[:, 0:1], sred[:], ALU.mult, ALU.add)
            P.copy(Sb[:], S32[:])
            yield

    run_streams([stream(0, [0, 1, 2, 3]), stream(1, [4, 5, 6, 7])])
    ph.close()


def gla_fin(nc, P, G, l):
    W = G.w
    ph = Phase(nc, P)
    nw = ph.sb("gnw", [128, 256]); of = ph.sb("gof", [128, 2, 256]); ob = ph.sb("gob", [128, 2, 256]); og = ph.sb("gog", [128, 2, 256])
    sq = ph.sb("gsq", [128, 256]); ss = ph.sb("gss", [128, 2, 4]); y = ph.sb("gy", [128, 2, 256])
    P.dma("sync", nw[:], bc_rows(W["gla_norm_w"][l:l + 1, :]))
    for i in range(NT):
        b = i % 2
        rows = slice(i * 128, (i + 1) * 128)
        P.dma("sync", of[:, b, :], G.gla_o[0, rows, :])
        P.dma("sync", ob[:, b, :], G.gla_o[1, rows, :])
        P.dma("sync", og[:, b, :], G.zin[rows, C_OG:C_OG + 256])
        o = of[:, b, :]
        P.tt(o, o, ob[:, b, :], ALU.add)
        P.tt(sq[:], o, o, ALU.mult, eng="gpsimd")
        P.op("vector", lambda e, b=b: e.tensor_reduce(ss[:, b, :], sq[:].rearrange("p (h q) -> p h q", h=4), AX.X, ALU.add),
             reads=[sq[:]], writes=[ss[:, b, :]])
        P.act(ss[:, b, :], ss[:, b, :], AF.Sqrt, bias=1e-5, scale=1.0 / 64)
        P.op("vector", lambda e, b=b: e.reciprocal(ss[:, b, :], ss[:, b, :]), reads=[ss[:, b, :]], writes=[ss[:, b, :]])
        y3 = y[:, b, :].rearrange("p (h q) -> p h q", h=4)
        P.tt(y3, o.rearrange("p (h q) -> p h q", h=4), ss[:, b, :].unsqueeze(2).to_broadcast([128, 4, 64]), ALU.mult)
        P.tt(y[:, b, :], y[:, b, :], nw[:], ALU.mult, eng="gpsimd")
        P.act(og[:, b, :], og[:, b, :], AF.Silu)
        P.tt(y[:, b, :], y[:, b, :], og[:, b, :], ALU.mult)
        P.dma("sync", G.ymix[rows, 768:1024], y[:, b, :])
    ph.close()
def phase_rwkv(nc, P, G, l):
    rwkv_prep(nc, P, G, l); rwkv_dir(nc, P, G, l); rwkv_fin(nc, P, G, l)


def rwkv_prep(nc, P, G, l):
    W = G.w
    ph = Phase(nc, P)
    mu = ph.sb("mu", [128, 4, 1152])
    w2a2 = ph.sb("w2a2", [128, 2, 256]); g2 = ph.sb("g2", [128, 256]); brow = ph.sb("rbrow", [1, 4, 256])
    kkw = ph.sb("kkw", [128, 256]); ka = ph.sb("ka", [128, 256]); rk = ph.sb("rk", [128, 256])
    c = ph.sb("rc", [128, 2, 1152]); nb = ph.sb("rnb", [128, 4, 1152])
    tws = ph.sb("tws", [128, 3, 128]); twT = ph.sb("twT", [128, 3, 128])
    pk = ph.sb("pk", [128, 2, 11, 256]); tmp = ph.sb("rtmp", [128, 3, 256]); s4 = ph.sb("rs4", [128, 8])
    for j in range(4):
        P.dma("sync", mu[:, j, :], bc_rows(W["rwkv_mu"][l, j:j + 1, :]))
    for d in range(2):
        P.dma("sync", w2a2[64 * d:64 * d + 64, 0, :], W["rwkv_w2"][l, d])
        P.dma("sync", w2a2[64 * d:64 * d + 64, 1, :], W["rwkv_a2"][l, d])
    P.dma("sync", g2[:], W["rwkv_g2"][l])
    P.dma("sync", brow[0:1, 0:2, :], W["rwkv_w0"][l:l + 1])
    P.dma("sync", brow[0:1, 2:4, :], W["rwkv_a0"][l:l + 1])
    P.dma("sync", kkw[:], bc_rows(W["rwkv_kk"][l:l + 1, :]))
    P.dma("sync", ka[:], bc_rows(W["rwkv_ka"][l:l + 1, :]))
    P.dma("sync", rk[:], bc_rows(W["rwkv_rk"][l:l + 1].rearrange("a b c -> a (b c)")))
    zr = G.zin[:, C_RW:C_RW + 1152]
    for i in range(NT):
        b = i % 2
        rows = slice(i * 128, (i + 1) * 128)
        lat = i >= 2
        cc = c[:, b, :]
        P.dma("sync", cc, zr[rows, :])
        offs = [-1, 1, -64, 64] if lat else [-1, 1]
        for j, off in enumerate(offs):
            X = nb[:, j, :]
            load_shift(P, X, zr, i, off)
            if lat and j < 2:
                P.stt(X, X, G.cmask[:, j:j + 1], cc, ALU.mult, ALU.subtract, eng=("vector"))
            else:
                P.tt(X, X, cc, ALU.subtract, eng="gpsimd")
            P.tt(X, X, mu[:, j, :], ALU.mult, eng=("gpsimd" if j % 2 else "vector"))
        for j in range(len(offs)):
            P.tt(cc, cc, nb[:, j, :], ALU.add)
        PK = pk[:, b]
        P.act(tws[:, 0, :], cc[:, 768:896], AF.Tanh)
        P.copy(tws[:, 1, :], cc[:, 896:1024], eng="gpsimd")
        P.act(tws[:, 2, :], cc[:, 1024:1152], AF.Sigmoid)
        for q in range(3):
            P.tr(G.psf[:, q * 128:(q + 1) * 128], tws[:, q, :], G.identf[:])
        P.copy(twT[:].rearrange("p a b -> p (a b)"), G.psf[:, 0:384])
        for d in range(2):
            ds = slice(64 * d, 64 * d + 64)
            xp = G.psf[:, (1 + 2 * d) * 512:(1 + 2 * d) * 512 + 256]
            P.mm(xp, twT[ds, 0, :], w2a2[ds, 0, :], start=True, stop=False)
            P.mm(xp, G.onesf[0:1, :], brow[0:1, d, :], start=False, stop=True)
            ap_ = G.psf[:, (1 + 2 * d) * 512 + 256:(1 + 2 * d) * 512 + 512]
            P.mm(ap_, twT[ds, 1, :], w2a2[ds, 1, :], start=True, stop=False)
            P.mm(ap_, G.onesf[0:1, :], brow[0:1, 2 + d, :], start=False, stop=True)
        gp = G.psf[:, 2048:2304]
        P.mm(gp, twT[:, 2, :], g2[:])
        for d in range(2):
            P.act(PK[:, 7 + d, :], G.psf[:, (1 + 2 * d) * 512:(1 + 2 * d) * 512 + 256], AF.Sigmoid)
        P.ts(PK[:, 7:9, :], PK[:, 7:9, :], -0.6065306597126334, ALU.mult, eng="gpsimd")
        adir = tmp[:, 0:2, :]
        for d in range(2):
            P.act(adir[:, d, :], G.psf[:, (1 + 2 * d) * 512 + 256:(1 + 2 * d) * 512 + 512], AF.Sigmoid)
        P.copy(PK[:, 9, :], gp, eng="scalar")
        P.copy(PK[:, 0, :], cc[:, 0:256], eng="gpsimd")
        P.copy(PK[:, 1, :], cc[:, 512:768], eng="gpsimd")
        kx = cc[:, 256:512]
        kk = PK[:, 2, :]
        P.tt(kk, kx, kkw[:], ALU.mult)
        P.tt(tmp[:, 2, :], kk, kk, ALU.mult, eng="gpsimd")
        P.op("vector", lambda e: e.tensor_reduce(s4[:, 0:4], tmp[:, 2, :].rearrange("p (h q) -> p h q", h=4), AX.X, ALU.add),
             reads=[tmp[:, 2, :]], writes=[s4[:, 0:4]])
        P.act(s4[:, 0:4], s4[:, 0:4], AF.Sqrt)
        P.ts(s4[:, 0:4], s4[:, 0:4], 1e-12, ALU.max)
        P.op("vector", lambda e: e.reciprocal(s4[:, 0:4], s4[:, 0:4]), reads=[s4[:, 0:4]], writes=[s4[:, 0:4]])
        kk3 = kk.rearrange("p (h q) -> p h q", h=4)
        P.tt(kk3, kk3, s4[:, 0:4].unsqueeze(2).to_broadcast([128, 4, 64]), ALU.mult)
        for d in range(2):
            t1 = tmp[:, 2, :]
            P.stt(t1, adir[:, d, :], -1.0, ka[:], ALU.add, ALU.mult)
            P.stt(PK[:, 3 + d, :], t1, 1.0, kx, ALU.add, ALU.mult)
            P.tt(PK[:, 5 + d, :], kk, adir[:, d, :], ALU.mult, eng="gpsimd")
        t1 = tmp[:, 2, :]
        P.tt(t1, PK[:, 3, :], PK[:, 4, :], ALU.add)
        P.tt(t1, t1, PK[:, 0, :], ALU.mult)
        P.tt(t1, t1, rk[:], ALU.mult)
        P.op("vector", lambda e: e.tensor_reduce(s4[:, 4:8], tmp[:, 2, :].rearrange("p (h q) -> p h q", h=4), AX.X, ALU.add),
             reads=[tmp[:, 2, :]], writes=[s4[:, 4:8]])
        P.tt(PK[:, 10, :].rearrange("p (h q) -> p h q", h=4), PK[:, 1, :].rearrange("p (h q) -> p h q", h=4),
             s4[:, 4:8].unsqueeze(2).to_broadcast([128, 4, 64]), ALU.mult)
        P.dma("sync", G.rwp[rows, :], PK.rearrange("p a b -> p (a b)"))
    ph.close()


def rwkv_dir(nc, P, G, l):
    ph = Phase(nc, P)
    ps = G.psf

    def stream(d, kb):
        sfx = "f" if d == 0 else "b"
        ld = ph.sb("rld" + sfx, [128, 2, 6, 256])
        ee = ph.sb("ree" + sfx, [128, 3, 256])
        tok = ph.sb("rtok" + sfx, [128, 4, 256])
        fmh = ph.sb("fmh" + sfx, [64, 4, 4, 128])
        Aarb = ph.sb("Aarb" + sfx, [128, 4, 128]); Aak = ph.sb("Aak" + sfx, [128, 4, 128]); Aark = ph.sb("Aark" + sfx, [128, 4, 128])
        Q = ph.sb("rQ" + sfx, [128, 2, 4, 128]); QT = ph.sb("rQT" + sfx, [128, 2, 4, 128]); IQT = ph.sb("rIQT" + sfx, [128, 4, 128])
        Z = ph.sb("rZ" + sfx, [128, 2, 4, 128])
        Wsb = ph.sb("rW" + sfx, [128, 4, 64]); nAkV = ph.sb("nAkV" + sfx, [128, 4, 64]); Uloc = ph.sb("Uloc" + sfx, [128, 4, 64])
        GpT = ph.sb("GpT" + sfx, [64, 4, 2, 64]); RwT = ph.sb("RwT" + sfx, [64, 4, 128]); gC = ph.sb("gC" + sfx, [64, 4, 2])
        H32 = ph.sb("H32" + sfx, [64, 4, 64]); Yt = ph.sb("Yt" + sfx, [64, 2, 2, 256])
        btc = ph.sb("btc" + sfx, [128, 2, 256]); ktc = ph.sb("ktc" + sfx, [128, 2, 256])

        def bank(k, w=512, o=0, p0=0, p1=128):
            kk_ = kb[k]
            return ps[p0:p1, kk_ * 512 + o:kk_ * 512 + o + w]

        def b4(k):
            return bank(k).rearrange("p (h q) -> p h q", h=4)

        def bc4(m):
            return m[:].unsqueeze(1).to_broadcast([128, 4, 128])
        idb4 = G.identf[:].unsqueeze(1).to_broadcast([128, 4, 128])
        ea = "scalar"
        P.memset(H32[:], 0.0)
        BD = G.IU if d == 0 else G.IL
        m_abT = G.nSU if d == 0 else G.nSL
        m_ab = G.nSL if d == 0 else G.nSU
        m_s = G.SU if d == 0 else G.SL
        m_i = G.IU if d == 0 else G.IL
        for i in dir_order(d):
            b = i % 2
            rows = slice(i * 128, (i + 1) * 128)
            L = ld[:, b]
            for q, col in enumerate([0, 1, 2, 3 + d, 5 + d, 7 + d]):
                P.dma("sync", L[:, q, :], G.rwp[rows, col * 256:(col + 1) * 256])
            r_, v_, kk_, k_, b_, lw_ = [L[:, q, :] for q in range(6)]
            cs_ps = bank(3, 256)
            P.mm(cs_ps, BD[:], lw_)
            yield
            P.act(ee[:, 0, :], cs_ps, AF.Exp)
            P.act(ee[:, 1, :], cs_ps, AF.Exp, scale=-1.0)
            P.copy(ee[:, 2, :], cs_ps, eng="scalar")
            P.tt(ee[:, 2, :], ee[:, 2, :], lw_, ALU.subtract)
            P.act(ee[:, 2, :], ee[:, 2, :], AF.Exp)
            P.tt(tok[:, 0, :], kk_, ee[:, 2, :], ALU.mult)
            P.tt(tok[:, 1, :], r_, ee[:, 0, :], ALU.mult, eng="gpsimd")
            P.tt(tok[:, 2, :], b_, ee[:, 1, :], ALU.mult)
            P.tt(tok[:, 3, :], k_, ee[:, 1, :], ALU.mult, eng="gpsimd")
            for c in range(2):
                P.ts(btc[:, c, :], tok[:, 2, :], G.cind[:, c:c + 1], ALU.mult, eng=("gpsimd" if c else "vector"))
                P.ts(ktc[:, c, :], tok[:, 3, :], G.cind[:, c:c + 1], ALU.mult, eng=("gpsimd" if c else "vector"))
            yield
            for q in range(4):
                for h in range(4):
                    P.tr(bank(q, 128, h * 128, 0, 64), tok[:, q, h * 64:(h + 1) * 64], G.identf[:])
            yield
            for q in range(4):
                P.copy(fmh[:, q].rearrange("p h t -> p (h t)"), bank(q, 512, 0, 0, 64), eng=("scalar" if q % 2 else "vector"))
            for h in range(4):
                P.mm(bank(0, 2, 2 * h, 0, 64), lw_[:, h * 64:(h + 1) * 64], G.cind[:])
            for h in range(4):
                khT = fmh[:, 0, h, :]; btT = fmh[:, 2, h, :]
                P.mm(bank(1, 128, h * 128), btT, khT)
                P.mm(bank(2, 128, h * 128), khT, btT)
            yield
            P.act(gC[:].rearrange("p h c -> p (h c)"), bank(0, 8, 0, 0, 64), AF.Exp)
            P.tt(Q[:, 0], b4(1), bc4(m_abT), ALU.mult)
            P.tt(QT[:, 0], b4(2), bc4(m_ab), ALU.mult)
            P.tt(IQT[:], QT[:, 0], idb4, ALU.add, eng="gpsimd")
            P.tt(Z[:, 0], Q[:, 0], idb4, ALU.add, eng="gpsimd")
            for h in range(4):
                khT = fmh[:, 0, h, :]; rtT = fmh[:, 1, h, :]; btT = fmh[:, 2, h, :]; ktT = fmh[:, 3, h, :]
                P.mm(bank(3, 128, h * 128), btT, rtT)
                P.mm(bank(0, 128, h * 128), ktT, khT)
            yield
            P.tt(Aarb[:], b4(3), bc4(m_i), ALU.mult)
            P.tt(Aak[:], b4(0), bc4(m_s), ALU.mult)
            zc = 0
            for k in range(1, 6):
                a, bq = (k - 1) % 2, k % 2
                for h in range(4):
                    if k < 5:
                        P.mm(bank(0, 128, h * 128), QT[:, a, h, :], Q[:, a, h, :])
                    P.mm(bank(1, 128, h * 128), Q[:, a, h, :], QT[:, a, h, :])
                if k >= 2:
                    for h in range(4):
                        P.mm(bank(2, 128, h * 128), IQT[:, h, :], Z[:, zc, h, :])
                if k == 1:
                    for h in range(4):
                        P.mm(bank(3, 128, h * 128), fmh[:, 3, h, :], fmh[:, 1, h, :])
                yield
                if k == 1:
                    P.tt(Aark[:], b4(3), bc4(m_i), ALU.mult)
                if k < 5:
                    P.copy(Q[:, bq], b4(0), eng="scalar")
                P.copy(QT[:, bq], b4(1))
                if k >= 2:
                    P.copy(Z[:, 1 - zc], b4(2), eng="scalar")
                    zc = 1 - zc
                P.tt(IQT[:], QT[:, bq], idb4, ALU.add, eng="gpsimd")
            for h in range(4):
                P.mm(bank(2, 128, h * 128), IQT[:, h, :], Z[:, zc, h, :])
            yield
            P.copy(Z[:, 1 - zc], b4(2), eng="scalar")
            zc = 1 - zc
            TT = Z[:, zc]
            for h in range(4):
                hc = slice(h * 64, (h + 1) * 64)
                P.mm(bank(3, 64, h * 64), TT[:, h, :], tok[:, 0, hc])
                P.mm(bank(0, 64, h * 64), Aak[:, h, :], v_[:, hc])
            yield
            P.copy(Wsb[:].rearrange("p h q -> p (h q)"), bank(3, 256), eng="scalar")
            P.ts(nAkV[:].rearrange("p h q -> p (h q)"), bank(0, 256), -1.0, ALU.mult)
            for h in range(4):
                P.mm(bank(1, 64, h * 64), TT[:, h, :], nAkV[:, h, :])
            for h in range(4):
                hc = slice(h * 64, (h + 1) * 64)
                for c in range(2):
                    P.mm(bank(2, 64, (h * 2 + c) * 64, 0, 64), Wsb[:, h, :], btc[:, c, hc])
                P.mm(bank(3, 128, h * 128, 0, 64), Wsb[:, h, :], Aarb[:, h, :])
            yield
            P.copy(Uloc[:].rearrange("p h q -> p (h q)"), bank(1, 256), eng="scalar")
            id8 = G.identf[0:64, 0:64].unsqueeze(1).to_broadcast([64, 8, 64])
            P.tt(GpT[:].rearrange("p h c q -> p (h c) q"), id8, bank(2, 512, 0, 0, 64).rearrange("p (a q) -> p a q", a=8), ALU.subtract)
            P.tt(RwT[:], fmh[:, 1], bank(3, 512, 0, 0, 64).rearrange("p (h q) -> p h q", h=4), ALU.subtract)
            for c in ([0, 1] if d == 0 else [1, 0]):
                cs_ = slice(64 * c, 64 * c + 64)
                for h in range(4):
                    hc = slice(h * 64, (h + 1) * 64)
                    yp = bank(1, 64, (c * 4 + h) * 64, 0, 64)
                    P.mm(yp, Aarb[:, h, cs_], Uloc[:, h, :], start=True, stop=False)
                    P.mm(yp, Aark[:, h, cs_], v_[:, hc], start=False, stop=False)
                    P.mm(yp, RwT[:, h, cs_], H32[:, h, :], start=False, stop=True)
                for h in range(4):
                    hc = slice(h * 64, (h + 1) * 64)
                    hp = bank(0, 64, h * 64, 0, 64)
                    P.mm(hp, btc[:, c, hc], Uloc[:, h, :], start=True, stop=False)
                    P.mm(hp, ktc[:, c, hc], v_[:, hc], start=False, stop=False)
                    P.mm(hp, GpT[:, h, c, :], H32[:, h, :], start=False, stop=True)
                yield
                P.tt(H32[:], bank(0, 256, 0, 0, 64).rearrange("p (h q) -> p h q", h=4),
                     gC[:, :, c].unsqueeze(2).to_broadcast([64, 4, 64]), ALU.mult)
            P.copy(Yt[:, b].rearrange("p c q -> p (c q)"), bank(1, 512, 0, 0, 64), eng="scalar")
            P.dma("sync", G.rwy[d, rows, :].rearrange("(c p) q -> p c q", p=64), Yt[:, b])
            yield

    gens = [stream(0, [0, 1, 2, 3]), stream(1, [4, 5, 6, 7])]
    while gens:
        for g in list(gens):
            try:
                next(g)
            except StopIteration:
                gens.remove(g)
    ph.close()


def rwkv_fin(nc, P, G, l):
    W = G.w
    ph = Phase(nc, P)
    lw_ = ph.sb("lnxw", [128, 256]); lb_ = ph.sb("lnxb", [128, 256])
    yf = ph.sb("ryf", [128, 2, 256]); yb = ph.sb("ryb", [128, 2, 256]); gb = ph.sb("rgb", [128, 2, 2, 256])
    sq = ph.sb("rsq", [128, 256]); s4 = ph.sb("rfs4", [128, 2, 8])
    P.dma("sync", lw_[:], bc_rows(W["rwkv_lnx_w"][l:l + 1, :]))
    P.dma("sync", lb_[:], bc_rows(W["rwkv_lnx_b"][l:l + 1, :]))
    for i in range(NT):
        b = i % 2
        rows = slice(i * 128, (i + 1) * 128)
        P.dma("sync", yf[:, b, :], G.rwy[0, rows, :])
        P.dma("sync", yb[:, b, :], G.rwy[1, rows, :])
        P.dma("sync", gb[:, b].rearrange("p a b -> p (a b)"), G.rwp[rows, 9 * 256:11 * 256])
        y = yf[:, b, :]
        y3 = y.rearrange("p (h q) -> p h q", h=4)
        S = s4[:, b, :]
        P.tt(y, y, yb[:, b, :], ALU.add)
        P.op("vector", lambda e, y3=y3, S=S: e.tensor_reduce(S[:, 0:4], y3, AX.X, ALU.add), reads=[y], writes=[S[:, 0:4]])
        P.ts(S[:, 0:4], S[:, 0:4], 1.0 / 64, ALU.mult)
        P.tt(y3, y3, S[:, 0:4].unsqueeze(2).to_broadcast([128, 4, 64]), ALU.subtract)
        P.tt(sq[:], y, y, ALU.mult, eng="gpsimd")
        P.op("vector", lambda e, S=S: e.tensor_reduce(S[:, 4:8], sq[:].rearrange("p (h q) -> p h q", h=4), AX.X, ALU.add),
             reads=[sq[:]], writes=[S[:, 4:8]])
        P.act(S[:, 4:8], S[:, 4:8], AF.Sqrt, bias=64e-5, scale=1.0 / 64)
        P.op("vector", lambda e, S=S: e.reciprocal(S[:, 4:8], S[:, 4:8]), reads=[S[:, 4:8]], writes=[S[:, 4:8]])
        P.tt(y3, y3, S[:, 4:8].unsqueeze(2).to_broadcast([128, 4, 64]), ALU.mult)
        P.tt(y, y, lw_[:], ALU.mult, eng="gpsimd")
        P.tt(y, y, lb_[:], ALU.add)
        P.tt(y, y, gb[:, b, 1, :], ALU.add)
        P.tt(y, y, gb[:, b, 0, :], ALU.mult)
        P.dma("sync", G.ymix[rows, 512:768], y)
    ph.close()
def tiles_for(l, n_layers_total=4):
    return list(range(2, NT)) if l == n_layers_total - 1 else list(range(NT))


def phase_outproj(nc, P, G, l):
    W = G.w
    ph = Phase(nc, P)
    woutb = ph.sb("woutb", [128, 8, 1024], BF16); wst = ph.sb("owst", [128, 8, 512])
    rw32 = ph.sb("rw32", [128, 8, 64]); rbias = ph.sb("rbias", [128, 64])
    mg1 = ph.sb("mg1", [128, 2, 1024]); msc2 = ph.sb("msc2", [128, 2, 1024]); msh2 = ph.sb("msh2", [128, 2, 1024])
    l1w = ph.sb("l1w", [128, 1024]); l1b = ph.sb("l1b", [128, 1024])
    ym = ph.sb("oym", [128, 2, 1024]); ymb = ph.sb("oymb", [128, 1024], BF16); ymT = ph.sb("oymT", [128, 8, 128], BF16)
    xt = ph.sb("oxt", [128, 2, 1024]); t = ph.sb("ot", [128, 1024]); hh = ph.sb("ohh", [128, 1024])
    hT32 = ph.sb("hT32", [128, 8, 128]); hTb = ph.sb("hTb", [128, 2, 8, 128], BF16)
    st6 = ph.sb("ost6", [128, 2, 6]); mv = ph.sb("omv", [128, 2]); rstd = ph.sb("orstd", [128, 1])
    sc = ph.sb("rsc", [128, 64]); sel = ph.sb("rsel", [128, 64]); m8 = ph.sb("rm8", [128, 8, 8]); gs = ph.sb("rgs", [128, 8])
    g8 = ph.sb("rg8", [128, 8]); gm = ph.sb("rgm", [128, 8]); pen = ph.sb("rpen", [128, 8]); s8 = ph.sb("rs8", [128, 8])
    selm = ph.sb("rselm", [128, 64]); wts = ph.sb("rwts", [128, 64]); den = ph.sb("rden", [128, 1]); gT = ph.sb("rgT", [128, 2, 64])
    for n in range(2):
        P.dma("sync", wst[:], W["w_out"][l, :, n * 512:(n + 1) * 512].rearrange("(kc p) j -> p kc j", p=128))
        P.copy(woutb[:, :, n * 512:(n + 1) * 512], wst[:], eng="gpsimd")
    P.dma("sync", rw32[:], W["router_w"][l].rearrange("(kc p) e -> p kc e", p=128))
    P.dma("sync", rbias[:], bc_rows(W["router_b"][l:l + 1, :]))
    for s in range(2):
        P.dma("sync", mg1[:, s, :], G.modd[s, 2])
        P.dma("sync", msc2[:, s, :], G.modd[s, 4])
        P.dma("sync", msh2[:, s, :], G.modd[s, 3])
    P.dma("sync", l1w[:], bc_rows(W["ln1_w"][l:l + 1, :]))
    P.dma("sync", l1b[:], bc_rows(W["ln1_b"][l:l + 1, :]))
    for i in tiles_for(l):
        b = i % 2
        s = seg_of(i)
        rows = slice(i * 128, (i + 1) * 128)
        P.dma("sync", ym[:, b, :], G.ymix[rows, :])
        P.dma("sync", xt[:, b, :], G.xres[rows, :])
        P.copy(ymb[:], ym[:, b, :], eng="gpsimd")
        for kc in range(8):
            P.tr(G.psb[:, kc * 128:(kc + 1) * 128], ymb[:, kc * 128:(kc + 1) * 128], G.identb[:])
        P.copy(ymT[:].rearrange("p a b -> p (a b)"), G.psb[:, 0:1024], eng="scalar")
        for n in range(2):
            pso = G.psf[:, n * 512:(n + 1) * 512]
            for kc in range(8):
                P.mm(pso, ymT[:, kc, :], woutb[:, kc, n * 512:(n + 1) * 512], start=(kc == 0), stop=(kc == 7))
            P.tt(t[:, n * 512:(n + 1) * 512], pso, mg1[:, s, n * 512:(n + 1) * 512], ALU.mult)
        x = xt[:, b, :]
        P.stt(t[:], x, DN_ALPHA, t[:], ALU.mult, ALU.add)
        ln_stats(P, ph, t[:], "o1", st6[:], mv[:], rstd[:])
        P.ts(t[:], t[:], mv[:, 0:1], ALU.subtract, rstd[:, 0:1], ALU.mult)
        P.tt(t[:], t[:], l1w[:], ALU.mult, eng="gpsimd")
        P.tt(x, t[:], l1b[:], ALU.add)
        P.dma("sync", G.xres[rows, :], x)
        ln_stats(P, ph, x, "o2", st6[:], mv[:], rstd[:])
        P.ts(hh[:], x, mv[:, 0:1], ALU.subtract, rstd[:, 0:1], ALU.mult)
        P.tt(hh[:], hh[:], msc2[:, s, :], ALU.mult, eng="gpsimd")
        P.tt(hh[:], hh[:], msh2[:, s, :], ALU.add)
        if G.dbg is not None and "hdbg" in G.dbg:
            P.dma("sync", G.dbg["hdbg"][rows, :], hh[:])
        for half in range(2):
            for kk in range(4):
                kc = half * 4 + kk
                P.tr(G.psf[:, (2 + half) * 512 + kk * 128:(2 + half) * 512 + (kk + 1) * 128], hh[:, kc * 128:(kc + 1) * 128], G.identf[:])
            P.copy(hT32[:, half * 4:half * 4 + 4, :].rearrange("p a b -> p (a b)"), G.psf[:, (2 + half) * 512:(3 + half) * 512], eng="scalar")
        P.copy(hTb[:, b], hT32[:], eng="gpsimd")
        P.dma("sync", G.hT[:, :, rows].rearrange("kc p t -> p kc t"), hTb[:, b])
        lg = G.psf[:, 4 * 512:4 * 512 + 64]
        for kc in range(8):
            P.mm(lg, hT32[:, kc, :], rw32[:, kc, :], start=(kc == 0), stop=(kc == 7))
        P.act(sc[:], lg, AF.Sigmoid)
        P.tt(sel[:], sc[:], rbias[:], ALU.add)
        for g in range(8):
            P.op("vector", lambda e, g=g: e.max(m8[:, g, :], sel[:, g * 8:(g + 1) * 8]), reads=[sel[:, g * 8:(g + 1) * 8]], writes=[m8[:, g, :]])
        P.tt(gs[:], m8[:, :, 0], m8[:, :, 1], ALU.add)
        P.op("vector", lambda e: e.max(g8[:], gs[:]), reads=[gs[:]], writes=[g8[:]])
        P.ts(gm[:], gs[:], g8[:, 3:4], ALU.is_ge)
        P.ts(pen[:], gm[:], 1.0, ALU.subtract, 1e30, ALU.mult)
        sel3 = sel[:].rearrange("p (g q) -> p g q", g=8)
        selm3 = selm[:].rearrange("p (g q) -> p g q", g=8)
        P.tt(selm3, sel3, gm[:].unsqueeze(2).to_broadcast([128, 8, 8]), ALU.mult)
        P.tt(selm3, selm3, pen[:].unsqueeze(2).to_broadcast([128, 8, 8]), ALU.add)
        P.op("vector", lambda e: e.max(s8[:], selm[:]), reads=[selm[:]], writes=[s8[:]])
        P.ts(wts[:], selm[:], s8[:, 7:8], ALU.is_ge)
        P.tt(wts[:], wts[:], sc[:], ALU.mult)
        P.op("vector", lambda e: e.tensor_reduce(den[:], wts[:], AX.X, ALU.add), reads=[wts[:]], writes=[den[:]])
        P.op("vector", lambda e: e.reciprocal(den[:], den[:]), reads=[den[:]], writes=[den[:]])
        P.ts(wts[:], wts[:], den[:, 0:1], ALU.mult, 2.5, ALU.mult)
        P.copy(gT[:, b, :], wts[:], eng="gpsimd")
        P.dma("sync", G.gtok[rows, :], gT[:, b, :])
    ph.close()


def phase_moe(nc, P, G, l):
    W = G.w
    tl = tiles_for(l)
    half = (len(tl) + 1) // 2
    for (pa, tiles) in enumerate((tl[:half], tl[half:])):
        ph = Phase(nc, P)
        ntl = len(tiles)
        ntok = ntl * 128
        t0 = tiles[0] * 128
        acc = ph.sb("macc", [128, ntl, 1024])
        hTp = ph.sb("mhT", [128, 8, ntok], BF16); gk = ph.sb("mgk", [128, ntl, 64])
        w13s = ph.sb("w13s", [128, 8, 512]); w13b = ph.sb("w13b", [128, 2, 8, 512], BF16)
        w2s = ph.sb("w2s", [128, 2, 1024]); w2b = ph.sb("w2b", [128, 2, 2, 1024], BF16)
        hid = ph.sb("mhid", [128, 2, 2, 512], BF16); sil = ph.sb("msil", [128, 2, 512])
        P.dma("sync", hTp[:], G.hT[:, :, t0:t0 + ntok].rearrange("kc p t -> p kc t"))
        P.dma("sync", gk[:], G.gtok[t0:t0 + ntok, :].rearrange("(k p) e -> p k e", p=128))
        groups = []
        c0 = 0
        while c0 < ntok:
            n = min(512, ntok - c0)
            groups.append((c0, n)); c0 += n
        NE = 65
        orot = [0]
        pending = [None]

        def emit_down(e, eb, hb, c0, n):
            for sub in range(n // 128):
                ti = (c0 // 128) + sub
                for cc in range(2):
                    ob = 4 + (orot[0] % 4)
                    orot[0] += 1
                    o = G.psf[:, ob * 512:(ob + 1) * 512]
                    for fc in range(2):
                        P.mm(o, hid[:, hb, fc, sub * 128:(sub + 1) * 128], w2b[:, eb, fc, cc * 512:(cc + 1) * 512],
                             start=(fc == 0), stop=(fc == 1))
                    a = acc[:, ti, cc * 512:(cc + 1) * 512]
                    if e == 0:
                        P.ts(a, o, gk[:, ti, 0:1], ALU.mult)
                    elif e < 64:
                        P.stt(a, o, gk[:, ti, e:e + 1], a, ALU.mult, ALU.add)
                    else:
                        P.tt(a, a, o, ALU.add)
        for e in range(NE):
            eb = e % 2
            if e < 64:
                s1, s3, s2 = W["exp_w1"][l, e], W["exp_w3"][l, e], W["exp_w2"][l, e]
            else:
                s1, s3, s2 = W["sh_w1"][l], W["sh_w3"][l], W["sh_w2"][l]
            import os
            if e < 2 or not os.environ.get("KNOW"):
                P.dma("sync", w13s[:, :, 0:256], s1.rearrange("(kc p) f -> p kc f", p=128))
                P.dma("sync", w13s[:, :, 256:512], s3.rearrange("(kc p) f -> p kc f", p=128))
                P.dma("sync", w2s[:], s2.rearrange("(fc p) c -> p fc c", p=128))
                P.copy(w13b[:, eb], w13s[:], eng="gpsimd")
                P.copy(w2b[:, eb], w2s[:], eng="gpsimd")
            for gi, (c0, n) in enumerate(groups):
                hb = (e * len(groups) + gi) % 2
                cols = slice(c0, c0 + n)
                for fc in range(2):
                    h1 = G.psf[:, fc * 512:fc * 512 + n]
                    h3 = G.psf[:, (2 + fc) * 512:(2 + fc) * 512 + n]
                    for kc in range(8):
                        P.mm(h1, w13b[:, eb, kc, fc * 128:(fc + 1) * 128], hTp[:, kc, cols], start=(kc == 0), stop=(kc == 7))
                    for kc in range(8):
                        P.mm(h3, w13b[:, eb, kc, 256 + fc * 128:256 + (fc + 1) * 128], hTp[:, kc, cols], start=(kc == 0), stop=(kc == 7))
                    P.act(sil[:, fc, 0:n], h1, AF.Silu)
                    P.tt(hid[:, hb, fc, 0:n], sil[:, fc, 0:n], h3, ALU.mult)
                if pending[0] is not None:
                    emit_down(*pending[0])
                pending[0] = (e, eb, hb, c0, n)
        emit_down(*pending[0])
        pending[0] = None
        mg2 = ph2 = None
        fin = Phase(nc, P)
        mg2 = fin.sb("mg2", [128, 2, 1024]); l2w = fin.sb("l2w", [128, 1024]); l2b = fin.sb("l2b", [128, 1024])
        xt = fin.sb("mxt", [128, 2, 1024]); st6 = fin.sb("mst6", [128, 2, 6]); mv = fin.sb("mmv", [128, 2]); rstd = fin.sb("mrstd", [128, 1])
        for s in range(2):
            P.dma("sync", mg2[:, s, :], G.modd[s, 5])
        P.dma("sync", l2w[:], bc_rows(W["ln2_w"][l:l + 1, :]))
        P.dma("sync", l2b[:], bc_rows(W["ln2_b"][l:l + 1, :]))
        for k, i in enumerate(tiles):
            b = k % 2
            s = seg_of(i)
            rows = slice(i * 128, (i + 1) * 128)
            x = xt[:, b, :]
            P.dma("sync", x, G.xres[rows, :])
            f = acc[:, k, :]
            if G.dbg is not None and "fdbg" in G.dbg:
                P.dma("sync", G.dbg["fdbg"][rows, :], f)
            P.tt(f, f, mg2[:, s, :], ALU.mult, eng="gpsimd")
            P.stt(f, x, DN_ALPHA, f, ALU.mult, ALU.add)
            ln_stats(P, fin, f, "m", st6[:], mv[:], rstd[:])
            P.ts(f, f, mv[:, 0:1], ALU.subtract, rstd[:, 0:1], ALU.mult)
            P.tt(f, f, l2w[:], ALU.mult, eng="gpsimd")
            P.tt(x, f, l2b[:], ALU.add)
            if G.dbg is not None and "xout" in G.dbg:
                P.dma("sync", G.dbg["xout"][rows, :], x)
            if l == 3 and G.final_out:
                P.dma("sync", G.out[(i - 2) * 128:(i - 1) * 128, :], x, final=True)
            else:
                P.dma("sync", G.xres[rows, :], x)
        fin.st.close()
        ph.close()
def alloc_scratch(nc, G, scratch):
    G.xbca = scratch("xbca", [T_ALL, 1024])
    G.dtla = scratch("dtla", [T_ALL, 32])
    G.ydir = scratch("ydir", [2, T_ALL, 512])
    G.gla_la = scratch("gla_la", [T_ALL, 256])
    G.gla_o = scratch("gla_o", [2, T_ALL, 256])
    G.rwp = scratch("rwp", [T_ALL, 11 * 256])
    G.rwy = scratch("rwy", [2, T_ALL, 256])
    G.hT = scratch("hT", [8, 128, T_ALL], BF16)
    G.gtok = scratch("gtok", [T_ALL, 64])

PHASES = {"mod": phase_mod, "inproj": phase_inproj, "ssd": phase_ssd, "ssd_prep": ssd_prep, "ssd_dir": ssd_dir, "ssd_fin": ssd_fin,
          "gla": phase_gla, "rwkv": phase_rwkv, "rwkv_prep": rwkv_prep, "rwkv_dir": rwkv_dir, "rwkv_fin": rwkv_fin, "outproj": phase_outproj, "moe": phase_moe}
WNAMES = ["ada_w", "ada_b", "w_in", "ssd_conv_w", "ssd_conv_b", "ssd_dt_bias", "ssd_a_log", "ssd_d", "ssd_norm_w",
          "rwkv_mu", "rwkv_w0", "rwkv_w2", "rwkv_a0", "rwkv_a2", "rwkv_g2", "rwkv_kk", "rwkv_ka", "rwkv_rk",
          "rwkv_lnx_w", "rwkv_lnx_b", "gla_gu", "gla_gb", "gla_norm_w", "w_out", "ln1_w", "ln1_b", "ln2_w", "ln2_b",
          "router_w", "router_b", "exp_w1", "exp_w3", "exp_w2", "sh_w1", "sh_w3", "sh_w2"]
WSHAPES = {"ada_w": [4, 1024, 6144], "ada_b": [4, 6144], "w_in": [4, 1024, 3504], "ssd_conv_w": [4, 3, 1024],
           "ssd_conv_b": [4, 1024], "ssd_dt_bias": [4, 2, 8], "ssd_a_log": [4, 2, 8], "ssd_d": [4, 8],
           "ssd_norm_w": [4, 512], "rwkv_mu": [4, 4, 1152], "rwkv_w0": [4, 2, 256], "rwkv_w2": [4, 2, 64, 256],
           "rwkv_a0": [4, 2, 256], "rwkv_a2": [4, 2, 64, 256], "rwkv_g2": [4, 128, 256], "rwkv_kk": [4, 256],
           "rwkv_ka": [4, 256], "rwkv_rk": [4, 4, 64], "rwkv_lnx_w": [4, 256], "rwkv_lnx_b": [4, 256],
           "gla_gu": [4, 2, 16, 128], "gla_gb": [4, 2, 128], "gla_norm_w": [4, 256], "w_out": [4, 1024, 1024],
           "ln1_w": [4, 1024], "ln1_b": [4, 1024], "ln2_w": [4, 1024], "ln2_b": [4, 1024],
           "router_w": [4, 1024, 64], "router_b": [4, 64], "exp_w1": [4, 64, 1024, 256], "exp_w3": [4, 64, 1024, 256],
           "exp_w2": [4, 64, 256, 1024], "sh_w1": [4, 1024, 256], "sh_w3": [4, 1024, 256], "sh_w2": [4, 256, 1024]}


def build_program(layers=(0, 1, 2, 3), phases=None, dbg=(), wnames=None, feed=()):
    nc = bass.Bass("TRN2", target_bir_lowering=False)
    P = Prog(nc)
    G = Ctx()
    G.w = {}
    import os
    G.stop = float(os.environ.get('KSTOP', '99'))
    for n in (wnames or WNAMES):
        G.w[n] = nc.dram_tensor(n, WSHAPES[n], F32, kind="ExternalInput").ap()
    G.x_in = nc.dram_tensor("x", [4096, 1024], F32, kind="ExternalInput").ap()
    G.ctx_in = nc.dram_tensor("ctx", [256, 1024], F32, kind="ExternalInput").ap()
    G.cvec = nc.dram_tensor("cvec", [2, 1024], F32, kind="ExternalInput").ap()
    G.out = nc.dram_tensor("out", [4096, 1024], F32, kind="ExternalOutput").ap()
    DBG_SHAPES = {"u": ([T_ALL, 1024], BF16), "modd": ([2, 6, 128, 1024], F32), "zin": ([T_ALL, NIN], F32),
                  "ymix": ([T_ALL, 1024], F32), "xres": ([T_ALL, 1024], F32)}
    G.dbg = {}
    G.final_out = (phases is None or "moe" in phases)

    def scratch(name, shape, dt=F32):
        kind = "ExternalOutput" if name in dbg else ("ExternalInput" if name in feed else "Internal")
        t = nc.dram_tensor(name, list(shape), dt, kind=kind).ap()
        return t
    for n in dbg:
        if n in ("u",):
            G.dbg[n] = nc.dram_tensor(n, DBG_SHAPES[n][0], DBG_SHAPES[n][1], kind="ExternalOutput").ap()
        if n in ("hdbg", "fdbg", "xout"):
            G.dbg[n] = nc.dram_tensor(n, [T_ALL, 1024], F32, kind="ExternalOutput").ap()
    G.xres = scratch("xres", [T_ALL, 1024])
    G.modd = scratch("modd", [2, 6, 128, 1024])
    G.zin = scratch("zin", [T_ALL, NIN])
    G.ymix = scratch("ymix", [T_ALL, 1024])
    G.psf = nc.alloc_psum_tensor("psa", [128, 8 * 512], F32)
    G.psb = G.psf.bitcast(BF16)[:, 6 * 1024:8 * 1024]
    make_consts(nc, P, G)
    alloc_scratch(nc, G, scratch)
    if "xres" not in feed:
        P.dma("sync", G.xres[0:256, :], G.ctx_in)
        P.dma("sync", G.xres[256:T_ALL, :], G.x_in)
    P.barrier()
    allp = ["mod", "inproj", "ssd", "gla", "rwkv", "outproj", "moe"]
    for l in layers:
        for pn in (phases or allp):
            PHASES[pn](nc, P, G, l)
    if phases is None or "moe" in phases:
        pass
    else:
        pass
    P.barrier()
    P.dma("sync", G.out[0:128, 0:8], G.xres[0:128, 0:8], final=True) if (phases is not None and "moe" not in phases) else None
    P.finish()
    return nc, P


_NC_CACHE = {}


def kernel(**inputs):
    from concourse.bass_utils import run_bass_kernel_spmd
    if "nc" not in _NC_CACHE:
        nc, P = build_program()
        _NC_CACHE["nc"] = nc
    nc = _NC_CACHE["nc"]
    B = 4
    base = {n: np.ascontiguousarray(np.asarray(inputs[n], dtype=np.float32)) for n in WNAMES}
    x = np.asarray(inputs["x"], dtype=np.float32)
    ctx = np.asarray(inputs["ctx"], dtype=np.float32)
    c = np.asarray(inputs["c"], dtype=np.float32)
    c_ctx = np.asarray(inputs["c_ctx"], dtype=np.float32)
    in_maps = []
    for b in range(B):
        m = dict(base)
        m["x"] = np.ascontiguousarray(x[b])
        m["ctx"] = np.ascontiguousarray(ctx[b])
        m["cvec"] = np.ascontiguousarray(np.stack([c_ctx, c[b]]))
        in_maps.append(m)
    res = run_bass_kernel_spmd(nc, in_maps, core_ids=list(range(B)))
    out = np.stack([np.asarray(res.results[b]["out"], dtype=np.float32) for b in range(B)])
    return out
```

```python
import numpy as np
import concourse.bass as bass
import concourse.mybir as mybir

F32 = mybir.dt.float32
BF16 = mybir.dt.bfloat16
I32 = mybir.dt.int32
U32 = mybir.dt.uint32
AF = mybir.ActivationFunctionType
ALU = mybir.AluOpType
AX = mybir.AxisListType

_DSZ = {F32: 4, BF16: 2, I32: 4, U32: 4}

ENGS = ("tensor", "vector", "scalar", "gpsimd", "sync")
DMA_K = {"sync": 16, "gpsimd": 8, "scalar": 4}


def _box(ap):
    t = ap.tensor
    name = t.name
    dsz = _DSZ.get(ap.dtype, 4)
    steps = list(ap.ap)
    off = ap.offset
    space = str(ap.space) if hasattr(ap, "space") else ""
    if "DRAM" in space.upper() or "dram" in space.lower() or type(t).__name__.startswith("DRam"):
        lo = off + sum(min(0, s * (c - 1)) for s, c in steps)
        hi = off + sum(max(0, s * (c - 1)) for s, c in steps) + 1
        return name, 0, 1, lo * dsz, hi * dsz
    pstep, pcnt = steps[0]
    if pstep == 0:
        pstep = 1 << 30
    p_lo = off // pstep
    f_off = off - p_lo * pstep
    rest = steps[1:]
    lo = f_off + sum(min(0, s * (c - 1)) for s, c in rest)
    hi = f_off + sum(max(0, s * (c - 1)) for s, c in rest) + 1
    lo *= dsz
    hi *= dsz
    if name.startswith("ps"):
        lo = (lo // 2048) * 2048
        hi = ((hi + 2047) // 2048) * 2048
        p_lo, pcnt = 0, 128
    return name, p_lo, p_lo + pcnt, lo, hi


class Prog:
    def __init__(self, nc):
        self.nc = nc
        self.ops = {e: [] for e in ENGS}
        self.cnt = {e: 0 for e in ENGS}
        self.dcnt = {e: 0 for e in DMA_K}
        self.waited = {e: {} for e in ENGS}
        self.recs = {}
        self.semkeys = set()
        self.final_tokens = []
        self.n_ops = 0

    def _deps(self, reads, writes, pe_accum=False, eng=None):
        deps = set()
        rb = [_box(a) for a in reads]
        wb = [_box(a) for a in writes]
        for (name, p0, p1, f0, f1) in rb:
            isps = name.startswith("ps")
            for r in self.recs.get(name, ()):
                if r[0] < p1 and p0 < r[1] and r[2] < f1 and f0 < r[3]:
                    if r[4] == "w" or (isps and r[5][0] != eng):
                        deps.add(r[5])
        for (name, p0, p1, f0, f1) in wb:
            for r in self.recs.get(name, ()):
                if r[0] < p1 and p0 < r[1] and r[2] < f1 and f0 < r[3]:
                    if r[4] == "w" and r[5][0] == "tensor" and eng == "tensor":
                        continue
                    deps.add(r[5])
        return deps, rb, wb

    def _update(self, rb, wb, tok):
        for (name, p0, p1, f0, f1) in wb:
            lst = self.recs.setdefault(name, [])
            lst[:] = [r for r in lst if not (p0 <= r[0] and r[1] <= p1 and f0 <= r[2] and r[3] <= f1)]
            lst.append((p0, p1, f0, f1, "w", tok))
        for (name, p0, p1, f0, f1) in rb:
            lst = self.recs.setdefault(name, [])
            lst[:] = [r for r in lst if not (r[4] == "r" and r[5][0] == tok[0] and p0 <= r[0] and r[1] <= p1
                                             and f0 <= r[2] and r[3] <= f1)]
            lst.append((p0, p1, f0, f1, "r", tok))

    def _waits(self, eng, deps):
        w = self.waited[eng]
        best = {}
        for (k, v) in deps:
            if v > best.get(k, 0):
                best[k] = v
        out = []
        for k, v in best.items():
            if w.get(k, 0) >= v:
                continue
            w[k] = v
            out.append((k, v))
        return out

    def op(self, eng, fn, reads=(), writes=(), pe_accum=False):
        deps, rb, wb = self._deps(reads, writes, pe_accum, eng)
        self.cnt[eng] += 1
        tok = (eng, self.cnt[eng])
        waits = self._waits(eng, deps)
        self.semkeys.add(eng)
        self.ops[eng].append((waits, fn, (eng, 1)))
        self._update(rb, wb, tok)
        self.n_ops += 1
        return tok

    def dma(self, q, out, in_, final=False, **kw):
        deps, rb, wb = self._deps([in_], [out])
        n = self.dcnt[q]
        self.dcnt[q] += 1
        K = DMA_K[q]
        key = ("dma", q, n % K)
        tok = (key, 16 * (n // K + 1))
        if n >= K:
            deps.add((key, 16 * (n // K)))
        waits = self._waits(q, deps)
        self.semkeys.add(key)
        self.ops[q].append((waits, (lambda e, o=out, i=in_, kw=kw: e.dma_start(out=o, in_=i, **kw)), (key, 16)))
        self._update(rb, wb, tok)
        if final:
            self.final_tokens.append(tok)
        self.n_ops += 1
        return tok

    def barrier(self):
        toks = set()
        for e in ENGS:
            if self.cnt[e] > 0:
                toks.add((e, self.cnt[e]))
        for q, K in DMA_K.items():
            n = self.dcnt[q]
            for j in range(min(n, K)):
                last = ((n - 1 - j) // K) * K + j
                toks.add((("dma", q, j), 16 * (last // K + 1)))
        for e in ENGS:
            waits = self._waits(e, set(toks))
            if waits:
                self.ops[e].append((waits, None, None))
        self.recs.clear()

    def mm(self, out, lhsT, rhs, start=True, stop=True):
        return self.op("tensor", lambda e: e.matmul(out, lhsT, rhs, start=start, stop=stop),
                       reads=[lhsT, rhs], writes=[out], pe_accum=not start)

    def tr(self, out, in_, ident):
        return self.op("tensor", lambda e: e.transpose(out, in_, ident), reads=[in_, ident], writes=[out])

    def act(self, out, in_, func, bias=None, scale=None, eng="scalar", accum_out=None):
        reads = [in_]
        kw = {}
        if bias is not None:
            kw["bias"] = bias
            if not isinstance(bias, (int, float)):
                reads.append(bias)
        if scale is not None:
            kw["scale"] = scale
            if not isinstance(scale, (int, float)):
                reads.append(scale)
        writes = [out]
        if accum_out is not None:
            kw["accum_out"] = accum_out
            writes.append(accum_out)
        return self.op(eng, lambda e: e.activation(out, in_, func, **kw), reads=reads, writes=writes)

    def tt(self, out, in0, in1, op, eng="vector"):
        return self.op(eng, lambda e: e.tensor_tensor(out, in0, in1, op), reads=[in0, in1], writes=[out])

    def ts(self, out, in0, s1, op0, s2=None, op1=None, eng="vector", accum_out=None):
        reads = [in0]
        if not isinstance(s1, (int, float)):
            reads.append(s1)
        if s2 is not None and not isinstance(s2, (int, float)):
            reads.append(s2)
        writes = [out]
        kw = {}
        if accum_out is not None:
            kw["accum_out"] = accum_out
            writes.append(accum_out)
        if op1 is None:
            return self.op(eng, lambda e: e.tensor_scalar(out, in0, s1, None, op0, **kw), reads=reads, writes=writes)
        return self.op(eng, lambda e: e.tensor_scalar(out, in0, s1, s2, op0, op1, **kw), reads=reads, writes=writes)

    def stt(self, out, in0, scalar, in1, op0, op1, eng="vector"):
        reads = [in0, in1]
        if not isinstance(scalar, (int, float)):
            reads.append(scalar)
        return self.op(eng, lambda e: e.scalar_tensor_tensor(out, in0, scalar, in1, op0, op1), reads=reads, writes=[out])

    def copy(self, out, in_, eng="vector"):
        if eng == "scalar":
            return self.op(eng, lambda e: e.copy(out, in_), reads=[in_], writes=[out])
        return self.op(eng, lambda e: e.tensor_copy(out, in_), reads=[in_], writes=[out])

    def memset(self, out, val, eng="vector"):
        return self.op(eng, lambda e: e.memset(out, val), writes=[out])

    def finish(self):
        nc = self.nc
        sems = {}
        import contextlib
        with contextlib.ExitStack() as st:
            for k in sorted(self.semkeys, key=str):
                nm = "s_" + "_".join(str(x) for x in (k if isinstance(k, tuple) else (k,)))
                sems[k] = st.enter_context(nc.semaphore(nm))
            block = st.enter_context(nc.Block())
            fin = self._waits("sync", set(self.final_tokens))
            ops = self.ops

            def make(engname, extra=None):
                def body(e):
                    for waits, fn, inc in ops[engname]:
                        for (k, v) in waits:
                            e.wait_ge(sems[k], v)
                        if fn is None:
                            continue
                        ins = fn(e)
                        ins.then_inc(sems[inc[0]], inc[1])
                    if extra:
                        for (k, v) in extra:
                            e.wait_ge(sems[k], v)
                return body

            block.tensor(make("tensor"))
            block.vector(make("vector"))
            block.scalar(make("scalar"))
            block.gpsimd(make("gpsimd"))
            block.sync(make("sync", fin))
        return nc
import contextlib

T_ALL = 4352
NT = 34
D = 1024
NIN = 3504
LN_EPS = 1e-5
DN_ALPHA = 8 ** 0.25
NEGM = -30000.0

C_Z = 0
C_XBC = 512
C_DT = 1536
C_RW = 1552
C_GQ = 2704
C_GK = 2832
C_GV = 2960
C_GD = 3216
C_OG = 3248


def seg_of(i):
    return 0 if i < 2 else 1


class Phase:
    _n = [0]

    def __init__(self, nc, P):
        self.nc = nc
        self.P = P
        self.st = contextlib.ExitStack()

    def sb(self, name, shape, dt=F32):
        Phase._n[0] += 1
        return self.st.enter_context(self.nc.sbuf_tensor(f"{name}_{Phase._n[0]}", list(shape), dt))

    def close(self):
        self.P.barrier()
        self.st.close()


def bc_rows(ap2d_row, n=128):
    steps = list(ap2d_row.ap)
    last = steps[-1]
    return bass.AP(ap2d_row.tensor, ap2d_row.offset, [[0, n], [last[0], last[1]]])


class Ctx:
    pass


def make_consts(nc, P, G):
    def sbp(name, shape, dt=F32):
        return nc.alloc_sbuf_tensor(name, list(shape), dt)
    G.identf = sbp("identf", [128, 128])
    G.identb = sbp("identb", [128, 128], BF16)
    G.onesf = sbp("onesf", [128, 128])
    G.tri = [sbp("tri_f", [128, 128]), sbp("tri_b", [128, 128])]
    G.negm = [sbp("negm_f", [128, 4, 128]), sbp("negm_b", [128, 4, 128])]
    G.trib = [sbp("trib_f", [128, 128], BF16), sbp("trib_b", [128, 128], BF16)]
    G.SL = sbp("m_SL", [128, 128]); G.SU = sbp("m_SU", [128, 128]); G.IU = sbp("m_IU", [128, 128]); G.IL = sbp("m_IL", [128, 128])
    gp = "gpsimd"
    G.nSU = sbp("m_nSU", [128, 128]); G.nSL = sbp("m_nSL", [128, 128])
    G.cind = sbp("cind", [128, 2]); G.cmask = sbp("cmask", [128, 2])
    G.hm4 = sbp("hm4", [128, 4])
    P.memset(G.hm4[:], 1.0, eng=gp)
    P.op(gp, lambda e: e.affine_select(G.hm4[:], G.hm4[:], [[-32, 4]], ALU.is_ge, 0.0, base=0, channel_multiplier=1),
         reads=[G.hm4[:]], writes=[G.hm4[:]])
    P.op(gp, lambda e: e.affine_select(G.hm4[:], G.hm4[:], [[32, 4]], ALU.is_ge, 0.0, base=31, channel_multiplier=-1),
         reads=[G.hm4[:]], writes=[G.hm4[:]])

    def asel(t, cm, step, cmp, base=0):
        P.op(gp, lambda e: e.affine_select(t, t, [[step, 128]], cmp, 0.0, base=base, channel_multiplier=cm),
             reads=[t], writes=[t])
    for t in (G.identf, G.onesf, G.tri[0], G.tri[1], G.SL, G.SU, G.IU, G.IL):
        P.memset(t[:], 1.0, eng=gp)
    asel(G.identf[:], -1, 1, ALU.is_equal)
    asel(G.tri[0][:], -1, 1, ALU.is_ge)
    asel(G.tri[1][:], 1, -1, ALU.is_ge)
    asel(G.IU[:], -1, 1, ALU.is_ge)
    asel(G.IL[:], 1, -1, ALU.is_ge)
    asel(G.SU[:], -1, 1, ALU.is_gt)
    asel(G.SL[:], 1, -1, ALU.is_gt)
    for t in (G.SL, G.SU, G.IU, G.IL):
        P.memset(t[0:64, 64:128], 0.0, eng=gp)
        P.memset(t[64:128, 0:64], 0.0, eng=gp)
    P.ts(G.nSU[:], G.SU[:], -1.0, ALU.mult, eng=gp)
    P.ts(G.nSL[:], G.SL[:], -1.0, ALU.mult, eng=gp)
    P.memset(G.cind[:], 0.0, eng=gp)
    P.memset(G.cind[0:64, 0:1], 1.0, eng=gp)
    P.memset(G.cind[64:128, 1:2], 1.0, eng=gp)
    P.memset(G.cmask[:], 1.0, eng=gp)
    for (col, base) in ((0, 0), (0, -64), (1, -63), (1, -127)):
        P.op(gp, lambda e, col=col, base=base: e.affine_select(G.cmask[:, col:col + 1], G.cmask[:, col:col + 1], [[0, 1]],
                                                             ALU.not_equal, 0.0, base=base, channel_multiplier=1),
             reads=[G.cmask[:, col:col + 1]], writes=[G.cmask[:, col:col + 1]])
    P.copy(G.identb[:], G.identf[:])
    for d in range(2):
        P.copy(G.trib[d][:], G.tri[d][:])
        for h in range(4):
            P.ts(G.negm[d][:, h, :], G.tri[d][:], 1.0, ALU.subtract, -NEGM, ALU.mult)


def ln_stats(P, ph, x_ap, tag, st6, mv, rstd, eps=LN_EPS):
    for hh in range(2):
        P.op("vector", lambda e, hh=hh: e.bn_stats(st6[:, hh, :], x_ap[:, hh * 512:(hh + 1) * 512]),
             reads=[x_ap[:, hh * 512:(hh + 1) * 512]], writes=[st6[:, hh, :]])
    P.op("vector", lambda e: e.bn_aggr(mv, st6), reads=[st6], writes=[mv])
    P.act(rstd, mv[:, 1:2], AF.Sqrt, bias=eps)
    P.op("vector", lambda e: e.reciprocal(rstd, rstd), reads=[rstd], writes=[rstd])


def phase_mod(nc, P, G, l):
    ph = Phase(nc, P)
    cT = ph.sb("cT", [128, 2, 8]); csil = ph.sb("csil", [128, 2, 8])
    wst = ph.sb("adaw", [128, 2, 8, 512]); brow = ph.sb("brow", [1, 6144]); mst = ph.sb("mst", [128, 2, 2, 512])
    for s in range(2):
        P.dma("sync", cT[:, s, :], G.cvec[s, :].rearrange("(kc p) -> p kc", p=128), allow_slow_non_contiguous=True)
    P.act(csil[:], cT[:], AF.Silu)
    P.dma("sync", brow[:], G.w["ada_b"][l:l + 1, :])
    for n in range(12):
        b = n % 2
        P.dma("sync", wst[:, b], G.w["ada_w"][l, :, n * 512:(n + 1) * 512].rearrange("(kc p) j -> p kc j", p=128))
        for s in range(2):
            pso = G.psf[:, (n % 2 * 2 + s) * 512:(n % 2 * 2 + s + 1) * 512]
            for kc in range(8):
                P.mm(pso, csil[:, s, kc:kc + 1].to_broadcast([128, 128]), wst[:, b, kc, :], start=(kc == 0), stop=False)
            P.mm(pso, G.onesf[0:1, :], brow[0:1, n * 512:(n + 1) * 512], start=False, stop=True)
            which = n // 2
            if which in (1, 4):
                P.act(mst[:, b, s, :], pso, AF.Identity, bias=1.0)
            else:
                P.copy(mst[:, b, s, :], pso)
            P.dma(G.stq, G.modd[s, which, :, (n % 2) * 512:(n % 2 + 1) * 512], mst[:, b, s, :])
    ph.close()


def phase_inproj(nc, P, G, l):
    ph = Phase(nc, P)
    winb = ph.sb("winb", [128, 8, NIN], BF16)
    wst = ph.sb("wst", [128, 2, 8, 512])
    msc = ph.sb("msc", [128, 2, 1024]); msh = ph.sb("msh", [128, 2, 1024])
    xt = ph.sb("xt", [128, 2, 1024]); ub = ph.sb("ub", [128, 2, 1024], BF16)
    uT = ph.sb("uT", [128, 2, 8, 128], BF16); zt = ph.sb("zt", [128, 2, NIN])
    st6 = ph.sb("st6", [128, 2, 6]); mv = ph.sb("mv", [128, 2]); rstd = ph.sb("rstd", [128, 1])
    for n in range(7):
        w = min(512, NIN - n * 512)
        P.dma("sync", wst[:, n % 2, :, 0:w], G.w["w_in"][l, :, n * 512:n * 512 + w].rearrange("(kc p) j -> p kc j", p=128))
        P.copy(winb[:, :, n * 512:n * 512 + w], wst[:, n % 2, :, 0:w], eng="gpsimd")
    for s in range(2):
        P.dma("sync", msc[:, s, :], G.modd[s, 1])
        P.dma("sync", msh[:, s, :], G.modd[s, 0])
    def stage_a(i):
        b = i % 2
        s = seg_of(i)
        x = xt[:, b, :]
        P.dma("sync", x, G.xres[i * 128:(i + 1) * 128, :])
        ln_stats(P, ph, x, "a", st6[:], mv[:], rstd[:])
        P.ts(x, x, mv[:, 0:1], ALU.subtract, rstd[:, 0:1], ALU.mult)
        P.tt(x, x, msc[:, s, :], ALU.mult, eng="gpsimd")
        P.tt(ub[:, b, :], x, msh[:, s, :], ALU.add)
        if G.dbg is not None and "u" in G.dbg:
            P.dma("sync", G.dbg["u"][i * 128:(i + 1) * 128, :], ub[:, b, :])
        for kc in range(8):
            P.tr(G.psb[:, kc * 128:(kc + 1) * 128], ub[:, b, kc * 128:(kc + 1) * 128], G.identb[:])
        P.copy(uT[:, b].rearrange("p a b -> p (a b)"), G.psb[:, 0:1024], eng="scalar")

    def stage_b(i):
        b = i % 2
        for n in range(7):
            w = min(512, NIN - n * 512)
            bk = (i * 7 + n) % 6
            pso = G.psf[:, bk * 512:bk * 512 + w]
            for kc in range(8):
                P.mm(pso, uT[:, b, kc, :], winb[:, kc, n * 512:n * 512 + w], start=(kc == 0), stop=(kc == 7))
            P.copy(zt[:, b, n * 512:n * 512 + w], pso, eng=("scalar" if n % 2 else "vector"))
        P.dma(G.stq, G.zin[i * 128:(i + 1) * 128, :], zt[:, b, :])
    stage_a(0)
    for i in range(NT):
        if i + 1 < NT:
            stage_a(i + 1)
        stage_b(i)
    ph.close()
def dir_order(d):
    if d == 0:
        return list(range(NT))
    return [1, 0] + list(range(NT - 1, 1, -1))


def load_shift(P, dst, src2d, i, off, eng_ms="gpsimd"):
    lo, hi = (0, 256) if i < 2 else (256, T_ALL)
    r0 = i * 128 + off
    a = max(r0, lo); bnd = min(r0 + 128, hi)
    if a > r0:
        n = a - r0
        if n >= 128:
            P.memset(dst, 0.0, eng=eng_ms); return
        P.memset(dst[0:((n + 31) // 32) * 32, :], 0.0, eng=eng_ms)
    if bnd < r0 + 128:
        n = r0 + 128 - bnd
        if n >= 128:
            P.memset(dst, 0.0, eng=eng_ms); return
        st = ((128 - n) // 32) * 32
        P.memset(dst[st:128, :], 0.0, eng=eng_ms)
    P.dma("sync", dst[a - r0:bnd - r0, :], src2d[a:bnd, :])


def phase_ssd(nc, P, G, l):
    ssd_prep(nc, P, G, l); ssd_dir(nc, P, G, l); ssd_fin(nc, P, G, l)


def ssd_prep(nc, P, G, l):
    W = G.w
    ph = Phase(nc, P)
    cw = ph.sb("cw", [128, 3, 1024]); cb = ph.sb("cb", [128, 1024])
    dtb = ph.sb("dtb", [128, 16]); nega = ph.sb("nega", [128, 16])
    xc = ph.sb("xc", [128, 2, 1024]); xl = ph.sb("xl", [128, 2, 1024]); xr = ph.sb("xr", [128, 2, 1024])
    acc = ph.sb("acc", [128, 2, 1024]); dl = ph.sb("dl", [128, 2, 32]); dr = ph.sb("dr", [128, 2, 16])
    for j in range(3):
        P.dma("sync", cw[:, j, :], bc_rows(W["ssd_conv_w"][l, j:j + 1, :]))
    P.dma("sync", cb[:], bc_rows(W["ssd_conv_b"][l:l + 1, :]))
    P.dma("sync", dtb[:], bc_rows(W["ssd_dt_bias"][l:l + 1].rearrange("a b c -> a (b c)")))
    P.dma("sync", nega[:], bc_rows(W["ssd_a_log"][l:l + 1].rearrange("a b c -> a (b c)")))
    P.act(nega[:], nega[:], AF.Exp)
    P.ts(nega[:], nega[:], -1.0, ALU.mult)
    zx = G.zin[:, C_XBC:C_XBC + 1024]
    for i in range(NT):
        b = i % 2
        rows = slice(i * 128, (i + 1) * 128)
        P.dma("sync", xc[:, b, :], zx[rows, :])
        load_shift(P, xl[:, b, :], zx, i, -1)
        load_shift(P, xr[:, b, :], zx, i, +1)
        P.dma("sync", dr[:, b, :], G.zin[rows, C_DT:C_DT + 16])
        a = acc[:, b, :]
        P.tt(a, xc[:, b, :], cw[:, 1, :], ALU.mult)
        P.tt(xl[:, b, :], xl[:, b, :], cw[:, 0, :], ALU.mult, eng="gpsimd")
        P.tt(xr[:, b, :], xr[:, b, :], cw[:, 2, :], ALU.mult, eng="gpsimd")
        P.tt(a, a, xl[:, b, :], ALU.add)
        P.tt(a, a, xr[:, b, :], ALU.add)
        P.tt(a, a, cb[:], ALU.add)
        P.act(a, a, AF.Silu)
        P.dma(G.stq, G.xbca[rows, :], a)
        P.tt(dr[:, b, :], dr[:, b, :], dtb[:], ALU.add)
        P.act(dr[:, b, :], dr[:, b, :], AF.Exp)
        P.act(dl[:, b, 0:16], dr[:, b, :], AF.Ln, bias=1.0)
        P.tt(dl[:, b, 16:32], dl[:, b, 0:16], nega[:], ALU.mult)
        P.dma(G.stq, G.dtla[rows, :], dl[:, b, :])
    ph.close()


def run_streams(gens):
    gens = list(gens)
    while gens:
        for g in list(gens):
            try:
                next(g)
            except StopIteration:
                gens.remove(g)


def ssd_dir(nc, P, G, l):
    ph = Phase(nc, P)
    ps = G.psf
    psbf = G.psf.bitcast(BF16)

    def stream(d, kb):
        sfx = "f" if d == 0 else "b"
        xa = ph.sb("xa" + sfx, [128, 2, 1024]); dl = ph.sb("dl2" + sfx, [128, 2, 32])
        sm = ph.sb("sm" + sfx, [128, 2, 48])
        R1 = ph.sb("R1" + sfx, [128, 8, 128]); R3 = ph.sb("R3" + sfx, [128, 8, 128])
        LT = ph.sb("LT" + sfx, [128, 8, 128], BF16); MT = ph.sb("MT" + sfx, [128, 8, 128], BF16)
        bcb = ph.sb("bcb" + sfx, [128, 2, 512], BF16); bcT = ph.sb("bcT" + sfx, [128, 2, 4, 128], BF16)
        xdt = ph.sb("xdt" + sfx, [128, 2, 8, 64], BF16); xdd = ph.sb("xdd" + sfx, [128, 2, 8, 64], BF16)
        ytmp = ph.sb("ytmp" + sfx, [128, 8, 64]); yo = ph.sb("yo" + sfx, [128, 2, 512])
        hin32 = ph.sb("hin32" + sfx, [128, 512]); hinb = ph.sb("hinb" + sfx, [128, 512], BF16)

        def bank(k, w=512, o=0):
            return ps[:, kb[k] * 512 + o:kb[k] * 512 + o + w]
        P.memset(hin32[:], 0.0)
        P.memset(hinb[:], 0.0)
        tri = G.tri[d]
        for i in dir_order(d):
            b = i % 2
            rows = slice(i * 128, (i + 1) * 128)
            P.dma("sync", xa[:, b, :], G.xbca[rows, :])
            P.dma("sync", dl[:, b, :], G.dtla[rows, :])
            dt_d = dl[:, b, d * 8:(d + 1) * 8]
            la_d = dl[:, b, 16 + d * 8:16 + (d + 1) * 8]
            S = sm[:, b, :]
            acs_ps = bank(0, 8, 256); tot_ps = bank(0, 8, 264)
            P.mm(acs_ps, tri[:], la_d)
            P.mm(tot_ps, G.onesf[:], la_d)
            P.tt(R1[:], tri[:].unsqueeze(1).to_broadcast([128, 8, 128]), la_d.unsqueeze(2).to_broadcast([128, 8, 128]),
                 ALU.mult, eng="gpsimd")
            P.act(R3[:], la_d.unsqueeze(2).to_broadcast([128, 8, 128]), AF.Identity, scale=-1.0)
            P.copy(bcb[:, b, :], xa[:, b, 512:1024])
            yield
            P.copy(S[:, 0:8], acs_ps)
            P.act(S[:, 8:16], acs_ps, AF.Exp)
            P.tt(S[:, 40:48], tot_ps, S[:, 0:8], ALU.subtract)
            P.act(S[:, 16:24], S[:, 40:48], AF.Exp)
            P.act(S[:, 24:32], tot_ps, AF.Exp)
            P.tt(S[:, 32:40], dt_d, S[:, 16:24], ALU.mult)
            for q in range(4):
                P.tr(psbf[:, kb[2] * 1024 + q * 128:kb[2] * 1024 + (q + 1) * 128], bcb[:, b, q * 128:(q + 1) * 128], G.identb[:])
            for g in range(2):
                sp = bank(1)
                P.mm(sp, G.onesf[:], R1[:, 4 * g:4 * g + 4, :], start=True, stop=False)
                P.mm(sp, tri[:], R3[:, 4 * g:4 * g + 4, :], start=False, stop=False)
                P.mm(sp, G.identf[:], G.negm[d][:], start=False, stop=True)
                if g == 0:
                    yield
                    P.copy(bcT[:, b].rearrange("p a b -> p (a b)"), psbf[:, kb[2] * 1024:kb[2] * 1024 + 512], eng="scalar")
                P.act(LT[:, 4 * g:4 * g + 4, :], sp, AF.Exp)
            xs3 = xa[:, b, 0:512].rearrange("p (h q) -> p h q", h=8)
            P.tt(xdt[:, b], xs3, dt_d.unsqueeze(2).to_broadcast([128, 8, 64]), ALU.mult, eng="gpsimd")
            P.tt(xdd[:, b], xs3, S[:, 32:40].unsqueeze(2).to_broadcast([128, 8, 64]), ALU.mult, eng="gpsimd")
            for g in range(2):
                cbp = bank(0, 128, g * 128)
                P.mm(cbp, bcT[:, b, g, :], bcT[:, b, 2 + g, :])
            for g in range(2):
                P.mm(bank(3, 256, g * 256), bcT[:, b, 2 + g, :], hinb[:, g * 256:(g + 1) * 256])
            yield
            for g in range(2):
                cbp = bank(0, 128, g * 128)
                P.tt(MT[:, 4 * g:4 * g + 4, :], LT[:, 4 * g:4 * g + 4, :], cbp.unsqueeze(1).to_broadcast([128, 4, 128]), ALU.mult)
            P.tt(ytmp[:], bank(3).rearrange("p (h q) -> p h q", h=8), S[:, 8:16].unsqueeze(2).to_broadcast([128, 8, 64]), ALU.mult)
            for h in range(8):
                P.mm(bank(2, 64, h * 64), MT[:, h, :], xdt[:, b, h, :])
            for g in range(2):
                P.mm(bank(3, 256, g * 256), bcb[:, b, g * 128:(g + 1) * 128], xdd[:, b, 4 * g:4 * g + 4, :])
            yield
            P.tt(yo[:, b, :], ytmp[:].rearrange("p h q -> p (h q)"), bank(2), ALU.add)
            P.dma(G.stq, G.ydir[d, rows, :], yo[:, b, :])
            h3 = hin32[:].rearrange("p (h q) -> p h q", h=8)
            P.tt(h3, h3, S[:, 24:32].unsqueeze(2).to_broadcast([128, 8, 64]), ALU.mult)
            P.tt(hin32[:], hin32[:], bank(3), ALU.add)
            P.copy(hinb[:], hin32[:], eng="scalar")
            yield

    run_streams([stream(0, [0, 1, 2, 3]), stream(1, [4, 5, 6, 7])])
    ph.close()


def ssd_fin(nc, P, G, l):
    W = G.w
    ph = Phase(nc, P)
    dsm = ph.sb("dsm", [128, 8]); dex = ph.sb("dex", [128, 8, 64]); nw = ph.sb("nw", [128, 512])
    yf = ph.sb("yf", [128, 2, 512]); yb = ph.sb("yb", [128, 2, 512]); xs = ph.sb("xs", [128, 2, 512]); z = ph.sb("z", [128, 2, 512])
    junk = ph.sb("junk", [128, 256]); ss = ph.sb("ss", [128, 2, 2]); o = ph.sb("o", [128, 2, 512])
    P.dma("sync", dsm[:], bc_rows(W["ssd_d"][l:l + 1, :]))
    P.copy(dex[:], dsm[:].unsqueeze(2).to_broadcast([128, 8, 64]))
    P.dma("sync", nw[:], bc_rows(W["ssd_norm_w"][l:l + 1, :]))
    dexf = dex[:].rearrange("p h q -> p (h q)")
    for i in range(NT):
        b = i % 2
        rows = slice(i * 128, (i + 1) * 128)
        P.dma("sync", yf[:, b, :], G.ydir[0, rows, :])
        P.dma("sync", yb[:, b, :], G.ydir[1, rows, :])
        P.dma("sync", xs[:, b, :], G.xbca[rows, 0:512])
        P.dma("sync", z[:, b, :], G.zin[rows, C_Z:C_Z + 512])
        y = yf[:, b, :]
        P.tt(y, y, yb[:, b, :], ALU.add)
        P.tt(xs[:, b, :], xs[:, b, :], dexf, ALU.mult, eng="gpsimd")
        P.tt(y, y, xs[:, b, :], ALU.add)
        P.act(z[:, b, :], z[:, b, :], AF.Silu)
        P.tt(y, y, z[:, b, :], ALU.mult)
        for g in range(2):
            P.act(junk[:], y[:, g * 256:(g + 1) * 256], AF.Square, accum_out=ss[:, b, g:g + 1])
        P.act(ss[:, b, :], ss[:, b, :], AF.Sqrt, bias=1e-5, scale=1.0 / 256)
        P.op("vector", lambda e, b=b: e.reciprocal(ss[:, b, :], ss[:, b, :]), reads=[ss[:, b, :]], writes=[ss[:, b, :]])
        for g in range(2):
            P.stt(o[:, b, g * 256:(g + 1) * 256], y[:, g * 256:(g + 1) * 256], ss[:, b, g:g + 1], nw[:, g * 256:(g + 1) * 256],
                  ALU.mult, ALU.mult)
        P.dma(G.stq, G.ymix[rows, 0:512], o[:, b, :])
    ph.close()
def phase_gla(nc, P, G, l):
    gla_prep(nc, P, G, l); gla_dir(nc, P, G, l); gla_fin(nc, P, G, l)


def gla_prep(nc, P, G, l):
    W = G.w
    ph = Phase(nc, P)
    GU = ph.sb("GU", [32, 256]); gbrow = ph.sb("gbrow", [1, 256])
    gd = ph.sb("gd", [128, 2, 32]); gdT = ph.sb("gdT", [32, 2, 128]); e = ph.sb("ge", [128, 2, 256])
    P.memset(GU[:], 0.0)
    P.dma("sync", GU[0:16, 0:128], W["gla_gu"][l, 0])
    P.dma("sync", GU[16:32, 128:256], W["gla_gu"][l, 1])
    P.dma("sync", gbrow[:], W["gla_gb"][l:l + 1].rearrange("a b c -> a (b c)"))
    for i in range(NT):
        b = i % 2
        rows = slice(i * 128, (i + 1) * 128)
        P.dma("sync", gd[:, b, :], G.zin[rows, C_GD:C_GD + 32])
        tp = G.psf[0:32, 0:128]
        P.tr(tp, gd[:, b, :], G.identf[:])
        P.copy(gdT[:, b, :], tp)
        gp = G.psf[:, 512:768]
        P.mm(gp, gdT[:, b, :], GU[:], start=True, stop=False)
        P.mm(gp, G.onesf[0:1, :], gbrow[:], start=False, stop=True)
        P.act(e[:, b, :], gp, AF.Exp, scale=-1.0)
        P.act(e[:, b, :], e[:, b, :], AF.Ln, bias=1.0)
        P.ts(e[:, b, :], e[:, b, :], -1.0 / 16.0, ALU.mult)
        P.dma(G.stq, G.gla_la[rows, :], e[:, b, :])
    ph.close()


def gla_dir(nc, P, G, l):
    ph = Phase(nc, P)
    ps = G.psf
    psbf = G.psf.bitcast(BF16)

    def stream(d, kb):
        sfx = "f" if d == 0 else "b"
        qk = ph.sb("qk" + sfx, [128, 2, 256]); v = ph.sb("gv" + sfx, [128, 2, 256]); la = ph.sb("gla" + sfx, [128, 2, 128])
        acs = ph.sb("gacs" + sfx, [128, 128]); ee = ph.sb("gee" + sfx, [128, 3, 128]); ecd = ph.sb("gecd" + sfx, [128, 1])
        qkt = ph.sb("qkt" + sfx, [128, 2, 128], BF16); kdec = ph.sb("kdec" + sfx, [128, 128], BF16)
        qkT = ph.sb("qkT" + sfx, [128, 2, 128], BF16); smb = ph.sb("smb" + sfx, [128, 4, 128], BF16); vb = ph.sb("vb" + sfx, [128, 256], BF16)
        kTm = ph.sb("kTm" + sfx, [128, 4, 128], BF16); qTm = ph.sb("qTm" + sfx, [128, 4, 128], BF16)
        stm = ph.sb("stm" + sfx, [128, 4, 64]); sred = ph.sb("sred" + sfx, [128, 64])
        ot = ph.sb("got" + sfx, [128, 2, 256]); S32 = ph.sb("S32" + sfx, [128, 64]); Sb = ph.sb("Sb" + sfx, [128, 64], BF16)

        def bank(k, w=512, o=0):
            return ps[:, kb[k] * 512 + o:kb[k] * 512 + o + w]
        P.memset(S32[:], 0.0)
        P.memset(Sb[:], 0.0)
        tri = G.tri[d]
        for i in dir_order(d):
            b = i % 2
            rows = slice(i * 128, (i + 1) * 128)
            P.dma("sync", qk[:, b, :], G.zin[rows, C_GQ:C_GQ + 256])
            P.dma("sync", v[:, b, :], G.zin[rows, C_GV:C_GV + 256])
            P.dma("sync", la[:, b, :], G.gla_la[rows, d * 128:(d + 1) * 128])
            acs_ps = bank(0, 128, 0); tot_ps = bank(0, 128, 128); totT = bank(0, 2, 256)
            P.mm(acs_ps, tri[:], la[:, b, :])
            P.mm(tot_ps, G.onesf[:], la[:, b, :])
            P.mm(totT, la[:, b, :], G.onesf[:, 0:2])
            P.copy(vb[:], v[:, b, :], eng="gpsimd")
            yield
            P.copy(acs[:], acs_ps, eng="scalar")
            P.act(ee[:, 0, :], acs_ps, AF.Exp)
            P.act(ee[:, 1, :], acs_ps, AF.Exp, scale=-1.0)
            P.tt(ee[:, 2, :], tot_ps, acs[:], ALU.subtract)
            P.act(ee[:, 2, :], ee[:, 2, :], AF.Exp)
            P.act(ecd[:], totT[:, 0:1], AF.Exp)
            P.stt(qkt[:, 0, :], qk[:, b, 0:128], 32 ** -0.5, ee[:, 0, :], ALU.mult, ALU.mult)
            P.tt(qkt[:, 1, :], qk[:, b, 128:256], ee[:, 1, :], ALU.mult)
            P.tt(kdec[:], qk[:, b, 128:256], ee[:, 2, :], ALU.mult, eng="gpsimd")
            for j in range(2):
                P.tr(psbf[:, kb[0] * 1024 + j * 128:kb[0] * 1024 + (j + 1) * 128], qkt[:, j, :], G.identb[:])
            P.mm(bank(3, 256, 0), kdec[:], vb[:])
            yield
            P.copy(qkT[:].rearrange("p a b -> p (a b)"), psbf[:, kb[0] * 1024:kb[0] * 1024 + 256], eng="scalar")
            P.tt(kTm[:], qkT[:, 1, :].unsqueeze(1).to_broadcast([128, 4, 128]), G.hm4[:].unsqueeze(2).to_broadcast([128, 4, 128]), ALU.mult)
            P.tt(qTm[:], qkT[:, 0, :].unsqueeze(1).to_broadcast([128, 4, 128]), G.hm4[:].unsqueeze(2).to_broadcast([128, 4, 128]), ALU.mult, eng="gpsimd")
            for h in range(4):
                P.mm(bank(1, 128, h * 128), kTm[:, h, :], qkT[:, 0, :])
            yield
            P.tt(smb[:], bank(1).rearrange("p (h q) -> p h q", h=4), G.tri[d][:].unsqueeze(1).to_broadcast([128, 4, 128]), ALU.mult)
            for h in range(4):
                o_ps = bank(2, 64, h * 64)
                P.mm(o_ps, smb[:, h, :], vb[:, h * 64:(h + 1) * 64], start=True, stop=False)
                P.mm(o_ps, qTm[:, h, :], Sb[:], start=False, stop=True)
            yield
            P.copy(ot[:, b, :], bank(2, 256, 0), eng="scalar")
            P.dma(G.stq, G.gla_o[d, rows, :], ot[:, b, :])
            P.tt(stm[:], bank(3, 256, 0).rearrange("p (h q) -> p h q", h=4), G.hm4[:].unsqueeze(2).to_broadcast([128, 4, 64]), ALU.mult)
            P.op("vector", lambda e: e.tensor_reduce(sred[:], stm[:].rearrange("p h q -> p q h"), AX.X, ALU.add),
                 reads=[stm[:]], writes=[sred[:]])
            P.stt(S32[:], S32[:], ecd[:, 0:1], sred[:], ALU.mult, ALU.add)
            P.copy(Sb[:], S32[:])
            yield

    run_streams([stream(0, [0, 1, 2, 3]), stream(1, [4, 5, 6, 7])])
    ph.close()


def gla_fin(nc, P, G, l):
    W = G.w
    ph = Phase(nc, P)
    nw = ph.sb("gnw", [128, 256]); of = ph.sb("gof", [128, 2, 256]); ob = ph.sb("gob", [128, 2, 256]); og = ph.sb("gog", [128, 2, 256])
    sq = ph.sb("gsq", [128, 256]); ss = ph.sb("gss", [128, 2, 4]); y = ph.sb("gy", [128, 2, 256])
    P.dma("sync", nw[:], bc_rows(W["gla_norm_w"][l:l + 1, :]))
    for i in range(NT):
        b = i % 2
        rows = slice(i * 128, (i + 1) * 128)
        P.dma("sync", of[:, b, :], G.gla_o[0, rows, :])
        P.dma("sync", ob[:, b, :], G.gla_o[1, rows, :])
        P.dma("sync", og[:, b, :], G.zin[rows, C_OG:C_OG + 256])
        o = of[:, b, :]
        P.tt(o, o, ob[:, b, :], ALU.add)
        P.tt(sq[:], o, o, ALU.mult, eng="gpsimd")
        P.op("vector", lambda e, b=b: e.tensor_reduce(ss[:, b, :], sq[:].rearrange("p (h q) -> p h q", h=4), AX.X, ALU.add),
             reads=[sq[:]], writes=[ss[:, b, :]])
        P.act(ss[:, b, :], ss[:, b, :], AF.Sqrt, bias=1e-5, scale=1.0 / 64)
        P.op("vector", lambda e, b=b: e.reciprocal(ss[:, b, :], ss[:, b, :]), reads=[ss[:, b, :]], writes=[ss[:, b, :]])
        y3 = y[:, b, :].rearrange("p (h q) -> p h q", h=4)
        P.tt(y3, o.rearrange("p (h q) -> p h q", h=4), ss[:, b, :].unsqueeze(2).to_broadcast([128, 4, 64]), ALU.mult)
        P.tt(y[:, b, :], y[:, b, :], nw[:], ALU.mult, eng="gpsimd")
        P.act(og[:, b, :], og[:, b, :], AF.Silu)
        P.tt(y[:, b, :], y[:, b, :], og[:, b, :], ALU.mult)
        P.dma(G.stq, G.ymix[rows, 768:1024], y[:, b, :])
    ph.close()
def phase_rwkv(nc, P, G, l):
    rwkv_prep(nc, P, G, l); rwkv_dir(nc, P, G, l); rwkv_fin(nc, P, G, l)


def rwkv_prep(nc, P, G, l):
    W = G.w
    ph = Phase(nc, P)
    mu = ph.sb("mu", [128, 4, 1152])
    w2a2 = ph.sb("w2a2", [128, 2, 256]); g2 = ph.sb("g2", [128, 256]); brow = ph.sb("rbrow", [1, 4, 256])
    kkw = ph.sb("kkw", [128, 256]); ka = ph.sb("ka", [128, 256]); rk = ph.sb("rk", [128, 256])
    c = ph.sb("rc", [128, 2, 1152]); nb = ph.sb("rnb", [128, 4, 1152])
    tws = ph.sb("tws", [128, 3, 128]); twT = ph.sb("twT", [128, 3, 128])
    pk = ph.sb("pk", [128, 2, 11, 256]); tmp = ph.sb("rtmp", [128, 3, 256]); s4 = ph.sb("rs4", [128, 8])
    for j in range(4):
        P.dma("sync", mu[:, j, :], bc_rows(W["rwkv_mu"][l, j:j + 1, :]))
    for d in range(2):
        P.dma("sync", w2a2[64 * d:64 * d + 64, 0, :], W["rwkv_w2"][l, d])
        P.dma("sync", w2a2[64 * d:64 * d + 64, 1, :], W["rwkv_a2"][l, d])
    P.dma("sync", g2[:], W["rwkv_g2"][l])
    P.dma("sync", brow[0:1, 0:2, :], W["rwkv_w0"][l:l + 1])
    P.dma("sync", brow[0:1, 2:4, :], W["rwkv_a0"][l:l + 1])
    P.dma("sync", kkw[:], bc_rows(W["rwkv_kk"][l:l + 1, :]))
    P.dma("sync", ka[:], bc_rows(W["rwkv_ka"][l:l + 1, :]))
    P.dma("sync", rk[:], bc_rows(W["rwkv_rk"][l:l + 1].rearrange("a b c -> a (b c)")))
    zr = G.zin[:, C_RW:C_RW + 1152]
    for i in range(NT):
        b = i % 2
        rows = slice(i * 128, (i + 1) * 128)
        lat = i >= 2
        cc = c[:, b, :]
        P.dma("sync", cc, zr[rows, :])
        offs = [-1, 1, -64, 64] if lat else [-1, 1]
        for j, off in enumerate(offs):
            X = nb[:, j, :]
            load_shift(P, X, zr, i, off)
            if lat and j < 2:
                P.stt(X, X, G.cmask[:, j:j + 1], cc, ALU.mult, ALU.subtract, eng=("vector"))
            else:
                P.tt(X, X, cc, ALU.subtract, eng="gpsimd")
            P.tt(X, X, mu[:, j, :], ALU.mult, eng=("gpsimd" if j % 2 else "vector"))
        for j in range(len(offs)):
            P.tt(cc, cc, nb[:, j, :], ALU.add)
        PK = pk[:, b]
        P.act(tws[:, 0, :], cc[:, 768:896], AF.Tanh)
        P.copy(tws[:, 1, :], cc[:, 896:1024], eng="gpsimd")
        P.act(tws[:, 2, :], cc[:, 1024:1152], AF.Sigmoid)
        for q in range(3):
            P.tr(G.psf[:, q * 128:(q + 1) * 128], tws[:, q, :], G.identf[:])
        P.copy(twT[:].rearrange("p a b -> p (a b)"), G.psf[:, 0:384])
        for d in range(2):
            ds = slice(64 * d, 64 * d + 64)
            xp = G.psf[:, (1 + 2 * d) * 512:(1 + 2 * d) * 512 + 256]
            P.mm(xp, twT[ds, 0, :], w2a2[ds, 0, :], start=True, stop=False)
            P.mm(xp, G.onesf[0:1, :], brow[0:1, d, :], start=False, stop=True)
            ap_ = G.psf[:, (1 + 2 * d) * 512 + 256:(1 + 2 * d) * 512 + 512]
            P.mm(ap_, twT[ds, 1, :], w2a2[ds, 1, :], start=True, stop=False)
            P.mm(ap_, G.onesf[0:1, :], brow[0:1, 2 + d, :], start=False, stop=True)
        gp = G.psf[:, 2048:2304]
        P.mm(gp, twT[:, 2, :], g2[:])
        for d in range(2):
            P.act(PK[:, 7 + d, :], G.psf[:, (1 + 2 * d) * 512:(1 + 2 * d) * 512 + 256], AF.Sigmoid)
        P.ts(PK[:, 7:9, :], PK[:, 7:9, :], -0.6065306597126334, ALU.mult, eng="gpsimd")
        adir = tmp[:, 0:2, :]
        for d in range(2):
            P.act(adir[:, d, :], G.psf[:, (1 + 2 * d) * 512 + 256:(1 + 2 * d) * 512 + 512], AF.Sigmoid)
        P.copy(PK[:, 9, :], gp, eng="scalar")
        P.copy(PK[:, 0, :], cc[:, 0:256], eng="gpsimd")
        P.copy(PK[:, 1, :], cc[:, 512:768], eng="gpsimd")
        kx = cc[:, 256:512]
        kk = PK[:, 2, :]
        P.tt(kk, kx, kkw[:], ALU.mult)
        P.tt(tmp[:, 2, :], kk, kk, ALU.mult, eng="gpsimd")
        P.op("vector", lambda e: e.tensor_reduce(s4[:, 0:4], tmp[:, 2, :].rearrange("p (h q) -> p h q", h=4), AX.X, ALU.add),
             reads=[tmp[:, 2, :]], writes=[s4[:, 0:4]])
        P.act(s4[:, 0:4], s4[:, 0:4], AF.Sqrt)
        P.ts(s4[:, 0:4], s4[:, 0:4], 1e-12, ALU.max)
        P.op("vector", lambda e: e.reciprocal(s4[:, 0:4], s4[:, 0:4]), reads=[s4[:, 0:4]], writes=[s4[:, 0:4]])
        kk3 = kk.rearrange("p (h q) -> p h q", h=4)
        P.tt(kk3, kk3, s4[:, 0:4].unsqueeze(2).to_broadcast([128, 4, 64]), ALU.mult)
        for d in range(2):
            t1 = tmp[:, 2, :]
            P.stt(t1, adir[:, d, :], -1.0, ka[:], ALU.add, ALU.mult)
            P.stt(PK[:, 3 + d, :], t1, 1.0, kx, ALU.add, ALU.mult)
            P.tt(PK[:, 5 + d, :], kk, adir[:, d, :], ALU.mult, eng="gpsimd")
        t1 = tmp[:, 2, :]
        P.tt(t1, PK[:, 3, :], PK[:, 4, :], ALU.add)
        P.tt(t1, t1, PK[:, 0, :], ALU.mult)
        P.tt(t1, t1, rk[:], ALU.mult)
        P.op("vector", lambda e: e.tensor_reduce(s4[:, 4:8], tmp[:, 2, :].rearrange("p (h q) -> p h q", h=4), AX.X, ALU.add),
             reads=[tmp[:, 2, :]], writes=[s4[:, 4:8]])
        P.tt(PK[:, 10, :].rearrange("p (h q) -> p h q", h=4), PK[:, 1, :].rearrange("p (h q) -> p h q", h=4),
             s4[:, 4:8].unsqueeze(2).to_broadcast([128, 4, 64]), ALU.mult)
        P.dma(G.stq, G.rwp[rows, :], PK.rearrange("p a b -> p (a b)"))
    ph.close()


def rwkv_dir(nc, P, G, l):
    ph = Phase(nc, P)
    ps = G.psf

    def stream(d, kb):
        sfx = "f" if d == 0 else "b"
        ld = ph.sb("rld" + sfx, [128, 2, 6, 256])
        ee = ph.sb("ree" + sfx, [128, 3, 256])
        tok = ph.sb("rtok" + sfx, [128, 4, 256])
        fmh = ph.sb("fmh" + sfx, [64, 4, 4, 128])
        Aarb = ph.sb("Aarb" + sfx, [128, 4, 128]); Aak = ph.sb("Aak" + sfx, [128, 4, 128]); Aark = ph.sb("Aark" + sfx, [128, 4, 128])
        Q = ph.sb("rQ" + sfx, [128, 2, 4, 128]); QT = ph.sb("rQT" + sfx, [128, 2, 4, 128]); IQT = ph.sb("rIQT" + sfx, [128, 4, 128])
        Z = ph.sb("rZ" + sfx, [128, 2, 4, 128])
        Wsb = ph.sb("rW" + sfx, [128, 4, 64]); nAkV = ph.sb("nAkV" + sfx, [128, 4, 64]); Uloc = ph.sb("Uloc" + sfx, [128, 4, 64])
        GpT = ph.sb("GpT" + sfx, [64, 4, 2, 64]); RwT = ph.sb("RwT" + sfx, [64, 4, 128]); gC = ph.sb("gC" + sfx, [64, 4, 2])
        H32 = ph.sb("H32" + sfx, [64, 4, 64]); Yt = ph.sb("Yt" + sfx, [64, 2, 2, 256])
        btc = ph.sb("btc" + sfx, [128, 2, 256]); ktc = ph.sb("ktc" + sfx, [128, 2, 256])

        def bank(k, w=512, o=0, p0=0, p1=128):
            kk_ = kb[k]
            return ps[p0:p1, kk_ * 512 + o:kk_ * 512 + o + w]

        def b4(k):
            return bank(k).rearrange("p (h q) -> p h q", h=4)

        def bc4(m):
            return m[:].unsqueeze(1).to_broadcast([128, 4, 128])
        idb4 = G.identf[:].unsqueeze(1).to_broadcast([128, 4, 128])
        ea = "scalar"
        P.memset(H32[:], 0.0)
        BD = G.IU if d == 0 else G.IL
        m_abT = G.nSU if d == 0 else G.nSL
        m_ab = G.nSL if d == 0 else G.nSU
        m_s = G.SU if d == 0 else G.SL
        m_i = G.IU if d == 0 else G.IL
        for i in dir_order(d):
            b = i % 2
            rows = slice(i * 128, (i + 1) * 128)
            L = ld[:, b]
            for q, col in enumerate([0, 1, 2, 3 + d, 5 + d, 7 + d]):
                P.dma("sync", L[:, q, :], G.rwp[rows, col * 256:(col + 1) * 256])
            r_, v_, kk_, k_, b_, lw_ = [L[:, q, :] for q in range(6)]
            cs_ps = bank(3, 256)
            P.mm(cs_ps, BD[:], lw_)
            yield
            P.act(ee[:, 0, :], cs_ps, AF.Exp)
            P.act(ee[:, 1, :], cs_ps, AF.Exp, scale=-1.0)
            P.copy(ee[:, 2, :], cs_ps, eng="scalar")
            P.tt(ee[:, 2, :], ee[:, 2, :], lw_, ALU.subtract)
            P.act(ee[:, 2, :], ee[:, 2, :], AF.Exp)
            P.tt(tok[:, 0, :], kk_, ee[:, 2, :], ALU.mult)
            P.tt(tok[:, 1, :], r_, ee[:, 0, :], ALU.mult, eng="gpsimd")
            P.tt(tok[:, 2, :], b_, ee[:, 1, :], ALU.mult)
            P.tt(tok[:, 3, :], k_, ee[:, 1, :], ALU.mult, eng="gpsimd")
            for c in range(2):
                P.ts(btc[:, c, :], tok[:, 2, :], G.cind[:, c:c + 1], ALU.mult, eng=("gpsimd" if c else "vector"))
                P.ts(ktc[:, c, :], tok[:, 3, :], G.cind[:, c:c + 1], ALU.mult, eng=("gpsimd" if c else "vector"))
            yield
            for q in range(4):
                for h in range(4):
                    P.tr(bank(q, 128, h * 128, 0, 64), tok[:, q, h * 64:(h + 1) * 64], G.identf[:])
            yield
            for q in range(4):
                P.copy(fmh[:, q].rearrange("p h t -> p (h t)"), bank(q, 512, 0, 0, 64), eng=("scalar" if q % 2 else "vector"))
            for h in range(4):
                P.mm(bank(0, 2, 2 * h, 0, 64), lw_[:, h * 64:(h + 1) * 64], G.cind[:])
            for h in range(4):
                khT = fmh[:, 0, h, :]; btT = fmh[:, 2, h, :]
                P.mm(bank(1, 128, h * 128), btT, khT)
                P.mm(bank(2, 128, h * 128), khT, btT)
            yield
            P.act(gC[:].rearrange("p h c -> p (h c)"), bank(0, 8, 0, 0, 64), AF.Exp)
            P.tt(Q[:, 0], b4(1), bc4(m_abT), ALU.mult)
            P.tt(QT[:, 0], b4(2), bc4(m_ab), ALU.mult)
            P.tt(IQT[:], QT[:, 0], idb4, ALU.add, eng="gpsimd")
            P.tt(Z[:, 0], Q[:, 0], idb4, ALU.add, eng="gpsimd")
            for h in range(4):
                khT = fmh[:, 0, h, :]; rtT = fmh[:, 1, h, :]; btT = fmh[:, 2, h, :]; ktT = fmh[:, 3, h, :]
                P.mm(bank(3, 128, h * 128), btT, rtT)
                P.mm(bank(0, 128, h * 128), ktT, khT)
            yield
            P.tt(Aarb[:], b4(3), bc4(m_i), ALU.mult)
            P.tt(Aak[:], b4(0), bc4(m_s), ALU.mult)
            zc = 0
            for k in range(1, 6):
                a, bq = (k - 1) % 2, k % 2
                for h in range(4):
                    if k < 5:
                        P.mm(bank(0, 128, h * 128), QT[:, a, h, :], Q[:, a, h, :])
                    P.mm(bank(1, 128, h * 128), Q[:, a, h, :], QT[:, a, h, :])
                if k >= 2:
                    for h in range(4):
                        P.mm(bank(2, 128, h * 128), IQT[:, h, :], Z[:, zc, h, :])
                if k == 1:
                    for h in range(4):
                        P.mm(bank(3, 128, h * 128), fmh[:, 3, h, :], fmh[:, 1, h, :])
                yield
                if k == 1:
                    P.tt(Aark[:], b4(3), bc4(m_i), ALU.mult)
                if k < 5:
                    P.copy(Q[:, bq], b4(0), eng="scalar")
                P.copy(QT[:, bq], b4(1))
                if k >= 2:
                    P.copy(Z[:, 1 - zc], b4(2), eng="scalar")
                    zc = 1 - zc
                P.tt(IQT[:], QT[:, bq], idb4, ALU.add, eng="gpsimd")
            for h in range(4):
                P.mm(bank(2, 128, h * 128), IQT[:, h, :], Z[:, zc, h, :])
            yield
            P.copy(Z[:, 1 - zc], b4(2), eng="scalar")
            zc = 1 - zc
            TT = Z[:, zc]
            for h in range(4):
                hc = slice(h * 64, (h + 1) * 64)
                P.mm(bank(3, 64, h * 64), TT[:, h, :], tok[:, 0, hc])
                P.mm(bank(0, 64, h * 64), Aak[:, h, :], v_[:, hc])
            yield
            P.copy(Wsb[:].rearrange("p h q -> p (h q)"), bank(3, 256), eng="scalar")
            P.ts(nAkV[:].rearrange("p h q -> p (h q)"), bank(0, 256), -1.0, ALU.mult)
            for h in range(4):
                P.mm(bank(1, 64, h * 64), TT[:, h, :], nAkV[:, h, :])
            for h in range(4):
                hc = slice(h * 64, (h + 1) * 64)
                for c in range(2):
                    P.mm(bank(2, 64, (h * 2 + c) * 64, 0, 64), Wsb[:, h, :], btc[:, c, hc])
                P.mm(bank(3, 128, h * 128, 0, 64), Wsb[:, h, :], Aarb[:, h, :])
            yield
            P.copy(Uloc[:].rearrange("p h q -> p (h q)"), bank(1, 256), eng="scalar")
            id8 = G.identf[0:64, 0:64].unsqueeze(1).to_broadcast([64, 8, 64])
            P.tt(GpT[:].rearrange("p h c q -> p (h c) q"), id8, bank(2, 512, 0, 0, 64).rearrange("p (a q) -> p a q", a=8), ALU.subtract)
            P.tt(RwT[:], fmh[:, 1], bank(3, 512, 0, 0, 64).rearrange("p (h q) -> p h q", h=4), ALU.subtract)
            for c in ([0, 1] if d == 0 else [1, 0]):
                cs_ = slice(64 * c, 64 * c + 64)
                for h in range(4):
                    hc = slice(h * 64, (h + 1) * 64)
                    yp = bank(1, 64, (c * 4 + h) * 64, 0, 64)
                    P.mm(yp, Aarb[:, h, cs_], Uloc[:, h, :], start=True, stop=False)
                    P.mm(yp, Aark[:, h, cs_], v_[:, hc], start=False, stop=False)
                    P.mm(yp, RwT[:, h, cs_], H32[:, h, :], start=False, stop=True)
                for h in range(4):
                    hc = slice(h * 64, (h + 1) * 64)
                    hp = bank(0, 64, h * 64, 0, 64)
                    P.mm(hp, btc[:, c, hc], Uloc[:, h, :], start=True, stop=False)
                    P.mm(hp, ktc[:, c, hc], v_[:, hc], start=False, stop=False)
                    P.mm(hp, GpT[:, h, c, :], H32[:, h, :], start=False, stop=True)
                yield
                P.tt(H32[:], bank(0, 256, 0, 0, 64).rearrange("p (h q) -> p h q", h=4),
                     gC[:, :, c].unsqueeze(2).to_broadcast([64, 4, 64]), ALU.mult)
            P.copy(Yt[:, b].rearrange("p c q -> p (c q)"), bank(1, 512, 0, 0, 64), eng="scalar")
            P.dma(G.stq, G.rwy[d, rows, :].rearrange("(c p) q -> p c q", p=64), Yt[:, b])
            yield

    gens = [stream(0, [0, 1, 2, 3]), stream(1, [4, 5, 6, 7])]
    while gens:
        for g in list(gens):
            try:
                next(g)
            except StopIteration:
                gens.remove(g)
    ph.close()


def rwkv_fin(nc, P, G, l):
    W = G.w
    ph = Phase(nc, P)
    lw_ = ph.sb("lnxw", [128, 256]); lb_ = ph.sb("lnxb", [128, 256])
    yf = ph.sb("ryf", [128, 2, 256]); yb = ph.sb("ryb", [128, 2, 256]); gb = ph.sb("rgb", [128, 2, 2, 256])
    sq = ph.sb("rsq", [128, 256]); s4 = ph.sb("rfs4", [128, 2, 8])
    P.dma("sync", lw_[:], bc_rows(W["rwkv_lnx_w"][l:l + 1, :]))
    P.dma("sync", lb_[:], bc_rows(W["rwkv_lnx_b"][l:l + 1, :]))
    for i in range(NT):
        b = i % 2
        rows = slice(i * 128, (i + 1) * 128)
        P.dma("sync", yf[:, b, :], G.rwy[0, rows, :])
        P.dma("sync", yb[:, b, :], G.rwy[1, rows, :])
        P.dma("sync", gb[:, b].rearrange("p a b -> p (a b)"), G.rwp[rows, 9 * 256:11 * 256])
        y = yf[:, b, :]
        y3 = y.rearrange("p (h q) -> p h q", h=4)
        S = s4[:, b, :]
        P.tt(y, y, yb[:, b, :], ALU.add)
        P.op("vector", lambda e, y3=y3, S=S: e.tensor_reduce(S[:, 0:4], y3, AX.X, ALU.add), reads=[y], writes=[S[:, 0:4]])
        P.ts(S[:, 0:4], S[:, 0:4], 1.0 / 64, ALU.mult)
        P.tt(y3, y3, S[:, 0:4].unsqueeze(2).to_broadcast([128, 4, 64]), ALU.subtract)
        P.tt(sq[:], y, y, ALU.mult, eng="gpsimd")
        P.op("vector", lambda e, S=S: e.tensor_reduce(S[:, 4:8], sq[:].rearrange("p (h q) -> p h q", h=4), AX.X, ALU.add),
             reads=[sq[:]], writes=[S[:, 4:8]])
        P.act(S[:, 4:8], S[:, 4:8], AF.Sqrt, bias=64e-5, scale=1.0 / 64)
        P.op("vector", lambda e, S=S: e.reciprocal(S[:, 4:8], S[:, 4:8]), reads=[S[:, 4:8]], writes=[S[:, 4:8]])
        P.tt(y3, y3, S[:, 4:8].unsqueeze(2).to_broadcast([128, 4, 64]), ALU.mult)
        P.tt(y, y, lw_[:], ALU.mult, eng="gpsimd")
        P.tt(y, y, lb_[:], ALU.add)
        P.tt(y, y, gb[:, b, 1, :], ALU.add)
        P.tt(y, y, gb[:, b, 0, :], ALU.mult)
        P.dma(G.stq, G.ymix[rows, 512:768], y)
    ph.close()
def tiles_for(l, n_layers_total=4):
    return list(range(2, NT)) if l == n_layers_total - 1 else list(range(NT))


def phase_outproj(nc, P, G, l):
    W = G.w
    ph = Phase(nc, P)
    psbf = G.psf.bitcast(BF16)
    woutb = ph.sb("woutb", [128, 8, 1024], BF16); wst = ph.sb("owst", [128, 8, 512])
    rw32 = ph.sb("rw32", [128, 8, 64]); rbias = ph.sb("rbias", [128, 64])
    mg1 = ph.sb("mg1", [128, 2, 1024]); msc2 = ph.sb("msc2", [128, 2, 1024]); msh2 = ph.sb("msh2", [128, 2, 1024])
    l1w = ph.sb("l1w", [128, 1024]); l1b = ph.sb("l1b", [128, 1024])
    for n in range(2):
        P.dma("sync", wst[:], W["w_out"][l, :, n * 512:(n + 1) * 512].rearrange("(kc p) j -> p kc j", p=128))
        P.copy(woutb[:, :, n * 512:(n + 1) * 512], wst[:], eng="gpsimd")
    P.dma("sync", rw32[:], W["router_w"][l].rearrange("(kc p) e -> p kc e", p=128))
    P.dma("sync", rbias[:], bc_rows(W["router_b"][l:l + 1, :]))
    for s in range(2):
        P.dma("sync", mg1[:, s, :], G.modd[s, 2])
        P.dma("sync", msc2[:, s, :], G.modd[s, 4])
        P.dma("sync", msh2[:, s, :], G.modd[s, 3])
    P.dma("sync", l1w[:], bc_rows(W["ln1_w"][l:l + 1, :]))
    P.dma("sync", l1b[:], bc_rows(W["ln1_b"][l:l + 1, :]))

    def stream(sid, tiles, kb):
        sfx = str(sid)
        ym = ph.sb("oym" + sfx, [128, 1024]); ymb = ph.sb("oymb" + sfx, [128, 1024], BF16); ymT = ph.sb("oymT" + sfx, [128, 8, 128], BF16)
        xt = ph.sb("oxt" + sfx, [128, 1024]); t = ph.sb("ot" + sfx, [128, 1024]); hh = ph.sb("ohh" + sfx, [128, 1024])
        hT32 = ph.sb("hT32" + sfx, [128, 8, 128]); hTb = ph.sb("hTb" + sfx, [128, 8, 128], BF16)
        st6 = ph.sb("ost6" + sfx, [128, 2, 6]); mv = ph.sb("omv" + sfx, [128, 2]); rstd = ph.sb("orstd" + sfx, [128, 1])
        sc = ph.sb("rsc" + sfx, [128, 64]); sel = ph.sb("rsel" + sfx, [128, 64]); m8 = ph.sb("rm8" + sfx, [128, 8, 8]); gs = ph.sb("rgs" + sfx, [128, 8])
        g8 = ph.sb("rg8" + sfx, [128, 8]); gm = ph.sb("rgm" + sfx, [128, 8]); pen = ph.sb("rpen" + sfx, [128, 8]); s8 = ph.sb("rs8" + sfx, [128, 8])
        selm = ph.sb("rselm" + sfx, [128, 64]); wts = ph.sb("rwts" + sfx, [128, 64]); den = ph.sb("rden" + sfx, [128, 1]); gT = ph.sb("rgT" + sfx, [128, 64])

        def bank(k, w=512, o=0):
            return G.psf[:, kb[k] * 512 + o:kb[k] * 512 + o + w]
        for i in tiles:
            s = seg_of(i)
            rows = slice(i * 128, (i + 1) * 128)
            P.dma("sync", ym[:], G.ymix[rows, :])
            P.dma("sync", xt[:], G.xres[rows, :])
            P.copy(ymb[:], ym[:], eng="gpsimd")
            for kc in range(8):
                P.tr(psbf[:, kb[2] * 1024 + kc * 128:kb[2] * 1024 + (kc + 1) * 128], ymb[:, kc * 128:(kc + 1) * 128], G.identb[:])
            yield
            P.copy(ymT[:].rearrange("p a b -> p (a b)"), psbf[:, kb[2] * 1024:kb[2] * 1024 + 1024], eng="scalar")
            for n in range(2):
                pso = bank(n)
                for kc in range(8):
                    P.mm(pso, ymT[:, kc, :], woutb[:, kc, n * 512:(n + 1) * 512], start=(kc == 0), stop=(kc == 7))
            yield
            for n in range(2):
                P.tt(t[:, n * 512:(n + 1) * 512], bank(n), mg1[:, s, n * 512:(n + 1) * 512], ALU.mult)
            x = xt[:]
            P.stt(t[:], x, DN_ALPHA, t[:], ALU.mult, ALU.add)
            ln_stats(P, ph, t[:], "o1", st6[:], mv[:], rstd[:])
            yield
            P.ts(t[:], t[:], mv[:, 0:1], ALU.subtract, rstd[:, 0:1], ALU.mult)
            P.tt(t[:], t[:], l1w[:], ALU.mult, eng="gpsimd")
            P.tt(x, t[:], l1b[:], ALU.add)
            P.dma(G.stq, G.xres[rows, :], x)
            ln_stats(P, ph, x, "o2", st6[:], mv[:], rstd[:])
            yield
            P.ts(hh[:], x, mv[:, 0:1], ALU.subtract, rstd[:, 0:1], ALU.mult)
            P.tt(hh[:], hh[:], msc2[:, s, :], ALU.mult, eng="gpsimd")
            P.tt(hh[:], hh[:], msh2[:, s, :], ALU.add)
            if G.dbg is not None and "hdbg" in G.dbg:
                P.dma("sync", G.dbg["hdbg"][rows, :], hh[:])
            for half in range(2):
                for kk in range(4):
                    kc = half * 4 + kk
                    P.tr(bank(half, 128, kk * 128), hh[:, kc * 128:(kc + 1) * 128], G.identf[:])
            yield
            for half in range(2):
                P.copy(hT32[:, half * 4:half * 4 + 4, :].rearrange("p a b -> p (a b)"), bank(half), eng="scalar")
            P.copy(hTb[:], hT32[:], eng="gpsimd")
            P.dma(G.stq, G.hT[:, :, rows].rearrange("kc p t -> p kc t"), hTb[:])
            lg = bank(3, 64)
            for kc in range(8):
                P.mm(lg, hT32[:, kc, :], rw32[:, kc, :], start=(kc == 0), stop=(kc == 7))
            yield
            P.act(sc[:], lg, AF.Sigmoid)
            P.tt(sel[:], sc[:], rbias[:], ALU.add)
            for g in range(8):
                P.op("vector", lambda e, g=g: e.max(m8[:, g, :], sel[:, g * 8:(g + 1) * 8]), reads=[sel[:, g * 8:(g + 1) * 8]], writes=[m8[:, g, :]])
            P.tt(gs[:], m8[:, :, 0], m8[:, :, 1], ALU.add)
            P.op("vector", lambda e: e.max(g8[:], gs[:]), reads=[gs[:]], writes=[g8[:]])
            P.ts(gm[:], gs[:], g8[:, 3:4], ALU.is_ge)
            P.ts(pen[:], gm[:], 1.0, ALU.subtract, 1e30, ALU.mult)
            sel3 = sel[:].rearrange("p (g q) -> p g q", g=8)
            selm3 = selm[:].rearrange("p (g q) -> p g q", g=8)
            P.tt(selm3, sel3, gm[:].unsqueeze(2).to_broadcast([128, 8, 8]), ALU.mult)
            P.tt(selm3, selm3, pen[:].unsqueeze(2).to_broadcast([128, 8, 8]), ALU.add)
            yield
            P.op("vector", lambda e: e.max(s8[:], selm[:]), reads=[selm[:]], writes=[s8[:]])
            P.ts(wts[:], selm[:], s8[:, 7:8], ALU.is_ge)
            P.tt(wts[:], wts[:], sc[:], ALU.mult)
            P.op("vector", lambda e: e.tensor_reduce(den[:], wts[:], AX.X, ALU.add), reads=[wts[:]], writes=[den[:]])
            P.op("vector", lambda e: e.reciprocal(den[:], den[:]), reads=[den[:]], writes=[den[:]])
            P.ts(wts[:], wts[:], den[:, 0:1], ALU.mult, 2.5, ALU.mult)
            P.copy(gT[:], wts[:], eng="gpsimd")
            P.dma(G.stq, G.gtok[rows, :], gT[:])
            yield

    tl = tiles_for(l)
    run_streams([stream(0, tl[0::2], [0, 1, 2, 3]), stream(1, tl[1::2], [4, 5, 6, 7])])
    ph.close()


def phase_moe(nc, P, G, l):
    W = G.w
    tl = tiles_for(l)
    half = (len(tl) + 1) // 2
    for (pa, tiles) in enumerate((tl[:half], tl[half:])):
        ph = Phase(nc, P)
        ntl = len(tiles)
        ntok = ntl * 128
        t0 = tiles[0] * 128
        acc = ph.sb("macc", [128, ntl, 1024])
        hTp = ph.sb("mhT", [128, 8, ntok], BF16); gk = ph.sb("mgk", [128, ntl, 64])
        w13s = ph.sb("w13s", [128, 8, 512]); w13b = ph.sb("w13b", [128, 2, 8, 512], BF16)
        w2s = ph.sb("w2s", [128, 2, 1024]); w2b = ph.sb("w2b", [128, 2, 2, 1024], BF16)
        hid = ph.sb("mhid", [128, 2, 2, 512], BF16); sil = ph.sb("msil", [128, 2, 512])
        P.dma("sync", hTp[:], G.hT[:, :, t0:t0 + ntok].rearrange("kc p t -> p kc t"))
        P.dma("sync", gk[:], G.gtok[t0:t0 + ntok, :].rearrange("(k p) e -> p k e", p=128))
        groups = []
        c0 = 0
        while c0 < ntok:
            n = min(512, ntok - c0)
            groups.append((c0, n)); c0 += n
        NE = 65
        orot = [0]
        pending = [None]

        def emit_down(e, eb, hb, c0, n):
            for sub in range(n // 128):
                ti = (c0 // 128) + sub
                for cc in range(2):
                    ob = 4 + (orot[0] % 4)
                    orot[0] += 1
                    o = G.psf[:, ob * 512:(ob + 1) * 512]
                    for fc in range(2):
                        P.mm(o, hid[:, hb, fc, sub * 128:(sub + 1) * 128], w2b[:, eb, fc, cc * 512:(cc + 1) * 512],
                             start=(fc == 0), stop=(fc == 1))
                    a = acc[:, ti, cc * 512:(cc + 1) * 512]
                    if e == 0:
                        P.ts(a, o, gk[:, ti, 0:1], ALU.mult)
                    elif e < 64:
                        P.stt(a, o, gk[:, ti, e:e + 1], a, ALU.mult, ALU.add)
                    else:
                        P.tt(a, a, o, ALU.add)
        for e in range(NE):
            eb = e % 2
            if e < 64:
                s1, s3, s2 = W["exp_w1"][l, e], W["exp_w3"][l, e], W["exp_w2"][l, e]
            else:
                s1, s3, s2 = W["sh_w1"][l], W["sh_w3"][l], W["sh_w2"][l]
            import os
            if e < 2 or not os.environ.get("KNOW"):
                P.dma("sync", w13s[:, :, 0:256], s1.rearrange("(kc p) f -> p kc f", p=128))
                P.dma("sync", w13s[:, :, 256:512], s3.rearrange("(kc p) f -> p kc f", p=128))
                P.dma("sync", w2s[:], s2.rearrange("(fc p) c -> p fc c", p=128))
                P.copy(w13b[:, eb], w13s[:], eng="gpsimd")
                P.copy(w2b[:, eb], w2s[:], eng="gpsimd")
            for gi, (c0, n) in enumerate(groups):
                hb = (e * len(groups) + gi) % 2
                cols = slice(c0, c0 + n)
                for fc in range(2):
                    h1 = G.psf[:, fc * 512:fc * 512 + n]
                    h3 = G.psf[:, (2 + fc) * 512:(2 + fc) * 512 + n]
                    for kc in range(8):
                        P.mm(h1, w13b[:, eb, kc, fc * 128:(fc + 1) * 128], hTp[:, kc, cols], start=(kc == 0), stop=(kc == 7))
                    for kc in range(8):
                        P.mm(h3, w13b[:, eb, kc, 256 + fc * 128:256 + (fc + 1) * 128], hTp[:, kc, cols], start=(kc == 0), stop=(kc == 7))
                    P.act(sil[:, fc, 0:n], h1, AF.Silu)
                    P.tt(hid[:, hb, fc, 0:n], sil[:, fc, 0:n], h3, ALU.mult)
                if pending[0] is not None:
                    emit_down(*pending[0])
                pending[0] = (e, eb, hb, c0, n)
        emit_down(*pending[0])
        pending[0] = None
        mg2 = ph2 = None
        fin = Phase(nc, P)
        mg2 = fin.sb("mg2", [128, 2, 1024]); l2w = fin.sb("l2w", [128, 1024]); l2b = fin.sb("l2b", [128, 1024])
        xt = fin.sb("mxt", [128, 2, 1024]); st6 = fin.sb("mst6", [128, 2, 6]); mv = fin.sb("mmv", [128, 2]); rstd = fin.sb("mrstd", [128, 1])
        for s in range(2):
            P.dma("sync", mg2[:, s, :], G.modd[s, 5])
        P.dma("sync", l2w[:], bc_rows(W["ln2_w"][l:l + 1, :]))
        P.dma("sync", l2b[:], bc_rows(W["ln2_b"][l:l + 1, :]))
        for k, i in enumerate(tiles):
            b = k % 2
            s = seg_of(i)
            rows = slice(i * 128, (i + 1) * 128)
            x = xt[:, b, :]
            P.dma("sync", x, G.xres[rows, :])
            f = acc[:, k, :]
            if G.dbg is not None and "fdbg" in G.dbg:
                P.dma("sync", G.dbg["fdbg"][rows, :], f)
            P.tt(f, f, mg2[:, s, :], ALU.mult, eng="gpsimd")
            P.stt(f, x, DN_ALPHA, f, ALU.mult, ALU.add)
            ln_stats(P, fin, f, "m", st6[:], mv[:], rstd[:])
            P.ts(f, f, mv[:, 0:1], ALU.subtract, rstd[:, 0:1], ALU.mult)
            P.tt(f, f, l2w[:], ALU.mult, eng="gpsimd")
            P.tt(x, f, l2b[:], ALU.add)
            if G.dbg is not None and "xout" in G.dbg:
                P.dma("sync", G.dbg["xout"][rows, :], x)
            if l == 3 and G.final_out:
                P.dma(G.stq, G.out[(i - 2) * 128:(i - 1) * 128, :], x, final=True)
            else:
                P.dma(G.stq, G.xres[rows, :], x)
        fin.st.close()
        ph.close()
def alloc_scratch(nc, G, scratch):
    G.xbca = scratch("xbca", [T_ALL, 1024])
    G.dtla = scratch("dtla", [T_ALL, 32])
    G.ydir = scratch("ydir", [2, T_ALL, 512])
    G.gla_la = scratch("gla_la", [T_ALL, 256])
    G.gla_o = scratch("gla_o", [2, T_ALL, 256])
    G.rwp = scratch("rwp", [T_ALL, 11 * 256])
    G.rwy = scratch("rwy", [2, T_ALL, 256])
    G.hT = scratch("hT", [8, 128, T_ALL], BF16)
    G.gtok = scratch("gtok", [T_ALL, 64])

PHASES = {"mod": phase_mod, "inproj": phase_inproj, "ssd": phase_ssd, "ssd_prep": ssd_prep, "ssd_dir": ssd_dir, "ssd_fin": ssd_fin,
          "gla": phase_gla, "rwkv": phase_rwkv, "rwkv_prep": rwkv_prep, "rwkv_dir": rwkv_dir, "rwkv_fin": rwkv_fin, "outproj": phase_outproj, "moe": phase_moe}
WNAMES = ["ada_w", "ada_b", "w_in", "ssd_conv_w", "ssd_conv_b", "ssd_dt_bias", "ssd_a_log", "ssd_d", "ssd_norm_w",
          "rwkv_mu", "rwkv_w0", "rwkv_w2", "rwkv_a0", "rwkv_a2", "rwkv_g2", "rwkv_kk", "rwkv_ka", "rwkv_rk",
          "rwkv_lnx_w", "rwkv_lnx_b", "gla_gu", "gla_gb", "gla_norm_w", "w_out", "ln1_w", "ln1_b", "ln2_w", "ln2_b",
          "router_w", "router_b", "exp_w1", "exp_w3", "exp_w2", "sh_w1", "sh_w3", "sh_w2"]
WSHAPES = {"ada_w": [4, 1024, 6144], "ada_b": [4, 6144], "w_in": [4, 1024, 3504], "ssd_conv_w": [4, 3, 1024],
           "ssd_conv_b": [4, 1024], "ssd_dt_bias": [4, 2, 8], "ssd_a_log": [4, 2, 8], "ssd_d": [4, 8],
           "ssd_norm_w": [4, 512], "rwkv_mu": [4, 4, 1152], "rwkv_w0": [4, 2, 256], "rwkv_w2": [4, 2, 64, 256],
           "rwkv_a0": [4, 2, 256], "rwkv_a2": [4, 2, 64, 256], "rwkv_g2": [4, 128, 256], "rwkv_kk": [4, 256],
           "rwkv_ka": [4, 256], "rwkv_rk": [4, 4, 64], "rwkv_lnx_w": [4, 256], "rwkv_lnx_b": [4, 256],
           "gla_gu": [4, 2, 16, 128], "gla_gb": [4, 2, 128], "gla_norm_w": [4, 256], "w_out": [4, 1024, 1024],
           "ln1_w": [4, 1024], "ln1_b": [4, 1024], "ln2_w": [4, 1024], "ln2_b": [4, 1024],
           "router_w": [4, 1024, 64], "router_b": [4, 64], "exp_w1": [4, 64, 1024, 256], "exp_w3": [4, 64, 1024, 256],
           "exp_w2": [4, 64, 256, 1024], "sh_w1": [4, 1024, 256], "sh_w3": [4, 1024, 256], "sh_w2": [4, 256, 1024]}


def build_program(layers=(0, 1, 2, 3), phases=None, dbg=(), wnames=None, feed=()):
    nc = bass.Bass("TRN2", target_bir_lowering=False)
    P = Prog(nc)
    G = Ctx()
    G.w = {}
    import os as _os
    G.stq = _os.environ.get('KSTQ', 'gpsimd')
    import os
    G.stop = float(os.environ.get('KSTOP', '99'))
    for n in (wnames or WNAMES):
        G.w[n] = nc.dram_tensor(n, WSHAPES[n], F32, kind="ExternalInput").ap()
    G.x_in = nc.dram_tensor("x", [4096, 1024], F32, kind="ExternalInput").ap()
    G.ctx_in = nc.dram_tensor("ctx", [256, 1024], F32, kind="ExternalInput").ap()
    G.cvec = nc.dram_tensor("cvec", [2, 1024], F32, kind="ExternalInput").ap()
    G.out = nc.dram_tensor("out", [4096, 1024], F32, kind="ExternalOutput").ap()
    DBG_SHAPES = {"u": ([T_ALL, 1024], BF16), "modd": ([2, 6, 128, 1024], F32), "zin": ([T_ALL, NIN], F32),
                  "ymix": ([T_ALL, 1024], F32), "xres": ([T_ALL, 1024], F32)}
    G.dbg = {}
    G.final_out = (phases is None or "moe" in phases)

    def scratch(name, shape, dt=F32):
        kind = "ExternalOutput" if name in dbg else ("ExternalInput" if name in feed else "Internal")
        t = nc.dram_tensor(name, list(shape), dt, kind=kind).ap()
        return t
    for n in dbg:
        if n in ("u",):
            G.dbg[n] = nc.dram_tensor(n, DBG_SHAPES[n][0], DBG_SHAPES[n][1], kind="ExternalOutput").ap()
        if n in ("hdbg", "fdbg", "xout"):
            G.dbg[n] = nc.dram_tensor(n, [T_ALL, 1024], F32, kind="ExternalOutput").ap()
    G.xres = scratch("xres", [T_ALL, 1024])
    G.modd = scratch("modd", [2, 6, 128, 1024])
    G.zin = scratch("zin", [T_ALL, NIN])
    G.ymix = scratch("ymix", [T_ALL, 1024])
    G.psf = nc.alloc_psum_tensor("psa", [128, 8 * 512], F32)
    G.psb = G.psf.bitcast(BF16)[:, 6 * 1024:8 * 1024]
    make_consts(nc, P, G)
    alloc_scratch(nc, G, scratch)
    if "xres" not in feed:
        P.dma("sync", G.xres[0:256, :], G.ctx_in)
        P.dma("sync", G.xres[256:T_ALL, :], G.x_in)
    P.barrier()
    allp = ["mod", "inproj", "ssd", "gla", "rwkv", "outproj", "moe"]
    for l in layers:
        for pn in (phases or allp):
            PHASES[pn](nc, P, G, l)
    if phases is None or "moe" in phases:
        pass
    else:
        pass
    P.barrier()
    P.dma("sync", G.out[0:128, 0:8], G.xres[0:128, 0:8], final=True) if (phases is not None and "moe" not in phases) else None
    P.finish()
    return nc, P


_NC_CACHE = {}


def kernel(**inputs):
    from concourse.bass_utils import run_bass_kernel_spmd
    if "nc" not in _NC_CACHE:
        nc, P = build_program()
        _NC_CACHE["nc"] = nc
    nc = _NC_CACHE["nc"]
    B = 4
    base = {n: np.ascontiguousarray(np.asarray(inputs[n], dtype=np.float32)) for n in WNAMES}
    x = np.asarray(inputs["x"], dtype=np.float32)
    ctx = np.asarray(inputs["ctx"], dtype=np.float32)
    c = np.asarray(inputs["c"], dtype=np.float32)
    c_ctx = np.asarray(inputs["c_ctx"], dtype=np.float32)
    in_maps = []
    for b in range(B):
        m = dict(base)
        m["x"] = np.ascontiguousarray(x[b])
        m["ctx"] = np.ascontiguousarray(ctx[b])
        m["cvec"] = np.ascontiguousarray(np.stack([c_ctx, c[b]]))
        in_maps.append(m)
    res = run_bass_kernel_spmd(nc, in_maps, core_ids=list(range(B)))
    out = np.stack([np.asarray(res.results[b]["out"], dtype=np.float32) for b in range(B)])
    return out
```

```python
import numpy as np
import concourse.bass as bass
import concourse.mybir as mybir

F32 = mybir.dt.float32
BF16 = mybir.dt.bfloat16
I32 = mybir.dt.int32
U32 = mybir.dt.uint32
AF = mybir.ActivationFunctionType
ALU = mybir.AluOpType
AX = mybir.AxisListType

_DSZ = {F32: 4, BF16: 2, I32: 4, U32: 4}

ENGS = ("tensor", "vector", "scalar", "gpsimd", "sync")
DMA_K = {"sync": 16, "gpsimd": 8, "scalar": 4}


def _box(ap):
    t = ap.tensor
    name = t.name
    dsz = _DSZ.get(ap.dtype, 4)
    steps = list(ap.ap)
    off = ap.offset
    space = str(ap.space) if hasattr(ap, "space") else ""
    if "DRAM" in space.upper() or "dram" in space.lower() or type(t).__name__.startswith("DRam"):
        lo = off + sum(min(0, s * (c - 1)) for s, c in steps)
        hi = off + sum(max(0, s * (c - 1)) for s, c in steps) + 1
        return name, 0, 1, lo * dsz, hi * dsz
    pstep, pcnt = steps[0]
    if pstep == 0:
        pstep = 1 << 30
    p_lo = off // pstep
    f_off = off - p_lo * pstep
    rest = steps[1:]
    lo = f_off + sum(min(0, s * (c - 1)) for s, c in rest)
    hi = f_off + sum(max(0, s * (c - 1)) for s, c in rest) + 1
    lo *= dsz
    hi *= dsz
    if name.startswith("ps"):
        lo = (lo // 2048) * 2048
        hi = ((hi + 2047) // 2048) * 2048
        p_lo, pcnt = 0, 128
    return name, p_lo, p_lo + pcnt, lo, hi


class Prog:
    def __init__(self, nc):
        self.nc = nc
        self.ops = {e: [] for e in ENGS}
        self.cnt = {e: 0 for e in ENGS}
        self.dcnt = {e: 0 for e in DMA_K}
        self.waited = {e: {} for e in ENGS}
        self.recs = {}
        self.semkeys = set()
        self.final_tokens = []
        self.n_ops = 0

    def _deps(self, reads, writes, pe_accum=False, eng=None):
        deps = set()
        rb = [_box(a) for a in reads]
        wb = [_box(a) for a in writes]
        for (name, p0, p1, f0, f1) in rb:
            isps = name.startswith("ps")
            for r in self.recs.get(name, ()):
                if r[0] < p1 and p0 < r[1] and r[2] < f1 and f0 < r[3]:
                    if r[4] == "w" or (isps and r[5][0] != eng):
                        deps.add(r[5])
        for (name, p0, p1, f0, f1) in wb:
            for r in self.recs.get(name, ()):
                if r[0] < p1 and p0 < r[1] and r[2] < f1 and f0 < r[3]:
                    if r[4] == "w" and r[5][0] == "tensor" and eng == "tensor":
                        continue
                    deps.add(r[5])
        return deps, rb, wb

    def _update(self, rb, wb, tok):
        for (name, p0, p1, f0, f1) in wb:
            lst = self.recs.setdefault(name, [])
            lst[:] = [r for r in lst if not (p0 <= r[0] and r[1] <= p1 and f0 <= r[2] and r[3] <= f1)]
            lst.append((p0, p1, f0, f1, "w", tok))
        for (name, p0, p1, f0, f1) in rb:
            lst = self.recs.setdefault(name, [])
            lst[:] = [r for r in lst if not (r[4] == "r" and r[5][0] == tok[0] and p0 <= r[0] and r[1] <= p1
                                             and f0 <= r[2] and r[3] <= f1)]
            lst.append((p0, p1, f0, f1, "r", tok))

    def _waits(self, eng, deps):
        w = self.waited[eng]
        best = {}
        for (k, v) in deps:
            if v > best.get(k, 0):
                best[k] = v
        out = []
        for k, v in best.items():
            if w.get(k, 0) >= v:
                continue
            w[k] = v
            out.append((k, v))
        return out

    def op(self, eng, fn, reads=(), writes=(), pe_accum=False):
        deps, rb, wb = self._deps(reads, writes, pe_accum, eng)
        self.cnt[eng] += 1
        tok = (eng, self.cnt[eng])
        waits = self._waits(eng, deps)
        self.semkeys.add(eng)
        self.ops[eng].append((waits, fn, (eng, 1)))
        self._update(rb, wb, tok)
        self.n_ops += 1
        return tok

    def dma(self, q, out, in_, final=False, **kw):
        deps, rb, wb = self._deps([in_], [out])
        n = self.dcnt[q]
        self.dcnt[q] += 1
        K = DMA_K[q]
        key = ("dma", q, n % K)
        tok = (key, 16 * (n // K + 1))
        if n >= K:
            deps.add((key, 16 * (n // K)))
        waits = self._waits(q, deps)
        self.semkeys.add(key)
        self.ops[q].append((waits, (lambda e, o=out, i=in_, kw=kw: e.dma_start(out=o, in_=i, **kw)), (key, 16)))
        self._update(rb, wb, tok)
        if final:
            self.final_tokens.append(tok)
        self.n_ops += 1
        return tok

    def barrier(self):
        toks = set()
        for e in ENGS:
            if self.cnt[e] > 0:
                toks.add((e, self.cnt[e]))
        for q, K in DMA_K.items():
            n = self.dcnt[q]
            for j in range(min(n, K)):
                last = ((n - 1 - j) // K) * K + j
                toks.add((("dma", q, j), 16 * (last // K + 1)))
        for e in ENGS:
            waits = self._waits(e, set(toks))
            if waits:
                self.ops[e].append((waits, None, None))
        self.recs.clear()

    def mm(self, out, lhsT, rhs, start=True, stop=True):
        return self.op("tensor", lambda e: e.matmul(out, lhsT, rhs, start=start, stop=stop),
                       reads=[lhsT, rhs], writes=[out], pe_accum=not start)

    def tr(self, out, in_, ident):
        return self.op("tensor", lambda e: e.transpose(out, in_, ident), reads=[in_, ident], writes=[out])

    def act(self, out, in_, func, bias=None, scale=None, eng="scalar", accum_out=None):
        reads = [in_]
        kw = {}
        if bias is not None:
            kw["bias"] = bias
            if not isinstance(bias, (int, float)):
                reads.append(bias)
        if scale is not None:
            kw["scale"] = scale
            if not isinstance(scale, (int, float)):
                reads.append(scale)
        writes = [out]
        if accum_out is not None:
            kw["accum_out"] = accum_out
            writes.append(accum_out)
        return self.op(eng, lambda e: e.activation(out, in_, func, **kw), reads=reads, writes=writes)

    def tt(self, out, in0, in1, op, eng="vector"):
        return self.op(eng, lambda e: e.tensor_tensor(out, in0, in1, op), reads=[in0, in1], writes=[out])

    def ts(self, out, in0, s1, op0, s2=None, op1=None, eng="vector", accum_out=None):
        reads = [in0]
        if not isinstance(s1, (int, float)):
            reads.append(s1)
        if s2 is not None and not isinstance(s2, (int, float)):
            reads.append(s2)
        writes = [out]
        kw = {}
        if accum_out is not None:
            kw["accum_out"] = accum_out
            writes.append(accum_out)
        if op1 is None:
            return self.op(eng, lambda e: e.tensor_scalar(out, in0, s1, None, op0, **kw), reads=reads, writes=writes)
        return self.op(eng, lambda e: e.tensor_scalar(out, in0, s1, s2, op0, op1, **kw), reads=reads, writes=writes)

    def stt(self, out, in0, scalar, in1, op0, op1, eng="vector"):
        reads = [in0, in1]
        if not isinstance(scalar, (int, float)):
            reads.append(scalar)
        return self.op(eng, lambda e: e.scalar_tensor_tensor(out, in0, scalar, in1, op0, op1), reads=reads, writes=[out])

    def copy(self, out, in_, eng="vector"):
        if eng == "scalar":
            return self.op(eng, lambda e: e.copy(out, in_), reads=[in_], writes=[out])
        return self.op(eng, lambda e: e.tensor_copy(out, in_), reads=[in_], writes=[out])

    def memset(self, out, val, eng="vector"):
        return self.op(eng, lambda e: e.memset(out, val), writes=[out])

    def finish(self):
        nc = self.nc
        sems = {}
        import contextlib
        with contextlib.ExitStack() as st:
            for k in sorted(self.semkeys, key=str):
                nm = "s_" + "_".join(str(x) for x in (k if isinstance(k, tuple) else (k,)))
                sems[k] = st.enter_context(nc.semaphore(nm))
            block = st.enter_context(nc.Block())
            fin = self._waits("sync", set(self.final_tokens))
            ops = self.ops

            def make(engname, extra=None):
                def body(e):
                    for waits, fn, inc in ops[engname]:
                        for (k, v) in waits:
                            e.wait_ge(sems[k], v)
                        if fn is None:
                            continue
                        ins = fn(e)
                        ins.then_inc(sems[inc[0]], inc[1])
                    if extra:
                        for (k, v) in extra:
                            e.wait_ge(sems[k], v)
                return body

            block.tensor(make("tensor"))
            block.vector(make("vector"))
            block.scalar(make("scalar"))
            block.gpsimd(make("gpsimd"))
            block.sync(make("sync", fin))
        return nc
import contextlib

T_ALL = 4352
NT = 34
D = 1024
NIN = 3504
LN_EPS = 1e-5
DN_ALPHA = 8 ** 0.25
NEGM = -30000.0

C_Z = 0
C_XBC = 512
C_DT = 1536
C_RW = 1552
C_GQ = 2704
C_GK = 2832
C_GV = 2960
C_GD = 3216
C_OG = 3248


def seg_of(i):
    return 0 if i < 2 else 1


class Phase:
    _n = [0]

    def __init__(self, nc, P):
        self.nc = nc
        self.P = P
        self.st = contextlib.ExitStack()

    def sb(self, name, shape, dt=F32):
        Phase._n[0] += 1
        return self.st.enter_context(self.nc.sbuf_tensor(f"{name}_{Phase._n[0]}", list(shape), dt))

    def close(self):
        self.P.barrier()
        self.st.close()


def bc_rows(ap2d_row, n=128):
    steps = list(ap2d_row.ap)
    last = steps[-1]
    return bass.AP(ap2d_row.tensor, ap2d_row.offset, [[0, n], [last[0], last[1]]])


class Ctx:
    pass


def make_consts(nc, P, G):
    def sbp(name, shape, dt=F32):
        return nc.alloc_sbuf_tensor(name, list(shape), dt)
    G.identf = sbp("identf", [128, 128])
    G.identb = sbp("identb", [128, 128], BF16)
    G.onesf = sbp("onesf", [128, 128])
    G.tri = [sbp("tri_f", [128, 128]), sbp("tri_b", [128, 128])]
    G.negm = [sbp("negm_f", [128, 4, 128]), sbp("negm_b", [128, 4, 128])]
    G.trib = [sbp("trib_f", [128, 128], BF16), sbp("trib_b", [128, 128], BF16)]
    G.SL = sbp("m_SL", [128, 128]); G.SU = sbp("m_SU", [128, 128]); G.IU = sbp("m_IU", [128, 128]); G.IL = sbp("m_IL", [128, 128])
    gp = "gpsimd"
    G.nSU = sbp("m_nSU", [128, 128]); G.nSL = sbp("m_nSL", [128, 128])
    G.cind = sbp("cind", [128, 2]); G.cmask = sbp("cmask", [128, 2])
    G.hm4 = sbp("hm4", [128, 4])
    P.memset(G.hm4[:], 1.0, eng=gp)
    P.op(gp, lambda e: e.affine_select(G.hm4[:], G.hm4[:], [[-32, 4]], ALU.is_ge, 0.0, base=0, channel_multiplier=1),
         reads=[G.hm4[:]], writes=[G.hm4[:]])
    P.op(gp, lambda e: e.affine_select(G.hm4[:], G.hm4[:], [[32, 4]], ALU.is_ge, 0.0, base=31, channel_multiplier=-1),
         reads=[G.hm4[:]], writes=[G.hm4[:]])

    def asel(t, cm, step, cmp, base=0):
        P.op(gp, lambda e: e.affine_select(t, t, [[step, 128]], cmp, 0.0, base=base, channel_multiplier=cm),
             reads=[t], writes=[t])
    for t in (G.identf, G.onesf, G.tri[0], G.tri[1], G.SL, G.SU, G.IU, G.IL):
        P.memset(t[:], 1.0, eng=gp)
    asel(G.identf[:], -1, 1, ALU.is_equal)
    asel(G.tri[0][:], -1, 1, ALU.is_ge)
    asel(G.tri[1][:], 1, -1, ALU.is_ge)
    asel(G.IU[:], -1, 1, ALU.is_ge)
    asel(G.IL[:], 1, -1, ALU.is_ge)
    asel(G.SU[:], -1, 1, ALU.is_gt)
    asel(G.SL[:], 1, -1, ALU.is_gt)
    for t in (G.SL, G.SU, G.IU, G.IL):
        P.memset(t[0:64, 64:128], 0.0, eng=gp)
        P.memset(t[64:128, 0:64], 0.0, eng=gp)
    P.ts(G.nSU[:], G.SU[:], -1.0, ALU.mult, eng=gp)
    P.ts(G.nSL[:], G.SL[:], -1.0, ALU.mult, eng=gp)
    P.memset(G.cind[:], 0.0, eng=gp)
    P.memset(G.cind[0:64, 0:1], 1.0, eng=gp)
    P.memset(G.cind[64:128, 1:2], 1.0, eng=gp)
    P.memset(G.cmask[:], 1.0, eng=gp)
    for (col, base) in ((0, 0), (0, -64), (1, -63), (1, -127)):
        P.op(gp, lambda e, col=col, base=base: e.affine_select(G.cmask[:, col:col + 1], G.cmask[:, col:col + 1], [[0, 1]],
                                                             ALU.not_equal, 0.0, base=base, channel_multiplier=1),
             reads=[G.cmask[:, col:col + 1]], writes=[G.cmask[:, col:col + 1]])
    P.copy(G.identb[:], G.identf[:])
    for d in range(2):
        P.copy(G.trib[d][:], G.tri[d][:])
        for h in range(4):
            P.ts(G.negm[d][:, h, :], G.tri[d][:], 1.0, ALU.subtract, -NEGM, ALU.mult)


def ln_stats(P, ph, x_ap, tag, st6, mv, rstd, eps=LN_EPS):
    for hh in range(2):
        P.op("vector", lambda e, hh=hh: e.bn_stats(st6[:, hh, :], x_ap[:, hh * 512:(hh + 1) * 512]),
             reads=[x_ap[:, hh * 512:(hh + 1) * 512]], writes=[st6[:, hh, :]])
    P.op("vector", lambda e: e.bn_aggr(mv, st6), reads=[st6], writes=[mv])
    P.act(rstd, mv[:, 1:2], AF.Sqrt, bias=eps)
    P.op("vector", lambda e: e.reciprocal(rstd, rstd), reads=[rstd], writes=[rstd])


def phase_mod(nc, P, G, l):
    ph = Phase(nc, P)
    cT = ph.sb("cT", [128, 2, 8]); csil = ph.sb("csil", [128, 2, 8])
    wst = ph.sb("adaw", [128, 2, 8, 512]); brow = ph.sb("brow", [1, 6144]); mst = ph.sb("mst", [128, 2, 2, 512])
    for s in range(2):
        P.dma("sync", cT[:, s, :], G.cvec[s, :].rearrange("(kc p) -> p kc", p=128), allow_slow_non_contiguous=True)
    P.act(csil[:], cT[:], AF.Silu)
    P.dma("sync", brow[:], G.w["ada_b"][l:l + 1, :])
    for n in range(12):
        b = n % 2
        P.dma("sync", wst[:, b], G.w["ada_w"][l, :, n * 512:(n + 1) * 512].rearrange("(kc p) j -> p kc j", p=128))
        for s in range(2):
            pso = G.psf[:, (n % 2 * 2 + s) * 512:(n % 2 * 2 + s + 1) * 512]
            for kc in range(8):
                P.mm(pso, csil[:, s, kc:kc + 1].to_broadcast([128, 128]), wst[:, b, kc, :], start=(kc == 0), stop=False)
            P.mm(pso, G.onesf[0:1, :], brow[0:1, n * 512:(n + 1) * 512], start=False, stop=True)
            which = n // 2
            if which in (1, 4):
                P.act(mst[:, b, s, :], pso, AF.Identity, bias=1.0)
            else:
                P.copy(mst[:, b, s, :], pso)
            P.dma(G.stq, G.modd[s, which, :, (n % 2) * 512:(n % 2 + 1) * 512], mst[:, b, s, :])
    ph.close()


def phase_inproj(nc, P, G, l):
    ph = Phase(nc, P)
    winb = ph.sb("winb", [128, 8, NIN], BF16)
    wst = ph.sb("wst", [128, 2, 8, 512])
    msc = ph.sb("msc", [128, 2, 1024]); msh = ph.sb("msh", [128, 2, 1024])
    xt = ph.sb("xt", [128, 2, 1024]); ub = ph.sb("ub", [128, 2, 1024], BF16)
    uT = ph.sb("uT", [128, 2, 8, 128], BF16); zt = ph.sb("zt", [128, 2, NIN])
    st6 = ph.sb("st6", [128, 2, 6]); mv = ph.sb("mv", [128, 2]); rstd = ph.sb("rstd", [128, 1])
    for n in range(7):
        w = min(512, NIN - n * 512)
        P.dma("sync", wst[:, n % 2, :, 0:w], G.w["w_in"][l, :, n * 512:n * 512 + w].rearrange("(kc p) j -> p kc j", p=128))
        P.copy(winb[:, :, n * 512:n * 512 + w], wst[:, n % 2, :, 0:w], eng="gpsimd")
    for s in range(2):
        P.dma("sync", msc[:, s, :], G.modd[s, 1])
        P.dma("sync", msh[:, s, :], G.modd[s, 0])
    def stage_a(i):
        b = i % 2
        s = seg_of(i)
        x = xt[:, b, :]
        P.dma("sync", x, G.xres[i * 128:(i + 1) * 128, :])
        ln_stats(P, ph, x, "a", st6[:], mv[:], rstd[:])
        P.ts(x, x, mv[:, 0:1], ALU.subtract, rstd[:, 0:1], ALU.mult)
        P.tt(x, x, msc[:, s, :], ALU.mult, eng="gpsimd")
        P.tt(ub[:, b, :], x, msh[:, s, :], ALU.add)
        if G.dbg is not None and "u" in G.dbg:
            P.dma("sync", G.dbg["u"][i * 128:(i + 1) * 128, :], ub[:, b, :])
        for kc in range(8):
            P.tr(G.psb[:, kc * 128:(kc + 1) * 128], ub[:, b, kc * 128:(kc + 1) * 128], G.identb[:])
        P.copy(uT[:, b].rearrange("p a b -> p (a b)"), G.psb[:, 0:1024], eng="scalar")

    def stage_b(i):
        b = i % 2
        for n in range(7):
            w = min(512, NIN - n * 512)
            bk = (i * 7 + n) % 6
            pso = G.psf[:, bk * 512:bk * 512 + w]
            for kc in range(8):
                P.mm(pso, uT[:, b, kc, :], winb[:, kc, n * 512:n * 512 + w], start=(kc == 0), stop=(kc == 7))
            P.copy(zt[:, b, n * 512:n * 512 + w], pso, eng=("scalar" if n % 2 else "vector"))
        P.dma(G.stq, G.zin[i * 128:(i + 1) * 128, :], zt[:, b, :])
    stage_a(0)
    for i in range(NT):
        if i + 1 < NT:
            stage_a(i + 1)
        stage_b(i)
    ph.close()
def dir_order(d):
    if d == 0:
        return list(range(NT))
    return [1, 0] + list(range(NT - 1, 1, -1))


import os as _os2
G_LDQ = [_os2.environ.get("KLDQ", "sync")]


def load_shift(P, dst, src2d, i, off, eng_ms="gpsimd"):
    lo, hi = (0, 256) if i < 2 else (256, T_ALL)
    r0 = i * 128 + off
    a = max(r0, lo); bnd = min(r0 + 128, hi)
    if a > r0:
        n = a - r0
        if n >= 128:
            P.memset(dst, 0.0, eng=eng_ms); return
        P.memset(dst[0:((n + 31) // 32) * 32, :], 0.0, eng=eng_ms)
    if bnd < r0 + 128:
        n = r0 + 128 - bnd
        if n >= 128:
            P.memset(dst, 0.0, eng=eng_ms); return
        st = ((128 - n) // 32) * 32
        P.memset(dst[st:128, :], 0.0, eng=eng_ms)
    P.dma(G_LDQ[0], dst[a - r0:bnd - r0, :], src2d[a:bnd, :])


def phase_ssd(nc, P, G, l):
    ssd_prep(nc, P, G, l); ssd_dir(nc, P, G, l); ssd_fin(nc, P, G, l)


def ssd_prep(nc, P, G, l):
    W = G.w
    ph = Phase(nc, P)
    cw = ph.sb("cw", [128, 3, 1024]); cb = ph.sb("cb", [128, 1024])
    dtb = ph.sb("dtb", [128, 16]); nega = ph.sb("nega", [128, 16])
    xc = ph.sb("xc", [128, 2, 1024]); xl = ph.sb("xl", [128, 2, 1024]); xr = ph.sb("xr", [128, 2, 1024])
    acc = ph.sb("acc", [128, 2, 1024]); dl = ph.sb("dl", [128, 2, 32]); dr = ph.sb("dr", [128, 2, 16])
    for j in range(3):
        P.dma("sync", cw[:, j, :], bc_rows(W["ssd_conv_w"][l, j:j + 1, :]))
    P.dma("sync", cb[:], bc_rows(W["ssd_conv_b"][l:l + 1, :]))
    P.dma("sync", dtb[:], bc_rows(W["ssd_dt_bias"][l:l + 1].rearrange("a b c -> a (b c)")))
    P.dma("sync", nega[:], bc_rows(W["ssd_a_log"][l:l + 1].rearrange("a b c -> a (b c)")))
    P.act(nega[:], nega[:], AF.Exp)
    P.ts(nega[:], nega[:], -1.0, ALU.mult)
    zx = G.zin[:, C_XBC:C_XBC + 1024]
    for i in range(NT):
        b = i % 2
        rows = slice(i * 128, (i + 1) * 128)
        P.dma("sync", xc[:, b, :], zx[rows, :])
        load_shift(P, xl[:, b, :], zx, i, -1)
        load_shift(P, xr[:, b, :], zx, i, +1)
        P.dma("sync", dr[:, b, :], G.zin[rows, C_DT:C_DT + 16])
        a = acc[:, b, :]
        P.tt(a, xc[:, b, :], cw[:, 1, :], ALU.mult)
        P.tt(xl[:, b, :], xl[:, b, :], cw[:, 0, :], ALU.mult, eng="gpsimd")
        P.tt(xr[:, b, :], xr[:, b, :], cw[:, 2, :], ALU.mult, eng="gpsimd")
        P.tt(a, a, xl[:, b, :], ALU.add)
        P.tt(a, a, xr[:, b, :], ALU.add)
        P.tt(a, a, cb[:], ALU.add)
        P.act(a, a, AF.Silu)
        P.dma(G.stq, G.xbca[rows, :], a)
        P.tt(dr[:, b, :], dr[:, b, :], dtb[:], ALU.add)
        P.act(dr[:, b, :], dr[:, b, :], AF.Exp)
        P.act(dl[:, b, 0:16], dr[:, b, :], AF.Ln, bias=1.0)
        P.tt(dl[:, b, 16:32], dl[:, b, 0:16], nega[:], ALU.mult)
        P.dma(G.stq, G.dtla[rows, :], dl[:, b, :])
    ph.close()


def run_streams(gens):
    gens = list(gens)
    while gens:
        for g in list(gens):
            try:
                next(g)
            except StopIteration:
                gens.remove(g)


def ssd_dir(nc, P, G, l, with_rwkv_prep=False):
    ph = Phase(nc, P)
    ps = G.psf
    psbf = G.psf.bitcast(BF16)

    def stream(d, kb):
        sfx = "f" if d == 0 else "b"
        xa = ph.sb("xa" + sfx, [128, 2, 1024]); dl = ph.sb("dl2" + sfx, [128, 2, 32])
        sm = ph.sb("sm" + sfx, [128, 2, 48])
        R1 = ph.sb("R1" + sfx, [128, 8, 128]); R3 = ph.sb("R3" + sfx, [128, 8, 128])
        LT = ph.sb("LT" + sfx, [128, 8, 128], BF16); MT = ph.sb("MT" + sfx, [128, 8, 128], BF16)
        bcb = ph.sb("bcb" + sfx, [128, 2, 512], BF16); bcT = ph.sb("bcT" + sfx, [128, 2, 4, 128], BF16)
        xdt = ph.sb("xdt" + sfx, [128, 2, 8, 64], BF16); xdd = ph.sb("xdd" + sfx, [128, 2, 8, 64], BF16)
        ytmp = ph.sb("ytmp" + sfx, [128, 8, 64]); yo = ph.sb("yo" + sfx, [128, 2, 512])
        hin32 = ph.sb("hin32" + sfx, [128, 512]); hinb = ph.sb("hinb" + sfx, [128, 512], BF16)

        def bank(k, w=512, o=0):
            return ps[:, kb[k] * 512 + o:kb[k] * 512 + o + w]
        P.memset(hin32[:], 0.0)
        P.memset(hinb[:], 0.0)
        tri = G.tri[d]
        for i in dir_order(d):
            b = i % 2
            rows = slice(i * 128, (i + 1) * 128)
            P.dma("sync", xa[:, b, :], G.xbca[rows, :])
            P.dma("sync", dl[:, b, :], G.dtla[rows, :])
            dt_d = dl[:, b, d * 8:(d + 1) * 8]
            la_d = dl[:, b, 16 + d * 8:16 + (d + 1) * 8]
            S = sm[:, b, :]
            acs_ps = bank(0, 8, 256); tot_ps = bank(0, 8, 264)
            P.mm(acs_ps, tri[:], la_d)
            P.mm(tot_ps, G.onesf[:], la_d)
            P.tt(R1[:], tri[:].unsqueeze(1).to_broadcast([128, 8, 128]), la_d.unsqueeze(2).to_broadcast([128, 8, 128]),
                 ALU.mult, eng="gpsimd")
            P.act(R3[:], la_d.unsqueeze(2).to_broadcast([128, 8, 128]), AF.Identity, scale=-1.0)
            P.copy(bcb[:, b, :], xa[:, b, 512:1024])
            yield
            P.copy(S[:, 0:8], acs_ps)
            P.act(S[:, 8:16], acs_ps, AF.Exp)
            P.tt(S[:, 40:48], tot_ps, S[:, 0:8], ALU.subtract)
            P.act(S[:, 16:24], S[:, 40:48], AF.Exp)
            P.act(S[:, 24:32], tot_ps, AF.Exp)
            P.tt(S[:, 32:40], dt_d, S[:, 16:24], ALU.mult)
            for q in range(4):
                P.tr(psbf[:, kb[2] * 1024 + q * 128:kb[2] * 1024 + (q + 1) * 128], bcb[:, b, q * 128:(q + 1) * 128], G.identb[:])
            for g in range(2):
                sp = bank(1)
                P.mm(sp, G.onesf[:], R1[:, 4 * g:4 * g + 4, :], start=True, stop=False)
                P.mm(sp, tri[:], R3[:, 4 * g:4 * g + 4, :], start=False, stop=False)
                P.mm(sp, G.identf[:], G.negm[d][:], start=False, stop=True)
                if g == 0:
                    yield
                    P.copy(bcT[:, b].rearrange("p a b -> p (a b)"), psbf[:, kb[2] * 1024:kb[2] * 1024 + 512], eng="scalar")
                P.act(LT[:, 4 * g:4 * g + 4, :], sp, AF.Exp)
            xs3 = xa[:, b, 0:512].rearrange("p (h q) -> p h q", h=8)
            P.tt(xdt[:, b], xs3, dt_d.unsqueeze(2).to_broadcast([128, 8, 64]), ALU.mult, eng="gpsimd")
            P.tt(xdd[:, b], xs3, S[:, 32:40].unsqueeze(2).to_broadcast([128, 8, 64]), ALU.mult, eng="gpsimd")
            for g in range(2):
                cbp = bank(0, 128, g * 128)
                P.mm(cbp, bcT[:, b, g, :], bcT[:, b, 2 + g, :])
            for g in range(2):
                P.mm(bank(3, 256, g * 256), bcT[:, b, 2 + g, :], hinb[:, g * 256:(g + 1) * 256])
            yield
            for g in range(2):
                cbp = bank(0, 128, g * 128)
                P.tt(MT[:, 4 * g:4 * g + 4, :], LT[:, 4 * g:4 * g + 4, :], cbp.unsqueeze(1).to_broadcast([128, 4, 128]), ALU.mult)
            P.tt(ytmp[:], bank(3).rearrange("p (h q) -> p h q", h=8), S[:, 8:16].unsqueeze(2).to_broadcast([128, 8, 64]), ALU.mult)
            for h in range(8):
                P.mm(bank(2, 64, h * 64), MT[:, h, :], xdt[:, b, h, :])
            for g in range(2):
                P.mm(bank(3, 256, g * 256), bcb[:, b, g * 128:(g + 1) * 128], xdd[:, b, 4 * g:4 * g + 4, :])
            yield
            P.tt(yo[:, b, :], ytmp[:].rearrange("p h q -> p (h q)"), bank(2), ALU.add)
            P.dma(G.stq, G.ydir[d, rows, :], yo[:, b, :])
            h3 = hin32[:].rearrange("p (h q) -> p h q", h=8)
            P.tt(h3, h3, S[:, 24:32].unsqueeze(2).to_broadcast([128, 8, 64]), ALU.mult)
            P.tt(hin32[:], hin32[:], bank(3), ALU.add)
            P.copy(hinb[:], hin32[:], eng="scalar")
            yield

    gens = [stream(0, [0, 1, 2, 3]), stream(1, [4, 5, 6, 7])]
    if with_rwkv_prep:
        gens.append(rwkv_prep_stream(nc, P, G, l, ph))
    run_streams(gens)
    ph.close()


def ssd_fin(nc, P, G, l):
    W = G.w
    ph = Phase(nc, P)
    dsm = ph.sb("dsm", [128, 8]); dex = ph.sb("dex", [128, 8, 64]); nw = ph.sb("nw", [128, 512])
    yf = ph.sb("yf", [128, 2, 512]); yb = ph.sb("yb", [128, 2, 512]); xs = ph.sb("xs", [128, 2, 512]); z = ph.sb("z", [128, 2, 512])
    junk = ph.sb("junk", [128, 256]); ss = ph.sb("ss", [128, 2, 2]); o = ph.sb("o", [128, 2, 512])
    P.dma("sync", dsm[:], bc_rows(W["ssd_d"][l:l + 1, :]))
    P.copy(dex[:], dsm[:].unsqueeze(2).to_broadcast([128, 8, 64]))
    P.dma("sync", nw[:], bc_rows(W["ssd_norm_w"][l:l + 1, :]))
    dexf = dex[:].rearrange("p h q -> p (h q)")
    for i in range(NT):
        b = i % 2
        rows = slice(i * 128, (i + 1) * 128)
        P.dma("sync", yf[:, b, :], G.ydir[0, rows, :])
        P.dma("sync", yb[:, b, :], G.ydir[1, rows, :])
        P.dma("sync", xs[:, b, :], G.xbca[rows, 0:512])
        P.dma("sync", z[:, b, :], G.zin[rows, C_Z:C_Z + 512])
        y = yf[:, b, :]
        P.tt(y, y, yb[:, b, :], ALU.add)
        P.tt(xs[:, b, :], xs[:, b, :], dexf, ALU.mult, eng="gpsimd")
        P.tt(y, y, xs[:, b, :], ALU.add)
        P.act(z[:, b, :], z[:, b, :], AF.Silu)
        P.tt(y, y, z[:, b, :], ALU.mult)
        for g in range(2):
            P.act(junk[:], y[:, g * 256:(g + 1) * 256], AF.Square, accum_out=ss[:, b, g:g + 1])
        P.act(ss[:, b, :], ss[:, b, :], AF.Sqrt, bias=1e-5, scale=1.0 / 256)
        P.op("vector", lambda e, b=b: e.reciprocal(ss[:, b, :], ss[:, b, :]), reads=[ss[:, b, :]], writes=[ss[:, b, :]])
        for g in range(2):
            P.stt(o[:, b, g * 256:(g + 1) * 256], y[:, g * 256:(g + 1) * 256], ss[:, b, g:g + 1], nw[:, g * 256:(g + 1) * 256],
                  ALU.mult, ALU.mult)
        P.dma(G.stq, G.ymix[rows, 0:512], o[:, b, :])
    ph.close()
def phase_gla(nc, P, G, l):
    gla_prep(nc, P, G, l); gla_dir(nc, P, G, l); gla_fin(nc, P, G, l)


def gla_prep(nc, P, G, l):
    W = G.w
    ph = Phase(nc, P)
    GU = ph.sb("GU", [32, 256]); gbrow = ph.sb("gbrow", [1, 256])
    gd = ph.sb("gd", [128, 2, 32]); gdT = ph.sb("gdT", [32, 2, 128]); e = ph.sb("ge", [128, 2, 256])
    P.memset(GU[:], 0.0)
    P.dma("sync", GU[0:16, 0:128], W["gla_gu"][l, 0])
    P.dma("sync", GU[16:32, 128:256], W["gla_gu"][l, 1])
    P.dma("sync", gbrow[:], W["gla_gb"][l:l + 1].rearrange("a b c -> a (b c)"))
    for i in range(NT):
        b = i % 2
        rows = slice(i * 128, (i + 1) * 128)
        P.dma("sync", gd[:, b, :], G.zin[rows, C_GD:C_GD + 32])
        tp = G.psf[0:32, 0:128]
        P.tr(tp, gd[:, b, :], G.identf[:])
        P.copy(gdT[:, b, :], tp)
        gp = G.psf[:, 512:768]
        P.mm(gp, gdT[:, b, :], GU[:], start=True, stop=False)
        P.mm(gp, G.onesf[0:1, :], gbrow[:], start=False, stop=True)
        P.act(e[:, b, :], gp, AF.Exp, scale=-1.0)
        P.act(e[:, b, :], e[:, b, :], AF.Ln, bias=1.0)
        P.ts(e[:, b, :], e[:, b, :], -1.0 / 16.0, ALU.mult)
        P.dma(G.stq, G.gla_la[rows, :], e[:, b, :])
    ph.close()


def gla_dir(nc, P, G, l):
    ph = Phase(nc, P)
    ps = G.psf
    psbf = G.psf.bitcast(BF16)

    def stream(d, kb):
        sfx = "f" if d == 0 else "b"
        qk = ph.sb("qk" + sfx, [128, 2, 256]); v = ph.sb("gv" + sfx, [128, 2, 256]); la = ph.sb("gla" + sfx, [128, 2, 128])
        acs = ph.sb("gacs" + sfx, [128, 128]); ee = ph.sb("gee" + sfx, [128, 3, 128]); ecd = ph.sb("gecd" + sfx, [128, 1])
        qkt = ph.sb("qkt" + sfx, [128, 2, 128], BF16); kdec = ph.sb("kdec" + sfx, [128, 128], BF16)
        qkT = ph.sb("qkT" + sfx, [128, 2, 128], BF16); smb = ph.sb("smb" + sfx, [128, 4, 128], BF16); vb = ph.sb("vb" + sfx, [128, 256], BF16)
        kTm = ph.sb("kTm" + sfx, [128, 4, 128], BF16); qTm = ph.sb("qTm" + sfx, [128, 4, 128], BF16)
        stm = ph.sb("stm" + sfx, [128, 4, 64]); sred = ph.sb("sred" + sfx, [128, 64])
        ot = ph.sb("got" + sfx, [128, 2, 256]); S32 = ph.sb("S32" + sfx, [128, 64]); Sb = ph.sb("Sb" + sfx, [128, 64], BF16)

        def bank(k, w=512, o=0):
            return ps[:, kb[k] * 512 + o:kb[k] * 512 + o + w]
        P.memset(S32[:], 0.0)
        P.memset(Sb[:], 0.0)
        tri = G.tri[d]
        for i in dir_order(d):
            b = i % 2
            rows = slice(i * 128, (i + 1) * 128)
            P.dma("sync", qk[:, b, :], G.zin[rows, C_GQ:C_GQ + 256])
            P.dma("sync", v[:, b, :], G.zin[rows, C_GV:C_GV + 256])
            P.dma("sync", la[:, b, :], G.gla_la[rows, d * 128:(d + 1) * 128])
            acs_ps = bank(0, 128, 0); tot_ps = bank(0, 128, 128); totT = bank(0, 2, 256)
            P.mm(acs_ps, tri[:], la[:, b, :])
            P.mm(tot_ps, G.onesf[:], la[:, b, :])
            P.mm(totT, la[:, b, :], G.onesf[:, 0:2])
            P.copy(vb[:], v[:, b, :], eng="gpsimd")
            yield
            P.copy(acs[:], acs_ps, eng="scalar")
            P.act(ee[:, 0, :], acs_ps, AF.Exp)
            P.act(ee[:, 1, :], acs_ps, AF.Exp, scale=-1.0)
            P.tt(ee[:, 2, :], tot_ps, acs[:], ALU.subtract)
            P.act(ee[:, 2, :], ee[:, 2, :], AF.Exp)
            P.act(ecd[:], totT[:, 0:1], AF.Exp)
            P.stt(qkt[:, 0, :], qk[:, b, 0:128], 32 ** -0.5, ee[:, 0, :], ALU.mult, ALU.mult)
            P.tt(qkt[:, 1, :], qk[:, b, 128:256], ee[:, 1, :], ALU.mult)
            P.tt(kdec[:], qk[:, b, 128:256], ee[:, 2, :], ALU.mult, eng="gpsimd")
            for j in range(2):
                P.tr(psbf[:, kb[0] * 1024 + j * 128:kb[0] * 1024 + (j + 1) * 128], qkt[:, j, :], G.identb[:])
            P.mm(bank(3, 256, 0), kdec[:], vb[:])
            yield
            P.copy(qkT[:].rearrange("p a b -> p (a b)"), psbf[:, kb[0] * 1024:kb[0] * 1024 + 256], eng="scalar")
            P.tt(kTm[:], qkT[:, 1, :].unsqueeze(1).to_broadcast([128, 4, 128]), G.hm4[:].unsqueeze(2).to_broadcast([128, 4, 128]), ALU.mult)
            P.tt(qTm[:], qkT[:, 0, :].unsqueeze(1).to_broadcast([128, 4, 128]), G.hm4[:].unsqueeze(2).to_broadcast([128, 4, 128]), ALU.mult, eng="gpsimd")
            for h in range(4):
                P.mm(bank(1, 128, h * 128), kTm[:, h, :], qkT[:, 0, :])
            yield
            P.tt(smb[:], bank(1).rearrange("p (h q) -> p h q", h=4), G.tri[d][:].unsqueeze(1).to_broadcast([128, 4, 128]), ALU.mult)
            for h in range(4):
                o_ps = bank(2, 64, h * 64)
                P.mm(o_ps, smb[:, h, :], vb[:, h * 64:(h + 1) * 64], start=True, stop=False)
                P.mm(o_ps, qTm[:, h, :], Sb[:], start=False, stop=True)
            yield
            P.copy(ot[:, b, :], bank(2, 256, 0), eng="scalar")
            P.dma(G.stq, G.gla_o[d, rows, :], ot[:, b, :])
            P.tt(stm[:], bank(3, 256, 0).rearrange("p (h q) -> p h q", h=4), G.hm4[:].unsqueeze(2).to_broadcast([128, 4, 64]), ALU.mult)
            P.op("vector", lambda e: e.tensor_reduce(sred[:], stm[:].rearrange("p h q -> p q h"), AX.X, ALU.add),
                 reads=[stm[:]], writes=[sred[:]])
            P.stt(S32[:], S32[:], ecd[:, 0:1], sred[:], ALU.mult, ALU.add)
            P.copy(Sb[:], S32[:])
            yield

    run_streams([stream(0, [0, 1, 2, 3]), stream(1, [4, 5, 6, 7])])
    ph.close()


def gla_fin(nc, P, G, l):
    W = G.w
    ph = Phase(nc, P)
    nw = ph.sb("gnw", [128, 256]); of = ph.sb("gof", [128, 2, 256]); ob = ph.sb("gob", [128, 2, 256]); og = ph.sb("gog", [128, 2, 256])
    sq = ph.sb("gsq", [128, 256]); ss = ph.sb("gss", [128, 2, 4]); y = ph.sb("gy", [128, 2, 256])
    P.dma("sync", nw[:], bc_rows(W["gla_norm_w"][l:l + 1, :]))
    for i in range(NT):
        b = i % 2
        rows = slice(i * 128, (i + 1) * 128)
        P.dma("sync", of[:, b, :], G.gla_o[0, rows, :])
        P.dma("sync", ob[:, b, :], G.gla_o[1, rows, :])
        P.dma("sync", og[:, b, :], G.zin[rows, C_OG:C_OG + 256])
        o = of[:, b, :]
        P.tt(o, o, ob[:, b, :], ALU.add)
        P.tt(sq[:], o, o, ALU.mult, eng="gpsimd")
        P.op("vector", lambda e, b=b: e.tensor_reduce(ss[:, b, :], sq[:].rearrange("p (h q) -> p h q", h=4), AX.X, ALU.add),
             reads=[sq[:]], writes=[ss[:, b, :]])
        P.act(ss[:, b, :], ss[:, b, :], AF.Sqrt, bias=1e-5, scale=1.0 / 64)
        P.op("vector", lambda e, b=b: e.reciprocal(ss[:, b, :], ss[:, b, :]), reads=[ss[:, b, :]], writes=[ss[:, b, :]])
        y3 = y[:, b, :].rearrange("p (h q) -> p h q", h=4)
        P.tt(y3, o.rearrange("p (h q) -> p h q", h=4), ss[:, b, :].unsqueeze(2).to_broadcast([128, 4, 64]), ALU.mult)
        P.tt(y[:, b, :], y[:, b, :], nw[:], ALU.mult, eng="gpsimd")
        P.act(og[:, b, :], og[:, b, :], AF.Silu)
        P.tt(y[:, b, :], y[:, b, :], og[:, b, :], ALU.mult)
        P.dma(G.stq, G.ymix[rows, 768:1024], y[:, b, :])
    ph.close()
def phase_rwkv(nc, P, G, l):
    rwkv_prep(nc, P, G, l); rwkv_dir(nc, P, G, l); rwkv_fin(nc, P, G, l)


def rwkv_prep(nc, P, G, l):
    ph = Phase(nc, P)
    for _ in rwkv_prep_stream(nc, P, G, l, ph):
        pass
    ph.close()


def rwkv_prep_stream(nc, P, G, l, ph):
    W = G.w
    mu = ph.sb("mu", [128, 4, 1152])
    w2a2 = ph.sb("w2a2", [128, 2, 256]); g2 = ph.sb("g2", [128, 256]); brow = ph.sb("rbrow", [1, 4, 256])
    kkw = ph.sb("kkw", [128, 256]); ka = ph.sb("ka", [128, 256]); rk = ph.sb("rk", [128, 256])
    c = ph.sb("rc", [128, 2, 1152]); nb = ph.sb("rnb", [128, 4, 1152])
    tws = ph.sb("tws", [128, 3, 128]); twT = ph.sb("twT", [128, 3, 128])
    pk = ph.sb("pk", [128, 2, 11, 256]); tmp = ph.sb("rtmp", [128, 3, 256]); s4 = ph.sb("rs4", [128, 8])
    for j in range(4):
        P.dma("sync", mu[:, j, :], bc_rows(W["rwkv_mu"][l, j:j + 1, :]))
    for d in range(2):
        P.dma("sync", w2a2[64 * d:64 * d + 64, 0, :], W["rwkv_w2"][l, d])
        P.dma("sync", w2a2[64 * d:64 * d + 64, 1, :], W["rwkv_a2"][l, d])
    P.dma("sync", g2[:], W["rwkv_g2"][l])
    P.dma("sync", brow[0:1, 0:2, :], W["rwkv_w0"][l:l + 1])
    P.dma("sync", brow[0:1, 2:4, :], W["rwkv_a0"][l:l + 1])
    P.dma("sync", kkw[:], bc_rows(W["rwkv_kk"][l:l + 1, :]))
    P.dma("sync", ka[:], bc_rows(W["rwkv_ka"][l:l + 1, :]))
    P.dma("sync", rk[:], bc_rows(W["rwkv_rk"][l:l + 1].rearrange("a b c -> a (b c)")))
    zr = G.zin[:, C_RW:C_RW + 1152]
    for i in range(NT):
        b = i % 2
        rows = slice(i * 128, (i + 1) * 128)
        lat = i >= 2
        cc = c[:, b, :]
        P.dma("sync", cc, zr[rows, :])
        offs = [-1, 1, -64, 64] if lat else [-1, 1]
        for j, off in enumerate(offs):
            X = nb[:, j, :]
            load_shift(P, X, zr, i, off)
            if lat and j < 2:
                P.stt(X, X, G.cmask[:, j:j + 1], cc, ALU.mult, ALU.subtract, eng=("vector"))
            else:
                P.tt(X, X, cc, ALU.subtract, eng="gpsimd")
            P.tt(X, X, mu[:, j, :], ALU.mult, eng=("gpsimd" if j % 2 else "vector"))
            yield
        for j in range(len(offs)):
            P.tt(cc, cc, nb[:, j, :], ALU.add)
        yield
        PK = pk[:, b]
        P.act(tws[:, 0, :], cc[:, 768:896], AF.Tanh)
        P.copy(tws[:, 1, :], cc[:, 896:1024], eng="gpsimd")
        P.act(tws[:, 2, :], cc[:, 1024:1152], AF.Sigmoid)
        for q in range(3):
            P.tr(G.psf[:, q * 128:(q + 1) * 128], tws[:, q, :], G.identf[:])
        yield
        P.copy(twT[:].rearrange("p a b -> p (a b)"), G.psf[:, 0:384])
        for d in range(2):
            ds = slice(64 * d, 64 * d + 64)
            xp = G.psf[:, (1 + 2 * d) * 512:(1 + 2 * d) * 512 + 256]
            P.mm(xp, twT[ds, 0, :], w2a2[ds, 0, :], start=True, stop=False)
            P.mm(xp, G.onesf[0:1, :], brow[0:1, d, :], start=False, stop=True)
            ap_ = G.psf[:, (1 + 2 * d) * 512 + 256:(1 + 2 * d) * 512 + 512]
            P.mm(ap_, twT[ds, 1, :], w2a2[ds, 1, :], start=True, stop=False)
            P.mm(ap_, G.onesf[0:1, :], brow[0:1, 2 + d, :], start=False, stop=True)
        gp = G.psf[:, 2048:2304]
        P.mm(gp, twT[:, 2, :], g2[:])
        yield
        for d in range(2):
            P.act(PK[:, 7 + d, :], G.psf[:, (1 + 2 * d) * 512:(1 + 2 * d) * 512 + 256], AF.Sigmoid)
        P.ts(PK[:, 7:9, :], PK[:, 7:9, :], -0.6065306597126334, ALU.mult, eng="gpsimd")
        adir = tmp[:, 0:2, :]
        for d in range(2):
            P.act(adir[:, d, :], G.psf[:, (1 + 2 * d) * 512 + 256:(1 + 2 * d) * 512 + 512], AF.Sigmoid)
        P.copy(PK[:, 9, :], gp, eng="scalar")
        P.copy(PK[:, 0, :], cc[:, 0:256], eng="gpsimd")
        P.copy(PK[:, 1, :], cc[:, 512:768], eng="gpsimd")
        kx = cc[:, 256:512]
        kk = PK[:, 2, :]
        P.tt(kk, kx, kkw[:], ALU.mult)
        P.tt(tmp[:, 2, :], kk, kk, ALU.mult, eng="gpsimd")
        P.op("vector", lambda e: e.tensor_reduce(s4[:, 0:4], tmp[:, 2, :].rearrange("p (h q) -> p h q", h=4), AX.X, ALU.add),
             reads=[tmp[:, 2, :]], writes=[s4[:, 0:4]])
        P.act(s4[:, 0:4], s4[:, 0:4], AF.Sqrt)
        P.ts(s4[:, 0:4], s4[:, 0:4], 1e-12, ALU.max)
        P.op("vector", lambda e: e.reciprocal(s4[:, 0:4], s4[:, 0:4]), reads=[s4[:, 0:4]], writes=[s4[:, 0:4]])
        kk3 = kk.rearrange("p (h q) -> p h q", h=4)
        P.tt(kk3, kk3, s4[:, 0:4].unsqueeze(2).to_broadcast([128, 4, 64]), ALU.mult)
        for d in range(2):
            t1 = tmp[:, 2, :]
            P.stt(t1, adir[:, d, :], -1.0, ka[:], ALU.add, ALU.mult)
            P.stt(PK[:, 3 + d, :], t1, 1.0, kx, ALU.add, ALU.mult)
            P.tt(PK[:, 5 + d, :], kk, adir[:, d, :], ALU.mult, eng="gpsimd")
        t1 = tmp[:, 2, :]
        P.tt(t1, PK[:, 3, :], PK[:, 4, :], ALU.add)
        P.tt(t1, t1, PK[:, 0, :], ALU.mult)
        P.tt(t1, t1, rk[:], ALU.mult)
        P.op("vector", lambda e: e.tensor_reduce(s4[:, 4:8], tmp[:, 2, :].rearrange("p (h q) -> p h q", h=4), AX.X, ALU.add),
             reads=[tmp[:, 2, :]], writes=[s4[:, 4:8]])
        P.tt(PK[:, 10, :].rearrange("p (h q) -> p h q", h=4), PK[:, 1, :].rearrange("p (h q) -> p h q", h=4),
             s4[:, 4:8].unsqueeze(2).to_broadcast([128, 4, 64]), ALU.mult)
        P.dma(G.stq, G.rwp[rows, :], PK.rearrange("p a b -> p (a b)"))
        yield


def rwkv_dir(nc, P, G, l):
    ph = Phase(nc, P)
    ps = G.psf

    def stream(d, kb):
        sfx = "f" if d == 0 else "b"
        ld = ph.sb("rld" + sfx, [128, 2, 6, 256])
        ee = ph.sb("ree" + sfx, [128, 3, 256])
        tok = ph.sb("rtok" + sfx, [128, 4, 256])
        fmh = ph.sb("fmh" + sfx, [64, 4, 4, 128])
        Aarb = ph.sb("Aarb" + sfx, [128, 4, 128]); Aak = ph.sb("Aak" + sfx, [128, 4, 128]); Aark = ph.sb("Aark" + sfx, [128, 4, 128])
        Q = ph.sb("rQ" + sfx, [128, 2, 4, 128]); QT = ph.sb("rQT" + sfx, [128, 2, 4, 128]); IQT = ph.sb("rIQT" + sfx, [128, 4, 128])
        Z = ph.sb("rZ" + sfx, [128, 2, 4, 128])
        Wsb = ph.sb("rW" + sfx, [128, 4, 64]); nAkV = ph.sb("nAkV" + sfx, [128, 4, 64]); Uloc = ph.sb("Uloc" + sfx, [128, 4, 64])
        GpT = ph.sb("GpT" + sfx, [64, 4, 2, 64]); RwT = ph.sb("RwT" + sfx, [64, 4, 128]); gC = ph.sb("gC" + sfx, [64, 4, 2])
        H32 = ph.sb("H32" + sfx, [64, 4, 64]); Yt = ph.sb("Yt" + sfx, [64, 2, 2, 256])
        btc = ph.sb("btc" + sfx, [128, 2, 256]); ktc = ph.sb("ktc" + sfx, [128, 2, 256])

        def bank(k, w=512, o=0, p0=0, p1=128):
            kk_ = kb[k]
            return ps[p0:p1, kk_ * 512 + o:kk_ * 512 + o + w]

        def b4(k):
            return bank(k).rearrange("p (h q) -> p h q", h=4)

        def bc4(m):
            return m[:].unsqueeze(1).to_broadcast([128, 4, 128])
        idb4 = G.identf[:].unsqueeze(1).to_broadcast([128, 4, 128])
        ea = "scalar"
        P.memset(H32[:], 0.0)
        BD = G.IU if d == 0 else G.IL
        m_abT = G.nSU if d == 0 else G.nSL
        m_ab = G.nSL if d == 0 else G.nSU
        m_s = G.SU if d == 0 else G.SL
        m_i = G.IU if d == 0 else G.IL
        for i in dir_order(d):
            b = i % 2
            rows = slice(i * 128, (i + 1) * 128)
            L = ld[:, b]
            for q, col in enumerate([0, 1, 2, 3 + d, 5 + d, 7 + d]):
                P.dma("sync", L[:, q, :], G.rwp[rows, col * 256:(col + 1) * 256])
            r_, v_, kk_, k_, b_, lw_ = [L[:, q, :] for q in range(6)]
            cs_ps = bank(3, 256)
            P.mm(cs_ps, BD[:], lw_)
            yield
            P.act(ee[:, 0, :], cs_ps, AF.Exp)
            P.act(ee[:, 1, :], cs_ps, AF.Exp, scale=-1.0)
            P.copy(ee[:, 2, :], cs_ps, eng="scalar")
            P.tt(ee[:, 2, :], ee[:, 2, :], lw_, ALU.subtract)
            P.act(ee[:, 2, :], ee[:, 2, :], AF.Exp)
            P.tt(tok[:, 0, :], kk_, ee[:, 2, :], ALU.mult)
            P.tt(tok[:, 1, :], r_, ee[:, 0, :], ALU.mult, eng="gpsimd")
            P.tt(tok[:, 2, :], b_, ee[:, 1, :], ALU.mult)
            P.tt(tok[:, 3, :], k_, ee[:, 1, :], ALU.mult, eng="gpsimd")
            for c in range(2):
                P.ts(btc[:, c, :], tok[:, 2, :], G.cind[:, c:c + 1], ALU.mult, eng=("gpsimd" if c else "vector"))
                P.ts(ktc[:, c, :], tok[:, 3, :], G.cind[:, c:c + 1], ALU.mult, eng=("gpsimd" if c else "vector"))
            yield
            for q in range(4):
                for h in range(4):
                    P.tr(bank(q, 128, h * 128, 0, 64), tok[:, q, h * 64:(h + 1) * 64], G.identf[:])
            yield
            for q in range(4):
                P.copy(fmh[:, q].rearrange("p h t -> p (h t)"), bank(q, 512, 0, 0, 64), eng=("scalar" if q % 2 else "vector"))
            for h in range(4):
                P.mm(bank(0, 2, 2 * h, 0, 64), lw_[:, h * 64:(h + 1) * 64], G.cind[:])
            for h in range(4):
                khT = fmh[:, 0, h, :]; btT = fmh[:, 2, h, :]
                P.mm(bank(1, 128, h * 128), btT, khT)
                P.mm(bank(2, 128, h * 128), khT, btT)
            yield
            P.act(gC[:].rearrange("p h c -> p (h c)"), bank(0, 8, 0, 0, 64), AF.Exp)
            P.tt(Q[:, 0], b4(1), bc4(m_abT), ALU.mult)
            P.tt(QT[:, 0], b4(2), bc4(m_ab), ALU.mult)
            P.tt(IQT[:], QT[:, 0], idb4, ALU.add, eng="gpsimd")
            P.tt(Z[:, 0], Q[:, 0], idb4, ALU.add, eng="gpsimd")
            for h in range(4):
                khT = fmh[:, 0, h, :]; rtT = fmh[:, 1, h, :]; btT = fmh[:, 2, h, :]; ktT = fmh[:, 3, h, :]
                P.mm(bank(3, 128, h * 128), btT, rtT)
                P.mm(bank(0, 128, h * 128), ktT, khT)
            yield
            P.tt(Aarb[:], b4(3), bc4(m_i), ALU.mult)
            P.tt(Aak[:], b4(0), bc4(m_s), ALU.mult)
            zc = 0
            for k in range(1, 6):
                a, bq = (k - 1) % 2, k % 2
                for h in range(4):
                    if k < 5:
                        P.mm(bank(0, 128, h * 128), QT[:, a, h, :], Q[:, a, h, :])
                    P.mm(bank(1, 128, h * 128), Q[:, a, h, :], QT[:, a, h, :])
                if k >= 2:
                    for h in range(4):
                        P.mm(bank(2, 128, h * 128), IQT[:, h, :], Z[:, zc, h, :])
                if k == 1:
                    for h in range(4):
                        P.mm(bank(3, 128, h * 128), fmh[:, 3, h, :], fmh[:, 1, h, :])
                yield
                if k == 1:
                    P.tt(Aark[:], b4(3), bc4(m_i), ALU.mult)
                if k < 5:
                    P.copy(Q[:, bq], b4(0), eng="scalar")
                P.tt(IQT[:], b4(1), idb4, ALU.add)
                if k < 5:
                    P.copy(QT[:, bq], b4(1))
                if k >= 2:
                    P.copy(Z[:, 1 - zc], b4(2), eng="scalar")
                    zc = 1 - zc
            for h in range(4):
                P.mm(bank(2, 128, h * 128), IQT[:, h, :], Z[:, zc, h, :])
            yield
            P.copy(Z[:, 1 - zc], b4(2), eng="scalar")
            zc = 1 - zc
            TT = Z[:, zc]
            for h in range(4):
                hc = slice(h * 64, (h + 1) * 64)
                P.mm(bank(3, 64, h * 64), TT[:, h, :], tok[:, 0, hc])
                P.mm(bank(0, 64, h * 64), Aak[:, h, :], v_[:, hc])
            yield
            P.copy(Wsb[:].rearrange("p h q -> p (h q)"), bank(3, 256), eng="scalar")
            P.ts(nAkV[:].rearrange("p h q -> p (h q)"), bank(0, 256), -1.0, ALU.mult)
            for h in range(4):
                P.mm(bank(1, 64, h * 64), TT[:, h, :], nAkV[:, h, :])
            for h in range(4):
                hc = slice(h * 64, (h + 1) * 64)
                for c in range(2):
                    P.mm(bank(2, 64, (h * 2 + c) * 64, 0, 64), Wsb[:, h, :], btc[:, c, hc])
                P.mm(bank(3, 128, h * 128, 0, 64), Wsb[:, h, :], Aarb[:, h, :])
            yield
            P.copy(Uloc[:].rearrange("p h q -> p (h q)"), bank(1, 256), eng="scalar")
            id8 = G.identf[0:64, 0:64].unsqueeze(1).to_broadcast([64, 8, 64])
            P.tt(GpT[:].rearrange("p h c q -> p (h c) q"), id8, bank(2, 512, 0, 0, 64).rearrange("p (a q) -> p a q", a=8), ALU.subtract)
            P.tt(RwT[:], fmh[:, 1], bank(3, 512, 0, 0, 64).rearrange("p (h q) -> p h q", h=4), ALU.subtract)
            for c in ([0, 1] if d == 0 else [1, 0]):
                cs_ = slice(64 * c, 64 * c + 64)
                for h in range(4):
                    hc = slice(h * 64, (h + 1) * 64)
                    yp = bank(1, 64, (c * 4 + h) * 64, 0, 64)
                    P.mm(yp, Aarb[:, h, cs_], Uloc[:, h, :], start=True, stop=False)
                    P.mm(yp, Aark[:, h, cs_], v_[:, hc], start=False, stop=False)
                    P.mm(yp, RwT[:, h, cs_], H32[:, h, :], start=False, stop=True)
                for h in range(4):
                    hc = slice(h * 64, (h + 1) * 64)
                    hp = bank(0, 64, h * 64, 0, 64)
                    P.mm(hp, btc[:, c, hc], Uloc[:, h, :], start=True, stop=False)
                    P.mm(hp, ktc[:, c, hc], v_[:, hc], start=False, stop=False)
                    P.mm(hp, GpT[:, h, c, :], H32[:, h, :], start=False, stop=True)
                yield
                P.tt(H32[:], bank(0, 256, 0, 0, 64).rearrange("p (h q) -> p h q", h=4),
                     gC[:, :, c].unsqueeze(2).to_broadcast([64, 4, 64]), ALU.mult)
            P.copy(Yt[:, b].rearrange("p c q -> p (c q)"), bank(1, 512, 0, 0, 64), eng="scalar")
            P.dma(G.stq, G.rwy[d, rows, :].rearrange("(c p) q -> p c q", p=64), Yt[:, b])
            yield

    gens = [stream(0, [0, 1, 2, 3]), stream(1, [4, 5, 6, 7])]
    while gens:
        for g in list(gens):
            try:
                next(g)
            except StopIteration:
                gens.remove(g)
    ph.close()


def rwkv_fin(nc, P, G, l):
    W = G.w
    ph = Phase(nc, P)
    lw_ = ph.sb("lnxw", [128, 256]); lb_ = ph.sb("lnxb", [128, 256])
    yf = ph.sb("ryf", [128, 2, 256]); yb = ph.sb("ryb", [128, 2, 256]); gb = ph.sb("rgb", [128, 2, 2, 256])
    sq = ph.sb("rsq", [128, 256]); s4 = ph.sb("rfs4", [128, 2, 8])
    P.dma("sync", lw_[:], bc_rows(W["rwkv_lnx_w"][l:l + 1, :]))
    P.dma("sync", lb_[:], bc_rows(W["rwkv_lnx_b"][l:l + 1, :]))
    for i in range(NT):
        b = i % 2
        rows = slice(i * 128, (i + 1) * 128)
        P.dma("sync", yf[:, b, :], G.rwy[0, rows, :])
        P.dma("sync", yb[:, b, :], G.rwy[1, rows, :])
        P.dma("sync", gb[:, b].rearrange("p a b -> p (a b)"), G.rwp[rows, 9 * 256:11 * 256])
        y = yf[:, b, :]
        y3 = y.rearrange("p (h q) -> p h q", h=4)
        S = s4[:, b, :]
        P.tt(y, y, yb[:, b, :], ALU.add)
        P.op("vector", lambda e, y3=y3, S=S: e.tensor_reduce(S[:, 0:4], y3, AX.X, ALU.add), reads=[y], writes=[S[:, 0:4]])
        P.ts(S[:, 0:4], S[:, 0:4], 1.0 / 64, ALU.mult)
        P.tt(y3, y3, S[:, 0:4].unsqueeze(2).to_broadcast([128, 4, 64]), ALU.subtract)
        P.tt(sq[:], y, y, ALU.mult, eng="gpsimd")
        P.op("vector", lambda e, S=S: e.tensor_reduce(S[:, 4:8], sq[:].rearrange("p (h q) -> p h q", h=4), AX.X, ALU.add),
             reads=[sq[:]], writes=[S[:, 4:8]])
        P.act(S[:, 4:8], S[:, 4:8], AF.Sqrt, bias=64e-5, scale=1.0 / 64)
        P.op("vector", lambda e, S=S: e.reciprocal(S[:, 4:8], S[:, 4:8]), reads=[S[:, 4:8]], writes=[S[:, 4:8]])
        P.tt(y3, y3, S[:, 4:8].unsqueeze(2).to_broadcast([128, 4, 64]), ALU.mult)
        P.tt(y, y, lw_[:], ALU.mult, eng="gpsimd")
        P.tt(y, y, lb_[:], ALU.add)
        P.tt(y, y, gb[:, b, 1, :], ALU.add)
        P.tt(y, y, gb[:, b, 0, :], ALU.mult)
        P.dma(G.stq, G.ymix[rows, 512:768], y)
    ph.close()
def tiles_for(l, n_layers_total=4):
    return list(range(2, NT)) if l == n_layers_total - 1 else list(range(NT))


def phase_outproj(nc, P, G, l):
    W = G.w
    ph = Phase(nc, P)
    psbf = G.psf.bitcast(BF16)
    woutb = ph.sb("woutb", [128, 8, 1024], BF16); wst = ph.sb("owst", [128, 8, 512])
    rw32 = ph.sb("rw32", [128, 8, 64]); rbias = ph.sb("rbias", [128, 64])
    mg1 = ph.sb("mg1", [128, 2, 1024]); msc2 = ph.sb("msc2", [128, 2, 1024]); msh2 = ph.sb("msh2", [128, 2, 1024])
    l1w = ph.sb("l1w", [128, 1024]); l1b = ph.sb("l1b", [128, 1024])
    for n in range(2):
        P.dma("sync", wst[:], W["w_out"][l, :, n * 512:(n + 1) * 512].rearrange("(kc p) j -> p kc j", p=128))
        P.copy(woutb[:, :, n * 512:(n + 1) * 512], wst[:], eng="gpsimd")
    P.dma("sync", rw32[:], W["router_w"][l].rearrange("(kc p) e -> p kc e", p=128))
    P.dma("sync", rbias[:], bc_rows(W["router_b"][l:l + 1, :]))
    for s in range(2):
        P.dma("sync", mg1[:, s, :], G.modd[s, 2])
        P.dma("sync", msc2[:, s, :], G.modd[s, 4])
        P.dma("sync", msh2[:, s, :], G.modd[s, 3])
    P.dma("sync", l1w[:], bc_rows(W["ln1_w"][l:l + 1, :]))
    P.dma("sync", l1b[:], bc_rows(W["ln1_b"][l:l + 1, :]))

    def stream(sid, tiles, kb):
        sfx = str(sid)
        ym = ph.sb("oym" + sfx, [128, 1024]); ymb = ph.sb("oymb" + sfx, [128, 1024], BF16); ymT = ph.sb("oymT" + sfx, [128, 8, 128], BF16)
        xt = ph.sb("oxt" + sfx, [128, 1024]); t = ph.sb("ot" + sfx, [128, 1024]); hh = ph.sb("ohh" + sfx, [128, 1024])
        hT32 = ph.sb("hT32" + sfx, [128, 8, 128]); hTb = ph.sb("hTb" + sfx, [128, 8, 128], BF16)
        st6 = ph.sb("ost6" + sfx, [128, 2, 6]); mv = ph.sb("omv" + sfx, [128, 2]); rstd = ph.sb("orstd" + sfx, [128, 1])
        sc = ph.sb("rsc" + sfx, [128, 64]); sel = ph.sb("rsel" + sfx, [128, 64]); m8 = ph.sb("rm8" + sfx, [128, 8, 8]); gs = ph.sb("rgs" + sfx, [128, 8])
        g8 = ph.sb("rg8" + sfx, [128, 8]); gm = ph.sb("rgm" + sfx, [128, 8]); pen = ph.sb("rpen" + sfx, [128, 8]); s8 = ph.sb("rs8" + sfx, [128, 8])
        selm = ph.sb("rselm" + sfx, [128, 64]); wts = ph.sb("rwts" + sfx, [128, 64]); den = ph.sb("rden" + sfx, [128, 1]); gT = ph.sb("rgT" + sfx, [128, 64])

        def bank(k, w=512, o=0):
            return G.psf[:, kb[k] * 512 + o:kb[k] * 512 + o + w]
        for i in tiles:
            s = seg_of(i)
            rows = slice(i * 128, (i + 1) * 128)
            P.dma("sync", ym[:], G.ymix[rows, :])
            P.dma("sync", xt[:], G.xres[rows, :])
            P.copy(ymb[:], ym[:], eng="gpsimd")
            for kc in range(8):
                P.tr(psbf[:, kb[2] * 1024 + kc * 128:kb[2] * 1024 + (kc + 1) * 128], ymb[:, kc * 128:(kc + 1) * 128], G.identb[:])
            yield
            P.copy(ymT[:].rearrange("p a b -> p (a b)"), psbf[:, kb[2] * 1024:kb[2] * 1024 + 1024], eng="scalar")
            for n in range(2):
                pso = bank(n)
                for kc in range(8):
                    P.mm(pso, ymT[:, kc, :], woutb[:, kc, n * 512:(n + 1) * 512], start=(kc == 0), stop=(kc == 7))
            yield
            for n in range(2):
                P.tt(t[:, n * 512:(n + 1) * 512], bank(n), mg1[:, s, n * 512:(n + 1) * 512], ALU.mult)
            x = xt[:]
            P.stt(t[:], x, DN_ALPHA, t[:], ALU.mult, ALU.add)
            ln_stats(P, ph, t[:], "o1", st6[:], mv[:], rstd[:])
            yield
            P.ts(t[:], t[:], mv[:, 0:1], ALU.subtract, rstd[:, 0:1], ALU.mult)
            P.tt(t[:], t[:], l1w[:], ALU.mult, eng="gpsimd")
            P.tt(x, t[:], l1b[:], ALU.add)
            P.dma(G.stq, G.xres[rows, :], x)
            ln_stats(P, ph, x, "o2", st6[:], mv[:], rstd[:])
            yield
            P.ts(hh[:], x, mv[:, 0:1], ALU.subtract, rstd[:, 0:1], ALU.mult)
            P.tt(hh[:], hh[:], msc2[:, s, :], ALU.mult, eng="gpsimd")
            P.tt(hh[:], hh[:], msh2[:, s, :], ALU.add)
            if G.dbg is not None and "hdbg" in G.dbg:
                P.dma("sync", G.dbg["hdbg"][rows, :], hh[:])
            for half in range(2):
                for kk in range(4):
                    kc = half * 4 + kk
                    P.tr(bank(half, 128, kk * 128), hh[:, kc * 128:(kc + 1) * 128], G.identf[:])
            yield
            for half in range(2):
                P.copy(hT32[:, half * 4:half * 4 + 4, :].rearrange("p a b -> p (a b)"), bank(half), eng="scalar")
            P.copy(hTb[:], hT32[:], eng="gpsimd")
            P.dma(G.stq, G.hT[:, :, rows].rearrange("kc p t -> p kc t"), hTb[:])
            lg = bank(3, 64)
            for kc in range(8):
                P.mm(lg, hT32[:, kc, :], rw32[:, kc, :], start=(kc == 0), stop=(kc == 7))
            yield
            P.act(sc[:], lg, AF.Sigmoid)
            P.tt(sel[:], sc[:], rbias[:], ALU.add)
            for g in range(8):
                P.op("vector", lambda e, g=g: e.max(m8[:, g, :], sel[:, g * 8:(g + 1) * 8]), reads=[sel[:, g * 8:(g + 1) * 8]], writes=[m8[:, g, :]])
            P.tt(gs[:], m8[:, :, 0], m8[:, :, 1], ALU.add)
            P.op("vector", lambda e: e.max(g8[:], gs[:]), reads=[gs[:]], writes=[g8[:]])
            P.ts(gm[:], gs[:], g8[:, 3:4], ALU.is_ge)
            P.ts(pen[:], gm[:], 1.0, ALU.subtract, 1e30, ALU.mult)
            sel3 = sel[:].rearrange("p (g q) -> p g q", g=8)
            selm3 = selm[:].rearrange("p (g q) -> p g q", g=8)
            P.tt(selm3, sel3, gm[:].unsqueeze(2).to_broadcast([128, 8, 8]), ALU.mult)
            P.tt(selm3, selm3, pen[:].unsqueeze(2).to_broadcast([128, 8, 8]), ALU.add)
            yield
            P.op("vector", lambda e: e.max(s8[:], selm[:]), reads=[selm[:]], writes=[s8[:]])
            P.ts(wts[:], selm[:], s8[:, 7:8], ALU.is_ge)
            P.tt(wts[:], wts[:], sc[:], ALU.mult)
            P.op("vector", lambda e: e.tensor_reduce(den[:], wts[:], AX.X, ALU.add), reads=[wts[:]], writes=[den[:]])
            P.op("vector", lambda e: e.reciprocal(den[:], den[:]), reads=[den[:]], writes=[den[:]])
            P.ts(wts[:], wts[:], den[:, 0:1], ALU.mult, 2.5, ALU.mult)
            P.copy(gT[:], wts[:], eng="gpsimd")
            P.dma(G.stq, G.gtok[rows, :], gT[:])
            yield

    tl = tiles_for(l)
    run_streams([stream(0, tl[0::2], [0, 1, 2, 3]), stream(1, tl[1::2], [4, 5, 6, 7])])
    ph.close()


def phase_moe(nc, P, G, l):
    W = G.w
    tl = tiles_for(l)
    half = (len(tl) + 1) // 2
    for (pa, tiles) in enumerate((tl[:half], tl[half:])):
        ph = Phase(nc, P)
        ntl = len(tiles)
        ntok = ntl * 128
        t0 = tiles[0] * 128
        acc = ph.sb("macc", [128, ntl, 1024])
        hTp = ph.sb("mhT", [128, 8, ntok], BF16); gk = ph.sb("mgk", [128, ntl, 64])
        w13s = ph.sb("w13s", [128, 8, 512]); w13b = ph.sb("w13b", [128, 2, 8, 512], BF16)
        w2s = ph.sb("w2s", [128, 2, 1024]); w2b = ph.sb("w2b", [128, 2, 2, 1024], BF16)
        hid = ph.sb("mhid", [128, 2, 2, 512], BF16); sil = ph.sb("msil", [128, 2, 512])
        P.dma("sync", hTp[:], G.hT[:, :, t0:t0 + ntok].rearrange("kc p t -> p kc t"))
        P.dma("sync", gk[:], G.gtok[t0:t0 + ntok, :].rearrange("(k p) e -> p k e", p=128))
        groups = []
        c0 = 0
        while c0 < ntok:
            n = min(512, ntok - c0)
            groups.append((c0, n)); c0 += n
        NE = 65
        orot = [0]
        pending = [None]

        def emit_down(e, eb, hb, c0, n):
            for sub in range(n // 128):
                ti = (c0 // 128) + sub
                for cc in range(2):
                    ob = 4 + (orot[0] % 4)
                    orot[0] += 1
                    o = G.psf[:, ob * 512:(ob + 1) * 512]
                    for fc in range(2):
                        P.mm(o, hid[:, hb, fc, sub * 128:(sub + 1) * 128], w2b[:, eb, fc, cc * 512:(cc + 1) * 512],
                             start=(fc == 0), stop=(fc == 1))
                    a = acc[:, ti, cc * 512:(cc + 1) * 512]
                    if e == 0:
                        P.ts(a, o, gk[:, ti, 0:1], ALU.mult)
                    elif e < 64:
                        P.stt(a, o, gk[:, ti, e:e + 1], a, ALU.mult, ALU.add)
                    else:
                        P.tt(a, a, o, ALU.add)
        for e in range(NE):
            eb = e % 2
            if e < 64:
                s1, s3, s2 = W["exp_w1"][l, e], W["exp_w3"][l, e], W["exp_w2"][l, e]
            else:
                s1, s3, s2 = W["sh_w1"][l], W["sh_w3"][l], W["sh_w2"][l]
            import os
            if e < 2 or not os.environ.get("KNOW"):
                P.dma("sync", w13s[:, :, 0:256], s1.rearrange("(kc p) f -> p kc f", p=128))
                P.dma("sync", w13s[:, :, 256:512], s3.rearrange("(kc p) f -> p kc f", p=128))
                P.dma("sync", w2s[:], s2.rearrange("(fc p) c -> p fc c", p=128))
                P.copy(w13b[:, eb], w13s[:], eng="gpsimd")
                P.copy(w2b[:, eb], w2s[:], eng="gpsimd")
            for gi, (c0, n) in enumerate(groups):
                hb = (e * len(groups) + gi) % 2
                cols = slice(c0, c0 + n)
                for fc in range(2):
                    h1 = G.psf[:, fc * 512:fc * 512 + n]
                    h3 = G.psf[:, (2 + fc) * 512:(2 + fc) * 512 + n]
                    for kc in range(8):
                        P.mm(h1, w13b[:, eb, kc, fc * 128:(fc + 1) * 128], hTp[:, kc, cols], start=(kc == 0), stop=(kc == 7))
                    for kc in range(8):
                        P.mm(h3, w13b[:, eb, kc, 256 + fc * 128:256 + (fc + 1) * 128], hTp[:, kc, cols], start=(kc == 0), stop=(kc == 7))
                    P.act(sil[:, fc, 0:n], h1, AF.Silu)
                    P.tt(hid[:, hb, fc, 0:n], sil[:, fc, 0:n], h3, ALU.mult)
                if pending[0] is not None:
                    emit_down(*pending[0])
                pending[0] = (e, eb, hb, c0, n)
        emit_down(*pending[0])
        pending[0] = None
        mg2 = ph2 = None
        fin = Phase(nc, P)
        mg2 = fin.sb("mg2", [128, 2, 1024]); l2w = fin.sb("l2w", [128, 1024]); l2b = fin.sb("l2b", [128, 1024])
        xt = fin.sb("mxt", [128, 2, 1024]); st6 = fin.sb("mst6", [128, 2, 6]); mv = fin.sb("mmv", [128, 2]); rstd = fin.sb("mrstd", [128, 1])
        for s in range(2):
            P.dma("sync", mg2[:, s, :], G.modd[s, 5])
        P.dma("sync", l2w[:], bc_rows(W["ln2_w"][l:l + 1, :]))
        P.dma("sync", l2b[:], bc_rows(W["ln2_b"][l:l + 1, :]))
        for k, i in enumerate(tiles):
            b = k % 2
            s = seg_of(i)
            rows = slice(i * 128, (i + 1) * 128)
            x = xt[:, b, :]
            P.dma("sync", x, G.xres[rows, :])
            f = acc[:, k, :]
            if G.dbg is not None and "fdbg" in G.dbg:
                P.dma("sync", G.dbg["fdbg"][rows, :], f)
            P.tt(f, f, mg2[:, s, :], ALU.mult, eng="gpsimd")
            P.stt(f, x, DN_ALPHA, f, ALU.mult, ALU.add)
            ln_stats(P, fin, f, "m", st6[:], mv[:], rstd[:])
            P.ts(f, f, mv[:, 0:1], ALU.subtract, rstd[:, 0:1], ALU.mult)
            P.tt(f, f, l2w[:], ALU.mult, eng="gpsimd")
            P.tt(x, f, l2b[:], ALU.add)
            if G.dbg is not None and "xout" in G.dbg:
                P.dma("sync", G.dbg["xout"][rows, :], x)
            if l == 3 and G.final_out:
                P.dma(G.stq, G.out[(i - 2) * 128:(i - 1) * 128, :], x, final=True)
            else:
                P.dma(G.stq, G.xres[rows, :], x)
        fin.st.close()
        ph.close()
def alloc_scratch(nc, G, scratch):
    G.xbca = scratch("xbca", [T_ALL, 1024])
    G.dtla = scratch("dtla", [T_ALL, 32])
    G.ydir = scratch("ydir", [2, T_ALL, 512])
    G.gla_la = scratch("gla_la", [T_ALL, 256])
    G.gla_o = scratch("gla_o", [2, T_ALL, 256])
    G.rwp = scratch("rwp", [T_ALL, 11 * 256])
    G.rwy = scratch("rwy", [2, T_ALL, 256])
    G.hT = scratch("hT", [8, 128, T_ALL], BF16)
    G.gtok = scratch("gtok", [T_ALL, 64])

PHASES = {"mod": phase_mod, "inproj": phase_inproj, "ssd": phase_ssd, "ssd_prep": ssd_prep, "ssd_dir": ssd_dir, "ssd_fin": ssd_fin,
          "gla": phase_gla, "rwkv": phase_rwkv, "rwkv_prep": rwkv_prep, "rwkv_dir": rwkv_dir, "rwkv_fin": rwkv_fin, "outproj": phase_outproj, "moe": phase_moe}
WNAMES = ["ada_w", "ada_b", "w_in", "ssd_conv_w", "ssd_conv_b", "ssd_dt_bias", "ssd_a_log", "ssd_d", "ssd_norm_w",
          "rwkv_mu", "rwkv_w0", "rwkv_w2", "rwkv_a0", "rwkv_a2", "rwkv_g2", "rwkv_kk", "rwkv_ka", "rwkv_rk",
          "rwkv_lnx_w", "rwkv_lnx_b", "gla_gu", "gla_gb", "gla_norm_w", "w_out", "ln1_w", "ln1_b", "ln2_w", "ln2_b",
          "router_w", "router_b", "exp_w1", "exp_w3", "exp_w2", "sh_w1", "sh_w3", "sh_w2"]
WSHAPES = {"ada_w": [4, 1024, 6144], "ada_b": [4, 6144], "w_in": [4, 1024, 3504], "ssd_conv_w": [4, 3, 1024],
           "ssd_conv_b": [4, 1024], "ssd_dt_bias": [4, 2, 8], "ssd_a_log": [4, 2, 8], "ssd_d": [4, 8],
           "ssd_norm_w": [4, 512], "rwkv_mu": [4, 4, 1152], "rwkv_w0": [4, 2, 256], "rwkv_w2": [4, 2, 64, 256],
           "rwkv_a0": [4, 2, 256], "rwkv_a2": [4, 2, 64, 256], "rwkv_g2": [4, 128, 256], "rwkv_kk": [4, 256],
           "rwkv_ka": [4, 256], "rwkv_rk": [4, 4, 64], "rwkv_lnx_w": [4, 256], "rwkv_lnx_b": [4, 256],
           "gla_gu": [4, 2, 16, 128], "gla_gb": [4, 2, 128], "gla_norm_w": [4, 256], "w_out": [4, 1024, 1024],
           "ln1_w": [4, 1024], "ln1_b": [4, 1024], "ln2_w": [4, 1024], "ln2_b": [4, 1024],
           "router_w": [4, 1024, 64], "router_b": [4, 64], "exp_w1": [4, 64, 1024, 256], "exp_w3": [4, 64, 1024, 256],
           "exp_w2": [4, 64, 256, 1024], "sh_w1": [4, 1024, 256], "sh_w3": [4, 1024, 256], "sh_w2": [4, 256, 1024]}


def build_program(layers=(0, 1, 2, 3), phases=None, dbg=(), wnames=None, feed=()):
    nc = bass.Bass("TRN2", target_bir_lowering=False)
    P = Prog(nc)
    G = Ctx()
    G.w = {}
    import os as _os
    G.stq = _os.environ.get('KSTQ', 'gpsimd')
    import os
    G.stop = float(os.environ.get('KSTOP', '99'))
    for n in (wnames or WNAMES):
        G.w[n] = nc.dram_tensor(n, WSHAPES[n], F32, kind="ExternalInput").ap()
    G.x_in = nc.dram_tensor("x", [4096, 1024], F32, kind="ExternalInput").ap()
    G.ctx_in = nc.dram_tensor("ctx", [256, 1024], F32, kind="ExternalInput").ap()
    G.cvec = nc.dram_tensor("cvec", [2, 1024], F32, kind="ExternalInput").ap()
    G.out = nc.dram_tensor("out", [4096, 1024], F32, kind="ExternalOutput").ap()
    DBG_SHAPES = {"u": ([T_ALL, 1024], BF16), "modd": ([2, 6, 128, 1024], F32), "zin": ([T_ALL, NIN], F32),
                  "ymix": ([T_ALL, 1024], F32), "xres": ([T_ALL, 1024], F32)}
    G.dbg = {}
    G.final_out = (phases is None or "moe" in phases)

    def scratch(name, shape, dt=F32):
        kind = "ExternalOutput" if name in dbg else ("ExternalInput" if name in feed else "Internal")
        t = nc.dram_tensor(name, list(shape), dt, kind=kind).ap()
        return t
    for n in dbg:
        if n in ("u",):
            G.dbg[n] = nc.dram_tensor(n, DBG_SHAPES[n][0], DBG_SHAPES[n][1], kind="ExternalOutput").ap()
        if n in ("hdbg", "fdbg", "xout"):
            G.dbg[n] = nc.dram_tensor(n, [T_ALL, 1024], F32, kind="ExternalOutput").ap()
    G.xres = scratch("xres", [T_ALL, 1024])
    G.modd = scratch("modd", [2, 6, 128, 1024])
    G.zin = scratch("zin", [T_ALL, NIN])
    G.ymix = scratch("ymix", [T_ALL, 1024])
    G.psf = nc.alloc_psum_tensor("psa", [128, 8 * 512], F32)
    G.psb = G.psf.bitcast(BF16)[:, 6 * 1024:8 * 1024]
    make_consts(nc, P, G)
    alloc_scratch(nc, G, scratch)
    if "xres" not in feed:
        P.dma("sync", G.xres[0:256, :], G.ctx_in)
        P.dma("sync", G.xres[256:T_ALL, :], G.x_in)
    P.barrier()
    allp = ["mod", "inproj", "ssd", "gla", "rwkv", "outproj", "moe"]
    for l in layers:
        for pn in (phases or allp):
            PHASES[pn](nc, P, G, l)
    if phases is None or "moe" in phases:
        pass
    else:
        pass
    P.barrier()
    P.dma("sync", G.out[0:128, 0:8], G.xres[0:128, 0:8], final=True) if (phases is not None and "moe" not in phases) else None
    P.finish()
    return nc, P


_NC_CACHE = {}


def kernel(**inputs):
    from concourse.bass_utils import run_bass_kernel_spmd
    if "nc" not in _NC_CACHE:
        nc, P = build_program()
        _NC_CACHE["nc"] = nc
    nc = _NC_CACHE["nc"]
    B = 4
    base = {n: np.ascontiguousarray(np.asarray(inputs[n], dtype=np.float32)) for n in WNAMES}
    x = np.asarray(inputs["x"], dtype=np.float32)
    ctx = np.asarray(inputs["ctx"], dtype=np.float32)
    c = np.asarray(inputs["c"], dtype=np.float32)
    c_ctx = np.asarray(inputs["c_ctx"], dtype=np.float32)
    in_maps = []
    for b in range(B):
        m = dict(base)
        m["x"] = np.ascontiguousarray(x[b])
        m["ctx"] = np.ascontiguousarray(ctx[b])
        m["cvec"] = np.ascontiguousarray(np.stack([c_ctx, c[b]]))
        in_maps.append(m)
    res = run_bass_kernel_spmd(nc, in_maps, core_ids=list(range(B)))
    out = np.stack([np.asarray(res.results[b]["out"], dtype=np.float32) for b in range(B)])
    return out
```

```python
import numpy as np
import concourse.bass as bass
import concourse.mybir as mybir

F32 = mybir.dt.float32
BF16 = mybir.dt.bfloat16
I32 = mybir.dt.int32
U32 = mybir.dt.uint32
AF = mybir.ActivationFunctionType
ALU = mybir.AluOpType
AX = mybir.AxisListType

_DSZ = {F32: 4, BF16: 2, I32: 4, U32: 4}

ENGS = ("tensor", "vector", "scalar", "gpsimd", "sync")
DMA_K = {"sync": 16, "gpsimd": 8, "scalar": 4}


def _box(ap):
    t = ap.tensor
    name = t.name
    dsz = _DSZ.get(ap.dtype, 4)
    steps = list(ap.ap)
    off = ap.offset
    space = str(ap.space) if hasattr(ap, "space") else ""
    if "DRAM" in space.upper() or "dram" in space.lower() or type(t).__name__.startswith("DRam"):
        lo = off + sum(min(0, s * (c - 1)) for s, c in steps)
        hi = off + sum(max(0, s * (c - 1)) for s, c in steps) + 1
        return name, 0, 1, lo * dsz, hi * dsz
    pstep, pcnt = steps[0]
    if pstep == 0:
        pstep = 1 << 30
    p_lo = off // pstep
    f_off = off - p_lo * pstep
    rest = steps[1:]
    lo = f_off + sum(min(0, s * (c - 1)) for s, c in rest)
    hi = f_off + sum(max(0, s * (c - 1)) for s, c in rest) + 1
    lo *= dsz
    hi *= dsz
    if name.startswith("ps"):
        lo = (lo // 2048) * 2048
        hi = ((hi + 2047) // 2048) * 2048
        p_lo, pcnt = 0, 128
    return name, p_lo, p_lo + pcnt, lo, hi


class Prog:
    def __init__(self, nc):
        self.nc = nc
        self.ops = {e: [] for e in ENGS}
        self.cnt = {e: 0 for e in ENGS}
        self.dcnt = {e: 0 for e in DMA_K}
        self.waited = {e: {} for e in ENGS}
        self.recs = {}
        self.semkeys = set()
        self.final_tokens = []
        self.n_ops = 0

    def _deps(self, reads, writes, pe_accum=False, eng=None):
        deps = set()
        rb = [_box(a) for a in reads]
        wb = [_box(a) for a in writes]
        for (name, p0, p1, f0, f1) in rb:
            isps = name.startswith("ps")
            for r in self.recs.get(name, ()):
                if r[0] < p1 and p0 < r[1] and r[2] < f1 and f0 < r[3]:
                    if r[4] == "w" or (isps and r[5][0] != eng):
                        deps.add(r[5])
        for (name, p0, p1, f0, f1) in wb:
            for r in self.recs.get(name, ()):
                if r[0] < p1 and p0 < r[1] and r[2] < f1 and f0 < r[3]:
                    if r[4] == "w" and r[5][0] == "tensor" and eng == "tensor":
                        continue
                    deps.add(r[5])
        return deps, rb, wb

    def _update(self, rb, wb, tok):
        for (name, p0, p1, f0, f1) in wb:
            lst = self.recs.setdefault(name, [])
            lst[:] = [r for r in lst if not (p0 <= r[0] and r[1] <= p1 and f0 <= r[2] and r[3] <= f1)]
            lst.append((p0, p1, f0, f1, "w", tok))
        for (name, p0, p1, f0, f1) in rb:
            lst = self.recs.setdefault(name, [])
            lst[:] = [r for r in lst if not (r[4] == "r" and r[5][0] == tok[0] and p0 <= r[0] and r[1] <= p1
                                             and f0 <= r[2] and r[3] <= f1)]
            lst.append((p0, p1, f0, f1, "r", tok))

    def _waits(self, eng, deps):
        w = self.waited[eng]
        best = {}
        for (k, v) in deps:
            if v > best.get(k, 0):
                best[k] = v
        out = []
        for k, v in best.items():
            if w.get(k, 0) >= v:
                continue
            w[k] = v
            out.append((k, v))
        return out

    def op(self, eng, fn, reads=(), writes=(), pe_accum=False):
        deps, rb, wb = self._deps(reads, writes, pe_accum, eng)
        self.cnt[eng] += 1
        tok = (eng, self.cnt[eng])
        waits = self._waits(eng, deps)
        self.semkeys.add(eng)
        self.ops[eng].append((waits, fn, (eng, 1)))
        self._update(rb, wb, tok)
        self.n_ops += 1
        return tok

    def dma(self, q, out, in_, final=False, **kw):
        deps, rb, wb = self._deps([in_], [out])
        n = self.dcnt[q]
        self.dcnt[q] += 1
        K = DMA_K[q]
        key = ("dma", q, n % K)
        tok = (key, 16 * (n // K + 1))
        if n >= K:
            deps.add((key, 16 * (n // K)))
        waits = self._waits(q, deps)
        self.semkeys.add(key)
        self.ops[q].append((waits, (lambda e, o=out, i=in_, kw=kw: e.dma_start(out=o, in_=i, **kw)), (key, 16)))
        self._update(rb, wb, tok)
        if final:
            self.final_tokens.append(tok)
        self.n_ops += 1
        return tok

    def barrier(self):
        toks = set()
        for e in ENGS:
            if self.cnt[e] > 0:
                toks.add((e, self.cnt[e]))
        for q, K in DMA_K.items():
            n = self.dcnt[q]
            for j in range(min(n, K)):
                last = ((n - 1 - j) // K) * K + j
                toks.add((("dma", q, j), 16 * (last // K + 1)))
        for e in ENGS:
            waits = self._waits(e, set(toks))
            if waits:
                self.ops[e].append((waits, None, None))
        self.recs.clear()

    def mm(self, out, lhsT, rhs, start=True, stop=True):
        return self.op("tensor", lambda e: e.matmul(out, lhsT, rhs, start=start, stop=stop),
                       reads=[lhsT, rhs], writes=[out], pe_accum=not start)

    def tr(self, out, in_, ident):
        return self.op("tensor", lambda e: e.transpose(out, in_, ident), reads=[in_, ident], writes=[out])

    def act(self, out, in_, func, bias=None, scale=None, eng="scalar", accum_out=None):
        reads = [in_]
        kw = {}
        if bias is not None:
            kw["bias"] = bias
            if not isinstance(bias, (int, float)):
                reads.append(bias)
        if scale is not None:
            kw["scale"] = scale
            if not isinstance(scale, (int, float)):
                reads.append(scale)
        writes = [out]
        if accum_out is not None:
            kw["accum_out"] = accum_out
            writes.append(accum_out)
        return self.op(eng, lambda e: e.activation(out, in_, func, **kw), reads=reads, writes=writes)

    def tt(self, out, in0, in1, op, eng="vector"):
        return self.op(eng, lambda e: e.tensor_tensor(out, in0, in1, op), reads=[in0, in1], writes=[out])

    def ts(self, out, in0, s1, op0, s2=None, op1=None, eng="vector", accum_out=None):
        reads = [in0]
        if not isinstance(s1, (int, float)):
            reads.append(s1)
        if s2 is not None and not isinstance(s2, (int, float)):
            reads.append(s2)
        writes = [out]
        kw = {}
        if accum_out is not None:
            kw["accum_out"] = accum_out
            writes.append(accum_out)
        if op1 is None:
            return self.op(eng, lambda e: e.tensor_scalar(out, in0, s1, None, op0, **kw), reads=reads, writes=writes)
        return self.op(eng, lambda e: e.tensor_scalar(out, in0, s1, s2, op0, op1, **kw), reads=reads, writes=writes)

    def stt(self, out, in0, scalar, in1, op0, op1, eng="vector"):
        reads = [in0, in1]
        if not isinstance(scalar, (int, float)):
            reads.append(scalar)
        return self.op(eng, lambda e: e.scalar_tensor_tensor(out, in0, scalar, in1, op0, op1), reads=reads, writes=[out])

    def copy(self, out, in_, eng="vector"):
        if eng == "scalar":
            return self.op(eng, lambda e: e.copy(out, in_), reads=[in_], writes=[out])
        return self.op(eng, lambda e: e.tensor_copy(out, in_), reads=[in_], writes=[out])

    def memset(self, out, val, eng="vector"):
        return self.op(eng, lambda e: e.memset(out, val), writes=[out])

    def finish(self):
        nc = self.nc
        sems = {}
        import contextlib
        with contextlib.ExitStack() as st:
            for k in sorted(self.semkeys, key=str):
                nm = "s_" + "_".join(str(x) for x in (k if isinstance(k, tuple) else (k,)))
                sems[k] = st.enter_context(nc.semaphore(nm))
            block = st.enter_context(nc.Block())
            fin = self._waits("sync", set(self.final_tokens))
            ops = self.ops

            def make(engname, extra=None):
                def body(e):
                    for waits, fn, inc in ops[engname]:
                        for (k, v) in waits:
                            e.wait_ge(sems[k], v)
                        if fn is None:
                            continue
                        ins = fn(e)
                        ins.then_inc(sems[inc[0]], inc[1])
                    if extra:
                        for (k, v) in extra:
                            e.wait_ge(sems[k], v)
                return body

            block.tensor(make("tensor"))
            block.vector(make("vector"))
            block.scalar(make("scalar"))
            block.gpsimd(make("gpsimd"))
            block.sync(make("sync", fin))
        return nc
import contextlib

T_ALL = 4352
NT = 34
D = 1024
NIN = 3504
LN_EPS = 1e-5
DN_ALPHA = 8 ** 0.25
NEGM = -30000.0

C_Z = 0
C_XBC = 512
C_DT = 1536
C_RW = 1552
C_GQ = 2704
C_GK = 2832
C_GV = 2960
C_GD = 3216
C_OG = 3248


def seg_of(i):
    return 0 if i < 2 else 1


class Phase:
    _n = [0]

    def __init__(self, nc, P):
        self.nc = nc
        self.P = P
        self.st = contextlib.ExitStack()

    def sb(self, name, shape, dt=F32):
        Phase._n[0] += 1
        return self.st.enter_context(self.nc.sbuf_tensor(f"{name}_{Phase._n[0]}", list(shape), dt))

    def close(self):
        self.P.barrier()
        self.st.close()


def bc_rows(ap2d_row, n=128):
    steps = list(ap2d_row.ap)
    last = steps[-1]
    return bass.AP(ap2d_row.tensor, ap2d_row.offset, [[0, n], [last[0], last[1]]])


class Ctx:
    pass


def make_consts(nc, P, G):
    def sbp(name, shape, dt=F32):
        return nc.alloc_sbuf_tensor(name, list(shape), dt)
    G.identf = sbp("identf", [128, 128])
    G.identb = sbp("identb", [128, 128], BF16)
    G.onesf = sbp("onesf", [128, 128])
    G.tri = [sbp("tri_f", [128, 128]), sbp("tri_b", [128, 128])]
    G.negm = [sbp("negm_f", [128, 4, 128]), sbp("negm_b", [128, 4, 128])]
    G.trib = [sbp("trib_f", [128, 128], BF16), sbp("trib_b", [128, 128], BF16)]
    G.SL = sbp("m_SL", [128, 128]); G.SU = sbp("m_SU", [128, 128]); G.IU = sbp("m_IU", [128, 128]); G.IL = sbp("m_IL", [128, 128])
    gp = "gpsimd"
    G.nSU = sbp("m_nSU", [128, 128]); G.nSL = sbp("m_nSL", [128, 128])
    G.cind = sbp("cind", [128, 2]); G.cmask = sbp("cmask", [128, 2])
    G.hm4 = sbp("hm4", [128, 4])
    P.memset(G.hm4[:], 1.0, eng=gp)
    P.op(gp, lambda e: e.affine_select(G.hm4[:], G.hm4[:], [[-32, 4]], ALU.is_ge, 0.0, base=0, channel_multiplier=1),
         reads=[G.hm4[:]], writes=[G.hm4[:]])
    P.op(gp, lambda e: e.affine_select(G.hm4[:], G.hm4[:], [[32, 4]], ALU.is_ge, 0.0, base=31, channel_multiplier=-1),
         reads=[G.hm4[:]], writes=[G.hm4[:]])

    def asel(t, cm, step, cmp, base=0):
        P.op(gp, lambda e: e.affine_select(t, t, [[step, 128]], cmp, 0.0, base=base, channel_multiplier=cm),
             reads=[t], writes=[t])
    for t in (G.identf, G.onesf, G.tri[0], G.tri[1], G.SL, G.SU, G.IU, G.IL):
        P.memset(t[:], 1.0, eng=gp)
    asel(G.identf[:], -1, 1, ALU.is_equal)
    asel(G.tri[0][:], -1, 1, ALU.is_ge)
    asel(G.tri[1][:], 1, -1, ALU.is_ge)
    asel(G.IU[:], -1, 1, ALU.is_ge)
    asel(G.IL[:], 1, -1, ALU.is_ge)
    asel(G.SU[:], -1, 1, ALU.is_gt)
    asel(G.SL[:], 1, -1, ALU.is_gt)
    for t in (G.SL, G.SU, G.IU, G.IL):
        P.memset(t[0:64, 64:128], 0.0, eng=gp)
        P.memset(t[64:128, 0:64], 0.0, eng=gp)
    P.ts(G.nSU[:], G.SU[:], -1.0, ALU.mult, eng=gp)
    P.ts(G.nSL[:], G.SL[:], -1.0, ALU.mult, eng=gp)
    P.memset(G.cind[:], 0.0, eng=gp)
    P.memset(G.cind[0:64, 0:1], 1.0, eng=gp)
    P.memset(G.cind[64:128, 1:2], 1.0, eng=gp)
    P.memset(G.cmask[:], 1.0, eng=gp)
    for (col, base) in ((0, 0), (0, -64), (1, -63), (1, -127)):
        P.op(gp, lambda e, col=col, base=base: e.affine_select(G.cmask[:, col:col + 1], G.cmask[:, col:col + 1], [[0, 1]],
                                                             ALU.not_equal, 0.0, base=base, channel_multiplier=1),
             reads=[G.cmask[:, col:col + 1]], writes=[G.cmask[:, col:col + 1]])
    P.copy(G.identb[:], G.identf[:])
    for d in range(2):
        P.copy(G.trib[d][:], G.tri[d][:])
        for h in range(4):
            P.ts(G.negm[d][:, h, :], G.tri[d][:], 1.0, ALU.subtract, -NEGM, ALU.mult)


def ln_stats(P, ph, x_ap, tag, st6, mv, rstd, eps=LN_EPS):
    for hh in range(2):
        P.op("vector", lambda e, hh=hh: e.bn_stats(st6[:, hh, :], x_ap[:, hh * 512:(hh + 1) * 512]),
             reads=[x_ap[:, hh * 512:(hh + 1) * 512]], writes=[st6[:, hh, :]])
    P.op("vector", lambda e: e.bn_aggr(mv, st6), reads=[st6], writes=[mv])
    P.act(rstd, mv[:, 1:2], AF.Sqrt, bias=eps)
    P.op("vector", lambda e: e.reciprocal(rstd, rstd), reads=[rstd], writes=[rstd])


def phase_mod(nc, P, G, l):
    ph = Phase(nc, P)
    cT = ph.sb("cT", [128, 2, 8]); csil = ph.sb("csil", [128, 2, 8])
    wst = ph.sb("adaw", [128, 2, 8, 512]); brow = ph.sb("brow", [1, 6144]); mst = ph.sb("mst", [128, 2, 2, 512])
    for s in range(2):
        P.dma("sync", cT[:, s, :], G.cvec[s, :].rearrange("(kc p) -> p kc", p=128), allow_slow_non_contiguous=True)
    P.act(csil[:], cT[:], AF.Silu)
    P.dma("sync", brow[:], G.w["ada_b"][l:l + 1, :])
    for n in range(12):
        b = n % 2
        P.dma("sync", wst[:, b], G.w["ada_w"][l, :, n * 512:(n + 1) * 512].rearrange("(kc p) j -> p kc j", p=128))
        for s in range(2):
            pso = G.psf[:, (n % 2 * 2 + s) * 512:(n % 2 * 2 + s + 1) * 512]
            for kc in range(8):
                P.mm(pso, csil[:, s, kc:kc + 1].to_broadcast([128, 128]), wst[:, b, kc, :], start=(kc == 0), stop=False)
            P.mm(pso, G.onesf[0:1, :], brow[0:1, n * 512:(n + 1) * 512], start=False, stop=True)
            which = n // 2
            if which in (1, 4):
                P.act(mst[:, b, s, :], pso, AF.Identity, bias=1.0)
            else:
                P.copy(mst[:, b, s, :], pso)
            P.dma(G.stq, G.modd[s, which, :, (n % 2) * 512:(n % 2 + 1) * 512], mst[:, b, s, :])
    ph.close()


def phase_inproj(nc, P, G, l):
    ph = Phase(nc, P)
    winb = ph.sb("winb", [128, 8, NIN], BF16)
    wst = ph.sb("wst", [128, 2, 8, 512])
    msc = ph.sb("msc", [128, 2, 1024]); msh = ph.sb("msh", [128, 2, 1024])
    xt = ph.sb("xt", [128, 2, 1024]); ub = ph.sb("ub", [128, 2, 1024], BF16)
    uT = ph.sb("uT", [128, 2, 8, 128], BF16); zt = ph.sb("zt", [128, 2, NIN])
    st6 = ph.sb("st6", [128, 2, 6]); mv = ph.sb("mv", [128, 2]); rstd = ph.sb("rstd", [128, 1])
    for n in range(7):
        w = min(512, NIN - n * 512)
        P.dma("sync", wst[:, n % 2, :, 0:w], G.w["w_in"][l, :, n * 512:n * 512 + w].rearrange("(kc p) j -> p kc j", p=128))
        P.copy(winb[:, :, n * 512:n * 512 + w], wst[:, n % 2, :, 0:w], eng="gpsimd")
    for s in range(2):
        P.dma("sync", msc[:, s, :], G.modd[s, 1])
        P.dma("sync", msh[:, s, :], G.modd[s, 0])
    def stage_a(i):
        b = i % 2
        s = seg_of(i)
        x = xt[:, b, :]
        P.dma("sync", x, G.xres[i * 128:(i + 1) * 128, :])
        ln_stats(P, ph, x, "a", st6[:], mv[:], rstd[:])
        P.ts(mv[:, 1:2], mv[:, 0:1], rstd[:, 0:1], ALU.mult, -1.0, ALU.mult)
        P.act(x, x, AF.Identity, bias=mv[:, 1:2], scale=rstd[:, 0:1])
        P.tt(x, x, msc[:, s, :], ALU.mult)
        P.tt(ub[:, b, :], x, msh[:, s, :], ALU.add)
        if G.dbg is not None and "u" in G.dbg:
            P.dma("sync", G.dbg["u"][i * 128:(i + 1) * 128, :], ub[:, b, :])
        for kc in range(8):
            P.tr(G.psb[:, kc * 128:(kc + 1) * 128], ub[:, b, kc * 128:(kc + 1) * 128], G.identb[:])
        P.copy(uT[:, b].rearrange("p a b -> p (a b)"), G.psb[:, 0:1024], eng="scalar")

    def stage_b(i):
        b = i % 2
        for n in range(7):
            w = min(512, NIN - n * 512)
            bk = (i * 7 + n) % 6
            pso = G.psf[:, bk * 512:bk * 512 + w]
            for kc in range(8):
                P.mm(pso, uT[:, b, kc, :], winb[:, kc, n * 512:n * 512 + w], start=(kc == 0), stop=(kc == 7))
            P.copy(zt[:, b, n * 512:n * 512 + w], pso, eng=("scalar" if n % 2 else "vector"))
        P.dma(G.stq, G.zin[i * 128:(i + 1) * 128, :], zt[:, b, :])
    stage_a(0)
    for i in range(NT):
        if i + 1 < NT:
            stage_a(i + 1)
        stage_b(i)
    ph.close()
def dir_order(d):
    if d == 0:
        return list(range(NT))
    return [1, 0] + list(range(NT - 1, 1, -1))


import os as _os2
G_LDQ = [_os2.environ.get("KLDQ", "sync")]


def load_shift(P, dst, src2d, i, off, eng_ms="gpsimd"):
    lo, hi = (0, 256) if i < 2 else (256, T_ALL)
    r0 = i * 128 + off
    a = max(r0, lo); bnd = min(r0 + 128, hi)
    if a > r0:
        n = a - r0
        if n >= 128:
            P.memset(dst, 0.0, eng=eng_ms); return
        P.memset(dst[0:((n + 31) // 32) * 32, :], 0.0, eng=eng_ms)
    if bnd < r0 + 128:
        n = r0 + 128 - bnd
        if n >= 128:
            P.memset(dst, 0.0, eng=eng_ms); return
        st = ((128 - n) // 32) * 32
        P.memset(dst[st:128, :], 0.0, eng=eng_ms)
    P.dma(G_LDQ[0], dst[a - r0:bnd - r0, :], src2d[a:bnd, :])


def phase_ssd(nc, P, G, l):
    ssd_prep(nc, P, G, l); ssd_dir(nc, P, G, l); ssd_fin(nc, P, G, l)


def ssd_prep(nc, P, G, l):
    W = G.w
    ph = Phase(nc, P)
    cw = ph.sb("cw", [128, 3, 1024]); cb = ph.sb("cb", [128, 1024])
    dtb = ph.sb("dtb", [128, 16]); nega = ph.sb("nega", [128, 16])
    xc = ph.sb("xc", [128, 2, 1024]); xl = ph.sb("xl", [128, 2, 1024]); xr = ph.sb("xr", [128, 2, 1024])
    acc = ph.sb("acc", [128, 2, 1024]); dl = ph.sb("dl", [128, 2, 32]); dr = ph.sb("dr", [128, 2, 16])
    for j in range(3):
        P.dma("sync", cw[:, j, :], bc_rows(W["ssd_conv_w"][l, j:j + 1, :]))
    P.dma("sync", cb[:], bc_rows(W["ssd_conv_b"][l:l + 1, :]))
    P.dma("sync", dtb[:], bc_rows(W["ssd_dt_bias"][l:l + 1].rearrange("a b c -> a (b c)")))
    P.dma("sync", nega[:], bc_rows(W["ssd_a_log"][l:l + 1].rearrange("a b c -> a (b c)")))
    P.act(nega[:], nega[:], AF.Exp)
    P.ts(nega[:], nega[:], -1.0, ALU.mult)
    zx = G.zin[:, C_XBC:C_XBC + 1024]
    for i in range(NT):
        b = i % 2
        rows = slice(i * 128, (i + 1) * 128)
        P.dma("sync", xc[:, b, :], zx[rows, :])
        load_shift(P, xl[:, b, :], zx, i, -1)
        load_shift(P, xr[:, b, :], zx, i, +1)
        P.dma("sync", dr[:, b, :], G.zin[rows, C_DT:C_DT + 16])
        a = acc[:, b, :]
        P.tt(a, xc[:, b, :], cw[:, 1, :], ALU.mult)
        P.tt(xl[:, b, :], xl[:, b, :], cw[:, 0, :], ALU.mult, eng="gpsimd")
        P.tt(xr[:, b, :], xr[:, b, :], cw[:, 2, :], ALU.mult, eng="gpsimd")
        P.tt(a, a, xl[:, b, :], ALU.add)
        P.tt(a, a, xr[:, b, :], ALU.add)
        P.tt(a, a, cb[:], ALU.add)
        P.act(a, a, AF.Silu)
        P.dma(G.stq, G.xbca[rows, :], a)
        P.tt(dr[:, b, :], dr[:, b, :], dtb[:], ALU.add)
        P.act(dr[:, b, :], dr[:, b, :], AF.Exp)
        P.act(dl[:, b, 0:16], dr[:, b, :], AF.Ln, bias=1.0)
        P.tt(dl[:, b, 16:32], dl[:, b, 0:16], nega[:], ALU.mult)
        P.dma(G.stq, G.dtla[rows, :], dl[:, b, :])
    ph.close()


def run_streams(gens):
    gens = list(gens)
    while gens:
        for g in list(gens):
            try:
                next(g)
            except StopIteration:
                gens.remove(g)


def ssd_dir(nc, P, G, l, with_rwkv_prep=False):
    ph = Phase(nc, P)
    ps = G.psf
    psbf = G.psf.bitcast(BF16)

    def stream(d, kb):
        sfx = "f" if d == 0 else "b"
        xa = ph.sb("xa" + sfx, [128, 2, 1024]); dl = ph.sb("dl2" + sfx, [128, 2, 32])
        sm = ph.sb("sm" + sfx, [128, 2, 48])
        R1 = ph.sb("R1" + sfx, [128, 8, 128]); R3 = ph.sb("R3" + sfx, [128, 8, 128])
        LT = ph.sb("LT" + sfx, [128, 8, 128], BF16); MT = ph.sb("MT" + sfx, [128, 8, 128], BF16)
        bcb = ph.sb("bcb" + sfx, [128, 2, 512], BF16); bcT = ph.sb("bcT" + sfx, [128, 2, 4, 128], BF16)
        xdt = ph.sb("xdt" + sfx, [128, 2, 8, 64], BF16); xdd = ph.sb("xdd" + sfx, [128, 2, 8, 64], BF16)
        ytmp = ph.sb("ytmp" + sfx, [128, 8, 64]); yo = ph.sb("yo" + sfx, [128, 2, 512])
        hin32 = ph.sb("hin32" + sfx, [128, 512]); hinb = ph.sb("hinb" + sfx, [128, 512], BF16)

        def bank(k, w=512, o=0):
            return ps[:, kb[k] * 512 + o:kb[k] * 512 + o + w]
        P.memset(hin32[:], 0.0)
        P.memset(hinb[:], 0.0)
        tri = G.tri[d]
        for i in dir_order(d):
            b = i % 2
            rows = slice(i * 128, (i + 1) * 128)
            P.dma("sync", xa[:, b, :], G.xbca[rows, :])
            P.dma("sync", dl[:, b, :], G.dtla[rows, :])
            dt_d = dl[:, b, d * 8:(d + 1) * 8]
            la_d = dl[:, b, 16 + d * 8:16 + (d + 1) * 8]
            S = sm[:, b, :]
            acs_ps = bank(0, 8, 256); tot_ps = bank(0, 8, 264)
            P.mm(acs_ps, tri[:], la_d)
            P.mm(tot_ps, G.onesf[:], la_d)
            P.tt(R1[:], tri[:].unsqueeze(1).to_broadcast([128, 8, 128]), la_d.unsqueeze(2).to_broadcast([128, 8, 128]),
                 ALU.mult, eng="gpsimd")
            P.act(R3[:], la_d.unsqueeze(2).to_broadcast([128, 8, 128]), AF.Identity, scale=-1.0)
            P.copy(bcb[:, b, :], xa[:, b, 512:1024])
            yield
            P.copy(S[:, 0:8], acs_ps)
            P.act(S[:, 8:16], acs_ps, AF.Exp)
            P.tt(S[:, 40:48], tot_ps, S[:, 0:8], ALU.subtract)
            P.act(S[:, 16:24], S[:, 40:48], AF.Exp)
            P.act(S[:, 24:32], tot_ps, AF.Exp)
            P.tt(S[:, 32:40], dt_d, S[:, 16:24], ALU.mult)
            for q in range(4):
                P.tr(psbf[:, kb[2] * 1024 + q * 128:kb[2] * 1024 + (q + 1) * 128], bcb[:, b, q * 128:(q + 1) * 128], G.identb[:])
            for g in range(2):
                sp = bank(1)
                P.mm(sp, G.onesf[:], R1[:, 4 * g:4 * g + 4, :], start=True, stop=False)
                P.mm(sp, tri[:], R3[:, 4 * g:4 * g + 4, :], start=False, stop=False)
                P.mm(sp, G.identf[:], G.negm[d][:], start=False, stop=True)
                if g == 0:
                    yield
                    P.copy(bcT[:, b].rearrange("p a b -> p (a b)"), psbf[:, kb[2] * 1024:kb[2] * 1024 + 512], eng="scalar")
                P.act(LT[:, 4 * g:4 * g + 4, :], sp, AF.Exp)
            xs3 = xa[:, b, 0:512].rearrange("p (h q) -> p h q", h=8)
            P.tt(xdt[:, b], xs3, dt_d.unsqueeze(2).to_broadcast([128, 8, 64]), ALU.mult, eng="gpsimd")
            P.tt(xdd[:, b], xs3, S[:, 32:40].unsqueeze(2).to_broadcast([128, 8, 64]), ALU.mult, eng="gpsimd")
            for g in range(2):
                cbp = bank(0, 128, g * 128)
                P.mm(cbp, bcT[:, b, g, :], bcT[:, b, 2 + g, :])
            for g in range(2):
                P.mm(bank(3, 256, g * 256), bcT[:, b, 2 + g, :], hinb[:, g * 256:(g + 1) * 256])
            yield
            for g in range(2):
                cbp = bank(0, 128, g * 128)
                P.tt(MT[:, 4 * g:4 * g + 4, :], LT[:, 4 * g:4 * g + 4, :], cbp.unsqueeze(1).to_broadcast([128, 4, 128]), ALU.mult)
            P.tt(ytmp[:], bank(3).rearrange("p (h q) -> p h q", h=8), S[:, 8:16].unsqueeze(2).to_broadcast([128, 8, 64]), ALU.mult)
            for h in range(8):
                P.mm(bank(2, 64, h * 64), MT[:, h, :], xdt[:, b, h, :])
            for g in range(2):
                P.mm(bank(3, 256, g * 256), bcb[:, b, g * 128:(g + 1) * 128], xdd[:, b, 4 * g:4 * g + 4, :])
            yield
            P.tt(yo[:, b, :], ytmp[:].rearrange("p h q -> p (h q)"), bank(2), ALU.add)
            P.dma(G.stq, G.ydir[d, rows, :], yo[:, b, :])
            h3 = hin32[:].rearrange("p (h q) -> p h q", h=8)
            P.tt(h3, h3, S[:, 24:32].unsqueeze(2).to_broadcast([128, 8, 64]), ALU.mult)
            P.tt(hin32[:], hin32[:], bank(3), ALU.add)
            P.copy(hinb[:], hin32[:], eng="scalar")
            yield

    gens = [stream(0, [0, 1, 2, 3]), stream(1, [4, 5, 6, 7])]
    if with_rwkv_prep:
        gens.append(rwkv_prep_stream(nc, P, G, l, ph))
    run_streams(gens)
    ph.close()


def ssd_fin(nc, P, G, l):
    W = G.w
    ph = Phase(nc, P)
    dsm = ph.sb("dsm", [128, 8]); dex = ph.sb("dex", [128, 8, 64]); nw = ph.sb("nw", [128, 512])
    yf = ph.sb("yf", [128, 2, 512]); yb = ph.sb("yb", [128, 2, 512]); xs = ph.sb("xs", [128, 2, 512]); z = ph.sb("z", [128, 2, 512])
    junk = ph.sb("junk", [128, 256]); ss = ph.sb("ss", [128, 2, 2]); o = ph.sb("o", [128, 2, 512])
    P.dma("sync", dsm[:], bc_rows(W["ssd_d"][l:l + 1, :]))
    P.copy(dex[:], dsm[:].unsqueeze(2).to_broadcast([128, 8, 64]))
    P.dma("sync", nw[:], bc_rows(W["ssd_norm_w"][l:l + 1, :]))
    dexf = dex[:].rearrange("p h q -> p (h q)")
    for i in range(NT):
        b = i % 2
        rows = slice(i * 128, (i + 1) * 128)
        P.dma("sync", yf[:, b, :], G.ydir[0, rows, :])
        P.dma("sync", yb[:, b, :], G.ydir[1, rows, :])
        P.dma("sync", xs[:, b, :], G.xbca[rows, 0:512])
        P.dma("sync", z[:, b, :], G.zin[rows, C_Z:C_Z + 512])
        y = yf[:, b, :]
        P.tt(y, y, yb[:, b, :], ALU.add)
        P.tt(xs[:, b, :], xs[:, b, :], dexf, ALU.mult, eng="gpsimd")
        P.tt(y, y, xs[:, b, :], ALU.add)
        P.act(z[:, b, :], z[:, b, :], AF.Silu)
        P.tt(y, y, z[:, b, :], ALU.mult)
        for g in range(2):
            P.act(junk[:], y[:, g * 256:(g + 1) * 256], AF.Square, accum_out=ss[:, b, g:g + 1])
        P.act(ss[:, b, :], ss[:, b, :], AF.Sqrt, bias=1e-5, scale=1.0 / 256)
        P.op("vector", lambda e, b=b: e.reciprocal(ss[:, b, :], ss[:, b, :]), reads=[ss[:, b, :]], writes=[ss[:, b, :]])
        for g in range(2):
            P.stt(o[:, b, g * 256:(g + 1) * 256], y[:, g * 256:(g + 1) * 256], ss[:, b, g:g + 1], nw[:, g * 256:(g + 1) * 256],
                  ALU.mult, ALU.mult)
        P.dma(G.stq, G.ymix[rows, 0:512], o[:, b, :])
    ph.close()
def phase_gla(nc, P, G, l):
    gla_prep(nc, P, G, l); gla_dir(nc, P, G, l); gla_fin(nc, P, G, l)


def gla_prep(nc, P, G, l):
    W = G.w
    ph = Phase(nc, P)
    GU = ph.sb("GU", [32, 256]); gbrow = ph.sb("gbrow", [1, 256])
    gd = ph.sb("gd", [128, 2, 32]); gdT = ph.sb("gdT", [32, 2, 128]); e = ph.sb("ge", [128, 2, 256])
    P.memset(GU[:], 0.0)
    P.dma("sync", GU[0:16, 0:128], W["gla_gu"][l, 0])
    P.dma("sync", GU[16:32, 128:256], W["gla_gu"][l, 1])
    P.dma("sync", gbrow[:], W["gla_gb"][l:l + 1].rearrange("a b c -> a (b c)"))
    for i in range(NT):
        b = i % 2
        rows = slice(i * 128, (i + 1) * 128)
        P.dma("sync", gd[:, b, :], G.zin[rows, C_GD:C_GD + 32])
        tp = G.psf[0:32, 0:128]
        P.tr(tp, gd[:, b, :], G.identf[:])
        P.copy(gdT[:, b, :], tp)
        gp = G.psf[:, 512:768]
        P.mm(gp, gdT[:, b, :], GU[:], start=True, stop=False)
        P.mm(gp, G.onesf[0:1, :], gbrow[:], start=False, stop=True)
        P.act(e[:, b, :], gp, AF.Exp, scale=-1.0)
        P.act(e[:, b, :], e[:, b, :], AF.Ln, bias=1.0)
        P.ts(e[:, b, :], e[:, b, :], -1.0 / 16.0, ALU.mult)
        P.dma(G.stq, G.gla_la[rows, :], e[:, b, :])
    ph.close()


def gla_dir(nc, P, G, l):
    ph = Phase(nc, P)
    ps = G.psf
    psbf = G.psf.bitcast(BF16)

    def stream(d, kb):
        sfx = "f" if d == 0 else "b"
        qk = ph.sb("qk" + sfx, [128, 2, 256]); v = ph.sb("gv" + sfx, [128, 2, 256]); la = ph.sb("gla" + sfx, [128, 2, 128])
        acs = ph.sb("gacs" + sfx, [128, 128]); ee = ph.sb("gee" + sfx, [128, 3, 128]); ecd = ph.sb("gecd" + sfx, [128, 1])
        qkt = ph.sb("qkt" + sfx, [128, 2, 128], BF16); kdec = ph.sb("kdec" + sfx, [128, 128], BF16)
        qkT = ph.sb("qkT" + sfx, [128, 2, 128], BF16); smb = ph.sb("smb" + sfx, [128, 4, 128], BF16); vb = ph.sb("vb" + sfx, [128, 256], BF16)
        kTm = ph.sb("kTm" + sfx, [128, 4, 128], BF16); qTm = ph.sb("qTm" + sfx, [128, 4, 128], BF16)
        stm = ph.sb("stm" + sfx, [128, 4, 64]); sred = ph.sb("sred" + sfx, [128, 64])
        ot = ph.sb("got" + sfx, [128, 2, 256]); S32 = ph.sb("S32" + sfx, [128, 64]); Sb = ph.sb("Sb" + sfx, [128, 64], BF16)

        def bank(k, w=512, o=0):
            return ps[:, kb[k] * 512 + o:kb[k] * 512 + o + w]
        P.memset(S32[:], 0.0)
        P.memset(Sb[:], 0.0)
        tri = G.tri[d]
        for i in dir_order(d):
            b = i % 2
            rows = slice(i * 128, (i + 1) * 128)
            P.dma("sync", qk[:, b, :], G.zin[rows, C_GQ:C_GQ + 256])
            P.dma("sync", v[:, b, :], G.zin[rows, C_GV:C_GV + 256])
            P.dma("sync", la[:, b, :], G.gla_la[rows, d * 128:(d + 1) * 128])
            acs_ps = bank(0, 128, 0); tot_ps = bank(0, 128, 128); totT = bank(0, 2, 256)
            P.mm(acs_ps, tri[:], la[:, b, :])
            P.mm(tot_ps, G.onesf[:], la[:, b, :])
            P.mm(totT, la[:, b, :], G.onesf[:, 0:2])
            P.copy(vb[:], v[:, b, :], eng="gpsimd")
            yield
            P.copy(acs[:], acs_ps, eng="scalar")
            P.act(ee[:, 0, :], acs_ps, AF.Exp)
            P.act(ee[:, 1, :], acs_ps, AF.Exp, scale=-1.0)
            P.tt(ee[:, 2, :], tot_ps, acs[:], ALU.subtract)
            P.act(ee[:, 2, :], ee[:, 2, :], AF.Exp)
            P.act(ecd[:], totT[:, 0:1], AF.Exp)
            P.stt(qkt[:, 0, :], qk[:, b, 0:128], 32 ** -0.5, ee[:, 0, :], ALU.mult, ALU.mult)
            P.tt(qkt[:, 1, :], qk[:, b, 128:256], ee[:, 1, :], ALU.mult)
            P.tt(kdec[:], qk[:, b, 128:256], ee[:, 2, :], ALU.mult, eng="gpsimd")
            for j in range(2):
                P.tr(psbf[:, kb[0] * 1024 + j * 128:kb[0] * 1024 + (j + 1) * 128], qkt[:, j, :], G.identb[:])
            P.mm(bank(3, 256, 0), kdec[:], vb[:])
            yield
            P.copy(qkT[:].rearrange("p a b -> p (a b)"), psbf[:, kb[0] * 1024:kb[0] * 1024 + 256], eng="scalar")
            P.tt(kTm[:], qkT[:, 1, :].unsqueeze(1).to_broadcast([128, 4, 128]), G.hm4[:].unsqueeze(2).to_broadcast([128, 4, 128]), ALU.mult)
            P.tt(qTm[:], qkT[:, 0, :].unsqueeze(1).to_broadcast([128, 4, 128]), G.hm4[:].unsqueeze(2).to_broadcast([128, 4, 128]), ALU.mult, eng="gpsimd")
            for h in range(4):
                P.mm(bank(1, 128, h * 128), kTm[:, h, :], qkT[:, 0, :])
            yield
            P.tt(smb[:], bank(1).rearrange("p (h q) -> p h q", h=4), G.tri[d][:].unsqueeze(1).to_broadcast([128, 4, 128]), ALU.mult)
            for h in range(4):
                o_ps = bank(2, 64, h * 64)
                P.mm(o_ps, smb[:, h, :], vb[:, h * 64:(h + 1) * 64], start=True, stop=False)
                P.mm(o_ps, qTm[:, h, :], Sb[:], start=False, stop=True)
            yield
            P.copy(ot[:, b, :], bank(2, 256, 0), eng="scalar")
            P.dma(G.stq, G.gla_o[d, rows, :], ot[:, b, :])
            P.tt(stm[:], bank(3, 256, 0).rearrange("p (h q) -> p h q", h=4), G.hm4[:].unsqueeze(2).to_broadcast([128, 4, 64]), ALU.mult)
            P.op("vector", lambda e: e.tensor_reduce(sred[:], stm[:].rearrange("p h q -> p q h"), AX.X, ALU.add),
                 reads=[stm[:]], writes=[sred[:]])
            P.stt(S32[:], S32[:], ecd[:, 0:1], sred[:], ALU.mult, ALU.add)
            P.copy(Sb[:], S32[:])
            yield

    run_streams([stream(0, [0, 1, 2, 3]), stream(1, [4, 5, 6, 7])])
    ph.close()


def gla_fin(nc, P, G, l):
    W = G.w
    ph = Phase(nc, P)
    nw = ph.sb("gnw", [128, 256]); of = ph.sb("gof", [128, 2, 256]); ob = ph.sb("gob", [128, 2, 256]); og = ph.sb("gog", [128, 2, 256])
    sq = ph.sb("gsq", [128, 256]); ss = ph.sb("gss", [128, 2, 4]); y = ph.sb("gy", [128, 2, 256])
    P.dma("sync", nw[:], bc_rows(W["gla_norm_w"][l:l + 1, :]))
    for i in range(NT):
        b = i % 2
        rows = slice(i * 128, (i + 1) * 128)
        P.dma("sync", of[:, b, :], G.gla_o[0, rows, :])
        P.dma("sync", ob[:, b, :], G.gla_o[1, rows, :])
        P.dma("sync", og[:, b, :], G.zin[rows, C_OG:C_OG + 256])
        o = of[:, b, :]
        P.tt(o, o, ob[:, b, :], ALU.add)
        P.tt(sq[:], o, o, ALU.mult, eng="gpsimd")
        P.op("vector", lambda e, b=b: e.tensor_reduce(ss[:, b, :], sq[:].rearrange("p (h q) -> p h q", h=4), AX.X, ALU.add),
             reads=[sq[:]], writes=[ss[:, b, :]])
        P.act(ss[:, b, :], ss[:, b, :], AF.Sqrt, bias=1e-5, scale=1.0 / 64)
        P.op("vector", lambda e, b=b: e.reciprocal(ss[:, b, :], ss[:, b, :]), reads=[ss[:, b, :]], writes=[ss[:, b, :]])
        y3 = y[:, b, :].rearrange("p (h q) -> p h q", h=4)
        P.tt(y3, o.rearrange("p (h q) -> p h q", h=4), ss[:, b, :].unsqueeze(2).to_broadcast([128, 4, 64]), ALU.mult)
        P.tt(y[:, b, :], y[:, b, :], nw[:], ALU.mult, eng="gpsimd")
        P.act(og[:, b, :], og[:, b, :], AF.Silu)
        P.tt(y[:, b, :], y[:, b, :], og[:, b, :], ALU.mult)
        P.dma(G.stq, G.ymix[rows, 768:1024], y[:, b, :])
    ph.close()
def phase_rwkv(nc, P, G, l):
    rwkv_prep(nc, P, G, l); rwkv_dir(nc, P, G, l); rwkv_fin(nc, P, G, l)


def rwkv_prep(nc, P, G, l):
    ph = Phase(nc, P)
    for _ in rwkv_prep_stream(nc, P, G, l, ph):
        pass
    ph.close()


def rwkv_prep_stream(nc, P, G, l, ph):
    W = G.w
    mu = ph.sb("mu", [128, 4, 1152])
    w2a2 = ph.sb("w2a2", [128, 2, 256]); g2 = ph.sb("g2", [128, 256]); brow = ph.sb("rbrow", [1, 4, 256])
    kkw = ph.sb("kkw", [128, 256]); ka = ph.sb("ka", [128, 256]); rk = ph.sb("rk", [128, 256])
    c = ph.sb("rc", [128, 2, 1152]); nb = ph.sb("rnb", [128, 4, 1152])
    tws = ph.sb("tws", [128, 3, 128]); twT = ph.sb("twT", [128, 3, 128])
    pk = ph.sb("pk", [128, 2, 11, 256]); tmp = ph.sb("rtmp", [128, 3, 256]); s4 = ph.sb("rs4", [128, 8])
    for j in range(4):
        P.dma("sync", mu[:, j, :], bc_rows(W["rwkv_mu"][l, j:j + 1, :]))
    for d in range(2):
        P.dma("sync", w2a2[64 * d:64 * d + 64, 0, :], W["rwkv_w2"][l, d])
        P.dma("sync", w2a2[64 * d:64 * d + 64, 1, :], W["rwkv_a2"][l, d])
    P.dma("sync", g2[:], W["rwkv_g2"][l])
    P.dma("sync", brow[0:1, 0:2, :], W["rwkv_w0"][l:l + 1])
    P.dma("sync", brow[0:1, 2:4, :], W["rwkv_a0"][l:l + 1])
    P.dma("sync", kkw[:], bc_rows(W["rwkv_kk"][l:l + 1, :]))
    P.dma("sync", ka[:], bc_rows(W["rwkv_ka"][l:l + 1, :]))
    P.dma("sync", rk[:], bc_rows(W["rwkv_rk"][l:l + 1].rearrange("a b c -> a (b c)")))
    zr = G.zin[:, C_RW:C_RW + 1152]
    for i in range(NT):
        b = i % 2
        rows = slice(i * 128, (i + 1) * 128)
        lat = i >= 2
        cc = c[:, b, :]
        P.dma("sync", cc, zr[rows, :])
        offs = [-1, 1, -64, 64] if lat else [-1, 1]
        for j, off in enumerate(offs):
            X = nb[:, j, :]
            load_shift(P, X, zr, i, off)
            if lat and j < 2:
                P.stt(X, X, G.cmask[:, j:j + 1], cc, ALU.mult, ALU.subtract, eng=("vector"))
            else:
                P.tt(X, X, cc, ALU.subtract, eng="gpsimd")
            P.tt(X, X, mu[:, j, :], ALU.mult, eng=("gpsimd" if j % 2 else "vector"))
            yield
        for j in range(len(offs)):
            P.tt(cc, cc, nb[:, j, :], ALU.add)
        yield
        PK = pk[:, b]
        P.act(tws[:, 0, :], cc[:, 768:896], AF.Tanh)
        P.copy(tws[:, 1, :], cc[:, 896:1024], eng="gpsimd")
        P.act(tws[:, 2, :], cc[:, 1024:1152], AF.Sigmoid)
        for q in range(3):
            P.tr(G.psf[:, q * 128:(q + 1) * 128], tws[:, q, :], G.identf[:])
        yield
        P.copy(twT[:].rearrange("p a b -> p (a b)"), G.psf[:, 0:384])
        for d in range(2):
            ds = slice(64 * d, 64 * d + 64)
            xp = G.psf[:, (1 + 2 * d) * 512:(1 + 2 * d) * 512 + 256]
            P.mm(xp, twT[ds, 0, :], w2a2[ds, 0, :], start=True, stop=False)
            P.mm(xp, G.onesf[0:1, :], brow[0:1, d, :], start=False, stop=True)
            ap_ = G.psf[:, (1 + 2 * d) * 512 + 256:(1 + 2 * d) * 512 + 512]
            P.mm(ap_, twT[ds, 1, :], w2a2[ds, 1, :], start=True, stop=False)
            P.mm(ap_, G.onesf[0:1, :], brow[0:1, 2 + d, :], start=False, stop=True)
        gp = G.psf[:, 2048:2304]
        P.mm(gp, twT[:, 2, :], g2[:])
        yield
        for d in range(2):
            P.act(PK[:, 7 + d, :], G.psf[:, (1 + 2 * d) * 512:(1 + 2 * d) * 512 + 256], AF.Sigmoid)
        P.ts(PK[:, 7:9, :], PK[:, 7:9, :], -0.6065306597126334, ALU.mult, eng="gpsimd")
        adir = tmp[:, 0:2, :]
        for d in range(2):
            P.act(adir[:, d, :], G.psf[:, (1 + 2 * d) * 512 + 256:(1 + 2 * d) * 512 + 512], AF.Sigmoid)
        P.copy(PK[:, 9, :], gp, eng="scalar")
        P.copy(PK[:, 0, :], cc[:, 0:256], eng="gpsimd")
        P.copy(PK[:, 1, :], cc[:, 512:768], eng="gpsimd")
        kx = cc[:, 256:512]
        kk = PK[:, 2, :]
        P.tt(kk, kx, kkw[:], ALU.mult)
        P.tt(tmp[:, 2, :], kk, kk, ALU.mult, eng="gpsimd")
        P.op("vector", lambda e: e.tensor_reduce(s4[:, 0:4], tmp[:, 2, :].rearrange("p (h q) -> p h q", h=4), AX.X, ALU.add),
             reads=[tmp[:, 2, :]], writes=[s4[:, 0:4]])
        P.act(s4[:, 0:4], s4[:, 0:4], AF.Sqrt)
        P.ts(s4[:, 0:4], s4[:, 0:4], 1e-12, ALU.max)
        P.op("vector", lambda e: e.reciprocal(s4[:, 0:4], s4[:, 0:4]), reads=[s4[:, 0:4]], writes=[s4[:, 0:4]])
        kk3 = kk.rearrange("p (h q) -> p h q", h=4)
        P.tt(kk3, kk3, s4[:, 0:4].unsqueeze(2).to_broadcast([128, 4, 64]), ALU.mult)
        for d in range(2):
            t1 = tmp[:, 2, :]
            P.stt(t1, adir[:, d, :], -1.0, ka[:], ALU.add, ALU.mult)
            P.stt(PK[:, 3 + d, :], t1, 1.0, kx, ALU.add, ALU.mult)
            P.tt(PK[:, 5 + d, :], kk, adir[:, d, :], ALU.mult, eng="gpsimd")
        t1 = tmp[:, 2, :]
        P.tt(t1, PK[:, 3, :], PK[:, 4, :], ALU.add)
        P.tt(t1, t1, PK[:, 0, :], ALU.mult)
        P.tt(t1, t1, rk[:], ALU.mult)
        P.op("vector", lambda e: e.tensor_reduce(s4[:, 4:8], tmp[:, 2, :].rearrange("p (h q) -> p h q", h=4), AX.X, ALU.add),
             reads=[tmp[:, 2, :]], writes=[s4[:, 4:8]])
        P.tt(PK[:, 10, :].rearrange("p (h q) -> p h q", h=4), PK[:, 1, :].rearrange("p (h q) -> p h q", h=4),
             s4[:, 4:8].unsqueeze(2).to_broadcast([128, 4, 64]), ALU.mult)
        P.dma(G.stq, G.rwp[rows, :], PK.rearrange("p a b -> p (a b)"))
        yield


def rwkv_dir(nc, P, G, l):
    ph = Phase(nc, P)
    ps = G.psf

    def stream(d, kb):
        sfx = "f" if d == 0 else "b"
        ld = ph.sb("rld" + sfx, [128, 2, 6, 256])
        ee = ph.sb("ree" + sfx, [128, 3, 256])
        tok = ph.sb("rtok" + sfx, [128, 4, 256])
        fmh = ph.sb("fmh" + sfx, [64, 4, 4, 128])
        Aarb = ph.sb("Aarb" + sfx, [128, 4, 128]); Aak = ph.sb("Aak" + sfx, [128, 4, 128]); Aark = ph.sb("Aark" + sfx, [128, 4, 128])
        Q = ph.sb("rQ" + sfx, [128, 2, 4, 128]); QT = ph.sb("rQT" + sfx, [128, 2, 4, 128]); IQT = ph.sb("rIQT" + sfx, [128, 4, 128])
        Z = ph.sb("rZ" + sfx, [128, 2, 4, 128])
        Wsb = ph.sb("rW" + sfx, [128, 4, 64]); nAkV = ph.sb("nAkV" + sfx, [128, 4, 64]); Uloc = ph.sb("Uloc" + sfx, [128, 4, 64])
        GpT = ph.sb("GpT" + sfx, [64, 4, 2, 64]); RwT = ph.sb("RwT" + sfx, [64, 4, 128]); gC = ph.sb("gC" + sfx, [64, 4, 2])
        H32 = ph.sb("H32" + sfx, [64, 4, 64]); Yt = ph.sb("Yt" + sfx, [64, 2, 2, 256])
        btc = ph.sb("btc" + sfx, [128, 2, 256]); ktc = ph.sb("ktc" + sfx, [128, 2, 256])

        def bank(k, w=512, o=0, p0=0, p1=128):
            kk_ = kb[k]
            return ps[p0:p1, kk_ * 512 + o:kk_ * 512 + o + w]

        def b4(k):
            return bank(k).rearrange("p (h q) -> p h q", h=4)

        def bc4(m):
            return m[:].unsqueeze(1).to_broadcast([128, 4, 128])
        idb4 = G.identf[:].unsqueeze(1).to_broadcast([128, 4, 128])
        ea = "scalar"
        P.memset(H32[:], 0.0)
        BD = G.IU if d == 0 else G.IL
        m_abT = G.nSU if d == 0 else G.nSL
        m_ab = G.nSL if d == 0 else G.nSU
        m_s = G.SU if d == 0 else G.SL
        m_i = G.IU if d == 0 else G.IL
        for i in dir_order(d):
            b = i % 2
            rows = slice(i * 128, (i + 1) * 128)
            L = ld[:, b]
            for q, col in enumerate([0, 1, 2, 3 + d, 5 + d, 7 + d]):
                P.dma("sync", L[:, q, :], G.rwp[rows, col * 256:(col + 1) * 256])
            r_, v_, kk_, k_, b_, lw_ = [L[:, q, :] for q in range(6)]
            cs_ps = bank(3, 256)
            P.mm(cs_ps, BD[:], lw_)
            yield
            P.act(ee[:, 0, :], cs_ps, AF.Exp)
            P.act(ee[:, 1, :], cs_ps, AF.Exp, scale=-1.0)
            P.copy(ee[:, 2, :], cs_ps, eng="scalar")
            P.tt(ee[:, 2, :], ee[:, 2, :], lw_, ALU.subtract)
            P.act(ee[:, 2, :], ee[:, 2, :], AF.Exp)
            P.tt(tok[:, 0, :], kk_, ee[:, 2, :], ALU.mult)
            P.tt(tok[:, 1, :], r_, ee[:, 0, :], ALU.mult, eng="gpsimd")
            P.tt(tok[:, 2, :], b_, ee[:, 1, :], ALU.mult)
            P.tt(tok[:, 3, :], k_, ee[:, 1, :], ALU.mult, eng="gpsimd")
            for c in range(2):
                P.ts(btc[:, c, :], tok[:, 2, :], G.cind[:, c:c + 1], ALU.mult, eng=("gpsimd" if c else "vector"))
                P.ts(ktc[:, c, :], tok[:, 3, :], G.cind[:, c:c + 1], ALU.mult, eng=("gpsimd" if c else "vector"))
            yield
            for q in range(4):
                for h in range(4):
                    P.tr(bank(q, 128, h * 128, 0, 64), tok[:, q, h * 64:(h + 1) * 64], G.identf[:])
            yield
            for q in range(4):
                P.copy(fmh[:, q].rearrange("p h t -> p (h t)"), bank(q, 512, 0, 0, 64), eng=("scalar" if q % 2 else "vector"))
            for h in range(4):
                P.mm(bank(0, 2, 2 * h, 0, 64), lw_[:, h * 64:(h + 1) * 64], G.cind[:])
            for h in range(4):
                khT = fmh[:, 0, h, :]; btT = fmh[:, 2, h, :]
                P.mm(bank(1, 128, h * 128), btT, khT)
                P.mm(bank(2, 128, h * 128), khT, btT)
            yield
            P.act(gC[:].rearrange("p h c -> p (h c)"), bank(0, 8, 0, 0, 64), AF.Exp)
            P.tt(Q[:, 0], b4(1), bc4(m_abT), ALU.mult)
            P.tt(QT[:, 0], b4(2), bc4(m_ab), ALU.mult)
            P.tt(IQT[:], QT[:, 0], idb4, ALU.add, eng="gpsimd")
            P.tt(Z[:, 0], Q[:, 0], idb4, ALU.add, eng="gpsimd")
            for h in range(4):
                khT = fmh[:, 0, h, :]; rtT = fmh[:, 1, h, :]; btT = fmh[:, 2, h, :]; ktT = fmh[:, 3, h, :]
                P.mm(bank(3, 128, h * 128), btT, rtT)
                P.mm(bank(0, 128, h * 128), ktT, khT)
            yield
            P.tt(Aarb[:], b4(3), bc4(m_i), ALU.mult)
            P.tt(Aak[:], b4(0), bc4(m_s), ALU.mult)
            zc = 0
            for k in range(1, 6):
                a, bq = (k - 1) % 2, k % 2
                for h in range(4):
                    if k < 5:
                        P.mm(bank(0, 128, h * 128), QT[:, a, h, :], Q[:, a, h, :])
                    P.mm(bank(1, 128, h * 128), Q[:, a, h, :], QT[:, a, h, :])
                if k >= 2:
                    for h in range(4):
                        P.mm(bank(2, 128, h * 128), IQT[:, h, :], Z[:, zc, h, :])
                if k == 1:
                    for h in range(4):
                        P.mm(bank(3, 128, h * 128), fmh[:, 3, h, :], fmh[:, 1, h, :])
                yield
                if k == 1:
                    P.tt(Aark[:], b4(3), bc4(m_i), ALU.mult)
                if k < 5:
                    P.copy(Q[:, bq], b4(0), eng="scalar")
                P.tt(IQT[:], b4(1), idb4, ALU.add)
                if k < 5:
                    P.copy(QT[:, bq], b4(1))
                if k >= 2:
                    P.copy(Z[:, 1 - zc], b4(2), eng="scalar")
                    zc = 1 - zc
            for h in range(4):
                P.mm(bank(2, 128, h * 128), IQT[:, h, :], Z[:, zc, h, :])
            yield
            P.copy(Z[:, 1 - zc], b4(2), eng="scalar")
            zc = 1 - zc
            TT = Z[:, zc]
            for h in range(4):
                hc = slice(h * 64, (h + 1) * 64)
                P.mm(bank(3, 64, h * 64), TT[:, h, :], tok[:, 0, hc])
                P.mm(bank(0, 64, h * 64), Aak[:, h, :], v_[:, hc])
            yield
            P.copy(Wsb[:].rearrange("p h q -> p (h q)"), bank(3, 256), eng="scalar")
            P.ts(nAkV[:].rearrange("p h q -> p (h q)"), bank(0, 256), -1.0, ALU.mult)
            for h in range(4):
                P.mm(bank(1, 64, h * 64), TT[:, h, :], nAkV[:, h, :])
            for h in range(4):
                hc = slice(h * 64, (h + 1) * 64)
                for c in range(2):
                    P.mm(bank(2, 64, (h * 2 + c) * 64, 0, 64), Wsb[:, h, :], btc[:, c, hc])
                P.mm(bank(3, 128, h * 128, 0, 64), Wsb[:, h, :], Aarb[:, h, :])
            yield
            P.copy(Uloc[:].rearrange("p h q -> p (h q)"), bank(1, 256), eng="scalar")
            id8 = G.identf[0:64, 0:64].unsqueeze(1).to_broadcast([64, 8, 64])
            P.tt(GpT[:].rearrange("p h c q -> p (h c) q"), id8, bank(2, 512, 0, 0, 64).rearrange("p (a q) -> p a q", a=8), ALU.subtract)
            P.tt(RwT[:], fmh[:, 1], bank(3, 512, 0, 0, 64).rearrange("p (h q) -> p h q", h=4), ALU.subtract)
            for c in ([0, 1] if d == 0 else [1, 0]):
                cs_ = slice(64 * c, 64 * c + 64)
                for h in range(4):
                    hc = slice(h * 64, (h + 1) * 64)
                    yp = bank(1, 64, (c * 4 + h) * 64, 0, 64)
                    P.mm(yp, Aarb[:, h, cs_], Uloc[:, h, :], start=True, stop=False)
                    P.mm(yp, Aark[:, h, cs_], v_[:, hc], start=False, stop=False)
                    P.mm(yp, RwT[:, h, cs_], H32[:, h, :], start=False, stop=True)
                for h in range(4):
                    hc = slice(h * 64, (h + 1) * 64)
                    hp = bank(0, 64, h * 64, 0, 64)
                    P.mm(hp, btc[:, c, hc], Uloc[:, h, :], start=True, stop=False)
                    P.mm(hp, ktc[:, c, hc], v_[:, hc], start=False, stop=False)
                    P.mm(hp, GpT[:, h, c, :], H32[:, h, :], start=False, stop=True)
                yield
                P.tt(H32[:], bank(0, 256, 0, 0, 64).rearrange("p (h q) -> p h q", h=4),
                     gC[:, :, c].unsqueeze(2).to_broadcast([64, 4, 64]), ALU.mult)
            P.copy(Yt[:, b].rearrange("p c q -> p (c q)"), bank(1, 512, 0, 0, 64), eng="scalar")
            P.dma(G.stq, G.rwy[d, rows, :].rearrange("(c p) q -> p c q", p=64), Yt[:, b])
            yield

    gens = [stream(0, [0, 1, 2, 3]), stream(1, [4, 5, 6, 7])]
    while gens:
        for g in list(gens):
            try:
                next(g)
            except StopIteration:
                gens.remove(g)
    ph.close()


def rwkv_fin(nc, P, G, l):
    W = G.w
    ph = Phase(nc, P)
    lw_ = ph.sb("lnxw", [128, 256]); lb_ = ph.sb("lnxb", [128, 256])
    yf = ph.sb("ryf", [128, 2, 256]); yb = ph.sb("ryb", [128, 2, 256]); gb = ph.sb("rgb", [128, 2, 2, 256])
    sq = ph.sb("rsq", [128, 256]); s4 = ph.sb("rfs4", [128, 2, 8])
    P.dma("sync", lw_[:], bc_rows(W["rwkv_lnx_w"][l:l + 1, :]))
    P.dma("sync", lb_[:], bc_rows(W["rwkv_lnx_b"][l:l + 1, :]))
    for i in range(NT):
        b = i % 2
        rows = slice(i * 128, (i + 1) * 128)
        P.dma("sync", yf[:, b, :], G.rwy[0, rows, :])
        P.dma("sync", yb[:, b, :], G.rwy[1, rows, :])
        P.dma("sync", gb[:, b].rearrange("p a b -> p (a b)"), G.rwp[rows, 9 * 256:11 * 256])
        y = yf[:, b, :]
        y3 = y.rearrange("p (h q) -> p h q", h=4)
        S = s4[:, b, :]
        P.tt(y, y, yb[:, b, :], ALU.add)
        P.op("vector", lambda e, y3=y3, S=S: e.tensor_reduce(S[:, 0:4], y3, AX.X, ALU.add), reads=[y], writes=[S[:, 0:4]])
        P.ts(S[:, 0:4], S[:, 0:4], 1.0 / 64, ALU.mult)
        P.tt(y3, y3, S[:, 0:4].unsqueeze(2).to_broadcast([128, 4, 64]), ALU.subtract)
        P.tt(sq[:], y, y, ALU.mult, eng="gpsimd")
        P.op("vector", lambda e, S=S: e.tensor_reduce(S[:, 4:8], sq[:].rearrange("p (h q) -> p h q", h=4), AX.X, ALU.add),
             reads=[sq[:]], writes=[S[:, 4:8]])
        P.act(S[:, 4:8], S[:, 4:8], AF.Sqrt, bias=64e-5, scale=1.0 / 64)
        P.op("vector", lambda e, S=S: e.reciprocal(S[:, 4:8], S[:, 4:8]), reads=[S[:, 4:8]], writes=[S[:, 4:8]])
        P.tt(y3, y3, S[:, 4:8].unsqueeze(2).to_broadcast([128, 4, 64]), ALU.mult)
        P.tt(y, y, lw_[:], ALU.mult, eng="gpsimd")
        P.tt(y, y, lb_[:], ALU.add)
        P.tt(y, y, gb[:, b, 1, :], ALU.add)
        P.tt(y, y, gb[:, b, 0, :], ALU.mult)
        P.dma(G.stq, G.ymix[rows, 512:768], y)
    ph.close()
def tiles_for(l, n_layers_total=4):
    return list(range(2, NT)) if l == n_layers_total - 1 else list(range(NT))


def phase_outproj(nc, P, G, l):
    W = G.w
    ph = Phase(nc, P)
    psbf = G.psf.bitcast(BF16)
    woutb = ph.sb("woutb", [128, 8, 1024], BF16); wst = ph.sb("owst", [128, 8, 512])
    rw32 = ph.sb("rw32", [128, 8, 64]); rbias = ph.sb("rbias", [128, 64])
    mg1 = ph.sb("mg1", [128, 2, 1024]); msc2 = ph.sb("msc2", [128, 2, 1024]); msh2 = ph.sb("msh2", [128, 2, 1024])
    l1w = ph.sb("l1w", [128, 1024]); l1b = ph.sb("l1b", [128, 1024])
    for n in range(2):
        P.dma("sync", wst[:], W["w_out"][l, :, n * 512:(n + 1) * 512].rearrange("(kc p) j -> p kc j", p=128))
        P.copy(woutb[:, :, n * 512:(n + 1) * 512], wst[:], eng="gpsimd")
    P.dma("sync", rw32[:], W["router_w"][l].rearrange("(kc p) e -> p kc e", p=128))
    P.dma("sync", rbias[:], bc_rows(W["router_b"][l:l + 1, :]))
    for s in range(2):
        P.dma("sync", mg1[:, s, :], G.modd[s, 2])
        P.dma("sync", msc2[:, s, :], G.modd[s, 4])
        P.dma("sync", msh2[:, s, :], G.modd[s, 3])
    P.dma("sync", l1w[:], bc_rows(W["ln1_w"][l:l + 1, :]))
    P.dma("sync", l1b[:], bc_rows(W["ln1_b"][l:l + 1, :]))

    def stream(sid, tiles, kb):
        sfx = str(sid)
        ym = ph.sb("oym" + sfx, [128, 1024]); ymb = ph.sb("oymb" + sfx, [128, 1024], BF16); ymT = ph.sb("oymT" + sfx, [128, 8, 128], BF16)
        xt = ph.sb("oxt" + sfx, [128, 1024]); t = ph.sb("ot" + sfx, [128, 1024]); hh = ph.sb("ohh" + sfx, [128, 1024])
        hT32 = ph.sb("hT32" + sfx, [128, 8, 128]); hTb = ph.sb("hTb" + sfx, [128, 8, 128], BF16)
        st6 = ph.sb("ost6" + sfx, [128, 2, 6]); mv = ph.sb("omv" + sfx, [128, 2]); rstd = ph.sb("orstd" + sfx, [128, 1])
        sc = ph.sb("rsc" + sfx, [128, 64]); sel = ph.sb("rsel" + sfx, [128, 64]); m8 = ph.sb("rm8" + sfx, [128, 8, 8]); gs = ph.sb("rgs" + sfx, [128, 8])
        g8 = ph.sb("rg8" + sfx, [128, 8]); gm = ph.sb("rgm" + sfx, [128, 8]); pen = ph.sb("rpen" + sfx, [128, 8]); s8 = ph.sb("rs8" + sfx, [128, 8])
        selm = ph.sb("rselm" + sfx, [128, 64]); wts = ph.sb("rwts" + sfx, [128, 64]); den = ph.sb("rden" + sfx, [128, 1]); gT = ph.sb("rgT" + sfx, [128, 64])

        def bank(k, w=512, o=0):
            return G.psf[:, kb[k] * 512 + o:kb[k] * 512 + o + w]
        for i in tiles:
            s = seg_of(i)
            rows = slice(i * 128, (i + 1) * 128)
            P.dma("sync", ym[:], G.ymix[rows, :])
            P.dma("sync", xt[:], G.xres[rows, :])
            P.copy(ymb[:], ym[:], eng="scalar")
            for kc in range(8):
                P.tr(psbf[:, kb[2] * 1024 + kc * 128:kb[2] * 1024 + (kc + 1) * 128], ymb[:, kc * 128:(kc + 1) * 128], G.identb[:])
            yield
            P.copy(ymT[:].rearrange("p a b -> p (a b)"), psbf[:, kb[2] * 1024:kb[2] * 1024 + 1024], eng="scalar")
            for n in range(2):
                pso = bank(n)
                for kc in range(8):
                    P.mm(pso, ymT[:, kc, :], woutb[:, kc, n * 512:(n + 1) * 512], start=(kc == 0), stop=(kc == 7))
            yield
            for n in range(2):
                P.tt(t[:, n * 512:(n + 1) * 512], bank(n), mg1[:, s, n * 512:(n + 1) * 512], ALU.mult)
            x = xt[:]
            P.stt(t[:], x, DN_ALPHA, t[:], ALU.mult, ALU.add)
            ln_stats(P, ph, t[:], "o1", st6[:], mv[:], rstd[:])
            yield
            P.ts(t[:], t[:], mv[:, 0:1], ALU.subtract, rstd[:, 0:1], ALU.mult)
            P.tt(t[:], t[:], l1w[:], ALU.mult, eng="gpsimd")
            P.tt(x, t[:], l1b[:], ALU.add)
            P.dma(G.stq, G.xres[rows, :], x)
            ln_stats(P, ph, x, "o2", st6[:], mv[:], rstd[:])
            yield
            P.ts(hh[:], x, mv[:, 0:1], ALU.subtract, rstd[:, 0:1], ALU.mult)
            P.tt(hh[:], hh[:], msc2[:, s, :], ALU.mult, eng="gpsimd")
            P.tt(hh[:], hh[:], msh2[:, s, :], ALU.add)
            if G.dbg is not None and "hdbg" in G.dbg:
                P.dma("sync", G.dbg["hdbg"][rows, :], hh[:])
            for half in range(2):
                for kk in range(4):
                    kc = half * 4 + kk
                    P.tr(bank(half, 128, kk * 128), hh[:, kc * 128:(kc + 1) * 128], G.identf[:])
            yield
            for half in range(2):
                P.copy(hT32[:, half * 4:half * 4 + 4, :].rearrange("p a b -> p (a b)"), bank(half), eng="scalar")
            P.copy(hTb[:], hT32[:], eng="scalar")
            P.dma(G.stq, G.hT[:, :, rows].rearrange("kc p t -> p kc t"), hTb[:])
            lg = bank(3, 64)
            for kc in range(8):
                P.mm(lg, hT32[:, kc, :], rw32[:, kc, :], start=(kc == 0), stop=(kc == 7))
            yield
            P.act(sc[:], lg, AF.Sigmoid)
            P.tt(sel[:], sc[:], rbias[:], ALU.add)
            for g in range(8):
                P.op("vector", lambda e, g=g: e.max(m8[:, g, :], sel[:, g * 8:(g + 1) * 8]), reads=[sel[:, g * 8:(g + 1) * 8]], writes=[m8[:, g, :]])
            P.tt(gs[:], m8[:, :, 0], m8[:, :, 1], ALU.add)
            P.op("vector", lambda e: e.max(g8[:], gs[:]), reads=[gs[:]], writes=[g8[:]])
            P.ts(gm[:], gs[:], g8[:, 3:4], ALU.is_ge)
            P.ts(pen[:], gm[:], 1.0, ALU.subtract, 1e30, ALU.mult)
            sel3 = sel[:].rearrange("p (g q) -> p g q", g=8)
            selm3 = selm[:].rearrange("p (g q) -> p g q", g=8)
            P.tt(selm3, sel3, gm[:].unsqueeze(2).to_broadcast([128, 8, 8]), ALU.mult)
            P.tt(selm3, selm3, pen[:].unsqueeze(2).to_broadcast([128, 8, 8]), ALU.add)
            yield
            P.op("vector", lambda e: e.max(s8[:], selm[:]), reads=[selm[:]], writes=[s8[:]])
            P.ts(wts[:], selm[:], s8[:, 7:8], ALU.is_ge)
            P.tt(wts[:], wts[:], sc[:], ALU.mult)
            P.op("vector", lambda e: e.tensor_reduce(den[:], wts[:], AX.X, ALU.add), reads=[wts[:]], writes=[den[:]])
            P.op("vector", lambda e: e.reciprocal(den[:], den[:]), reads=[den[:]], writes=[den[:]])
            P.ts(wts[:], wts[:], den[:, 0:1], ALU.mult, 2.5, ALU.mult)
            P.dma(G.stq, G.gtok[rows, :], wts[:])
            yield

    tl = tiles_for(l)
    run_streams([stream(0, tl[0::2], [0, 1, 2, 3]), stream(1, tl[1::2], [4, 5, 6, 7])])
    ph.close()


def phase_moe(nc, P, G, l):
    W = G.w
    tl = tiles_for(l)
    half = (len(tl) + 1) // 2
    for (pa, tiles) in enumerate((tl[:half], tl[half:])):
        ph = Phase(nc, P)
        ntl = len(tiles)
        ntok = ntl * 128
        t0 = tiles[0] * 128
        acc = ph.sb("macc", [128, ntl, 1024])
        hTp = ph.sb("mhT", [128, 8, ntok], BF16); gk = ph.sb("mgk", [128, ntl, 64])
        w13s = ph.sb("w13s", [128, 8, 512]); w13b = ph.sb("w13b", [128, 2, 8, 512], BF16)
        w2s = ph.sb("w2s", [128, 2, 1024]); w2b = ph.sb("w2b", [128, 2, 2, 1024], BF16)
        hid = ph.sb("mhid", [128, 2, 2, 512], BF16); sil = ph.sb("msil", [128, 2, 512])
        P.dma("sync", hTp[:], G.hT[:, :, t0:t0 + ntok].rearrange("kc p t -> p kc t"))
        P.dma("sync", gk[:], G.gtok[t0:t0 + ntok, :].rearrange("(k p) e -> p k e", p=128))
        groups = []
        c0 = 0
        while c0 < ntok:
            n = min(512, ntok - c0)
            groups.append((c0, n)); c0 += n
        NE = 65
        orot = [0]
        pending = [None]

        def emit_down(e, eb, hb, c0, n):
            for sub in range(n // 128):
                ti = (c0 // 128) + sub
                for cc in range(2):
                    ob = 4 + (orot[0] % 4)
                    orot[0] += 1
                    o = G.psf[:, ob * 512:(ob + 1) * 512]
                    for fc in range(2):
                        P.mm(o, hid[:, hb, fc, sub * 128:(sub + 1) * 128], w2b[:, eb, fc, cc * 512:(cc + 1) * 512],
                             start=(fc == 0), stop=(fc == 1))
                    a = acc[:, ti, cc * 512:(cc + 1) * 512]
                    if e == 0:
                        P.ts(a, o, gk[:, ti, 0:1], ALU.mult)
                    elif e < 64:
                        P.stt(a, o, gk[:, ti, e:e + 1], a, ALU.mult, ALU.add)
                    else:
                        P.tt(a, a, o, ALU.add)
        for e in range(NE):
            eb = e % 2
            if e < 64:
                s1, s3, s2 = W["exp_w1"][l, e], W["exp_w3"][l, e], W["exp_w2"][l, e]
            else:
                s1, s3, s2 = W["sh_w1"][l], W["sh_w3"][l], W["sh_w2"][l]
            import os
            if e < 2 or not os.environ.get("KNOW"):
                P.dma("sync", w13s[:, :, 0:256], s1.rearrange("(kc p) f -> p kc f", p=128))
                P.dma("sync", w13s[:, :, 256:512], s3.rearrange("(kc p) f -> p kc f", p=128))
                P.dma("sync", w2s[:], s2.rearrange("(fc p) c -> p fc c", p=128))
                P.copy(w13b[:, eb], w13s[:], eng="gpsimd")
                P.copy(w2b[:, eb], w2s[:], eng="gpsimd")
            for gi, (c0, n) in enumerate(groups):
                hb = (e * len(groups) + gi) % 2
                cols = slice(c0, c0 + n)
                for fc in range(2):
                    h1 = G.psf[:, fc * 512:fc * 512 + n]
                    h3 = G.psf[:, (2 + fc) * 512:(2 + fc) * 512 + n]
                    for kc in range(8):
                        P.mm(h1, w13b[:, eb, kc, fc * 128:(fc + 1) * 128], hTp[:, kc, cols], start=(kc == 0), stop=(kc == 7))
                    for kc in range(8):
                        P.mm(h3, w13b[:, eb, kc, 256 + fc * 128:256 + (fc + 1) * 128], hTp[:, kc, cols], start=(kc == 0), stop=(kc == 7))
                    P.act(sil[:, fc, 0:n], h1, AF.Silu)
                    P.tt(hid[:, hb, fc, 0:n], sil[:, fc, 0:n], h3, ALU.mult)
                if pending[0] is not None:
                    emit_down(*pending[0])
                pending[0] = (e, eb, hb, c0, n)
        emit_down(*pending[0])
        pending[0] = None
        mg2 = ph2 = None
        fin = Phase(nc, P)
        mg2 = fin.sb("mg2", [128, 2, 1024]); l2w = fin.sb("l2w", [128, 1024]); l2b = fin.sb("l2b", [128, 1024])
        xt = fin.sb("mxt", [128, 2, 1024]); st6 = fin.sb("mst6", [128, 2, 6]); mv = fin.sb("mmv", [128, 2]); rstd = fin.sb("mrstd", [128, 1])
        for s in range(2):
            P.dma("sync", mg2[:, s, :], G.modd[s, 5])
        P.dma("sync", l2w[:], bc_rows(W["ln2_w"][l:l + 1, :]))
        P.dma("sync", l2b[:], bc_rows(W["ln2_b"][l:l + 1, :]))
        for k, i in enumerate(tiles):
            b = k % 2
            s = seg_of(i)
            rows = slice(i * 128, (i + 1) * 128)
            x = xt[:, b, :]
            P.dma("sync", x, G.xres[rows, :])
            f = acc[:, k, :]
            if G.dbg is not None and "fdbg" in G.dbg:
                P.dma("sync", G.dbg["fdbg"][rows, :], f)
            P.tt(f, f, mg2[:, s, :], ALU.mult)
            P.stt(f, x, DN_ALPHA, f, ALU.mult, ALU.add)
            ln_stats(P, fin, f, "m", st6[:], mv[:], rstd[:])
            P.ts(f, f, mv[:, 0:1], ALU.subtract, rstd[:, 0:1], ALU.mult)
            P.tt(f, f, l2w[:], ALU.mult)
            P.tt(x, f, l2b[:], ALU.add)
            if G.dbg is not None and "xout" in G.dbg:
                P.dma("sync", G.dbg["xout"][rows, :], x)
            if l == 3 and G.final_out:
                P.dma(G.stq, G.out[(i - 2) * 128:(i - 1) * 128, :], x, final=True)
            else:
                P.dma(G.stq, G.xres[rows, :], x)
        fin.st.close()
        ph.close()
def alloc_scratch(nc, G, scratch):
    G.xbca = scratch("xbca", [T_ALL, 1024])
    G.dtla = scratch("dtla", [T_ALL, 32])
    G.ydir = scratch("ydir", [2, T_ALL, 512])
    G.gla_la = scratch("gla_la", [T_ALL, 256])
    G.gla_o = scratch("gla_o", [2, T_ALL, 256])
    G.rwp = scratch("rwp", [T_ALL, 11 * 256])
    G.rwy = scratch("rwy", [2, T_ALL, 256])
    G.hT = scratch("hT", [8, 128, T_ALL], BF16)
    G.gtok = scratch("gtok", [T_ALL, 64])

PHASES = {"mod": phase_mod, "inproj": phase_inproj, "ssd": phase_ssd, "ssd_prep": ssd_prep, "ssd_dir": ssd_dir, "ssd_fin": ssd_fin,
          "gla": phase_gla, "rwkv": phase_rwkv, "rwkv_prep": rwkv_prep, "rwkv_dir": rwkv_dir, "rwkv_fin": rwkv_fin, "outproj": phase_outproj, "moe": phase_moe}
WNAMES = ["ada_w", "ada_b", "w_in", "ssd_conv_w", "ssd_conv_b", "ssd_dt_bias", "ssd_a_log", "ssd_d", "ssd_norm_w",
          "rwkv_mu", "rwkv_w0", "rwkv_w2", "rwkv_a0", "rwkv_a2", "rwkv_g2", "rwkv_kk", "rwkv_ka", "rwkv_rk",
          "rwkv_lnx_w", "rwkv_lnx_b", "gla_gu", "gla_gb", "gla_norm_w", "w_out", "ln1_w", "ln1_b", "ln2_w", "ln2_b",
          "router_w", "router_b", "exp_w1", "exp_w3", "exp_w2", "sh_w1", "sh_w3", "sh_w2"]
WSHAPES = {"ada_w": [4, 1024, 6144], "ada_b": [4, 6144], "w_in": [4, 1024, 3504], "ssd_conv_w": [4, 3, 1024],
           "ssd_conv_b": [4, 1024], "ssd_dt_bias": [4, 2, 8], "ssd_a_log": [4, 2, 8], "ssd_d": [4, 8],
           "ssd_norm_w": [4, 512], "rwkv_mu": [4, 4, 1152], "rwkv_w0": [4, 2, 256], "rwkv_w2": [4, 2, 64, 256],
           "rwkv_a0": [4, 2, 256], "rwkv_a2": [4, 2, 64, 256], "rwkv_g2": [4, 128, 256], "rwkv_kk": [4, 256],
           "rwkv_ka": [4, 256], "rwkv_rk": [4, 4, 64], "rwkv_lnx_w": [4, 256], "rwkv_lnx_b": [4, 256],
           "gla_gu": [4, 2, 16, 128], "gla_gb": [4, 2, 128], "gla_norm_w": [4, 256], "w_out": [4, 1024, 1024],
           "ln1_w": [4, 1024], "ln1_b": [4, 1024], "ln2_w": [4, 1024], "ln2_b": [4, 1024],
           "router_w": [4, 1024, 64], "router_b": [4, 64], "exp_w1": [4, 64, 1024, 256], "exp_w3": [4, 64, 1024, 256],
           "exp_w2": [4, 64, 256, 1024], "sh_w1": [4, 1024, 256], "sh_w3": [4, 1024, 256], "sh_w2": [4, 256, 1024]}


def build_program(layers=(0, 1, 2, 3), phases=None, dbg=(), wnames=None, feed=()):
    nc = bass.Bass("TRN2", target_bir_lowering=False)
    P = Prog(nc)
    G = Ctx()
    G.w = {}
    import os as _os
    G.stq = _os.environ.get('KSTQ', 'gpsimd')
    import os
    G.stop = float(os.environ.get('KSTOP', '99'))
    for n in (wnames or WNAMES):
        G.w[n] = nc.dram_tensor(n, WSHAPES[n], F32, kind="ExternalInput").ap()
    G.x_in = nc.dram_tensor("x", [4096, 1024], F32, kind="ExternalInput").ap()
    G.ctx_in = nc.dram_tensor("ctx", [256, 1024], F32, kind="ExternalInput").ap()
    G.cvec = nc.dram_tensor("cvec", [2, 1024], F32, kind="ExternalInput").ap()
    G.out = nc.dram_tensor("out", [4096, 1024], F32, kind="ExternalOutput").ap()
    DBG_SHAPES = {"u": ([T_ALL, 1024], BF16), "modd": ([2, 6, 128, 1024], F32), "zin": ([T_ALL, NIN], F32),
                  "ymix": ([T_ALL, 1024], F32), "xres": ([T_ALL, 1024], F32)}
    G.dbg = {}
    G.final_out = (phases is None or "moe" in phases)

    def scratch(name, shape, dt=F32):
        kind = "ExternalOutput" if name in dbg else ("ExternalInput" if name in feed else "Internal")
        t = nc.dram_tensor(name, list(shape), dt, kind=kind).ap()
        return t
    for n in dbg:
        if n in ("u",):
            G.dbg[n] = nc.dram_tensor(n, DBG_SHAPES[n][0], DBG_SHAPES[n][1], kind="ExternalOutput").ap()
        if n in ("hdbg", "fdbg", "xout"):
            G.dbg[n] = nc.dram_tensor(n, [T_ALL, 1024], F32, kind="ExternalOutput").ap()
    G.xres = scratch("xres", [T_ALL, 1024])
    G.modd = scratch("modd", [2, 6, 128, 1024])
    G.zin = scratch("zin", [T_ALL, NIN])
    G.ymix = scratch("ymix", [T_ALL, 1024])
    G.psf = nc.alloc_psum_tensor("psa", [128, 8 * 512], F32)
    G.psb = G.psf.bitcast(BF16)[:, 6 * 1024:8 * 1024]
    make_consts(nc, P, G)
    alloc_scratch(nc, G, scratch)
    if "xres" not in feed:
        P.dma("sync", G.xres[0:256, :], G.ctx_in)
        P.dma("sync", G.xres[256:T_ALL, :], G.x_in)
    P.barrier()
    allp = ["mod", "inproj", "ssd", "gla", "rwkv", "outproj", "moe"]
    for l in layers:
        for pn in (phases or allp):
            PHASES[pn](nc, P, G, l)
    if phases is None or "moe" in phases:
        pass
    else:
        pass
    P.barrier()
    P.dma("sync", G.out[0:128, 0:8], G.xres[0:128, 0:8], final=True) if (phases is not None and "moe" not in phases) else None
    P.finish()
    return nc, P


_NC_CACHE = {}


def kernel(**inputs):
    from concourse.bass_utils import run_bass_kernel_spmd
    if "nc" not in _NC_CACHE:
        nc, P = build_program()
        _NC_CACHE["nc"] = nc
    nc = _NC_CACHE["nc"]
    B = 4
    base = {n: np.ascontiguousarray(np.asarray(inputs[n], dtype=np.float32)) for n in WNAMES}
    x = np.asarray(inputs["x"], dtype=np.float32)
    ctx = np.asarray(inputs["ctx"], dtype=np.float32)
    c = np.asarray(inputs["c"], dtype=np.float32)
    c_ctx = np.asarray(inputs["c_ctx"], dtype=np.float32)
    in_maps = []
    for b in range(B):
        m = dict(base)
        m["x"] = np.ascontiguousarray(x[b])
        m["ctx"] = np.ascontiguousarray(ctx[b])
        m["cvec"] = np.ascontiguousarray(np.stack([c_ctx, c[b]]))
        in_maps.append(m)
    res = run_bass_kernel_spmd(nc, in_maps, core_ids=list(range(B)))
    out = np.stack([np.asarray(res.results[b]["out"], dtype=np.float32) for b in range(B)])
    return out
```
